# Optimizing a Trainium2 kernel written in Bass

```python
import math
import jax
import jax.numpy as jnp
from jax import lax
import numpy as np

D_MODEL = 1024
BATCH = 8
SEQ = 8192
DEPTH = 2

F32 = jnp.float32
N_EVEN = (DEPTH + 1) // 2
N_ODD = DEPTH // 2

RK_HEADS = 8
RK_HEAD = 64
RK_W = RK_HEADS * RK_HEAD
RK_DECAY_LORA = 64
RK_AAA_LORA = 64
RK_GATE_LORA = 128
RK_DECAY_SCALE = 0.606531
RK_LN_EPS = 64e-5

NS_HEADS = 8
NS_KV = 2
NS_HPG = NS_HEADS // NS_KV
NS_HEAD = 64
NS_W = NS_HEADS * NS_HEAD
CMP_LEN = 32
CMP_STRIDE = 16
CMP_HIDDEN = 128
SEL_BLOCK = 64
SEL_TOPK = 16
WINDOW = 512
Q_BLOCK = 128
ROPE_THETA = 500000.0
ROPE_DIM = NS_HEAD // 4
AB_COLS = 4 * RK_W + NS_W + 6 * NS_KV * NS_HEAD + 3 * NS_HEADS

RT_HEADS = 8
RT_QK = 128
RT_V = 256
RT_QKW = RT_HEADS * RT_QK
RT_VW = RT_HEADS * RT_V
RT_COLS = 2 * RT_QKW + 2 * RT_VW
RT_CHUNK = 128
RT_THETA = 10000.0
RT_GN_EPS = 1e-5

N_EXPERTS = 16
N_GROUPS = 4
EXP_PER_GROUP = N_EXPERTS // N_GROUPS
TOP_K = 2
D_EXPERT = 1024
MOE_BLOCK = 512

ALPHA = (2.0 * DEPTH) ** 0.25
BETA = (8.0 * DEPTH) ** -0.25
LN_EPS = 1e-5
NEG = -1e30

kernel_name = 'hybrid_rwkv7_nsa_retnet_grouped_moe_deepnorm'


def _layer_norm(x, g, b):
    xf = x.astype(F32)
    mu = xf.mean(-1, keepdims=True)
    var = jnp.square(xf - mu).mean(-1, keepdims=True)
    return ((xf - mu) * lax.rsqrt(var + LN_EPS) * g + b).astype(x.dtype)


def _head_norm(o, g, b, eps):
    h, n = o.shape[-2:]
    of = o.astype(F32)
    mu = of.mean(-1, keepdims=True)
    var = jnp.square(of - mu).mean(-1, keepdims=True)
    return (of - mu) * lax.rsqrt(var + eps) * g.reshape(h, n) + b.reshape(h, n)


def _rotate_half(x, cos, sin):
    x1, x2 = jnp.split(x, 2, axis=-1)
    return jnp.concatenate([x1 * cos - x2 * sin, x2 * cos + x1 * sin], axis=-1)


def _partial_rope(x):
    s = x.shape[1]
    half = ROPE_DIM // 2
    inv = ROPE_THETA ** (-jnp.arange(half, dtype=F32) / half)
    ang = jnp.arange(s, dtype=F32)[:, None] * inv[None, :]
    cos = jnp.cos(ang)[:, None, :]
    sin = jnp.sin(ang)[:, None, :]
    rot = _rotate_half(x[..., :ROPE_DIM].astype(F32), cos, sin).astype(x.dtype)
    return jnp.concatenate([rot, x[..., ROPE_DIM:]], axis=-1)


def _masked_softmax(s, mask, axis=-1):
    p = jax.nn.softmax(jnp.where(mask, s, NEG), axis=axis)
    return jnp.where(mask, p, 0.0)


def _token_shift(p):
    return jnp.pad(p[:, :-1], ((0, 0), (1, 0), (0, 0)))


def _rwkv7(p, mu, w0, w1, w2, a0, a1, a2, g1, g2, k_k, k_a, r_k, ln_gb):
    B, S, _ = p.shape
    dp = _token_shift(p) - p
    pr, pk, pv, pz = jnp.split(p, 4, axis=-1)
    dr, dk, dv, dz = jnp.split(dp, 4, axis=-1)
    r = pr + dr * mu[0]
    k = pk + dk * mu[1]
    v = pv + dv * mu[2]
    xw = pz + dz * mu[3]
    xa = pz + dz * mu[4]
    xg = pz + dz * mu[5]
    w = jnp.exp(-RK_DECAY_SCALE * jax.nn.sigmoid((w0 + jnp.tanh(xw @ w1) @ w2).astype(F32)))
    a = jax.nn.sigmoid((a0 + (xa @ a1) @ a2).astype(F32))
    g = jax.nn.sigmoid(xg @ g1) @ g2
    hd = lambda t: t.astype(F32).reshape(B, S, RK_HEADS, RK_HEAD)
    kk = hd(k * k_k)
    kk = kk / jnp.maximum(jnp.sqrt(jnp.sum(kk * kk, axis=-1, keepdims=True)), 1e-12)
    k = hd(k.astype(F32) * (1.0 + (a - 1.0) * k_a))
    r, v, w, a = hd(r), hd(v), hd(w), hd(a)

    def step(state, inp):
        r_t, w_t, k_t, v_t, kk_t, b_t = inp
        sa = jnp.einsum('bhvk,bhk->bhv', state, -kk_t)
        state = (state * w_t[:, :, None, :] + sa[..., None] * b_t[:, :, None, :]
                 + v_t[..., None] * k_t[:, :, None, :])
        return state, jnp.einsum('bhvk,bhk->bhv', state, r_t)

    tm = lambda t: jnp.moveaxis(t, 1, 0)
    s0 = jnp.zeros((B, RK_HEADS, RK_HEAD, RK_HEAD), F32)
    _, o = lax.scan(step, s0, (tm(r), tm(w), tm(k), tm(v), tm(kk), tm(kk * a)))
    o = jnp.moveaxis(o, 0, 1)
    o = _head_norm(o, ln_gb[0], ln_gb[1], RK_LN_EPS)
    bonus = jnp.sum(r * k * r_k.astype(F32).reshape(RK_HEADS, RK_HEAD), axis=-1, keepdims=True) * v
    o = (o + bonus).reshape(B, S, RK_W) * g
    return o.astype(p.dtype)


def _nsa(p, pe, c_w1, c_w2):
    B, S, _ = p.shape
    q = _partial_rope(p[..., :NS_W].reshape(B, S, NS_HEADS, NS_HEAD))
    kv = p[..., NS_W:NS_W + 6 * NS_KV * NS_HEAD].reshape(B, S, 6, NS_KV, NS_HEAD)
    kc = _partial_rope(kv[:, :, 0])
    vc = kv[:, :, 1]
    ks = _partial_rope(kv[:, :, 2])
    vs = kv[:, :, 3]
    kw = _partial_rope(kv[:, :, 4])
    vw = kv[:, :, 5]
    gates = jax.nn.sigmoid(p[..., -3 * NS_HEADS:].astype(F32)).reshape(B, S, 3, NS_KV, NS_HPG)

    n_cmp = (S - CMP_LEN) // CMP_STRIDE + 1
    cidx = jnp.arange(n_cmp)[:, None] * CMP_STRIDE + jnp.arange(CMP_LEN)[None, :]

    def compress(t, pe_i, w1, w2):
        blk = t[:, cidx] + pe_i[None, None, :, None, :]
        blk = jnp.moveaxis(blk, 3, 2).reshape(B, n_cmp, NS_KV, CMP_LEN * NS_HEAD)
        return jax.nn.gelu(blk @ w1) @ w2

    k_cmp = compress(kc, pe[0], c_w1[0], c_w2[0])
    v_cmp = compress(vc, pe[1], c_w1[1], c_w2[1])
    cmp_end = jnp.arange(n_cmp) * CMP_STRIDE + CMP_LEN - 1

    n_sel = S // SEL_BLOCK
    cs = jnp.arange(n_cmp) * CMP_STRIDE
    ss = jnp.arange(n_sel) * SEL_BLOCK
    ov = jnp.clip(jnp.minimum(cs[:, None] + CMP_LEN, ss[None, :] + SEL_BLOCK)
                  - jnp.maximum(cs[:, None], ss[None, :]), 0, None).astype(F32) / CMP_LEN
    top = min(SEL_TOPK, n_sel)
    ks_blk = ks.reshape(B, n_sel, SEL_BLOCK, NS_KV, NS_HEAD).transpose(0, 3, 1, 2, 4)
    vs_blk = vs.reshape(B, n_sel, SEL_BLOCK, NS_KV, NS_HEAD).transpose(0, 3, 1, 2, 4)
    kw_pad = jnp.pad(kw, ((0, 0), (WINDOW, 0), (0, 0), (0, 0)))
    vw_pad = jnp.pad(vw, ((0, 0), (WINDOW, 0), (0, 0), (0, 0)))
    scale = NS_HEAD ** -0.5
    b_ix = jnp.arange(B)[:, None, None, None]
    g_ix = jnp.arange(NS_KV)[None, None, :, None]
    blk_id = jnp.arange(n_sel)

    def block(s0):
        t = s0 + jnp.arange(Q_BLOCK)
        qb = lax.dynamic_slice_in_dim(q, s0, Q_BLOCK, 1).reshape(B, Q_BLOCK, NS_KV, NS_HPG, NS_HEAD)
        gb = lax.dynamic_slice_in_dim(gates, s0, Q_BLOCK, 1)
        s = jnp.einsum('bqghd,bngd->bqghn', qb, k_cmp).astype(F32) * scale
        pc = _masked_softmax(s, (cmp_end[None, :] <= t[:, None])[None, :, None, None, :])
        o_c = jnp.einsum('bqghn,bngd->bqghd', pc, v_cmp.astype(F32))
        imp = jnp.einsum('bqgn,nj->bqgj', pc.sum(3), ov)
        cur = t // SEL_BLOCK
        valid = (blk_id[None, :] <= cur[:, None])[None, :, None, :]
        forced = ((blk_id[None, :] == 0) | (blk_id[None, :] == cur[:, None])
                  | (blk_id[None, :] == cur[:, None] - 1))[None, :, None, :]
        pri = jnp.where(valid, jnp.where(forced, jnp.inf, imp), -jnp.inf)
        _, sel = lax.top_k(pri, top)
        sel_ok = sel <= cur[None, :, None, None]
        k_sel = ks_blk[b_ix, g_ix, sel]
        v_sel = vs_blk[b_ix, g_ix, sel]
        s = jnp.einsum('bqghd,bqgnld->bqghnl', qb, k_sel).astype(F32) * scale
        kpos = sel[..., None] * SEL_BLOCK + jnp.arange(SEL_BLOCK)
        m = (sel_ok[..., None] & (kpos <= t[None, :, None, None, None]))[:, :, :, None]
        ps = _masked_softmax(s, m, axis=(-2, -1))
        o_s = jnp.einsum('bqghnl,bqgnld->bqghd', ps, v_sel.astype(F32))
        kwb = lax.dynamic_slice_in_dim(kw_pad, s0, WINDOW + Q_BLOCK, 1)
        vwb = lax.dynamic_slice_in_dim(vw_pad, s0, WINDOW + Q_BLOCK, 1)
        wpos = s0 - WINDOW + jnp.arange(WINDOW + Q_BLOCK)
        wm = ((wpos[None, :] <= t[:, None]) & (wpos[None, :] > t[:, None] - WINDOW)
              & (wpos[None, :] >= 0))[None, :, None, None, :]
        s = jnp.einsum('bqghd,bkgd->bqghk', qb, kwb).astype(F32) * scale
        pw = _masked_softmax(s, wm)
        o_w = jnp.einsum('bqghk,bkgd->bqghd', pw, vwb.astype(F32))
        o = (gb[:, :, 0][..., None] * o_c + gb[:, :, 1][..., None] * o_s
             + gb[:, :, 2][..., None] * o_w)
        return o.reshape(B, Q_BLOCK, NS_W).astype(p.dtype)

    out = lax.map(block, jnp.arange(S // Q_BLOCK) * Q_BLOCK)
    return jnp.moveaxis(out, 0, 1).reshape(B, S, NS_W)


def _retention(p, gn_gb):
    B, S, _ = p.shape
    q, k, v, g = jnp.split(p, [RT_QKW, 2 * RT_QKW, 2 * RT_QKW + RT_VW], axis=-1)
    inv = RT_THETA ** (-jnp.linspace(0.0, 1.0, RT_QK // 2, dtype=F32))
    ang = jnp.arange(S, dtype=F32)[:, None] * inv[None, :]
    cos = jnp.cos(ang)[:, None, :]
    sin = jnp.sin(ang)[:, None, :]
    q = _rotate_half(q.astype(F32).reshape(B, S, RT_HEADS, RT_QK), cos, sin)
    k = _rotate_half(k.astype(F32).reshape(B, S, RT_HEADS, RT_QK), cos, sin) * RT_QK ** -0.5
    v = v.astype(F32).reshape(B, S, RT_HEADS, RT_V)
    log_g = jnp.log(1.0 - 2.0 ** (-5.0 - jnp.arange(RT_HEADS, dtype=F32)))
    idx = jnp.arange(RT_CHUNK, dtype=F32)
    diff = idx[:, None] - idx[None, :]
    inner_decay = jnp.where(diff >= 0, jnp.exp(jnp.maximum(diff, 0.0) * log_g[:, None, None]), 0.0)
    q_decay = jnp.exp((idx + 1.0) * log_g[:, None])[..., None]
    k_decay = jnp.exp((RT_CHUNK - 1.0 - idx) * log_g[:, None])[..., None]
    chunk_decay = jnp.exp(RT_CHUNK * log_g)[:, None, None]
    n_ch = S // RT_CHUNK
    chunks = lambda t: t.reshape(B, n_ch, RT_CHUNK, RT_HEADS, t.shape[-1]).transpose(1, 0, 3, 2, 4)

    def step(R, inp):
        qc, kc, vc = inp
        att = jnp.einsum('bhid,bhjd->bhij', qc, kc) * inner_decay
        inner = jnp.einsum('bhij,bhjv->bhiv', att, vc)
        cross = jnp.einsum('bhid,bhdv->bhiv', qc, R) * q_decay
        R = R * chunk_decay + jnp.einsum('bhjd,bhjv->bhdv', kc * k_decay, vc)
        return R, inner + cross

    R0 = jnp.zeros((B, RT_HEADS, RT_QK, RT_V), F32)
    _, o = lax.scan(step, R0, (chunks(q), chunks(k), chunks(v)))
    o = o.transpose(1, 0, 3, 2, 4).reshape(B, S, RT_HEADS, RT_V)
    o = _head_norm(o, gn_gb[0], gn_gb[1], RT_GN_EPS).reshape(B, S, RT_VW)
    return (jax.nn.silu(g.astype(F32)) * o).astype(p.dtype)


def _moe(h, router_w, router_b, w_gate, w_up, w_down):
    B, S, D = h.shape
    T = B * S
    xt = h.reshape(T, D)
    aff = jax.nn.sigmoid((xt @ router_w).astype(F32))
    grouped = (aff + router_b.astype(F32)).reshape(T, N_GROUPS, EXP_PER_GROUP)
    grp = jnp.argmax(lax.top_k(grouped, TOP_K)[0].sum(-1), axis=-1)
    in_grp = jnp.take_along_axis(grouped, grp[:, None, None], axis=1)[:, 0]
    _, loc = lax.top_k(in_grp, TOP_K)
    eidx = grp[:, None] * EXP_PER_GROUP + loc
    wts = jnp.take_along_axis(aff, eidx, axis=1)
    wts = wts / wts.sum(-1, keepdims=True)
    A = T * TOP_K
    fe = eidx.reshape(A)
    ftok = jnp.repeat(jnp.arange(T, dtype=jnp.int32), TOP_K)
    fw = wts.reshape(A)
    order = jnp.argsort(fe)
    se, stok, sw = fe[order], ftok[order], fw[order]
    counts = jnp.bincount(fe, length=N_EXPERTS)
    padded = (counts + MOE_BLOCK - 1) // MOE_BLOCK * MOE_BLOCK
    pad_end = jnp.cumsum(padded)
    pad_start = pad_end - padded
    start = jnp.cumsum(counts) - counts
    dest = pad_start[se] + jnp.arange(A) - start[se]
    n_blocks = -(-A // MOE_BLOCK) + N_EXPERTS
    rows = n_blocks * MOE_BLOCK
    row_tok = jnp.full((rows,), T, jnp.int32).at[dest].set(stok)
    row_w = jnp.zeros((rows,), F32).at[dest].set(sw)
    blk_exp = jnp.minimum(jnp.searchsorted(pad_end, jnp.arange(n_blocks) * MOE_BLOCK, side='right'),
                          N_EXPERTS - 1)
    x_pad = jnp.concatenate([xt, jnp.zeros((1, D), xt.dtype)], axis=0)

    def run(args):
        tok, wr, e = args
        xb = x_pad[tok]
        hb = jax.nn.silu(xb @ w_gate[e]) * (xb @ w_up[e])
        return (hb @ w_down[e]) * wr[:, None]

    y = lax.map(run, (row_tok.reshape(n_blocks, MOE_BLOCK), row_w.reshape(n_blocks, MOE_BLOCK), blk_exp))
    out = jax.ops.segment_sum(y.reshape(rows, D), row_tok, num_segments=T + 1)[:T]
    return out.reshape(B, S, D).astype(h.dtype)


def _gain_bias(key, lead, width):
    kg, kb = jax.random.split(key)
    g = 1.0 + 0.05 * jax.random.normal(kg, lead + (1, width), F32)
    b = 0.02 * jax.random.normal(kb, lead + (1, width), F32)
    return jnp.concatenate([g, b], axis=-2)


def setup_inputs(seed: int = 0) -> dict:
    key = jax.random.key(seed)
    ks = jax.random.split(key, 32)
    nrm = lambda k, shape, s: jax.random.normal(k, shape, F32) * s
    NE, NO = N_EVEN, N_ODD
    mix_w = RK_W + NS_W
    return {
        'x': nrm(ks[0], (BATCH, SEQ, D_MODEL), 1.0),
        'ab_w_in': nrm(ks[1], (NE, D_MODEL, AB_COLS), D_MODEL ** -0.5),
        'ab_w_out': nrm(ks[2], (NE, mix_w, D_MODEL), BETA * mix_w ** -0.5),
        'rk_mu': jax.random.uniform(ks[3], (NE, 6, RK_W), F32),
        'rk_w0': -2.0 + nrm(ks[4], (NE, RK_W), 1.5),
        'rk_w1': nrm(ks[5], (NE, RK_W, RK_DECAY_LORA), RK_W ** -0.5),
        'rk_w2': nrm(ks[6], (NE, RK_DECAY_LORA, RK_W), 0.5 * RK_DECAY_LORA ** -0.5),
        'rk_a0': nrm(ks[7], (NE, RK_W), 0.5),
        'rk_a1': nrm(ks[8], (NE, RK_W, RK_AAA_LORA), RK_W ** -0.5),
        'rk_a2': nrm(ks[9], (NE, RK_AAA_LORA, RK_W), 0.5 * RK_AAA_LORA ** -0.5),
        'rk_g1': nrm(ks[10], (NE, RK_W, RK_GATE_LORA), RK_W ** -0.5),
        'rk_g2': nrm(ks[11], (NE, RK_GATE_LORA, RK_W), RK_GATE_LORA ** -0.5),
        'rk_kk': 0.85 + nrm(ks[12], (NE, RK_W), 0.05),
        'rk_ka': 1.0 + nrm(ks[13], (NE, RK_W), 0.05),
        'rk_rk': nrm(ks[14], (NE, RK_W), 0.1),
        'rk_ln': _gain_bias(ks[15], (NE,), RK_W),
        'ns_pe': nrm(ks[16], (NE, 2, CMP_LEN, NS_HEAD), 0.1),
        'ns_c_w1': nrm(ks[17], (NE, 2, CMP_LEN * NS_HEAD, CMP_HIDDEN), (CMP_LEN * NS_HEAD) ** -0.5),
        'ns_c_w2': nrm(ks[18], (NE, 2, CMP_HIDDEN, NS_HEAD), CMP_HIDDEN ** -0.5),
        'rt_w_in': nrm(ks[19], (NO, D_MODEL, RT_COLS), D_MODEL ** -0.5),
        'rt_w_out': nrm(ks[20], (NO, RT_VW, D_MODEL), BETA * RT_VW ** -0.5),
        'rt_gn': _gain_bias(ks[21], (NO,), RT_VW),
        'router_w': nrm(ks[22], (D_MODEL, N_EXPERTS), D_MODEL ** -0.5),
        'router_b': nrm(ks[23], (N_EXPERTS,), 0.01),
        'moe_w_gate': nrm(ks[24], (DEPTH, N_EXPERTS, D_MODEL, D_EXPERT), D_MODEL ** -0.5),
        'moe_w_up': nrm(ks[25], (DEPTH, N_EXPERTS, D_MODEL, D_EXPERT), D_MODEL ** -0.5),
        'moe_w_down': nrm(ks[26], (DEPTH, N_EXPERTS, D_EXPERT, D_MODEL), BETA * D_EXPERT ** -0.5),
        'ln': _gain_bias(ks[27], (DEPTH, 2), D_MODEL),
    }


def reference(x, ab_w_in, ab_w_out, rk_mu, rk_w0, rk_w1, rk_w2, rk_a0, rk_a1, rk_a2, rk_g1, rk_g2,
              rk_kk, rk_ka, rk_rk, rk_ln, ns_pe, ns_c_w1, ns_c_w2, rt_w_in, rt_w_out, rt_gn,
              router_w, router_b, moe_w_gate, moe_w_up, moe_w_down, ln):
    for layer in range(DEPTH):
        i = layer // 2
        if layer % 2 == 0:
            p = x @ ab_w_in[i]
            o_a = _rwkv7(p[..., :4 * RK_W], rk_mu[i], rk_w0[i], rk_w1[i], rk_w2[i], rk_a0[i],
                         rk_a1[i], rk_a2[i], rk_g1[i], rk_g2[i], rk_kk[i], rk_ka[i], rk_rk[i], rk_ln[i])
            o_b = _nsa(p[..., 4 * RK_W:], ns_pe[i], ns_c_w1[i], ns_c_w2[i])
            mix = jnp.concatenate([o_a, o_b], axis=-1) @ ab_w_out[i]
        else:
            p = x @ rt_w_in[i]
            mix = _retention(p, rt_gn[i]) @ rt_w_out[i]
        x = _layer_norm(ALPHA * x + mix, ln[layer, 0, 0], ln[layer, 0, 1])
        ffn = _moe(x, router_w, router_b, moe_w_gate[layer], moe_w_up[layer], moe_w_down[layer])
        x = _layer_norm(ALPHA * x + ffn, ln[layer, 1, 0], ln[layer, 1, 1])
    return x
```

```python
import numpy as np
import ml_dtypes
from contextlib import ExitStack
import concourse.bass as bass
import concourse.mybir as mybir
from concourse.bass_utils import run_bass_kernel_spmd

F32 = mybir.dt.float32
BF16 = mybir.dt.bfloat16
I32 = mybir.dt.int32
U32 = mybir.dt.uint32
AF = mybir.ActivationFunctionType
ALU = mybir.AluOpType
AX = mybir.AxisListType

D = 1024
ALPHA = (2.0 * 2) ** 0.25
LN_EPS = 1e-5
NEG = -30000.0


class Ctx:
    NDMA = 10

    def __init__(self, nc):
        self.nc = nc
        self.eng = {"pe": nc.tensor, "dve": nc.vector, "act": nc.scalar, "pool": nc.gpsimd, "sp": nc.sync}
        self.sem = {}
        self.cnt = {}
        for e in self.eng:
            self.sem["e_" + e] = nc.alloc_semaphore("sem_e_" + e)
            self.cnt["e_" + e] = 0
        self.dma_pool = {}
        for q in ("sp", "pool", "act"):
            names = []
            for i in range(self.NDMA):
                n = "d_%s_%d" % (q, i)
                self.sem[n] = nc.alloc_semaphore("sem_" + n)
                self.cnt[n] = 0
                names.append(n)
            self.dma_pool[q] = [names, 0]
        self.known = {e: {} for e in self.eng}
        self.last_w = {}
        self.readers = {}
        self.n_inst = 0
        self.n_wait = 0

    def _wait(self, e, semname, val):
        kn = self.known[e]
        if kn.get(semname, 0) >= val:
            return
        self.eng[e].wait_ge(self.sem[semname], val)
        kn[semname] = val
        self.n_wait += 1

    def _deps(self, e, reads, writes, is_dma=False):
        own = "e_" + e if not is_dma else None
        need = {}

        def add(ev, raw):
            s, v = ev
            if s == own and e == "pe":
                return
            if need.get(s, 0) < v:
                need[s] = v

        for k in reads:
            ev = self.last_w.get(k)
            if ev is not None:
                add(ev, True)
        for k in writes:
            ev = self.last_w.get(k)
            if ev is not None:
                add(ev, False)
            for s, v in self.readers.get(k, {}).items():
                add((s, v), False)
        for s, v in need.items():
            self._wait(e, s, v)

    def _commit(self, ev, reads, writes):
        s, v = ev
        for k in writes:
            self.last_w[k] = ev
            self.readers[k] = {}
        for k in reads:
            if k in writes:
                continue
            r = self.readers.setdefault(k, {})
            if r.get(s, 0) < v:
                r[s] = v

    def op(self, e, fn, reads=(), writes=()):
        reads = list(reads)
        writes = list(writes)
        self._deps(e, reads, writes)
        ins = fn(self.eng[e])
        s = "e_" + e
        self.cnt[s] += 1
        ins.then_inc(self.sem[s], 1)
        self._commit((s, self.cnt[s]), reads, writes)
        self.n_inst += 1
        return ins

    def dma(self, q, out, in_, reads=(), writes=(), indirect=None, **kw):
        reads = list(reads)
        writes = list(writes)
        self._deps(q, reads, writes, is_dma=True)
        names, i = self.dma_pool[q]
        s = names[i % len(names)]
        self.dma_pool[q][1] = i + 1
        if self.cnt[s] > 0:
            self._wait(q, s, self.cnt[s])
        if indirect is None:
            ins = self.eng[q].dma_start(out=out, in_=in_, **kw)
        else:
            ins = self.eng[q].indirect_dma_start(out, indirect[0], in_, indirect[1], **kw)
        self.cnt[s] += 16
        ins.then_inc(self.sem[s], 16)
        self._commit((s, self.cnt[s]), reads, writes)
        self.n_inst += 1
        return ins

    def barrier(self):
        for e in self.eng:
            for s, c in self.cnt.items():
                if c > 0:
                    self._wait(e, s, c)

    def finish(self):
        for s, c in self.cnt.items():
            if c > 0:
                self._wait("sp", s, c)


class B:
    def __init__(self, S, dbg=()):
        self.S = S
        self.NT = S // 128
        self.dbg = set(dbg)
        nc = self.nc = bass.Bass("TRN2", target_bir_lowering=False)
        self.c = Ctx(nc)
        self.inp = {}
        self.ps = [nc.alloc_psum_tensor("psb%d" % i, [128, 512], F32) for i in range(8)]
        self.ps_i = 0
        self._uid = 0
        import os
        self.cut = int(os.environ['CUT']) if 'CUT' in os.environ else None

    def din(self, name, shape, dt=F32):
        t = self.nc.dram_tensor(name, list(shape), dt, kind="ExternalInput").ap()
        self.inp[name] = t
        return t

    def dscr(self, name, shape, dt=F32):
        kind = "ExternalOutput" if name in self.dbg else "Internal"
        return self.nc.dram_tensor(name, list(shape), dt, kind=kind).ap()

    def sb(self, st, name, shape, dt=F32):
        self._uid += 1
        return st.enter_context(self.nc.sbuf_tensor("%s_%d" % (name, self._uid), list(shape), dt))

    def nps(self):
        i = self.ps_i % 8
        self.ps_i += 1
        return self.ps[i], "ps%d" % i

    def mm(self, out, lhsT, rhs, start, stop, reads, pk):
        return self.c.op("pe", lambda e: e.matmul(out, lhsT, rhs, start=start, stop=stop), reads=reads, writes=[pk])

    def tr(self, out, in_, ident, reads, pk):
        return self.c.op("pe", lambda e: e.transpose(out, in_, ident), reads=reads, writes=[pk])

    def cp(self, eng, out, in_, reads, writes):
        if eng == "act":
            return self.c.op("act", lambda e: e.copy(out=out, in_=in_), reads=reads, writes=writes)
        return self.c.op(eng, lambda e: e.tensor_copy(out=out, in_=in_), reads=reads, writes=writes)

    def act(self, out, in_, func, reads, writes, bias=0.0, scale=1.0, accum_out=None):
        kw = {}
        if accum_out is not None:
            kw["accum_out"] = accum_out
        return self.c.op("act", lambda e: e.activation(out=out, in_=in_, func=func, bias=bias, scale=scale, **kw),
                         reads=reads, writes=writes)

    def tt(self, eng, out, in0, in1, op, reads, writes):
        return self.c.op(eng, lambda e: e.tensor_tensor(out=out, in0=in0, in1=in1, op=op), reads=reads, writes=writes)

    def ts(self, eng, out, in0, s1, op0, reads, writes, s2=None, op1=None):
        if op0 in (ALU.pow, ALU.divide) or op1 in (ALU.pow, ALU.divide):
            eng = "pool"
        if op1 is None:
            return self.c.op(eng, lambda e: e.tensor_scalar(out=out, in0=in0, scalar1=s1, scalar2=None, op0=op0),
                             reads=reads, writes=writes)
        return self.c.op(eng, lambda e: e.tensor_scalar(out=out, in0=in0, scalar1=s1, scalar2=s2, op0=op0, op1=op1),
                         reads=reads, writes=writes)

    def rsqrt(self, out, in_, eps, reads, writes, scale=1.0):
        self.act(out, in_, AF.Sqrt, reads, writes, bias=eps, scale=scale)
        return self.c.op("dve", lambda e: e.reciprocal(out=out, in_=out), reads=writes, writes=writes)

    def stt(self, eng, out, in0, scalar, in1, op0, op1, reads, writes):
        eng = "dve"
        return self.c.op(eng, lambda e: e.scalar_tensor_tensor(out=out, in0=in0, scalar=scalar, in1=in1, op0=op0, op1=op1),
                         reads=reads, writes=writes)

    def red(self, eng, out, in_, op, reads, writes, axis=AX.X):
        return self.c.op(eng, lambda e: e.tensor_reduce(out=out, in_=in_, axis=axis, op=op), reads=reads, writes=writes)

    def load_consts(self, st):
        self.ident = self.sb(st, "ident", [128, 128], F32)
        self.c.dma("sp", self.ident[:], self.inp["c_ident"][:, :], reads=[], writes=["ident"])

    def load_w(self, dst, w_ap, K, N, key, q="pool"):
        for kc in range(K // 128):
            for n0 in range(0, N, 2048):
                n1 = min(N, n0 + 2048)
                self.c.dma(q, dst[:, kc, n0:n1], w_ap[kc * 128:(kc + 1) * 128, n0:n1], reads=[], writes=[key])

    def load_w_fast(self, st, dst, w_ap, K, N, key):
        stg = [self.sb(st, "wstg", [128, 1024]) for _ in range(4)]
        i = 0
        for kc in range(K // 128):
            for n0 in range(0, N, 1024):
                n1 = min(N, n0 + 1024)
                s_ = stg[i % 4]
                sk = "wstg%d_%s" % (i % 4, key)
                self.c.dma("sp", s_[:, 0:n1 - n0], w_ap[kc * 128:(kc + 1) * 128, n0:n1], reads=[], writes=[sk])
                self.cp("act" if i % 2 == 0 else "dve", dst[:, kc, n0:n1], s_[:, 0:n1 - n0], [sk], [key])
                i += 1

    def transpose_in(self, xT, xin, nch, rkey, wkey, col0=0, ch0=0):
        j = 0
        k = 0
        while j < nch:
            g = min(4, nch - j)
            ps, pk = self.nps()
            for i in range(g):
                self.tr(ps[:, i * 128:(i + 1) * 128], xin[:, col0 + (j + i) * 128: col0 + (j + i + 1) * 128],
                        self.ident[:], [rkey, "ident"], pk)
            eng = "act" if k % 2 == 0 else "dve"
            self.cp(eng, xT[:, ch0 + j:ch0 + j + g, :], ps[:, 0:g * 128].rearrange("p (g t) -> p g t", g=g), [pk], [wkey])
            j += g
            k += 1

    def layer_norm(self, st_tiles, z, zkey, gam, bet, out, okey):
        stats, mv, rstd = st_tiles
        nc = self.nc
        c = self.c
        for i in range(2):
            c.op("dve", lambda e: e.bn_stats(out=stats[:, i, :], in_=z[:, i * 512:(i + 1) * 512]), reads=[zkey], writes=["ln_stats"])
        c.op("dve", lambda e: e.bn_aggr(out=mv[:], in_=stats[:]), reads=["ln_stats"], writes=["ln_mv"])
        self.rsqrt(rstd[:], mv[:, 1:2], LN_EPS, ["ln_mv"], ["ln_rstd"])
        self.ts("dve", out, z, mv[:, 0:1], ALU.subtract, [zkey, "ln_mv", "ln_rstd"], [okey], s2=rstd[:, 0:1], op1=ALU.mult)
        self.tt("pool", out, out, gam, ALU.mult, [okey, "lnp"], [okey])
        self.tt("pool", out, out, bet, ALU.add, [okey, "lnp"], [okey])

    def stage_proj(self, src, w_ap, dst, K, N):
        with ExitStack() as st:
            wb = self.sb(st, "wproj", [128, K // 128, N], BF16)
            self.load_w_fast(st, wb, w_ap, K, N, "wproj")
            xin = [self.sb(st, "xin", [128, K], F32) for _ in range(2)]
            xT = [self.sb(st, "xT", [128, K // 128, 128], BF16) for _ in range(2)]
            ot = [self.sb(st, "ot", [128, N], F32) for _ in range(2)]
            for t in range(self.NT):
                b = t % 2
                self.c.dma("sp", xin[b][:], src[t * 128:(t + 1) * 128, :], reads=[src.tensor.name], writes=["xin%d" % b])
                self.transpose_in(xT[b], xin[b], K // 128, "xin%d" % b, "xT%d" % b)
                k = 0
                for n0 in range(0, N, 512):
                    w = min(512, N - n0)
                    ps, pk = self.nps()
                    for kc in range(K // 128):
                        self.mm(ps[:, 0:w], xT[b][:, kc, :], wb[:, kc, n0:n0 + w], kc == 0, kc == K // 128 - 1,
                                ["xT%d" % b, "wproj"], pk)
                    self.cp("act" if k % 2 == 0 else "dve", ot[b][:, n0:n0 + w], ps[:, 0:w], [pk], ["ot%d" % b])
                    k += 1
                self.c.dma("sp", dst[t * 128:(t + 1) * 128, :], ot[b][:], reads=["ot%d" % b], writes=[dst.tensor.name])
            self.c.barrier()


RK_C = 0.606531


def stage_rwkv(self, p0, oab, W):
    c = self.c
    NT = self.NT
    with ExitStack() as st:
        sb = lambda n, shp, dt=F32: self.sb(st, n, shp, dt)
        pv = sb("rkv", [128, 13 * 512])
        c.dma("sp", pv[:], W["c_rkv"][:, :], writes=["rkv"])
        MU = lambda i: pv[:, i * 512:(i + 1) * 512]
        W0, A0, KK_, KA_, RKk, LNG, LNB = [pv[:, (6 + i) * 512:(7 + i) * 512] for i in range(7)]
        trib = sb("trib", [128, 128], BF16)
        c.dma("pool", trib[:], W["c_tri"][:, :], writes=["trib"])
        mask4 = sb("mask4", [128, 512])
        c.dma("sp", mask4[:], W["c_mask4"][:, :], writes=["mask4"])
        maskL = sb("maskL", [128, 128])
        c.dma("sp", maskL[:], W["c_maskL"][:, :], writes=["maskL"])
        identb = sb("identb", [128, 128], BF16)
        c.dma("pool", identb[:], W["c_ident"][:, :], writes=["identb"])
        w1 = sb("w1", [128, 4, 64], BF16)
        a1 = sb("a1", [128, 4, 64], BF16)
        g1 = sb("g1", [128, 4, 128], BF16)
        self.load_w(w1, W["rk_w1"], 512, 64, "w1")
        self.load_w(a1, W["rk_a1"], 512, 64, "a1")
        self.load_w(g1, W["rk_g1"], 512, 128, "g1")
        w2 = sb("w2", [64, 512], BF16)
        a2 = sb("a2", [64, 512], BF16)
        g2 = sb("g2", [128, 512], BF16)
        c.dma("pool", w2[:], W["rk_w2"][:, :], writes=["w2"])
        c.dma("pool", a2[:], W["rk_a2"][:, :], writes=["a2"])
        c.dma("pool", g2[:], W["rk_g2"][:, :], writes=["g2"])
        H = sb("H", [128, 4, 128])
        Hb = sb("Hb", [128, 4, 128], BF16)
        bd = sb("bd", [128, 128])
        c.dma("sp", bd[:], W["c_bd"][:, :], writes=["bd"])
        c.op("dve", lambda e: e.memset(H[:], 0.0), writes=["H"])
        c.op("dve", lambda e: e.memset(Hb[:], 0.0), writes=["Hb"])
        SINGLE = {"Pmm", "swh", "P", "Ps", "X", "xT", "hT", "sw", "a", "kk", "kp", "tmp", "ss", "cs", "e", "T", "BT", "KT", "Q"}

        def two(n, shp, dt=F32):
            return [sb(n, shp, dt) for _ in range(2)]

        def one(n, shp, dt=F32):
            x = sb(n, shp, dt)
            return [x, x]
        P_ = one("P", [128, 2048])
        Ps_ = one("Ps", [128, 2048])
        X6_ = one("X6", [128, 6, 512])
        xT_ = one("xT3", [128, 12, 128], BF16)
        hT_ = one("hT", [128, 384], BF16)
        sw_ = one("sw", [128, 512])
        swh = sb("swh", [128, 2, 512], BF16)
        a_ = one("a", [128, 512])
        g_ = two("g", [128, 512])
        kk_ = one("kk", [128, 512])
        kp_ = one("kp", [128, 512])
        tmp_ = one("tmp", [128, 512])
        tmp2_ = two("tq", [128, 512])
        ss_ = one("ss", [128, 8])
        bon_ = two("bon", [128, 512])
        sq_ = two("sq", [128, 8])
        bdg_ = [[sb("bdg", [128, 4, 128]) for _ in range(2)] for _ in range(2)]
        cs_ = one("cs", [128, 512])
        e_ = one("e3", [128, 3, 512])
        T4_ = one("T4", [128, 4, 512])
        Bt_ = two("Bt", [128, 512], BF16)
        Kt_ = two("Kt", [128, 512], BF16)
        Vt_ = two("Vt", [128, 512], BF16)
        ART_ = two("ART", [128, 4, 256], BF16)
        BT_ = one("BT", [128, 4, 128], BF16)
        KT_ = one("KT", [128, 4, 128], BF16)
        ET_ = two("ET", [128, 4, 128])
        G_ = two("G", [128, 8, 512], BF16)
        Wm_ = two("Wm", [128, 8, 128], BF16)
        Pm_ = one("Pm", [128, 8, 128], BF16)
        Qm_ = one("Qm", [128, 8, 128], BF16)
        Xs_ = two("Xs", [128, 512], BF16)
        Ub_ = [[sb("Ub", [128, 512], BF16) for _ in range(2)] for _ in range(2)]
        Vm_ = [[sb("Vm", [128, 512], BF16) for _ in range(2)] for _ in range(2)]
        for bb in range(2):
            c.op("pool", lambda e: e.memset(Xs_[bb][:], 0.0), writes=["Xs%d" % bb])
            for cc in range(2):
                c.op("pool", lambda e: e.memset(Ub_[bb][cc][:], 0.0), writes=["Ub%d_%d" % (cc, bb)])
                c.op("pool", lambda e: e.memset(Vm_[bb][cc][:], 0.0), writes=["Vm%d%d" % (cc, bb)])
        Os_ = two("Os", [128, 512])
        oo_ = two("oo", [128, 512])
        for t in range(NT if self.cut is None else 1):
            b = t % 2
            K = lambda n: n if n.rstrip("0123456789_") in SINGLE else "%s%d" % (n, b)
            P, Ps, X6, xT, hT = P_[b], Ps_[b], X6_[b], xT_[b], hT_[b]
            sw, a, g, kk, kp, tmp, tmp2, ss, bon, cs, e3, T4 = sw_[b], a_[b], g_[b], kk_[b], kp_[b], tmp_[b], tmp2_[b], ss_[b], bon_[b], cs_[b], e_[b], T4_[b]
            Bt, Kt, Vt, ART, BT, KT, ET, G, Wm, Pm, Qm, Xs, Ub, Os, oo = Bt_[b], Kt_[b], Vt_[b], ART_[b], BT_[b], KT_[b], ET_[b], G_[b], Wm_[b], Pm_[b], Qm_[b], Xs_[b], Ub_[b], Os_[b], oo_[b]
            sq = sq_[b]
            bdg = bdg_[b]
            Vm = Vm_[b]
            r0 = t * 128
            c.dma("sp", P[:], p0[r0:r0 + 128, 0:2048], reads=["p0"], writes=[K("Pmm")])
            if t == 0:
                c.op("pool", lambda e: e.memset(Ps[0:1, :], 0.0), writes=[K("Ps")])
                c.dma("sp", Ps[1:128, :], p0[0:127, 0:2048], reads=["p0"], writes=[K("Ps")])
            else:
                c.dma("sp", Ps[:], p0[r0 - 1:r0 + 127, 0:2048], reads=["p0"], writes=[K("Ps")])
            self.tt("dve", Ps[:], Ps[:], P[:], ALU.subtract, [K("Ps"), K("Pmm")], [K("Ps")])
            srcs = [0, 1, 2, 3, 3, 3]
            for i in range(6):
                eng = "dve" if i % 2 == 0 else "pool"
                sc = srcs[i] * 512
                self.tt(eng, X6[:, i, :], Ps[:, sc:sc + 512], MU(i), ALU.mult, [K("Ps"), "rkv"], [K("X6_%d" % i)])
                self.tt(eng, X6[:, i, :], X6[:, i, :], P[:, sc:sc + 512], ALU.add, [K("X6_%d" % i), K("Pmm")], [K("X6_%d" % i)])
            if self.cut == 1:
                return
            r, k, v = X6[:, 0, :], X6[:, 1, :], X6[:, 2, :]
            for i in range(3):
                self.transpose_in(xT, X6[:, 3 + i, :], 4, K("X6_%d" % (3 + i)), K("xT3"), ch0=4 * i)
            ps, pk = self.nps()
            for kc in range(4):
                self.mm(ps[0:64, 0:128], w1[:, kc, :], xT[:, kc, :], kc == 0, kc == 3, ["w1", K("xT3")], pk)
            for kc in range(4):
                self.mm(ps[0:64, 128:256], a1[:, kc, :], xT[:, 4 + kc, :], kc == 0, kc == 3, ["a1", K("xT3")], pk)
            for kc in range(4):
                self.mm(ps[:, 256:384], g1[:, kc, :], xT[:, 8 + kc, :], kc == 0, kc == 3, ["g1", K("xT3")], pk)
            self.act(hT[0:64, 0:128], ps[0:64, 0:128], AF.Tanh, [pk], [K("hT")])
            self.act(hT[0:64, 128:256], ps[0:64, 128:256], AF.Identity, [pk], [K("hT")])
            self.act(hT[:, 256:384], ps[:, 256:384], AF.Sigmoid, [pk], [K("hT")])
            ps, pk = self.nps()
            self.mm(ps[:, :], hT[0:64, 0:128], w2[:, :], True, True, [K("hT"), "w2"], pk)
            self.tt("dve", sw[:], ps[:, :], W0, ALU.add, [pk, "rkv"], [K("sw")])
            self.act(sw[:], sw[:], AF.Sigmoid, [K("sw")], [K("sw")])
            ps, pk = self.nps()
            self.mm(ps[:, :], hT[0:64, 128:256], a2[:, :], True, True, [K("hT"), "a2"], pk)
            self.tt("dve", a[:], ps[:, :], A0, ALU.add, [pk, "rkv"], [K("a")])
            self.act(a[:], a[:], AF.Sigmoid, [K("a")], [K("a")])
            ps, pk = self.nps()
            self.mm(ps[:, :], hT[:, 256:384], g2[:, :], True, True, [K("hT"), "g2"], pk)
            self.cp("act", g[:], ps[:, :], [pk], [K("g")])
            if self.cut == 2:
                return
            self.tt("pool", kk[:], k, KK_, ALU.mult, [K("X6_1"), "rkv"], [K("kk")])
            self.tt("pool", tmp[:], kk[:], kk[:], ALU.mult, [K("kk")], [K("tmp")])
            self.red("dve", ss[:], tmp[:].rearrange("p (h n) -> p h n", h=8), ALU.add, [K("tmp")], [K("ss")])
            self.rsqrt(ss[:], ss[:], 1e-24, [K("ss")], [K("ss")])
            kk3 = kk[:].rearrange("p (h n) -> p h n", h=8)
            self.tt("dve", kk3, kk3, ss[:].unsqueeze(2).to_broadcast([128, 8, 64]), ALU.mult, [K("kk"), K("ss")], [K("kk")])
            self.stt("pool", tmp[:], a[:], -1.0, KA_, ALU.add, ALU.mult, [K("a"), "rkv", K("tmp")], [K("tmp")])
            self.stt("pool", kp[:], tmp[:], 1.0, k, ALU.add, ALU.mult, [K("tmp"), K("X6_1")], [K("kp")])
            self.tt("dve", tmp[:], r, kp[:], ALU.mult, [K("X6_0"), K("kp")], [K("tmp")])
            self.tt("dve", tmp[:], tmp[:], RKk, ALU.mult, [K("tmp"), "rkv"], [K("tmp")])
            self.red("dve", ss[:], tmp[:].rearrange("p (h n) -> p h n", h=8), ALU.add, [K("tmp")], [K("ss")])
            self.tt("dve", bon[:].rearrange("p (h n) -> p h n", h=8), v.rearrange("p (h n) -> p h n", h=8),
                    ss[:].unsqueeze(2).to_broadcast([128, 8, 64]), ALU.mult, [K("X6_2"), K("ss")], [K("bon")])
            self.cp("pool", Vt[:], v, [K("X6_2")], [K("Vt")])
            if self.cut == 3:
                return
            self.cp("pool", swh[:, 0, :], sw[:], [K("sw")], [K("swh")])
            self.tt("pool", tmp[:], sw[:], swh[:, 0, :], ALU.subtract, [K("sw"), K("swh"), K("tmp")], [K("tmp")])
            self.cp("pool", swh[:, 1, :], tmp[:], [K("tmp")], [K("swh")])
            ps, pk = self.nps()
            self.mm(ps[:, :], trib[:], swh[:, 0, :], True, False, ["trib", K("swh")], pk)
            self.mm(ps[:, :], trib[:], swh[:, 1, :], False, True, ["trib", K("swh")], pk)
            self.cp("dve", cs[:], ps[:, :], [pk], [K("cs")])
            self.act(e3[:, 0, :], cs[:], AF.Exp, [K("cs")], [K("e3")], scale=-RK_C)
            self.act(e3[:, 2, :], cs[:], AF.Exp, [K("cs")], [K("e3")], scale=RK_C)
            self.tt("dve", cs[:], cs[:], sw[:], ALU.subtract, [K("cs"), K("sw")], [K("cs")])
            self.act(e3[:, 1, :], cs[:], AF.Exp, [K("cs")], [K("e3")], scale=-RK_C)
            self.tt("dve", T4[:, 0, :], kk[:], e3[:, 1, :], ALU.mult, [K("kk"), K("e3")], [K("T4")])
            self.tt("pool", T4[:, 1, :], r, e3[:, 0, :], ALU.mult, [K("X6_0"), K("e3")], [K("T4")])
            self.tt("dve", tmp[:], kk[:], a[:], ALU.mult, [K("kk"), K("a"), K("tmp")], [K("tmp")])
            self.tt("dve", T4[:, 2, :], tmp[:], e3[:, 2, :], ALU.mult, [K("tmp"), K("e3")], [K("T4")])
            self.tt("pool", T4[:, 3, :], kp[:], e3[:, 2, :], ALU.mult, [K("kp"), K("e3")], [K("T4")])
            self.cp("act", Bt[:], T4[:, 2, :], [K("T4")], [K("Bt")])
            self.cp("act", Kt[:], T4[:, 3, :], [K("T4")], [K("Kt")])
            if self.cut == 4:
                d4 = self.dscr("dbgT4", [128, 2048])
                d3 = self.dscr("dbge3", [128, 1536])
                dsw = self.dscr("dbgsw", [128, 512])
                c.dma("sp", d4[:, :], T4[:].rearrange("p i n -> p (i n)"), reads=[K("T4")], writes=["dbgT4"])
                c.dma("sp", d3[:, :], e3[:].rearrange("p i n -> p (i n)"), reads=[K("e3")], writes=["dbge3"])
                c.dma("sp", dsw[:, :], sw[:], reads=[K("sw")], writes=["dbgsw"])
                return
            ART4 = ART[:].rearrange("p j (a t) -> p j a t", a=2)
            self.transpose_in(ART4[:, :, 0, :], T4[:, 0, :], 4, K("T4"), K("ART"))
            self.transpose_in(ART4[:, :, 1, :], T4[:, 1, :], 4, K("T4"), K("ART"))
            self.transpose_in(BT, T4[:, 2, :], 4, K("T4"), K("BT"))
            self.transpose_in(KT, T4[:, 3, :], 4, K("T4"), K("KT"))
            if self.cut in (45, 46):
                return
            self.transpose_in(ET, e3[:, 0, :], 4, K("e3"), K("ET"))
            if self.cut == 5:
                return
            for h in range(8):
                j, po = h // 2, (h % 2) * 64
                ps, pk = self.nps()
                self.mm(ps[:, 0:256], BT[po:po + 64, j, :], ART[po:po + 64, j, :], True, True, [K("BT"), K("ART")], pk)
                self.mm(ps[:, 256:512], KT[po:po + 64, j, :], ART[po:po + 64, j, :], True, True, [K("KT"), K("ART")], pk)
                self.tt("dve", G[:, h, :], ps[:, :], mask4[:], ALU.mult, [pk, "mask4"], [K("G%d" % h)])
            for par in range(2):
                ps, pk = self.nps()
                for hh in range(4):
                    h = 2 * hh + par
                    j, po = h // 2, par * 64
                    self.mm(ps[:, hh * 128:(hh + 1) * 128], ART[po:po + 64, j, 0:128], BT[po:po + 64, j, :], True, True,
                            [K("BT"), K("ART")], pk)
                self.tt("dve", Qm[:, par:8:2, :], ps[:, :].rearrange("p (h t) -> p h t", h=4),
                        maskL[:].unsqueeze(1).to_broadcast([128, 4, 128]), ALU.mult, [pk, "maskL"], [K("Q")])
            for h in range(8):
                self.tt("pool", Wm[:, h, :], identb[:], G[:, h, 0:128], ALU.subtract, ["identb", K("G%d" % h)], [K("W")])
            Pg = [None, None]
            for lvl in range(1, 6):
                for hq in range(2):
                    hsl = slice(hq * 4, (hq + 1) * 4)
                    psq, pkq = self.nps()
                    psp, pkp = (self.nps() if lvl < 5 else (None, None))
                    for hh in range(4):
                        h = hq * 4 + hh
                        Pc = G[:, h, 0:128] if Pg[hq] is None else Pm[:, h, :]
                        pkey = K("G%d" % h) if Pg[hq] is None else K("Pmm")
                        csl = slice(hh * 128, (hh + 1) * 128)
                        self.mm(psq[:, csl], Pc, Qm[:, h, :], True, True, [pkey, K("Q")], pkq)
                        if lvl < 5:
                            self.mm(psp[:, csl], Qm[:, h, :], Pc, True, True, [pkey, K("Q")], pkp)
                    self.cp("act", Qm[:, hsl, :], psq[:, :].rearrange("p (h t) -> p h t", h=4), [pkq], [K("Q")])
                    if lvl < 5:
                        self.cp("dve", Pm[:, hsl, :], psp[:, :].rearrange("p (h t) -> p h t", h=4), [pkp], [K("Pmm")])
                        Pg[hq] = 1
                for hq in range(2):
                    hsl = slice(hq * 4, (hq + 1) * 4)
                    psw, pkw = self.nps()
                    for hh in range(4):
                        h = hq * 4 + hh
                        self.mm(psw[:, hh * 128:(hh + 1) * 128], Qm[:, h, :], Wm[:, h, :], True, True, [K("Q"), K("W")], pkw)
                    self.tt("dve", Wm[:, hsl, :], Wm[:, hsl, :], psw[:, :].rearrange("p (h t) -> p h t", h=4), ALU.add,
                            [pkw, K("W")], [K("W")])
            if self.cut == 6:
                return
            for cc in range(2):
                self.tt("pool", bdg[cc][:], bd[:].unsqueeze(1).to_broadcast([128, 4, 128]),
                        ET[:, :, cc * 64 + 63:cc * 64 + 64].to_broadcast([128, 4, 128]), ALU.mult, ["bd", K("ET")], [K("bdg%d" % cc)])
            self.cp("pool", Vm[0][0:64, :], v[0:64, :], [K("X6_2")], [K("Vm0")])
            self.cp("pool", Vm[1][64:128, :], v[64:128, :], [K("X6_2")], [K("Vm1")])
            for cc in range(2):
                q0 = cc * 64
                rs = slice(q0, q0 + 64)
                Ubc, Vtc = Ub[cc], Vm[cc]
                ku, kv = K("Ub%d_" % cc), K("Vm%d" % cc)
                psX, pkX = self.nps()
                for j in range(4):
                    self.mm(psX[:, j * 128:(j + 1) * 128], ART[:, j, 0:128], Hb[:, j, :], True, False, [K("ART"), "Hb"], pkX)
                    for h in (2 * j, 2 * j + 1):
                        hs = slice(h * 64, h * 64 + 64)
                        self.mm(psX[:, hs], G[:, h, 256:384], Vt[:, hs], False, h == 2 * j + 1, [K("G%d" % h), K("Vt")], pkX)
                self.ts("dve", Xs[rs, :], psX[rs, :], -1.0, ALU.mult, [pkX], [K("Xs")])
                psU, pkU = self.nps()
                for h in range(8):
                    hs = slice(h * 64, h * 64 + 64)
                    self.mm(psU[:, hs], Wm[:, h, :], Xs[:, hs], True, True, [K("W"), K("Xs")], pkU)
                self.cp("act", Ubc[rs, :], psU[rs, :], [pkU], [ku])
                psO, pkO = self.nps()
                for j in range(4):
                    self.mm(psO[:, j * 128:(j + 1) * 128], ART[:, j, 128:256], Hb[:, j, :], True, False, [K("ART"), "Hb"], pkO)
                    for h in (2 * j, 2 * j + 1):
                        hs = slice(h * 64, h * 64 + 64)
                        self.mm(psO[:, hs], G[:, h, 128:256], Ubc[:, hs], False, False, [K("G%d" % h), ku], pkO)
                        self.mm(psO[:, hs], G[:, h, 384:512], Vt[:, hs], False, h == 2 * j + 1, [K("G%d" % h), K("Vt")], pkO)
                self.cp("act", Os[rs, :], psO[rs, :], [pkO], [K("Os")])
                psH, pkH = self.nps()
                for j in range(4):
                    js = slice(j * 128, (j + 1) * 128)
                    self.mm(psH[:, js], Bt[:, js], Ubc[:, js], True, False, [K("Bt"), ku], pkH)
                    self.mm(psH[:, js], Kt[:, js], Vtc[:, js], False, True, [K("Kt"), kv], pkH)
                H2 = H[:].rearrange("p j v -> p (j v)")
                self.tt("dve", H2, H2, psH[:, :], ALU.add, [pkH, "H"], ["H"])
                self.tt("dve", H[:], H[:], bdg[cc][:], ALU.mult, ["H", K("bdg%d" % cc)], ["H"])
                self.cp("act", Hb[:].rearrange("p j v -> p (j v)"), H2, ["H"], ["Hb"])
            O3 = Os[:].rearrange("p (h n) -> p h n", h=8)
            self.red("dve", sq[:], O3, ALU.add, [K("Os")], [K("sq")])
            self.ts("dve", sq[:], sq[:], 1.0 / 64, ALU.mult, [K("sq")], [K("sq")])
            self.tt("dve", O3, O3, sq[:].unsqueeze(2).to_broadcast([128, 8, 64]), ALU.subtract, [K("Os"), K("sq")], [K("Os")])
            self.tt("pool", tmp2[:], Os[:], Os[:], ALU.mult, [K("Os")], [K("tq")])
            self.red("dve", sq[:], tmp2[:].rearrange("p (h n) -> p h n", h=8), ALU.add, [K("tq")], [K("sq")])
            self.rsqrt(sq[:], sq[:], 64e-5, [K("sq")], [K("sq")], scale=1.0 / 64)
            self.tt("dve", O3, O3, sq[:].unsqueeze(2).to_broadcast([128, 8, 64]), ALU.mult, [K("Os"), K("sq")], [K("Os")])
            self.tt("pool", Os[:], Os[:], LNG, ALU.mult, [K("Os"), "rkv"], [K("Os")])
            self.tt("pool", Os[:], Os[:], LNB, ALU.add, [K("Os"), "rkv"], [K("Os")])
            self.tt("dve", Os[:], Os[:], bon[:], ALU.add, [K("Os"), K("bon")], [K("Os")])
            self.tt("dve", oo[:], Os[:], g[:], ALU.mult, [K("Os"), K("g")], [K("oo")])
            c.dma("sp", oab[r0:r0 + 128, 0:512], oo[:], reads=[K("oo")], writes=["oab"])
        c.barrier()


B.stage_rwkv = stage_rwkv


def stage_nsa(self, p0, oab, W):
    c = self.c
    S, NT = self.S, self.NT
    n_cmp = (S - 32) // 16 + 1
    NCT = (n_cmp + 127) // 128
    NCP = NCT * 128
    NQ = S // 512
    QC, KC0, GC0 = 2048, 2560, 3328
    with ExitStack() as st:
        sb = lambda n, shp, dt=F32: self.sb(st, n, shp, dt)
        identb = sb("identb", [128, 128], BF16)
        c.dma("pool", identb[:], W["c_ident"][:, :], writes=["identb"])
        KcT = sb("KcT", [128, NCP], BF16)
        Vca = sb("Vca", [128, NCT, 2, 193], BF16)
        c.op("pool", lambda e: e.memset(KcT[:], 0.0), writes=["KcT"])
        c.op("pool", lambda e: e.memset(Vca[:], 0.0), writes=["Vca"])
        with ExitStack() as st2:
            sb2 = lambda n, shp, dt=F32: self.sb(st2, n, shp, dt)
            kvT = sb2("kvT", [128, 2, S], BF16)
            W1p = sb2("W1p", [128, 2, 2, 32, 128], BF16)
            c.op("pool", lambda e: e.memset(W1p[:].rearrange("p a g l h -> p (a g l h)"), 0.0), writes=["W1p"])
            for kv in range(2):
                for g in range(2):
                    c.dma("pool", W1p[g * 64:(g + 1) * 64, kv, g, :, :],
                          W["ns_c_w1"][kv].rearrange("(l d) h -> d l h", d=64), writes=["W1p"])
            w2k = sb2("w2k", [128, 2, 128], BF16)
            c.op("pool", lambda e: e.memset(w2k[:].rearrange("p g n -> p (g n)"), 0.0), writes=["w2k"])
            for g in range(2):
                c.dma("pool", w2k[:, g, g * 64:(g + 1) * 64], W["ns_c_w2"][0], writes=["w2k"])
            w2v = sb2("w2v", [128, 64], BF16)
            c.dma("pool", w2v[:], W["ns_c_w2"][1], writes=["w2v"])
            peT = sb2("peT", [128, 2, 32], BF16)
            c.dma("pool", peT[:].rearrange("p a l -> p (a l)"), W["c_peT"][:, :], writes=["peT"])
            ropeA = sb2("ropeA", [128, 16])
            pa = [sb2("pa", [128, 256]) for _ in range(2)]
            pr = [sb2("pra", [128, 256]) for _ in range(2)]
            tA = [sb2("tA", [128, 2, 8]) for _ in range(2)]
            for t in range(NT):
                b = t % 2
                r0 = t * 128
                c.dma("sp", pa[b][:], p0[r0:r0 + 128, KC0:KC0 + 256], reads=["p0"], writes=["pa%d" % b])
                c.dma("sp", ropeA[:], W["c_rope"][r0:r0 + 128, :], writes=["ropeA"])
                self.cp("pool", pr[b][:], pa[b][:], ["pa%d" % b], ["pra%d" % b])
                self._rope(pa[b][:, 0:128], pr[b][:, 0:128], 2, ropeA, tA[b], "pa%d" % b, "pra%d" % b, "ropeA", "tA%d" % b)
                self.transpose_in(kvT[:, :, r0:r0 + 128], pr[b], 2, "pra%d" % b, "kvT")
            hT = sb2("hTc", [128, NCP], BF16)
            c.op("pool", lambda e: e.memset(hT[:], 0.0), writes=["hTc"])
            bia = sb2("bia", [128, 1])
            xh = sb2("xh", [128, 512])
            x2 = sb2("x2", [128, 512])
            for kv in range(2):
                for g in range(2):
                    ps, pk = self.nps()
                    for l in range(32):
                        self.mm(ps[:, 0:1], W1p[:, kv, g, l, :], peT[:, kv, l:l + 1], l == 0, l == 31, ["W1p", "peT"], pk)
                    self.cp("dve", bia[:], ps[:, 0:1], [pk], ["bia"])
                    ps, pk = self.nps()
                    for l in range(32):
                        self.mm(ps[:, 0:n_cmp], W1p[:, kv, g, l, :], kvT[:, kv, l:l + 16 * (n_cmp - 1) + 1:16], l == 0, l == 31,
                                ["W1p", "kvT"], pk)
                    X = xh[:, 0:n_cmp]
                    Y = x2[:, 0:n_cmp]
                    self.act(X, ps[:, 0:n_cmp], AF.Identity, [pk, "bia"], ["xh"], bias=bia[:, 0:1])
                    self.tt("dve", Y, X, X, ALU.mult, ["xh"], ["x2"])
                    self.ts("dve", Y, Y, 0.044715, ALU.mult, ["x2"], ["x2"], s2=1.0, op1=ALU.add)
                    self.tt("dve", Y, Y, X, ALU.mult, ["x2", "xh"], ["x2"])
                    self.act(Y, Y, AF.Tanh, ["x2"], ["x2"], scale=0.7978845608)
                    self.stt("dve", hT[:, 0:n_cmp], Y, 1.0, X, ALU.add, ALU.mult, ["x2", "xh"], ["hTc"])
                    if kv == 0:
                        ps, pk = self.nps()
                        self.mm(ps[:, 0:n_cmp], w2k[:, g, :], hT[:, 0:n_cmp], True, True, ["w2k", "hTc"], pk)
                        if g == 0:
                            self.act(KcT[:, 0:n_cmp], ps[:, 0:n_cmp], AF.Identity, [pk], ["KcT"], scale=0.5)
                        else:
                            self.stt("dve", KcT[:, 0:n_cmp], ps[:, 0:n_cmp], 0.5, KcT[:, 0:n_cmp], ALU.mult, ALU.add, [pk, "KcT"], ["KcT"])
                    else:
                        for i in range(NCT):
                            ps, pk = self.nps()
                            self.mm(ps[:, 0:64], hT[:, i * 128:(i + 1) * 128], w2v[:], True, True, ["w2v", "hTc"], pk)
                            self.act(Vca[:, i, g, 0:64], ps[:, 0:64], AF.Identity, [pk], ["Vca"], scale=0.5)
            for i in range(NCT):
                for g in range(2):
                    c.dma("pool", Vca[:, i, g, 65:193], W["c_ov"][i * 128:(i + 1) * 128, :], writes=["Vca"])
                    c.dma("pool", Vca[:, i, g, 64:65], W["c_ones"][:, 0:1], writes=["Vca"])
            c.barrier()
        QT = sb("QT", [128, 4, S], BF16)
        KT2 = sb("KT2", [128, 2, S], BF16)
        Va = sb("Va", [128, NT, 2, 2, 65], BF16)
        Eo = sb("Eo", [128, S], BF16)
        c.dma("pool", Eo[:], W["c_E"][:, :], writes=["Eo"])
        caus = sb("caus", [128, 4, 512], BF16)
        winb = sb("winb", [128, 8, 512], BF16)
        cmpb = sb("cmpb", [128, 5, 512], BF16)
        c.dma("pool", caus[:].rearrange("p a q -> p (a q)"), W["c_caus"][:, :], writes=["caus"])
        c.dma("pool", winb[:].rearrange("p a q -> p (a q)"), W["c_win"][:, :], writes=["winb"])
        c.dma("pool", cmpb[:].rearrange("p a q -> p (a q)"), W["c_cmpb"][:, :], writes=["cmpb"])
        c.op("pool", lambda e: e.memset(Va[:].rearrange("p t a g d -> p (t a g d)"), 1.0), writes=["Va"])
        with ExitStack() as st2:
            sb2 = lambda n, shp, dt=F32: self.sb(st2, n, shp, dt)
            ropeB = sb2("ropeB", [128, 16])
            pn = [sb2("pn", [128, 1280]) for _ in range(2)]
            qp = [sb2("qp", [128, 512]) for _ in range(2)]
            kp = [sb2("kpn", [128, 256]) for _ in range(2)]
            tB = [sb2("tB", [128, 8, 8]) for _ in range(2)]
            for t in range(NT):
                b = t % 2
                r0 = t * 128
                kn, kq, kk_ = "pn%d" % b, "qp%d" % b, "kpn%d" % b
                c.dma("sp", pn[b][:], p0[r0:r0 + 128, QC:QC + 1280], reads=["p0"], writes=[kn])
                c.dma("sp", ropeB[:], W["c_rope"][r0:r0 + 128, :], writes=["ropeB"])
                qsrc = pn[b][:, 0:512].rearrange("p (g j d) -> p g j d", g=2, j=4)
                qdst = qp[b][:].rearrange("p (j g d) -> p g j d", g=2, j=4)
                self.cp("pool", qdst, qsrc, [kn], [kq])
                self._rope(qsrc, qdst, 8, ropeB, tB[b], kn, kq, "ropeB", "tB%d" % b, four=True)
                for a in range(2):
                    o = 512 + 256 * (a + 1)
                    self.cp("pool", kp[b][:, a * 128:(a + 1) * 128], pn[b][:, o:o + 128], [kn], [kk_])
                    self._rope(pn[b][:, o:o + 128], kp[b][:, a * 128:(a + 1) * 128], 2, ropeB, tB[b], kn, kk_, "ropeB", "tB%d" % b)
                    self.cp("act", Va[:, t, a, :, 0:64], pn[b][:, o + 128:o + 256].rearrange("p (g d) -> p g d", g=2), [kn], ["Va"])
                self.transpose_in(QT[:, :, r0:r0 + 128], qp[b], 4, kq, "QT")
                self.transpose_in(KT2[:, :, r0:r0 + 128], kp[b], 2, kk_, "KT2")
            c.barrier()
        Qh = [[sb("Qh", [128, 512], BF16) for _ in range(2)] for _ in range(2)]
        for g in range(2):
            for k_ in range(2):
                c.op("pool", lambda e: e.memset(Qh[g][k_][:], 0.0), writes=["Qh%d_%d" % (g, k_)])
        qh_i = [0]
        qcur = [None, None]
        PT = [sb("PT", [128, 512], BF16) for _ in range(3)]
        MbT = [sb("MbT", [128, 512], BF16) for _ in range(2)]
        acc = [sb("acc", [128, 512]) for _ in range(4)]
        imp = [[sb("imp", [128, 128]) for _ in range(4)] for _ in range(2)]
        sig = [sb("sig", [128, 24]) for _ in range(4)]
        selF = [sb("selF", [128, 128]) for _ in range(4)]
        rz4 = [sb("rz", [128, 2]) for _ in range(4)]
        ot4 = [sb("oto", [128, 193]) for _ in range(4)]
        m8 = sb("m8", [128, 16])
        pri = sb("pri", [128, 128])
        pri2 = sb("pri2", [128, 128])
        mb = sb("mb", [128, 128])
        SPS = [(self.ps[i], "ps%d" % i) for i in range(4)]
        APS = [(self.ps[4 + i], "ps%d" % (4 + i)) for i in range(4)]
        sps_i = [0]
        pt_i = [0]

        pending = []

        def flush_pv():
            while pending:
                P, pkey, vaug, nv, subs = pending.pop(0)
                for (sub, first, last) in subs:
                    aps, apk = APS[sub]
                    self.mm(aps[:, 0:nv], P[:, sub * 128:(sub + 1) * 128], vaug, first, last, [pkey, "Va", "Vca"], apk)

        def unit(h, Q, kT, kcols, bias_terms, vaug, nv, subs, started):
            g = h // 4
            ps, pk = SPS[sps_i[0] % 4]
            sps_i[0] += 1
            nb = len(bias_terms)
            self.mm(ps[:, :], kT, qcur[0][:], True, nb == 0, ["KcT", "KT2", qcur[1]], pk)
            for bi, (lt, rt, rk) in enumerate(bias_terms):
                self.mm(ps[:, :], lt, rt, False, bi == nb - 1, rk, pk)
            P = PT[pt_i[0] % 3]
            pkey = "PT%d" % (pt_i[0] % 3)
            pt_i[0] += 1
            self.act(P[:], ps[:, :], AF.Exp, [pk], [pkey], scale=0.125)
            flush_pv()
            pending.append((P, pkey, vaug, nv, subs))

        def finish_branch(h, br, nv, g=None, hh=None):
            hs = slice(h * 64, (h + 1) * 64)
            for sub in range(4):
                aps, apk = APS[sub]
                self.cp("dve", ot4[sub][:, 0:nv], aps[:, 0:nv], [apk], ["oto%d" % sub])
            for sub in range(4):
                ot = ot4[sub]
                ko, kr = "oto%d" % sub, "rz%d" % sub
                rz = rz4[sub]
                self.ts("dve", rz[:, 0:1], ot[:, 64:65], 1e-30, ALU.max, [ko], [kr])
                c.op("dve", lambda e: e.reciprocal(out=rz[:, 0:1], in_=rz[:, 0:1]), reads=[kr], writes=[kr])
                self.tt("dve", rz[:, 1:2], rz[:, 0:1], sig[sub][:, br * 8 + h:br * 8 + h + 1], ALU.mult, [kr, "sig%d" % sub], [kr])
                if br == 0:
                    self.ts("dve", acc[sub][:, hs], ot[:, 0:64], rz[:, 1:2], ALU.mult, [ko, kr], ["acc%d" % sub])
                    if hh == 0:
                        self.ts("dve", imp[g][sub][:], ot[:, 65:193], rz[:, 0:1], ALU.mult, [ko, kr], ["imp%d%d" % (g, sub)])
                    else:
                        self.stt("dve", imp[g][sub][:], ot[:, 65:193], rz[:, 0:1], imp[g][sub][:], ALU.mult, ALU.add,
                                 [ko, kr, "imp%d%d" % (g, sub)], ["imp%d%d" % (g, sub)])
                else:
                    self.stt("dve", acc[sub][:, hs], ot[:, 0:64], rz[:, 1:2], acc[sub][:, hs], ALU.mult, ALU.add,
                             [ko, kr, "acc%d" % sub], ["acc%d" % sub])

        for Q in range(NQ):
            q0 = Q * 512
            for sub in range(4):
                r0 = q0 + sub * 128
                c.dma("sp", sig[sub][:], p0[r0:r0 + 128, GC0:GC0 + 24], reads=["p0"], writes=["sig%d" % sub])
                self.act(sig[sub][:], sig[sub][:], AF.Sigmoid, ["sig%d" % sub], ["sig%d" % sub])
                c.dma("sp", selF[sub][:], W["c_selF"][r0:r0 + 128, :], writes=["selF%d" % sub])
            for g in range(2):
                for hh in range(4):
                    h = g * 4 + hh
                    k_ = qh_i[0] % 2
                    qh_i[0] += 1
                    qcur[0], qcur[1] = Qh[g][k_], "Qh%d_%d" % (g, k_)
                    self.cp("pool", Qh[g][k_][g * 64:(g + 1) * 64, :], QT[g * 64:(g + 1) * 64, hh, q0:q0 + 512], ["QT"], [qcur[1]])
                    started = [False] * 4
                    for i in range(NCT):
                        dj = Q - 4 * i
                        if dj < 0:
                            continue
                        bt = [] if dj > 4 else [(identb[:], cmpb[:, dj, :], ["identb", "cmpb"])]
                        imax = min(NCT - 1, Q // 4)
                        unit(h, Q, KcT[:, i * 128:(i + 1) * 128], None, bt, Vca[:, i, g, :], 193,
                             [(s_, i == 0, i == imax) for s_ in range(4)], started)
                    flush_pv()
                    finish_branch(h, 0, 193, g, hh)
                psM, pkM = SPS[sps_i[0] % 4]
                sps_i[0] += 1
                for sub in range(4):
                    self.tt("dve", pri[:], imp[g][sub][:], selF[sub][:], ALU.add, ["imp%d%d" % (g, sub), "selF%d" % sub], ["pri"])
                    c.op("dve", lambda e: e.max(out=m8[:, 0:8], in_=pri[:]), reads=["pri"], writes=["m8"])
                    c.op("dve", lambda e: e.match_replace(out=pri2[:], in_to_replace=m8[:, 0:8], in_values=pri[:], imm_value=-1e9),
                         reads=["pri", "m8"], writes=["pri2"])
                    c.op("dve", lambda e: e.max(out=m8[:, 8:16], in_=pri2[:]), reads=["pri2"], writes=["m8"])
                    self.ts("dve", mb[:], pri[:], m8[:, 15:16], ALU.is_ge, ["pri", "m8"], ["mb"])
                    self.ts("dve", mb[:], mb[:], -1.0, ALU.add, ["mb"], ["mb"], s2=-NEG, op1=ALU.mult)
                    self.tr(psM[:, sub * 128:(sub + 1) * 128], mb[:], self.ident[:], ["mb", "ident"], pkM)
                self.cp("act", MbT[g][:], psM[:, :], [pkM], ["MbT%d" % g])
                for hh in range(4):
                    h = g * 4 + hh
                    k_ = qh_i[0] % 2
                    qh_i[0] += 1
                    qcur[0], qcur[1] = Qh[g][k_], "Qh%d_%d" % (g, k_)
                    self.cp("pool", Qh[g][k_][g * 64:(g + 1) * 64, :], QT[g * 64:(g + 1) * 64, hh, q0:q0 + 512], ["QT"], [qcur[1]])
                    started = [False] * 4
                    for kt in range(0, 4 * Q + 4):
                        d = kt - 4 * Q
                        bt = [(Eo[:, kt * 128:(kt + 1) * 128], MbT[g][:], ["Eo", "MbT%d" % g])]
                        if d >= 0:
                            bt.append((identb[:], caus[:, d, :], ["identb", "caus"]))
                        subs = [(s_, kt == 0, kt == 4 * Q + s_) for s_ in range(4) if s_ >= d]
                        unit(h, Q, KT2[:, 0, kt * 128:(kt + 1) * 128], None, bt, Va[:, kt, 0, g, :], 65, subs, started)
                    flush_pv()
                    finish_branch(h, 1, 65)
                    started = [False] * 4
                    for kt in range(max(0, 4 * Q - 4), 4 * Q + 4):
                        d = kt - 4 * Q
                        bt = [(identb[:], winb[:, d + 4, :], ["identb", "winb"])]
                        subs = [(s_, kt == max(0, 4 * Q + s_ - 4), kt == 4 * Q + s_) for s_ in range(4) if s_ - 4 <= d <= s_]
                        unit(h, Q, KT2[:, 1, kt * 128:(kt + 1) * 128], None, bt, Va[:, kt, 1, g, :], 65, subs, started)
                    flush_pv()
                    finish_branch(h, 2, 65)
            for sub in range(4):
                r0 = q0 + sub * 128
                c.dma("sp", oab[r0:r0 + 128, 512:1024], acc[sub][:], reads=["acc%d" % sub], writes=["oab"])
        c.barrier()


def _rope(self, src, dst, nh, rope, tmp, ksrc, kdst, krope, ktmp, four=False):
    if four:
        s4, d4 = src, dst
        x1, x2 = s4[:, :, :, 0:8], s4[:, :, :, 8:16]
        o1, o2 = d4[:, :, :, 0:8], d4[:, :, :, 8:16]
        cos = rope[:, 0:8].unsqueeze(1).unsqueeze(1).to_broadcast([128, 2, 4, 8])
        sin = rope[:, 8:16].unsqueeze(1).unsqueeze(1).to_broadcast([128, 2, 4, 8])
        tm = tmp[:].rearrange("p (g j) d -> p g j d", g=2)
    else:
        s3 = src.rearrange("p (h d) -> p h d", h=nh)
        d3 = dst.rearrange("p (h d) -> p h d", h=nh)
        x1, x2 = s3[:, :, 0:8], s3[:, :, 8:16]
        o1, o2 = d3[:, :, 0:8], d3[:, :, 8:16]
        cos = rope[:, 0:8].unsqueeze(1).to_broadcast([128, nh, 8])
        sin = rope[:, 8:16].unsqueeze(1).to_broadcast([128, nh, 8])
        tm = tmp[:, 0:nh, :]
    self.tt("dve", o1, x1, cos, ALU.mult, [ksrc, krope], [kdst])
    self.tt("dve", tm, x2, sin, ALU.mult, [ksrc, krope], [ktmp])
    self.tt("dve", o1, o1, tm, ALU.subtract, [kdst, ktmp], [kdst])
    self.tt("dve", o2, x2, cos, ALU.mult, [ksrc, krope], [kdst])
    self.tt("dve", tm, x1, sin, ALU.mult, [ksrc, krope], [ktmp])
    self.tt("dve", o2, o2, tm, ALU.add, [kdst, ktmp], [kdst])


B.stage_nsa = stage_nsa
B._rope = _rope


def ln_tile(self, z, zkey, lnp, out, okey, tl):
    s1, zc, sq = tl
    stats, mv, rstd, nb = s1[:, 0:12], s1[:, 12:14], s1[:, 14:15], s1[:, 15:16]
    for i in range(2):
        self.c.op("dve", lambda e: e.bn_stats(out=s1[:, i * 6:(i + 1) * 6], in_=z[:, i * 512:(i + 1) * 512]),
                  reads=[zkey], writes=["ln_st"])
    self.c.op("dve", lambda e: e.bn_aggr(out=mv, in_=stats), reads=["ln_st"], writes=["ln_mv"])
    self.rsqrt(rstd, mv[:, 1:2], LN_EPS, ["ln_mv"], ["ln_rs"])
    self.stt("dve", nb, mv[:, 0:1], -1.0, rstd, ALU.mult, ALU.mult, ["ln_mv", "ln_rs"], ["ln_nb"])
    self.act(zc[:], z, AF.Identity, [zkey, "ln_rs", "ln_nb"], ["ln_zc"], bias=nb, scale=rstd)
    self.tt("dve", zc[:], zc[:], lnp[:, 0:D], ALU.mult, ["ln_zc", "lnp"], ["ln_zc"])
    self.tt("dve", out, zc[:], lnp[:, D:2 * D], ALU.add, ["ln_zc", "lnp"], [okey])


def stage_mix(self, src, K, w_ap, resid, lnp_ap, dst):
    c = self.c
    with ExitStack() as st:
        sb = lambda n, shp, dt=F32: self.sb(st, n, shp, dt)
        wb = sb("wmix", [128, K // 128, D], BF16)
        self.load_w_fast(st, wb, w_ap, K, D, "wmix")
        lnp = sb("lnp", [128, 2 * D])
        c.dma("sp", lnp[:], lnp_ap[:, :], writes=["lnp"])
        tl = (sb("ln_s", [128, 16]), sb("ln_zc", [128, D]), None)
        xin = [sb("min", [128, K]) for _ in range(2)]
        xT = [sb("mxT", [128, K // 128, 128], BF16) for _ in range(2)]
        rs = [sb("mrs", [128, D]) for _ in range(2)]
        z = [sb("mz", [128, D]) for _ in range(2)]
        o = [sb("mo", [128, D]) for _ in range(2)]
        for t in range(self.NT):
            b = t % 2
            r0 = t * 128
            c.dma("sp", xin[b][:], src[r0:r0 + 128, :], reads=[src.tensor.name], writes=["min%d" % b])
            c.dma("sp", rs[b][:], resid[r0:r0 + 128, :], reads=[resid.tensor.name], writes=["mrs%d" % b])
            self.transpose_in(xT[b], xin[b], K // 128, "min%d" % b, "mxT%d" % b)
            for half in range(2):
                ps, pk = self.nps()
                for kc in range(K // 128):
                    self.mm(ps[:, :], xT[b][:, kc, :], wb[:, kc, half * 512:(half + 1) * 512], kc == 0, kc == K // 128 - 1,
                            ["mxT%d" % b, "wmix"], pk)
                self.stt("dve", z[b][:, half * 512:(half + 1) * 512], rs[b][:, half * 512:(half + 1) * 512], ALPHA, ps[:, :],
                         ALU.mult, ALU.add, [pk, "mrs%d" % b], ["mz%d" % b])
            self.ln_tile(z[b][:], "mz%d" % b, lnp, o[b][:], "mo%d" % b, tl)
            c.dma("sp", dst[r0:r0 + 128, :], o[b][:], reads=["mo%d" % b], writes=[dst.tensor.name])
        c.barrier()


def moe_cap(S):
    m = (S // 8) * 3 // 2
    return ((m + 511) // 512) * 512


def stage_moe(self, xin, W, layer, lnp_ap, dst, toklist, ybuf):
    c = self.c
    S, NT = self.S, self.NT
    CAP = moe_cap(S)
    NG = CAP // 512
    with ExitStack() as st:
        sb = lambda n, shp, dt=F32: self.sb(st, n, shp, dt)
        slotAB = sb("slotAB", [128, NT, 2], I32)
        wAB = sb("wAB", [128, NT, 2])
        tokid = sb("tokid", [128, NT, 16], I32)
        c.dma("sp", tokid[:].rearrange("p t r -> p (t r)"), W["c_tokid"][:, :], writes=["tokid"])
        c.dma("sp", toklist[:, :], W["c_tokinit"][:, :], reads=["toklist"], writes=["toklist"])
        with ExitStack() as st2:
            sb2 = lambda n, shp, dt=F32: self.sb(st2, n, shp, dt)
            rw = sb2("rw", [128, 8, 16])
            c.dma("sp", rw[:], W["router_w"].rearrange("(c p) e -> p c e", p=128), writes=["rw"])
            rb = sb2("rb", [128, 128])
            c.dma("sp", rb[:], W["c_rb"][:, :], writes=["rb"])
            ebase = sb2("ebase", [128, 128])
            c.dma("sp", ebase[:], W["c_ebase"][:, :], writes=["ebase"])
            SU = sb2("SU", [128, 128], BF16)
            ONES = sb2("ONESm", [128, 128], BF16)
            c.dma("pool", SU[:], W["c_su"][:, :], writes=["SU"])
            c.op("pool", lambda e: e.memset(ONES[:], 1.0), writes=["ONESm"])
            offs = sb2("offs", [128, 16])
            c.op("pool", lambda e: e.memset(offs[:], 0.0), writes=["offs"])
            TB = min(8, NT)
            TE = TB * 16
            xt = [sb2("rxt", [128, D]) for _ in range(2)]
            xT = [sb2("rxT", [128, 8, 128]) for _ in range(2)]
            aff = sb2("aff", [128, TE])
            s = sb2("s", [128, TE])
            s2 = sb2("s2", [128, TE])
            eq = sb2("eq", [128, TE])
            m1 = sb2("m1", [128, TB * 4])
            m2 = sb2("m2", [128, TB * 4])
            gs = sb2("gs", [128, TB * 4])
            gm = sb2("gm", [128, 2, TB])
            sel = sb2("sel", [128, TE])
            selb = sb2("selb", [128, TE], BF16)
            gate = sb2("gate", [128, TE])
            val = sb2("val", [128, TE])
            offT = sb2("offT", [128, TE])
            sl = sb2("sl", [128, 2, TB])
            g4 = lambda ap: ap.rearrange("p (a e) -> p a e", e=4)
            t16 = lambda ap: ap.rearrange("p (t e) -> p t e", e=16)
            bc4 = lambda ap: ap.unsqueeze(2).to_broadcast([128, TB * 4, 4])
            bc16 = lambda ap: ap.unsqueeze(2).to_broadcast([128, TB, 16])
            n = 0
            for tb in range(NT // TB):
                for i in range(TB):
                    t = tb * TB + i
                    b = n % 2
                    n += 1
                    r0 = t * 128
                    c.dma("sp", xt[b][:], xin[r0:r0 + 128, :], reads=[xin.tensor.name], writes=["rxt%d" % b])
                    self.transpose_in(xT[b], xt[b], 8, "rxt%d" % b, "rxT%d" % b)
                    psL, pkL = self.nps()
                    for kc in range(8):
                        self.mm(psL[:, 0:16], xT[b][:, kc, :], rw[:, kc, :], kc == 0, kc == 7, ["rxT%d" % b, "rw"], pkL)
                    self.act(aff[:, i * 16:(i + 1) * 16], psL[:, 0:16], AF.Sigmoid, [pkL], ["aff"])
                self.tt("dve", s[:], aff[:], rb[:, 0:TE], ALU.add, ["aff", "rb"], ["s"])
                self.red("dve", m1[:], g4(s[:]), ALU.max, ["s"], ["m1"])
                self.tt("dve", g4(eq[:]), g4(s[:]), bc4(m1[:]), ALU.is_ge, ["s", "m1"], ["eq"])
                self.stt("dve", s2[:], eq[:], -1e9, s[:], ALU.mult, ALU.add, ["eq", "s"], ["s2"])
                self.red("dve", m2[:], g4(s2[:]), ALU.max, ["s2"], ["m2"])
                self.tt("dve", gs[:], m1[:], m2[:], ALU.add, ["m1", "m2"], ["gs"])
                gs3 = gs[:].rearrange("p (t g) -> p t g", g=4)
                self.red("dve", gm[:, 0, :], gs3, ALU.max, ["gs"], ["gm"])
                self.tt("dve", gs3, gs3, gm[:, 0, :].unsqueeze(2).to_broadcast([128, TB, 4]), ALU.is_ge, ["gs", "gm"], ["gs"])
                self.tt("dve", g4(sel[:]), g4(s[:]), bc4(m2[:]), ALU.is_ge, ["s", "m2"], ["sel"])
                self.tt("dve", g4(sel[:]), g4(sel[:]), bc4(gs[:]), ALU.mult, ["sel", "gs"], ["sel"])
                self.tt("dve", gate[:], aff[:], sel[:], ALU.mult, ["aff", "sel"], ["gate"])
                self.red("dve", gm[:, 1, :], t16(gate[:]), ALU.add, ["gate"], ["gm"])
                c.op("dve", lambda e: e.reciprocal(out=gm[:, 1, :], in_=gm[:, 1, :]), reads=["gm"], writes=["gm"])
                self.tt("dve", t16(gate[:]), t16(gate[:]), bc16(gm[:, 1, :]), ALU.mult, ["gate", "gm"], ["gate"])
                self.cp("dve", selb[:], sel[:], ["sel"], ["selb"])
                psC, pkC = self.nps()
                for i in range(TB):
                    self.mm(psC[:, i * 16:(i + 1) * 16], SU[:], selb[:, i * 16:(i + 1) * 16], True, True, ["SU", "selb"], pkC)
                    self.mm(psC[:, 128 + i * 16:128 + (i + 1) * 16], ONES[:], selb[:, i * 16:(i + 1) * 16], True, True, ["ONESm", "selb"], pkC)
                self.cp("dve", offT[:, 0:16], offs[:], ["offs"], ["offT"])
                for i in range(1, TB):
                    self.tt("dve", offT[:, i * 16:(i + 1) * 16], offT[:, (i - 1) * 16:i * 16], psC[:, 128 + (i - 1) * 16:128 + i * 16],
                            ALU.add, [pkC, "offT"], ["offT"])
                self.tt("dve", offs[:], offT[:, (TB - 1) * 16:TB * 16], psC[:, 128 + (TB - 1) * 16:128 + TB * 16], ALU.add,
                        [pkC, "offT"], ["offs"])
                self.tt("dve", val[:], offT[:], psC[:, 0:TE], ALU.add, [pkC, "offT"], ["val"])
                self.ts("dve", val[:], val[:], float(CAP - 1), ALU.min, ["val"], ["val"])
                self.tt("dve", val[:], val[:], ebase[:, 0:TE], ALU.add, ["val", "ebase"], ["val"])
                self.tt("dve", val[:], val[:], sel[:], ALU.mult, ["val", "sel"], ["val"])
                self.ts("dve", val[:], val[:], -1.0, ALU.add, ["val"], ["val"])
                ts_ = slice(tb * TB, (tb + 1) * TB)
                for j in range(2):
                    self.red("dve", sl[:, j, :], t16(val[:]), ALU.max, ["val"], ["sl"])
                    self.tt("dve", t16(eq[:]), t16(val[:]), bc16(sl[:, j, :]), ALU.is_equal, ["val", "sl"], ["eq"])
                    self.tt("dve", s2[:], eq[:], gate[:], ALU.mult, ["eq", "gate"], ["s2"])
                    self.red("dve", wAB[:, ts_, j], t16(s2[:]), ALU.add, ["s2"], ["wAB"])
                    self.cp("dve", slotAB[:, ts_, j], sl[:, j, :], ["sl"], ["slotAB"])
                    if j == 0:
                        self.stt("dve", val[:], eq[:], -1e9, val[:], ALU.mult, ALU.add, ["eq", "val"], ["val"])
                if "dbg_aff" in self.dbg and tb == 0 and layer == 0:
                    for nm, tl_, w_ in (("dbg_aff", aff, TE), ("dbg_offT", offT, TE), ("dbg_gate", gate, TE), ("dbg_sel", sel, TE)):
                        dd = self.dscr(nm, [128, w_])
                        c.dma("sp", dd[:, :], tl_[:, 0:w_], reads=["aff", "offT", "gate", "sel"], writes=[nm])
                    dd = self.dscr("dbg_slotAB", [128, NT * 2], I32)
                    c.dma("sp", dd[:, :], slotAB[:].rearrange("p t j -> p (t j)"), reads=["slotAB"], writes=["dbg_slotAB"])
                    dd = self.dscr("dbg_wAB", [128, NT * 2])
                    c.dma("sp", dd[:, :], wAB[:].rearrange("p t j -> p (t j)"), reads=["wAB"], writes=["dbg_wAB"])
                    dd = self.dscr("dbg_sl", [128, 2 * TB])
                    c.dma("sp", dd[:, :], sl[:].rearrange("p a t -> p (a t)"), reads=["sl"], writes=["dbg_sl"])
                for i in range(TB):
                    t = tb * TB + i
                    for j in range(2):
                        c.dma("pool", toklist, tokid[:, t, :], reads=["tokid", "slotAB"], writes=["toklist"],
                              indirect=(bass.IndirectOffsetOnAxis(ap=slotAB[:, t, j:j + 1], axis=0), None))
            c.barrier()
        with ExitStack() as st2:
            sb2 = lambda n, shp, dt=F32: self.sb(st2, n, shp, dt)
            Wg = [sb2("Wg", [128, 8, D], BF16) for _ in range(2)]
            Wu = [sb2("Wu", [128, 8, D], BF16) for _ in range(2)]
            Wd = [sb2("Wd", [128, 8, D], BF16) for _ in range(2)]
            idx = [sb2("idx", [128, 16], I32) for _ in range(2)]
            X = [sb2("Xg", [128, D]) for _ in range(2)]
            xTg = [sb2("xTg", [128, 8, 512], BF16) for _ in range(2)]
            hs = [sb2("hs", [128, 512]) for _ in range(2)]
            hT = sb2("hTm", [128, 8, 512], BF16)
            ysb = [sb2("ysb", [128, D]) for _ in range(2)]
            n = 0
            gi = 0
            wstg = [sb2("wstg", [128, D]) for _ in range(8)]
            wsrc = [W["moe_w_gate"], W["moe_w_up"], W["moe_w_down"]]

            def chunk_dma(e, ci, si):
                m_, kc = ci // 8, ci % 8
                c.dma("sp", wstg[si][:], wsrc[m_][layer, e][kc * 128:(kc + 1) * 128, :], reads=[], writes=["mwstg%d" % si])

            def chunk_cast(e, ci, si):
                m_, kc = ci // 8, ci % 8
                dstw = (Wg, Wu, Wd)[m_][e % 2]
                self.cp("act" if ci % 2 == 0 else "dve", dstw[:, kc, :], wstg[si][:], ["mwstg%d" % si], ["W%d" % (e % 2)])

            for ci in range(24):
                chunk_dma(0, ci, ci % 8)
                chunk_cast(0, ci, ci % 8)
            per_slot = (24 + NG - 1) // NG
            for e in range(16):
                wbuf = e % 2
                kw = "W%d" % wbuf
                for grp in range(NG):
                    gb = gi % 2
                    gi += 1
                    nxt = [ci for ci in range(grp * per_slot, min(24, (grp + 1) * per_slot))] if e + 1 < 16 else []
                    if per_slot <= 8:
                        for k_, ci in enumerate(nxt):
                            chunk_dma(e + 1, ci, k_)
                    for i in range(4):
                        s0 = e * CAP + grp * 512 + i * 128
                        b = n % 2
                        n += 1
                        c.dma("pool", idx[b][:], toklist[s0:s0 + 128, :], reads=["toklist"], writes=["idx%d" % b])
                        c.dma("pool", X[b][:], xin, reads=[xin.tensor.name, "idx%d" % b], writes=["Xg%d" % b],
                              indirect=(None, bass.IndirectOffsetOnAxis(ap=idx[b][:, 0:1], axis=0)))
                        self.transpose_in(xTg[gb][:, :, i * 128:(i + 1) * 128], X[b], 8, "Xg%d" % b, "xTg%d" % gb)
                    for fc in range(8):
                        fs = slice(fc * 128, (fc + 1) * 128)
                        psG, pkG = self.nps()
                        for kc in range(8):
                            self.mm(psG[:, :], Wg[wbuf][:, kc, fs], xTg[gb][:, kc, :], kc == 0, kc == 7, [kw, "xTg%d" % gb], pkG)
                        psU, pkU = self.nps()
                        for kc in range(8):
                            self.mm(psU[:, :], Wu[wbuf][:, kc, fs], xTg[gb][:, kc, :], kc == 0, kc == 7, [kw, "xTg%d" % gb], pkU)
                        hb = fc % 2
                        self.act(hs[hb][:], psG[:, :], AF.Silu, [pkG], ["hs%d" % hb])
                        self.tt("dve", hT[:, fc, :], hs[hb][:], psU[:, :], ALU.mult, ["hs%d" % hb, pkU], ["hTm"])
                    for i in range(4):
                        s0 = e * CAP + grp * 512 + i * 128
                        yb = i % 2
                        for half in range(2):
                            ps, pk = self.nps()
                            for fc in range(8):
                                self.mm(ps[:, :], hT[:, fc, i * 128:(i + 1) * 128], Wd[wbuf][:, fc, half * 512:(half + 1) * 512],
                                        fc == 0, fc == 7, [kw, "hTm"], pk)
                            self.cp("act", ysb[yb][:, half * 512:(half + 1) * 512], ps[:, :], [pk], ["ysb%d" % yb])
                        c.dma("sp", ybuf[s0:s0 + 128, :], ysb[yb][:], reads=["ysb%d" % yb], writes=["ybuf"])
                    for k_, ci in enumerate(nxt):
                        if per_slot > 8:
                            chunk_dma(e + 1, ci, k_ % 8)
                        chunk_cast(e + 1, ci, k_ % 8)
            c.barrier()
        with ExitStack() as st2:
            sb2 = lambda n, shp, dt=F32: self.sb(st2, n, shp, dt)
            lnp = sb2("lnp", [128, 2 * D])
            c.dma("sp", lnp[:], lnp_ap[:, :], writes=["lnp"])
            tl = (sb2("ln_s", [128, 16]), sb2("ln_zc", [128, D]), None)
            xr = [sb2("cx", [128, D]) for _ in range(2)]
            yA = [sb2("cyA", [128, D]) for _ in range(2)]
            yB = [sb2("cyB", [128, D]) for _ in range(2)]
            z = [sb2("cz", [128, D]) for _ in range(2)]
            o = [sb2("co", [128, D]) for _ in range(2)]
            for t in range(NT):
                b = t % 2
                r0 = t * 128
                c.dma("sp", xr[b][:], xin[r0:r0 + 128, :], reads=[xin.tensor.name], writes=["cx%d" % b])
                c.dma("pool", yA[b][:], ybuf, reads=["ybuf", "slotAB"], writes=["cyA%d" % b],
                      indirect=(None, bass.IndirectOffsetOnAxis(ap=slotAB[:, t, 0:1], axis=0)))
                c.dma("pool", yB[b][:], ybuf, reads=["ybuf", "slotAB"], writes=["cyB%d" % b],
                      indirect=(None, bass.IndirectOffsetOnAxis(ap=slotAB[:, t, 1:2], axis=0)))
                self.ts("dve", z[b][:], xr[b][:], ALPHA, ALU.mult, ["cx%d" % b], ["cz%d" % b])
                self.stt("dve", z[b][:], yA[b][:], wAB[:, t, 0:1], z[b][:], ALU.mult, ALU.add, ["cyA%d" % b, "wAB", "cz%d" % b], ["cz%d" % b])
                self.stt("dve", z[b][:], yB[b][:], wAB[:, t, 1:2], z[b][:], ALU.mult, ALU.add, ["cyB%d" % b, "wAB", "cz%d" % b], ["cz%d" % b])
                self.ln_tile(z[b][:], "cz%d" % b, lnp, o[b][:], "co%d" % b, tl)
                c.dma("sp", dst[r0:r0 + 128, :], o[b][:], reads=["co%d" % b], writes=[dst.tensor.name])
            c.barrier()


B.ln_tile = ln_tile
B.stage_mix = stage_mix
B.stage_moe = stage_moe


def stage_ret(self, p1, ret, W):
    c = self.c
    NT = self.NT
    with ExitStack() as st:
        sb = lambda n, shp, dt=F32: self.sb(st, n, shp, dt)
        dec = sb("rtdec", [128, 8 * 128 + 24])
        c.dma("sp", dec[:], W["c_rtdec"][:, :], writes=["rtdec"])
        DT = lambda h: dec[:, h * 128:(h + 1) * 128]
        qd = dec[:, 1024:1032]
        kd = dec[:, 1032:1040]
        cd = dec[:, 1040:1048]
        gn = sb("rtgn", [128, 4096])
        c.dma("sp", gn[:], W["c_rtgn"][:, :], writes=["rtgn"])
        R = sb("R", [128, 8, 256])
        Rb = sb("Rb", [128, 8, 256], BF16)
        c.op("pool", lambda e: e.memset(R[:].rearrange("p h v -> p (h v)"), 0.0), writes=["R"])
        c.op("pool", lambda e: e.memset(Rb[:].rearrange("p h v -> p (h v)"), 0.0), writes=["Rb"])
        rope = sb("rtrope", [128, 256])
        Pq = [sb("Pq", [128, 2048]) for _ in range(2)]
        Vv = [sb("Vv", [128, 2048]) for _ in range(2)]
        Gg = [sb("Gg", [128, 2048]) for _ in range(2)]
        qk = sb("qkr", [128, 3, 1024])
        ktb = sb("ktb", [128, 1024], BF16)
        tmp = sb("rtmp", [128, 8, 64])
        T3 = sb("T3", [128, 24, 128], BF16)
        Vb = sb("Vb", [128, 2048], BF16)
        attm = sb("attm", [128, 1024], BF16)
        Os = sb("rOs", [128, 2048])
        sq = sb("rsq", [128, 2048])
        sg = sb("rsg", [128, 2048])
        st8 = sb("rst8", [128, 8])
        oo = sb("roo", [128, 2048])
        for t in range(NT):
            b = t % 2
            r0 = t * 128
            kp, kv, kg = "Pq%d" % b, "Vv%d" % b, "Gg%d" % b
            c.dma("sp", Pq[b][:], p1[r0:r0 + 128, 0:2048], reads=["p1"], writes=[kp])
            c.dma("sp", Vv[b][:], p1[r0:r0 + 128, 2048:4096], reads=["p1"], writes=[kv])
            c.dma("sp", Gg[b][:], p1[r0:r0 + 128, 4096:6144], reads=["p1"], writes=[kg])
            c.dma("sp", rope[:], W["c_rtrope"][r0:r0 + 128, :], writes=["rtrope"])
            for a in range(2):
                src = Pq[b][:, a * 1024:(a + 1) * 1024].rearrange("p (h d) -> p h d", h=8)
                dst = qk[:, a, :].rearrange("p (h d) -> p h d", h=8)
                cos = rope[:, a * 128:a * 128 + 64].unsqueeze(1).to_broadcast([128, 8, 64])
                sin = rope[:, a * 128 + 64:a * 128 + 128].unsqueeze(1).to_broadcast([128, 8, 64])
                x1, x2 = src[:, :, 0:64], src[:, :, 64:128]
                o1, o2 = dst[:, :, 0:64], dst[:, :, 64:128]
                kq = "qk%d" % a
                self.tt("dve", o1, x1, cos, ALU.mult, [kp, "rtrope"], [kq])
                self.tt("pool", tmp[:], x2, sin, ALU.mult, [kp, "rtrope"], ["rtmp"])
                self.tt("dve", o1, o1, tmp[:], ALU.subtract, [kq, "rtmp"], [kq])
                self.tt("dve", o2, x2, cos, ALU.mult, [kp, "rtrope"], [kq])
                self.tt("pool", tmp[:], x1, sin, ALU.mult, [kp, "rtrope"], ["rtmp"])
                self.tt("dve", o2, o2, tmp[:], ALU.add, [kq, "rtmp"], [kq])
            q3 = qk[:, 0, :].rearrange("p (h d) -> p h d", h=8)
            k3 = qk[:, 1, :].rearrange("p (h d) -> p h d", h=8)
            self.tt("pool", qk[:, 2, :].rearrange("p (h d) -> p h d", h=8), q3, qd.unsqueeze(2).to_broadcast([128, 8, 128]),
                    ALU.mult, ["qk0", "rtdec"], ["qk2"])
            self.tt("dve", ktb[:].rearrange("p (h d) -> p h d", h=8), k3, kd.unsqueeze(2).to_broadcast([128, 8, 128]),
                    ALU.mult, ["qk1", "rtdec"], ["ktb"])
            self.cp("act", Vb[:], Vv[b][:], [kv], ["Vb"])
            for a in range(3):
                self.transpose_in(T3, qk[:, a, :], 8, "qk%d" % a, "T3_%d" % a, ch0=8 * a)
            for hq in range(2):
                psA, pkA = self.nps()
                for hh in range(4):
                    h = hq * 4 + hh
                    self.mm(psA[:, hh * 128:(hh + 1) * 128], T3[:, 8 + h, :], T3[:, h, :], True, True, ["T3_0", "T3_1"], pkA)
                self.tt("dve", attm[:, hq * 512:(hq + 1) * 512], psA[:, :], dec[:, hq * 512:(hq + 1) * 512], ALU.mult,
                        [pkA, "rtdec"], ["attm%d" % hq])
            for hp in range(4):
                psO, pkO = self.nps()
                for hh in range(2):
                    h = hp * 2 + hh
                    vs = slice(h * 256, (h + 1) * 256)
                    self.mm(psO[:, hh * 256:(hh + 1) * 256], attm[:, h * 128:(h + 1) * 128], Vb[:, vs], True, False,
                            ["attm%d" % (h // 4), "Vb"], pkO)
                    self.mm(psO[:, hh * 256:(hh + 1) * 256], T3[:, 16 + h, :], Rb[:, h, :], False, True, ["T3_2", "Rb"], pkO)
                self.cp("act", Os[:, hp * 512:(hp + 1) * 512], psO[:, :], [pkO], ["rOs"])
            R2 = R[:].rearrange("p h v -> p (h v)")
            self.tt("pool", R[:], R[:], cd.unsqueeze(2).to_broadcast([128, 8, 256]), ALU.mult, ["R", "rtdec"], ["R"])
            for hp in range(4):
                psR, pkR = self.nps()
                for hh in range(2):
                    h = hp * 2 + hh
                    vs = slice(h * 256, (h + 1) * 256)
                    self.mm(psR[:, hh * 256:(hh + 1) * 256], ktb[:, h * 128:(h + 1) * 128], Vb[:, vs], True, True, ["ktb", "Vb"], pkR)
                self.tt("dve", R2[:, hp * 512:(hp + 1) * 512], R2[:, hp * 512:(hp + 1) * 512], psR[:, :], ALU.add, [pkR, "R"], ["R"])
            self.cp("act", Rb[:].rearrange("p h v -> p (h v)"), R2, ["R"], ["Rb"])
            O3 = Os[:].rearrange("p (h v) -> p h v", h=8)
            self.red("dve", st8[:], O3, ALU.add, ["rOs"], ["rst8"])
            self.ts("dve", st8[:], st8[:], 1.0 / 256, ALU.mult, ["rst8"], ["rst8"])
            self.tt("dve", O3, O3, st8[:].unsqueeze(2).to_broadcast([128, 8, 256]), ALU.subtract, ["rOs", "rst8"], ["rOs"])
            self.act(sq[:], Os[:], AF.Square, ["rOs"], ["rsq"])
            self.red("dve", st8[:], sq[:].rearrange("p (h v) -> p h v", h=8), ALU.add, ["rsq"], ["rst8"])
            self.rsqrt(st8[:], st8[:], 1e-5, ["rst8"], ["rst8"], scale=1.0 / 256)
            self.tt("dve", O3, O3, st8[:].unsqueeze(2).to_broadcast([128, 8, 256]), ALU.mult, ["rOs", "rst8"], ["rOs"])
            self.tt("pool", Os[:], Os[:], gn[:, 0:2048], ALU.mult, ["rOs", "rtgn"], ["rOs"])
            self.tt("pool", Os[:], Os[:], gn[:, 2048:4096], ALU.add, ["rOs", "rtgn"], ["rOs"])
            self.act(sg[:], Gg[b][:], AF.Silu, [kg], ["rsg"])
            self.tt("dve", oo[:], Os[:], sg[:], ALU.mult, ["rOs", "rsg"], ["roo"])
            c.dma("sp", ret[r0:r0 + 128, :], oo[:], reads=["roo"], writes=["ret"])
        c.barrier()


B.stage_ret = stage_ret


STAGES = ["proj0", "rwkv", "nsa", "mix0", "moe0", "proj1", "ret", "mix1", "moe1"]


def build(S, upto="moe1", dbg=()):
    b = B(S, dbg)
    n_st = STAGES.index(upto) + 1
    on = lambda s: STAGES.index(s) < n_st
    CAP = moe_cap(S)
    NSLOT = 16 * CAP
    ncp = ((((S - 32) // 16 + 1) + 127) // 128) * 128
    W = {}
    for name, shp, dt in [("c_ident", [128, 128], F32), ("c_tri", [128, 128], F32), ("c_mask4", [128, 512], F32),
                          ("c_maskL", [128, 128], F32), ("c_bd", [128, 128], F32),
                          ("c_rkv", [128, 13 * 512], F32), ("rk_w1", [512, 64], F32), ("rk_a1", [512, 64], F32),
                          ("rk_g1", [512, 128], F32), ("rk_w2", [64, 512], F32), ("rk_a2", [64, 512], F32),
                          ("rk_g2", [128, 512], F32),
                          ("ns_c_w1", [2, 2048, 128], F32), ("ns_c_w2", [2, 128, 64], F32), ("c_peT", [128, 64], F32),
                          ("c_ones", [128, 1], F32), ("c_rope", [S, 16], F32), ("c_selF", [S, 128], F32),
                          ("c_E", [128, S], F32), ("c_caus", [128, 4 * 512], F32), ("c_win", [128, 8 * 512], F32),
                          ("c_cmpb", [128, 5 * 512], F32), ("c_ov", [ncp, 128], F32),
                          ("ab_w_out", [D, D], F32), ("c_ln", [4, 128, 2 * D], F32),
                          ("router_w", [D, 16], F32), ("c_rb", [128, 128], F32), ("c_ebase", [128, 128], F32),
                          ("c_su", [128, 128], F32), ("c_tokid", [128, (S // 128) * 16], I32),
                          ("c_tokinit", [NSLOT + 1, 16], I32),
                          ("moe_w_gate", [2, 16, D, D], F32), ("moe_w_up", [2, 16, D, D], F32),
                          ("moe_w_down", [2, 16, D, D], F32),
                          ("rt_w_in", [D, 6144], F32), ("rt_w_out", [2048, D], F32), ("c_rtgn", [128, 2 * 2048], F32),
                          ("c_rtrope", [S, 256], F32), ("c_rtdec", [128, 8 * 128 + 24], F32)]:
        W[name] = b.din(name, shp, dt)
    x = b.din("x", [S, D])
    ab_w_in = b.din("ab_w_in", [D, 3352])
    p0 = b.dscr("p0", [S, 3352])
    oab = b.dscr("oab", [S, 1024])
    x1 = b.dscr("x1", [S + 1, D])
    x2 = b.dscr("x2", [S + 1, D])
    x3 = b.dscr("x3", [S + 1, D])
    p1 = b.dscr("p1", [S, 6144])
    ret = b.dscr("ret", [S, 2048])
    toklist = b.dscr("toklist", [NSLOT + 1, 16], I32)
    ybuf = b.dscr("ybuf", [NSLOT, D])
    out = b.nc.dram_tensor("out", [S, D], F32, kind="ExternalOutput").ap()
    with ExitStack() as st:
        b.load_consts(st)
        zrow = b.sb(st, "zrow", [1, D])
        b.c.op("pool", lambda e: e.memset(zrow[:], 0.0), writes=["zrow"])
        for xx in (x1, x3):
            b.c.dma("sp", xx[S:S + 1, :], zrow[:], reads=["zrow"], writes=[xx.tensor.name])
        b.stage_proj(x, ab_w_in, p0, D, 3352)
        if on("rwkv"):
            b.stage_rwkv(p0, oab, W)
        if on("nsa"):
            b.stage_nsa(p0, oab, W)
        if on("mix0"):
            b.stage_mix(oab, 1024, W["ab_w_out"], x, W["c_ln"][0], x1)
        if on("moe0"):
            b.stage_moe(x1, W, 0, W["c_ln"][1], x2, toklist, ybuf)
        if on("proj1"):
            b.stage_proj(x2, W["rt_w_in"], p1, D, 6144)
        if on("ret"):
            b.stage_ret(p1, ret, W)
        if on("mix1"):
            b.stage_mix(ret, 2048, W["rt_w_out"], x2, W["c_ln"][2], x3)
        if on("moe1"):
            b.stage_moe(x3, W, 1, W["c_ln"][3], out, toklist, ybuf)
        b.c.finish()
    return b


def consts(S):
    c = {}
    c["c_ident"] = np.eye(128, dtype=np.float32)
    i = np.arange(128)
    same = (i[:, None] // 64) == (i[None, :] // 64)
    strict = ((i[:, None] < i[None, :]) & same).astype(np.float32)
    incl = ((i[:, None] <= i[None, :]) & same).astype(np.float32)
    c["c_tri"] = incl
    c["c_mask4"] = np.concatenate([strict, incl, strict, incl], axis=1)
    c["c_bd"] = same.astype(np.float32)
    c["c_ones"] = np.ones((128, 1), np.float32)
    inv = 500000.0 ** (-np.arange(8, dtype=np.float32) / 8)
    ang = np.arange(S, dtype=np.float32)[:, None] * inv[None, :]
    c["c_rope"] = np.concatenate([np.cos(ang), np.sin(ang)], axis=1).astype(np.float32)
    tpos = np.arange(S)
    cur = tpos // 64
    jb = np.arange(128)
    F = np.zeros((S, 128), np.float32)
    F[jb[None, :] > cur[:, None]] = -10.0
    forced = (jb[None, :] == 0) | (jb[None, :] == cur[:, None]) | (jb[None, :] == cur[:, None] - 1)
    F[forced & (jb[None, :] <= cur[:, None])] = 10.0
    c["c_selF"] = F
    c["c_E"] = (np.arange(S)[None, :] // 64 == jb[:, None]).astype(np.float32)
    k = np.arange(128)[:, None]
    q = np.arange(512)[None, :]
    c["c_caus"] = np.concatenate([np.where(128 * d + k <= q, 0.0, NEG) for d in range(4)], axis=1).astype(np.float32)
    c["c_win"] = np.concatenate([np.where((128 * d + k <= q) & (128 * d + k > q - 512), 0.0, NEG) for d in range(-4, 4)],
                                axis=1).astype(np.float32)
    c["c_cmpb"] = np.concatenate([np.where(16 * k + 31 <= 512 * dj + q, 0.0, NEG) for dj in range(5)], axis=1).astype(np.float32)
    n_cmp = (S - 32) // 16 + 1
    ncp = ((n_cmp + 127) // 128) * 128
    cs = np.arange(ncp) * 16
    ss = np.arange(128) * 64
    ov = np.clip(np.minimum(cs[:, None] + 32, ss[None, :] + 64) - np.maximum(cs[:, None], ss[None, :]), 0, None).astype(np.float32) / 32
    ov[n_cmp:] = 0.0
    c["c_ov"] = ov
    CAP = moe_cap(S)
    c["c_ebase"] = np.ascontiguousarray(np.broadcast_to(np.tile((np.arange(16) * CAP + 1).astype(np.float32), 8)[None, :], (128, 128)))
    c["c_su"] = (i[:, None] < i[None, :]).astype(np.float32)
    NT = S // 128
    tok = (np.arange(NT)[None, :, None] * 128 + np.arange(128)[:, None, None] + np.zeros((1, 1, 16), np.int64))
    c["c_tokid"] = np.ascontiguousarray(tok.reshape(128, NT * 16)).astype(np.int32)
    inv = 10000.0 ** (-np.linspace(0.0, 1.0, 64, dtype=np.float32))
    ang = np.arange(S, dtype=np.float32)[:, None] * inv[None, :]
    cs_, sn_ = np.cos(ang), np.sin(ang)
    sc = 128.0 ** -0.5
    c["c_rtrope"] = np.concatenate([cs_, sn_, cs_ * sc, sn_ * sc], axis=1).astype(np.float32)
    log_g = np.log(1.0 - 2.0 ** (-5.0 - np.arange(8, dtype=np.float64)))
    ii = np.arange(128, dtype=np.float64)
    diff = ii[None, :] - ii[:, None]
    DTm = [np.where(diff >= 0, np.exp(np.maximum(diff, 0.0) * lg), 0.0) for lg in log_g]
    qd = np.exp((ii[:, None] + 1.0) * log_g[None, :])
    kd = np.exp((127.0 - ii[:, None]) * log_g[None, :])
    cd = np.broadcast_to(np.exp(128.0 * log_g)[None, :], (128, 8))
    c["c_rtdec"] = np.concatenate(DTm + [qd, kd, cd], axis=1).astype(np.float32)
    c["c_tokinit"] = np.full((16 * CAP + 1, 16), S, np.int32)
    c["c_maskL"] = np.ascontiguousarray(strict.T)
    return c


def derived(inputs):
    d = {}
    rk = np.concatenate([inputs["rk_mu"].reshape(-1), inputs["rk_w0"].reshape(-1), inputs["rk_a0"].reshape(-1),
                         inputs["rk_kk"].reshape(-1), inputs["rk_ka"].reshape(-1), inputs["rk_rk"].reshape(-1),
                         inputs["rk_ln"].reshape(-1)])
    pe = np.asarray(inputs["ns_pe"]).reshape(2, 32, 64)
    peT = np.transpose(pe, (2, 0, 1)).reshape(64, 64)
    d["c_peT"] = np.ascontiguousarray(np.concatenate([peT, peT], axis=0)).astype(np.float32)
    ln = np.asarray(inputs["ln"]).reshape(4, 2 * D)
    d["c_ln"] = np.ascontiguousarray(np.broadcast_to(ln[:, None, :], (4, 128, 2 * D))).astype(np.float32)
    d["c_rtgn"] = np.ascontiguousarray(np.broadcast_to(np.asarray(inputs["rt_gn"]).reshape(1, 4096), (128, 4096))).astype(np.float32)
    d["c_rb"] = np.ascontiguousarray(np.broadcast_to(np.tile(np.asarray(inputs["router_b"]).reshape(16), 8)[None, :], (128, 128))).astype(np.float32)
    d["c_rkv"] = np.ascontiguousarray(np.broadcast_to(rk[None, :], (128, rk.size))).astype(np.float32)
    return d


def make_inputs(b, inputs, bi, S):
    cs = consts(S)
    cs.update(derived(inputs))
    m = {}
    for name, ap in b.inp.items():
        if name in cs:
            m[name] = cs[name]
        elif name == "x":
            m[name] = np.ascontiguousarray(inputs["x"][bi, :S])
        else:
            a = np.asarray(inputs[name])
            m[name] = np.ascontiguousarray(a.reshape(ap.shape))
    return m


_BUILT = {}


def kernel(**inputs):
    S = 8192
    if S not in _BUILT:
        _BUILT[S] = build(S)
    b = _BUILT[S]
    shared = make_inputs(b, inputs, 0, S)
    in_maps = []
    for bi in range(8):
        m = dict(shared)
        m["x"] = np.ascontiguousarray(np.asarray(inputs["x"])[bi, :S]).astype(np.float32)
        in_maps.append(m)
    res = run_bass_kernel_spmd(b.nc, in_maps, core_ids=list(range(8)))
    return np.stack([np.asarray(r["out"]) for r in res.results], axis=0).astype(np.float32)
```

```python
import numpy as np
import ml_dtypes
from contextlib import ExitStack
import concourse.bass as bass
import concourse.mybir as mybir
from concourse.bass_utils import run_bass_kernel_spmd

F32 = mybir.dt.float32
BF16 = mybir.dt.bfloat16
I32 = mybir.dt.int32
U32 = mybir.dt.uint32
AF = mybir.ActivationFunctionType
ALU = mybir.AluOpType
AX = mybir.AxisListType

D = 1024
ALPHA = (2.0 * 2) ** 0.25
LN_EPS = 1e-5
NEG = -30000.0


class Ctx:
    NDMA = 10

    def __init__(self, nc):
        self.nc = nc
        self.eng = {"pe": nc.tensor, "dve": nc.vector, "act": nc.scalar, "pool": nc.gpsimd, "sp": nc.sync}
        self.sem = {}
        self.cnt = {}
        for e in self.eng:
            self.sem["e_" + e] = nc.alloc_semaphore("sem_e_" + e)
            self.cnt["e_" + e] = 0
        self.dma_pool = {}
        for q in ("sp", "pool", "act"):
            names = []
            for i in range(self.NDMA):
                n = "d_%s_%d" % (q, i)
                self.sem[n] = nc.alloc_semaphore("sem_" + n)
                self.cnt[n] = 0
                names.append(n)
            self.dma_pool[q] = [names, 0]
        self.known = {e: {} for e in self.eng}
        self.last_w = {}
        self.readers = {}
        self.n_inst = 0
        self.n_wait = 0

    def _wait(self, e, semname, val):
        kn = self.known[e]
        if kn.get(semname, 0) >= val:
            return
        self.eng[e].wait_ge(self.sem[semname], val)
        kn[semname] = val
        self.n_wait += 1

    def _deps(self, e, reads, writes, is_dma=False):
        own = "e_" + e if not is_dma else None
        need = {}

        def add(ev, raw):
            s, v = ev
            if s == own and e == "pe":
                return
            if need.get(s, 0) < v:
                need[s] = v

        for k in reads:
            ev = self.last_w.get(k)
            if ev is not None:
                add(ev, True)
        for k in writes:
            ev = self.last_w.get(k)
            if ev is not None:
                add(ev, False)
            for s, v in self.readers.get(k, {}).items():
                add((s, v), False)
        for s, v in need.items():
            self._wait(e, s, v)

    def _commit(self, ev, reads, writes):
        s, v = ev
        for k in writes:
            self.last_w[k] = ev
            self.readers[k] = {}
        for k in reads:
            if k in writes:
                continue
            r = self.readers.setdefault(k, {})
            if r.get(s, 0) < v:
                r[s] = v

    def op(self, e, fn, reads=(), writes=()):
        reads = list(reads)
        writes = list(writes)
        self._deps(e, reads, writes)
        ins = fn(self.eng[e])
        s = "e_" + e
        self.cnt[s] += 1
        ins.then_inc(self.sem[s], 1)
        self._commit((s, self.cnt[s]), reads, writes)
        self.n_inst += 1
        return ins

    def dma(self, q, out, in_, reads=(), writes=(), indirect=None, **kw):
        reads = list(reads)
        writes = list(writes)
        self._deps(q, reads, writes, is_dma=True)
        names, i = self.dma_pool[q]
        s = names[i % len(names)]
        self.dma_pool[q][1] = i + 1
        if self.cnt[s] > 0:
            self._wait(q, s, self.cnt[s])
        if indirect is None:
            ins = self.eng[q].dma_start(out=out, in_=in_, **kw)
        else:
            ins = self.eng[q].indirect_dma_start(out, indirect[0], in_, indirect[1], **kw)
        self.cnt[s] += 16
        ins.then_inc(self.sem[s], 16)
        self._commit((s, self.cnt[s]), reads, writes)
        self.n_inst += 1
        return ins

    def barrier(self):
        for e in self.eng:
            for s, c in self.cnt.items():
                if c > 0:
                    self._wait(e, s, c)

    def finish(self):
        for s, c in self.cnt.items():
            if c > 0:
                self._wait("sp", s, c)


class B:
    def __init__(self, S, dbg=()):
        self.S = S
        self.NT = S // 128
        self.dbg = set(dbg)
        nc = self.nc = bass.Bass("TRN2", target_bir_lowering=False)
        self.c = Ctx(nc)
        self.inp = {}
        self.ps = [nc.alloc_psum_tensor("psb%d" % i, [128, 512], F32) for i in range(8)]
        self.ps_i = 0
        self._uid = 0
        import os
        self.cut = int(os.environ['CUT']) if 'CUT' in os.environ else None

    def din(self, name, shape, dt=F32):
        t = self.nc.dram_tensor(name, list(shape), dt, kind="ExternalInput").ap()
        self.inp[name] = t
        return t

    def dscr(self, name, shape, dt=F32):
        kind = "ExternalOutput" if name in self.dbg else "Internal"
        return self.nc.dram_tensor(name, list(shape), dt, kind=kind).ap()

    def sb(self, st, name, shape, dt=F32):
        self._uid += 1
        return st.enter_context(self.nc.sbuf_tensor("%s_%d" % (name, self._uid), list(shape), dt))

    def nps(self):
        i = self.ps_i % 8
        self.ps_i += 1
        return self.ps[i], "ps%d" % i

    def mm(self, out, lhsT, rhs, start, stop, reads, pk):
        return self.c.op("pe", lambda e: e.matmul(out, lhsT, rhs, start=start, stop=stop), reads=reads, writes=[pk])

    def tr(self, out, in_, ident, reads, pk):
        return self.c.op("pe", lambda e: e.transpose(out, in_, ident), reads=reads, writes=[pk])

    def cp(self, eng, out, in_, reads, writes):
        if eng == "act":
            return self.c.op("act", lambda e: e.copy(out=out, in_=in_), reads=reads, writes=writes)
        return self.c.op(eng, lambda e: e.tensor_copy(out=out, in_=in_), reads=reads, writes=writes)

    def act(self, out, in_, func, reads, writes, bias=0.0, scale=1.0, accum_out=None):
        kw = {}
        if accum_out is not None:
            kw["accum_out"] = accum_out
        return self.c.op("act", lambda e: e.activation(out=out, in_=in_, func=func, bias=bias, scale=scale, **kw),
                         reads=reads, writes=writes)

    def tt(self, eng, out, in0, in1, op, reads, writes):
        return self.c.op(eng, lambda e: e.tensor_tensor(out=out, in0=in0, in1=in1, op=op), reads=reads, writes=writes)

    def ts(self, eng, out, in0, s1, op0, reads, writes, s2=None, op1=None):
        if op0 in (ALU.pow, ALU.divide) or op1 in (ALU.pow, ALU.divide):
            eng = "pool"
        if op1 is None:
            return self.c.op(eng, lambda e: e.tensor_scalar(out=out, in0=in0, scalar1=s1, scalar2=None, op0=op0),
                             reads=reads, writes=writes)
        return self.c.op(eng, lambda e: e.tensor_scalar(out=out, in0=in0, scalar1=s1, scalar2=s2, op0=op0, op1=op1),
                         reads=reads, writes=writes)

    def rsqrt(self, out, in_, eps, reads, writes, scale=1.0):
        self.act(out, in_, AF.Sqrt, reads, writes, bias=eps, scale=scale)
        return self.c.op("dve", lambda e: e.reciprocal(out=out, in_=out), reads=writes, writes=writes)

    def stt(self, eng, out, in0, scalar, in1, op0, op1, reads, writes):
        eng = "dve"
        return self.c.op(eng, lambda e: e.scalar_tensor_tensor(out=out, in0=in0, scalar=scalar, in1=in1, op0=op0, op1=op1),
                         reads=reads, writes=writes)

    def red(self, eng, out, in_, op, reads, writes, axis=AX.X):
        return self.c.op(eng, lambda e: e.tensor_reduce(out=out, in_=in_, axis=axis, op=op), reads=reads, writes=writes)

    def load_consts(self, st):
        self.ident = self.sb(st, "ident", [128, 128], F32)
        self.c.dma("sp", self.ident[:], self.inp["c_ident"][:, :], reads=[], writes=["ident"])

    def load_w(self, dst, w_ap, K, N, key, q="pool"):
        for kc in range(K // 128):
            for n0 in range(0, N, 2048):
                n1 = min(N, n0 + 2048)
                self.c.dma(q, dst[:, kc, n0:n1], w_ap[kc * 128:(kc + 1) * 128, n0:n1], reads=[], writes=[key])

    def load_w_fast(self, st, dst, w_ap, K, N, key):
        stg = [self.sb(st, "wstg", [128, 1024]) for _ in range(4)]
        i = 0
        for kc in range(K // 128):
            for n0 in range(0, N, 1024):
                n1 = min(N, n0 + 1024)
                s_ = stg[i % 4]
                sk = "wstg%d_%s" % (i % 4, key)
                self.c.dma("sp", s_[:, 0:n1 - n0], w_ap[kc * 128:(kc + 1) * 128, n0:n1], reads=[], writes=[sk])
                self.cp("act" if i % 2 == 0 else "dve", dst[:, kc, n0:n1], s_[:, 0:n1 - n0], [sk], [key])
                i += 1

    def transpose_in(self, xT, xin, nch, rkey, wkey, col0=0, ch0=0):
        j = 0
        k = 0
        while j < nch:
            g = min(4, nch - j)
            ps, pk = self.nps()
            for i in range(g):
                self.tr(ps[:, i * 128:(i + 1) * 128], xin[:, col0 + (j + i) * 128: col0 + (j + i + 1) * 128],
                        self.ident[:], [rkey, "ident"], pk)
            eng = "act" if k % 2 == 0 else "dve"
            self.cp(eng, xT[:, ch0 + j:ch0 + j + g, :], ps[:, 0:g * 128].rearrange("p (g t) -> p g t", g=g), [pk], [wkey])
            j += g
            k += 1

    def layer_norm(self, st_tiles, z, zkey, gam, bet, out, okey):
        stats, mv, rstd = st_tiles
        nc = self.nc
        c = self.c
        for i in range(2):
            c.op("dve", lambda e: e.bn_stats(out=stats[:, i, :], in_=z[:, i * 512:(i + 1) * 512]), reads=[zkey], writes=["ln_stats"])
        c.op("dve", lambda e: e.bn_aggr(out=mv[:], in_=stats[:]), reads=["ln_stats"], writes=["ln_mv"])
        self.rsqrt(rstd[:], mv[:, 1:2], LN_EPS, ["ln_mv"], ["ln_rstd"])
        self.ts("dve", out, z, mv[:, 0:1], ALU.subtract, [zkey, "ln_mv", "ln_rstd"], [okey], s2=rstd[:, 0:1], op1=ALU.mult)
        self.tt("pool", out, out, gam, ALU.mult, [okey, "lnp"], [okey])
        self.tt("pool", out, out, bet, ALU.add, [okey, "lnp"], [okey])

    def stage_proj(self, src, w_ap, dst, K, N):
        with ExitStack() as st:
            wb = self.sb(st, "wproj", [128, K // 128, N], BF16)
            self.load_w_fast(st, wb, w_ap, K, N, "wproj")
            xin = [self.sb(st, "xin", [128, K], F32) for _ in range(2)]
            xT = [self.sb(st, "xT", [128, K // 128, 128], BF16) for _ in range(2)]
            ot = [self.sb(st, "ot", [128, N], F32) for _ in range(2)]
            for t in range(self.NT):
                b = t % 2
                self.c.dma("sp", xin[b][:], src[t * 128:(t + 1) * 128, :], reads=[src.tensor.name], writes=["xin%d" % b])
                self.transpose_in(xT[b], xin[b], K // 128, "xin%d" % b, "xT%d" % b)
                k = 0
                for n0 in range(0, N, 512):
                    w = min(512, N - n0)
                    ps, pk = self.nps()
                    for kc in range(K // 128):
                        self.mm(ps[:, 0:w], xT[b][:, kc, :], wb[:, kc, n0:n0 + w], kc == 0, kc == K // 128 - 1,
                                ["xT%d" % b, "wproj"], pk)
                    self.cp("act" if k % 2 == 0 else "dve", ot[b][:, n0:n0 + w], ps[:, 0:w], [pk], ["ot%d" % b])
                    k += 1
                self.c.dma("sp", dst[t * 128:(t + 1) * 128, :], ot[b][:], reads=["ot%d" % b], writes=[dst.tensor.name])
            self.c.barrier()


RK_C = 0.606531


def stage_rwkv(self, p0, oab, W):
    c = self.c
    NT = self.NT
    with ExitStack() as st:
        sb = lambda n, shp, dt=F32: self.sb(st, n, shp, dt)
        pv = sb("rkv", [128, 13 * 512])
        c.dma("sp", pv[:], W["c_rkv"][:, :], writes=["rkv"])
        MU = lambda i: pv[:, i * 512:(i + 1) * 512]
        W0, A0, KK_, KA_, RKk, LNG, LNB = [pv[:, (6 + i) * 512:(7 + i) * 512] for i in range(7)]
        trib = sb("trib", [128, 128], BF16)
        c.dma("pool", trib[:], W["c_tri"][:, :], writes=["trib"])
        mask4 = sb("mask4", [128, 512])
        c.dma("sp", mask4[:], W["c_mask4"][:, :], writes=["mask4"])
        maskL = sb("maskL", [128, 128])
        c.dma("sp", maskL[:], W["c_maskL"][:, :], writes=["maskL"])
        identb = sb("identb", [128, 128], BF16)
        c.dma("pool", identb[:], W["c_ident"][:, :], writes=["identb"])
        w1 = sb("w1", [128, 4, 64], BF16)
        a1 = sb("a1", [128, 4, 64], BF16)
        g1 = sb("g1", [128, 4, 128], BF16)
        self.load_w(w1, W["rk_w1"], 512, 64, "w1")
        self.load_w(a1, W["rk_a1"], 512, 64, "a1")
        self.load_w(g1, W["rk_g1"], 512, 128, "g1")
        w2 = sb("w2", [64, 512], BF16)
        a2 = sb("a2", [64, 512], BF16)
        g2 = sb("g2", [128, 512], BF16)
        c.dma("pool", w2[:], W["rk_w2"][:, :], writes=["w2"])
        c.dma("pool", a2[:], W["rk_a2"][:, :], writes=["a2"])
        c.dma("pool", g2[:], W["rk_g2"][:, :], writes=["g2"])
        H = sb("H", [128, 4, 128])
        Hb = sb("Hb", [128, 4, 128], BF16)
        bd = sb("bd", [128, 128])
        c.dma("sp", bd[:], W["c_bd"][:, :], writes=["bd"])
        c.op("dve", lambda e: e.memset(H[:], 0.0), writes=["H"])
        c.op("dve", lambda e: e.memset(Hb[:], 0.0), writes=["Hb"])
        SINGLE = {"Pmm", "swh", "P", "Ps", "X", "xT", "hT", "sw", "a", "kk", "kp", "tmp", "ss", "cs", "e", "T", "BT", "KT", "Q"}

        def two(n, shp, dt=F32):
            return [sb(n, shp, dt) for _ in range(2)]

        def one(n, shp, dt=F32):
            x = sb(n, shp, dt)
            return [x, x]
        P_ = one("P", [128, 2048])
        Ps_ = one("Ps", [128, 2048])
        X6_ = one("X6", [128, 6, 512])
        xT_ = one("xT3", [128, 12, 128], BF16)
        hT_ = one("hT", [128, 384], BF16)
        sw_ = one("sw", [128, 512])
        swh = sb("swh", [128, 2, 512], BF16)
        a_ = one("a", [128, 512])
        g_ = two("g", [128, 512])
        kk_ = one("kk", [128, 512])
        kp_ = one("kp", [128, 512])
        tmp_ = one("tmp", [128, 512])
        tmp2_ = two("tq", [128, 512])
        ss_ = one("ss", [128, 8])
        bon_ = two("bon", [128, 512])
        sq_ = two("sq", [128, 8])
        bdg_ = [[sb("bdg", [128, 4, 128]) for _ in range(2)] for _ in range(2)]
        cs_ = one("cs", [128, 512])
        e_ = one("e3", [128, 3, 512])
        T4_ = one("T4", [128, 4, 512])
        Bt_ = two("Bt", [128, 512], BF16)
        Kt_ = two("Kt", [128, 512], BF16)
        Vt_ = two("Vt", [128, 512], BF16)
        ART_ = two("ART", [128, 4, 256], BF16)
        BT_ = one("BT", [128, 4, 128], BF16)
        KT_ = one("KT", [128, 4, 128], BF16)
        ET_ = two("ET", [128, 4, 128])
        G_ = two("G", [128, 8, 512], BF16)
        Wm_ = two("Wm", [128, 8, 128], BF16)
        Pm_ = one("Pm", [128, 8, 128], BF16)
        Qm_ = one("Qm", [128, 8, 128], BF16)
        Xs_ = two("Xs", [128, 512], BF16)
        Ub_ = [[sb("Ub", [128, 512], BF16) for _ in range(2)] for _ in range(2)]
        Vm_ = [[sb("Vm", [128, 512], BF16) for _ in range(2)] for _ in range(2)]
        for bb in range(2):
            c.op("pool", lambda e: e.memset(Xs_[bb][:], 0.0), writes=["Xs%d" % bb])
            for cc in range(2):
                c.op("pool", lambda e: e.memset(Ub_[bb][cc][:], 0.0), writes=["Ub%d_%d" % (cc, bb)])
                c.op("pool", lambda e: e.memset(Vm_[bb][cc][:], 0.0), writes=["Vm%d%d" % (cc, bb)])
        Os_ = two("Os", [128, 512])
        oo_ = two("oo", [128, 512])
        for t in range(NT if self.cut is None else 1):
            b = t % 2
            K = lambda n: n if n.rstrip("0123456789_") in SINGLE else "%s%d" % (n, b)
            P, Ps, X6, xT, hT = P_[b], Ps_[b], X6_[b], xT_[b], hT_[b]
            sw, a, g, kk, kp, tmp, tmp2, ss, bon, cs, e3, T4 = sw_[b], a_[b], g_[b], kk_[b], kp_[b], tmp_[b], tmp2_[b], ss_[b], bon_[b], cs_[b], e_[b], T4_[b]
            Bt, Kt, Vt, ART, BT, KT, ET, G, Wm, Pm, Qm, Xs, Ub, Os, oo = Bt_[b], Kt_[b], Vt_[b], ART_[b], BT_[b], KT_[b], ET_[b], G_[b], Wm_[b], Pm_[b], Qm_[b], Xs_[b], Ub_[b], Os_[b], oo_[b]
            sq = sq_[b]
            bdg = bdg_[b]
            Vm = Vm_[b]
            r0 = t * 128
            c.dma("sp", P[:], p0[r0:r0 + 128, 0:2048], reads=["p0"], writes=[K("Pmm")])
            if t == 0:
                c.op("pool", lambda e: e.memset(Ps[0:1, :], 0.0), writes=[K("Ps")])
                c.dma("sp", Ps[1:128, :], p0[0:127, 0:2048], reads=["p0"], writes=[K("Ps")])
            else:
                c.dma("sp", Ps[:], p0[r0 - 1:r0 + 127, 0:2048], reads=["p0"], writes=[K("Ps")])
            self.tt("dve", Ps[:], Ps[:], P[:], ALU.subtract, [K("Ps"), K("Pmm")], [K("Ps")])
            srcs = [0, 1, 2, 3, 3, 3]
            for i in range(6):
                eng = "dve" if i % 2 == 0 else "pool"
                sc = srcs[i] * 512
                self.tt(eng, X6[:, i, :], Ps[:, sc:sc + 512], MU(i), ALU.mult, [K("Ps"), "rkv"], [K("X6_%d" % i)])
                self.tt(eng, X6[:, i, :], X6[:, i, :], P[:, sc:sc + 512], ALU.add, [K("X6_%d" % i), K("Pmm")], [K("X6_%d" % i)])
            if self.cut == 1:
                return
            r, k, v = X6[:, 0, :], X6[:, 1, :], X6[:, 2, :]
            for i in range(3):
                self.transpose_in(xT, X6[:, 3 + i, :], 4, K("X6_%d" % (3 + i)), K("xT3"), ch0=4 * i)
            ps, pk = self.nps()
            for kc in range(4):
                self.mm(ps[0:64, 0:128], w1[:, kc, :], xT[:, kc, :], kc == 0, kc == 3, ["w1", K("xT3")], pk)
            for kc in range(4):
                self.mm(ps[0:64, 128:256], a1[:, kc, :], xT[:, 4 + kc, :], kc == 0, kc == 3, ["a1", K("xT3")], pk)
            for kc in range(4):
                self.mm(ps[:, 256:384], g1[:, kc, :], xT[:, 8 + kc, :], kc == 0, kc == 3, ["g1", K("xT3")], pk)
            self.act(hT[0:64, 0:128], ps[0:64, 0:128], AF.Tanh, [pk], [K("hT")])
            self.act(hT[0:64, 128:256], ps[0:64, 128:256], AF.Identity, [pk], [K("hT")])
            self.act(hT[:, 256:384], ps[:, 256:384], AF.Sigmoid, [pk], [K("hT")])
            ps, pk = self.nps()
            self.mm(ps[:, :], hT[0:64, 0:128], w2[:, :], True, True, [K("hT"), "w2"], pk)
            self.tt("dve", sw[:], ps[:, :], W0, ALU.add, [pk, "rkv"], [K("sw")])
            self.act(sw[:], sw[:], AF.Sigmoid, [K("sw")], [K("sw")])
            ps, pk = self.nps()
            self.mm(ps[:, :], hT[0:64, 128:256], a2[:, :], True, True, [K("hT"), "a2"], pk)
            self.tt("dve", a[:], ps[:, :], A0, ALU.add, [pk, "rkv"], [K("a")])
            self.act(a[:], a[:], AF.Sigmoid, [K("a")], [K("a")])
            ps, pk = self.nps()
            self.mm(ps[:, :], hT[:, 256:384], g2[:, :], True, True, [K("hT"), "g2"], pk)
            self.cp("act", g[:], ps[:, :], [pk], [K("g")])
            if self.cut == 2:
                return
            self.tt("pool", kk[:], k, KK_, ALU.mult, [K("X6_1"), "rkv"], [K("kk")])
            self.act(tmp[:], kk[:], AF.Square, [K("kk")], [K("tmp")])
            self.red("dve", ss[:], tmp[:].rearrange("p (h n) -> p h n", h=8), ALU.add, [K("tmp")], [K("ss")])
            self.rsqrt(ss[:], ss[:], 1e-24, [K("ss")], [K("ss")])
            kk3 = kk[:].rearrange("p (h n) -> p h n", h=8)
            self.tt("dve", kk3, kk3, ss[:].unsqueeze(2).to_broadcast([128, 8, 64]), ALU.mult, [K("kk"), K("ss")], [K("kk")])
            self.stt("pool", tmp[:], a[:], -1.0, KA_, ALU.add, ALU.mult, [K("a"), "rkv", K("tmp")], [K("tmp")])
            self.stt("pool", kp[:], tmp[:], 1.0, k, ALU.add, ALU.mult, [K("tmp"), K("X6_1")], [K("kp")])
            self.tt("dve", tmp[:], r, kp[:], ALU.mult, [K("X6_0"), K("kp")], [K("tmp")])
            self.tt("dve", tmp[:], tmp[:], RKk, ALU.mult, [K("tmp"), "rkv"], [K("tmp")])
            self.red("dve", ss[:], tmp[:].rearrange("p (h n) -> p h n", h=8), ALU.add, [K("tmp")], [K("ss")])
            self.tt("dve", bon[:].rearrange("p (h n) -> p h n", h=8), v.rearrange("p (h n) -> p h n", h=8),
                    ss[:].unsqueeze(2).to_broadcast([128, 8, 64]), ALU.mult, [K("X6_2"), K("ss")], [K("bon")])
            self.cp("act", Vt[:], v, [K("X6_2")], [K("Vt")])
            if self.cut == 3:
                return
            self.cp("act", swh[:, 0, :], sw[:], [K("sw")], [K("swh")])
            self.tt("pool", tmp[:], sw[:], swh[:, 0, :], ALU.subtract, [K("sw"), K("swh"), K("tmp")], [K("tmp")])
            self.cp("act", swh[:, 1, :], tmp[:], [K("tmp")], [K("swh")])
            ps, pk = self.nps()
            self.mm(ps[:, :], trib[:], swh[:, 0, :], True, False, ["trib", K("swh")], pk)
            self.mm(ps[:, :], trib[:], swh[:, 1, :], False, True, ["trib", K("swh")], pk)
            self.cp("dve", cs[:], ps[:, :], [pk], [K("cs")])
            self.act(e3[:, 0, :], cs[:], AF.Exp, [K("cs")], [K("e3")], scale=-RK_C)
            self.act(e3[:, 2, :], cs[:], AF.Exp, [K("cs")], [K("e3")], scale=RK_C)
            self.tt("dve", cs[:], cs[:], sw[:], ALU.subtract, [K("cs"), K("sw")], [K("cs")])
            self.act(e3[:, 1, :], cs[:], AF.Exp, [K("cs")], [K("e3")], scale=-RK_C)
            self.tt("dve", T4[:, 0, :], kk[:], e3[:, 1, :], ALU.mult, [K("kk"), K("e3")], [K("T4")])
            self.tt("pool", T4[:, 1, :], r, e3[:, 0, :], ALU.mult, [K("X6_0"), K("e3")], [K("T4")])
            self.tt("dve", tmp[:], kk[:], a[:], ALU.mult, [K("kk"), K("a"), K("tmp")], [K("tmp")])
            self.tt("dve", T4[:, 2, :], tmp[:], e3[:, 2, :], ALU.mult, [K("tmp"), K("e3")], [K("T4")])
            self.tt("pool", T4[:, 3, :], kp[:], e3[:, 2, :], ALU.mult, [K("kp"), K("e3")], [K("T4")])
            self.cp("act", Bt[:], T4[:, 2, :], [K("T4")], [K("Bt")])
            self.cp("act", Kt[:], T4[:, 3, :], [K("T4")], [K("Kt")])
            if self.cut == 4:
                d4 = self.dscr("dbgT4", [128, 2048])
                d3 = self.dscr("dbge3", [128, 1536])
                dsw = self.dscr("dbgsw", [128, 512])
                c.dma("sp", d4[:, :], T4[:].rearrange("p i n -> p (i n)"), reads=[K("T4")], writes=["dbgT4"])
                c.dma("sp", d3[:, :], e3[:].rearrange("p i n -> p (i n)"), reads=[K("e3")], writes=["dbge3"])
                c.dma("sp", dsw[:, :], sw[:], reads=[K("sw")], writes=["dbgsw"])
                return
            ART4 = ART[:].rearrange("p j (a t) -> p j a t", a=2)
            self.transpose_in(ART4[:, :, 0, :], T4[:, 0, :], 4, K("T4"), K("ART"))
            self.transpose_in(ART4[:, :, 1, :], T4[:, 1, :], 4, K("T4"), K("ART"))
            self.transpose_in(BT, T4[:, 2, :], 4, K("T4"), K("BT"))
            self.transpose_in(KT, T4[:, 3, :], 4, K("T4"), K("KT"))
            if self.cut in (45, 46):
                return
            self.transpose_in(ET, e3[:, 0, :], 4, K("e3"), K("ET"))
            if self.cut == 5:
                return
            for h in range(8):
                j, po = h // 2, (h % 2) * 64
                ps, pk = self.nps()
                self.mm(ps[:, 0:256], BT[po:po + 64, j, :], ART[po:po + 64, j, :], True, True, [K("BT"), K("ART")], pk)
                self.mm(ps[:, 256:512], KT[po:po + 64, j, :], ART[po:po + 64, j, :], True, True, [K("KT"), K("ART")], pk)
                self.tt("dve", G[:, h, :], ps[:, :], mask4[:], ALU.mult, [pk, "mask4"], [K("G%d" % h)])
            for par in range(2):
                ps, pk = self.nps()
                for hh in range(4):
                    h = 2 * hh + par
                    j, po = h // 2, par * 64
                    self.mm(ps[:, hh * 128:(hh + 1) * 128], ART[po:po + 64, j, 0:128], BT[po:po + 64, j, :], True, True,
                            [K("BT"), K("ART")], pk)
                self.tt("dve", Qm[:, par:8:2, :], ps[:, :].rearrange("p (h t) -> p h t", h=4),
                        maskL[:].unsqueeze(1).to_broadcast([128, 4, 128]), ALU.mult, [pk, "maskL"], [K("Q")])
            for h in range(8):
                self.tt("pool", Wm[:, h, :], identb[:], G[:, h, 0:128], ALU.subtract, ["identb", K("G%d" % h)], [K("W")])
            Pg = [None, None]
            for lvl in range(1, 6):
                for hq in range(2):
                    hsl = slice(hq * 4, (hq + 1) * 4)
                    psq, pkq = self.nps()
                    psp, pkp = (self.nps() if lvl < 5 else (None, None))
                    for hh in range(4):
                        h = hq * 4 + hh
                        Pc = G[:, h, 0:128] if Pg[hq] is None else Pm[:, h, :]
                        pkey = K("G%d" % h) if Pg[hq] is None else K("Pmm")
                        csl = slice(hh * 128, (hh + 1) * 128)
                        self.mm(psq[:, csl], Pc, Qm[:, h, :], True, True, [pkey, K("Q")], pkq)
                        if lvl < 5:
                            self.mm(psp[:, csl], Qm[:, h, :], Pc, True, True, [pkey, K("Q")], pkp)
                    self.cp("act", Qm[:, hsl, :], psq[:, :].rearrange("p (h t) -> p h t", h=4), [pkq], [K("Q")])
                    if lvl < 5:
                        self.cp("dve", Pm[:, hsl, :], psp[:, :].rearrange("p (h t) -> p h t", h=4), [pkp], [K("Pmm")])
                        Pg[hq] = 1
                for hq in range(2):
                    hsl = slice(hq * 4, (hq + 1) * 4)
                    psw, pkw = self.nps()
                    for hh in range(4):
                        h = hq * 4 + hh
                        self.mm(psw[:, hh * 128:(hh + 1) * 128], Qm[:, h, :], Wm[:, h, :], True, True, [K("Q"), K("W")], pkw)
                    self.tt("dve", Wm[:, hsl, :], Wm[:, hsl, :], psw[:, :].rearrange("p (h t) -> p h t", h=4), ALU.add,
                            [pkw, K("W")], [K("W")])
            if self.cut == 6:
                return
            for cc in range(2):
                self.tt("pool", bdg[cc][:], bd[:].unsqueeze(1).to_broadcast([128, 4, 128]),
                        ET[:, :, cc * 64 + 63:cc * 64 + 64].to_broadcast([128, 4, 128]), ALU.mult, ["bd", K("ET")], [K("bdg%d" % cc)])
            self.cp("act", Vm[0][0:64, :], v[0:64, :], [K("X6_2")], [K("Vm0")])
            self.cp("act", Vm[1][64:128, :], v[64:128, :], [K("X6_2")], [K("Vm1")])
            for cc in range(2):
                q0 = cc * 64
                rs = slice(q0, q0 + 64)
                Ubc, Vtc = Ub[cc], Vm[cc]
                ku, kv = K("Ub%d_" % cc), K("Vm%d" % cc)
                psX, pkX = self.nps()
                for j in range(4):
                    self.mm(psX[:, j * 128:(j + 1) * 128], ART[:, j, 0:128], Hb[:, j, :], True, False, [K("ART"), "Hb"], pkX)
                    for h in (2 * j, 2 * j + 1):
                        hs = slice(h * 64, h * 64 + 64)
                        self.mm(psX[:, hs], G[:, h, 256:384], Vt[:, hs], False, h == 2 * j + 1, [K("G%d" % h), K("Vt")], pkX)
                self.ts("dve", Xs[rs, :], psX[rs, :], -1.0, ALU.mult, [pkX], [K("Xs")])
                psU, pkU = self.nps()
                for h in range(8):
                    hs = slice(h * 64, h * 64 + 64)
                    self.mm(psU[:, hs], Wm[:, h, :], Xs[:, hs], True, True, [K("W"), K("Xs")], pkU)
                self.cp("act", Ubc[rs, :], psU[rs, :], [pkU], [ku])
                psO, pkO = self.nps()
                for j in range(4):
                    self.mm(psO[:, j * 128:(j + 1) * 128], ART[:, j, 128:256], Hb[:, j, :], True, False, [K("ART"), "Hb"], pkO)
                    for h in (2 * j, 2 * j + 1):
                        hs = slice(h * 64, h * 64 + 64)
                        self.mm(psO[:, hs], G[:, h, 128:256], Ubc[:, hs], False, False, [K("G%d" % h), ku], pkO)
                        self.mm(psO[:, hs], G[:, h, 384:512], Vt[:, hs], False, h == 2 * j + 1, [K("G%d" % h), K("Vt")], pkO)
                self.cp("act", Os[rs, :], psO[rs, :], [pkO], [K("Os")])
                psH, pkH = self.nps()
                for j in range(4):
                    js = slice(j * 128, (j + 1) * 128)
                    self.mm(psH[:, js], Bt[:, js], Ubc[:, js], True, False, [K("Bt"), ku], pkH)
                    self.mm(psH[:, js], Kt[:, js], Vtc[:, js], False, True, [K("Kt"), kv], pkH)
                H2 = H[:].rearrange("p j v -> p (j v)")
                self.tt("dve", H2, H2, psH[:, :], ALU.add, [pkH, "H"], ["H"])
                self.tt("dve", H[:], H[:], bdg[cc][:], ALU.mult, ["H", K("bdg%d" % cc)], ["H"])
                self.cp("act", Hb[:].rearrange("p j v -> p (j v)"), H2, ["H"], ["Hb"])
            O3 = Os[:].rearrange("p (h n) -> p h n", h=8)
            self.red("dve", sq[:], O3, ALU.add, [K("Os")], [K("sq")])
            self.ts("dve", sq[:], sq[:], 1.0 / 64, ALU.mult, [K("sq")], [K("sq")])
            self.tt("dve", O3, O3, sq[:].unsqueeze(2).to_broadcast([128, 8, 64]), ALU.subtract, [K("Os"), K("sq")], [K("Os")])
            self.act(tmp2[:], Os[:], AF.Square, [K("Os")], [K("tq")])
            self.red("dve", sq[:], tmp2[:].rearrange("p (h n) -> p h n", h=8), ALU.add, [K("tq")], [K("sq")])
            self.rsqrt(sq[:], sq[:], 64e-5, [K("sq")], [K("sq")], scale=1.0 / 64)
            self.tt("dve", O3, O3, sq[:].unsqueeze(2).to_broadcast([128, 8, 64]), ALU.mult, [K("Os"), K("sq")], [K("Os")])
            self.tt("pool", Os[:], Os[:], LNG, ALU.mult, [K("Os"), "rkv"], [K("Os")])
            self.tt("pool", Os[:], Os[:], LNB, ALU.add, [K("Os"), "rkv"], [K("Os")])
            self.tt("dve", Os[:], Os[:], bon[:], ALU.add, [K("Os"), K("bon")], [K("Os")])
            self.tt("dve", oo[:], Os[:], g[:], ALU.mult, [K("Os"), K("g")], [K("oo")])
            c.dma("sp", oab[r0:r0 + 128, 0:512], oo[:], reads=[K("oo")], writes=["oab"])
        c.barrier()


B.stage_rwkv = stage_rwkv


def stage_nsa(self, p0, oab, W):
    c = self.c
    S, NT = self.S, self.NT
    n_cmp = (S - 32) // 16 + 1
    NCT = (n_cmp + 127) // 128
    NCP = NCT * 128
    NQ = S // 512
    QC, KC0, GC0 = 2048, 2560, 3328
    with ExitStack() as st:
        sb = lambda n, shp, dt=F32: self.sb(st, n, shp, dt)
        identb = sb("identb", [128, 128], BF16)
        c.dma("pool", identb[:], W["c_ident"][:, :], writes=["identb"])
        KcT = sb("KcT", [128, NCP], BF16)
        Vca = sb("Vca", [128, NCT, 2, 193], BF16)
        c.op("pool", lambda e: e.memset(KcT[:], 0.0), writes=["KcT"])
        c.op("pool", lambda e: e.memset(Vca[:], 0.0), writes=["Vca"])
        with ExitStack() as st2:
            sb2 = lambda n, shp, dt=F32: self.sb(st2, n, shp, dt)
            kvT = sb2("kvT", [128, 2, S], BF16)
            W1p = sb2("W1p", [128, 2, 2, 32, 128], BF16)
            c.op("pool", lambda e: e.memset(W1p[:].rearrange("p a g l h -> p (a g l h)"), 0.0), writes=["W1p"])
            for kv in range(2):
                for g in range(2):
                    c.dma("pool", W1p[g * 64:(g + 1) * 64, kv, g, :, :],
                          W["ns_c_w1"][kv].rearrange("(l d) h -> d l h", d=64), writes=["W1p"])
            w2k = sb2("w2k", [128, 2, 128], BF16)
            c.op("pool", lambda e: e.memset(w2k[:].rearrange("p g n -> p (g n)"), 0.0), writes=["w2k"])
            for g in range(2):
                c.dma("pool", w2k[:, g, g * 64:(g + 1) * 64], W["ns_c_w2"][0], writes=["w2k"])
            w2v = sb2("w2v", [128, 64], BF16)
            c.dma("pool", w2v[:], W["ns_c_w2"][1], writes=["w2v"])
            peT = sb2("peT", [128, 2, 32], BF16)
            c.dma("pool", peT[:].rearrange("p a l -> p (a l)"), W["c_peT"][:, :], writes=["peT"])
            ropeA = sb2("ropeA", [128, 16])
            pa = [sb2("pa", [128, 256]) for _ in range(2)]
            pr = [sb2("pra", [128, 256]) for _ in range(2)]
            tA = [sb2("tA", [128, 2, 8]) for _ in range(2)]
            for t in range(NT):
                b = t % 2
                r0 = t * 128
                c.dma("sp", pa[b][:], p0[r0:r0 + 128, KC0:KC0 + 256], reads=["p0"], writes=["pa%d" % b])
                c.dma("sp", ropeA[:], W["c_rope"][r0:r0 + 128, :], writes=["ropeA"])
                self.cp("pool", pr[b][:], pa[b][:], ["pa%d" % b], ["pra%d" % b])
                self._rope(pa[b][:, 0:128], pr[b][:, 0:128], 2, ropeA, tA[b], "pa%d" % b, "pra%d" % b, "ropeA", "tA%d" % b)
                self.transpose_in(kvT[:, :, r0:r0 + 128], pr[b], 2, "pra%d" % b, "kvT")
            hT = sb2("hTc", [128, NCP], BF16)
            c.op("pool", lambda e: e.memset(hT[:], 0.0), writes=["hTc"])
            bia = sb2("bia", [128, 1])
            xh = sb2("xh", [128, 512])
            x2 = sb2("x2", [128, 512])
            for kv in range(2):
                for g in range(2):
                    ps, pk = self.nps()
                    for l in range(32):
                        self.mm(ps[:, 0:1], W1p[:, kv, g, l, :], peT[:, kv, l:l + 1], l == 0, l == 31, ["W1p", "peT"], pk)
                    self.cp("dve", bia[:], ps[:, 0:1], [pk], ["bia"])
                    ps, pk = self.nps()
                    for l in range(32):
                        self.mm(ps[:, 0:n_cmp], W1p[:, kv, g, l, :], kvT[:, kv, l:l + 16 * (n_cmp - 1) + 1:16], l == 0, l == 31,
                                ["W1p", "kvT"], pk)
                    X = xh[:, 0:n_cmp]
                    Y = x2[:, 0:n_cmp]
                    self.act(X, ps[:, 0:n_cmp], AF.Identity, [pk, "bia"], ["xh"], bias=bia[:, 0:1])
                    self.tt("dve", Y, X, X, ALU.mult, ["xh"], ["x2"])
                    self.ts("dve", Y, Y, 0.044715, ALU.mult, ["x2"], ["x2"], s2=1.0, op1=ALU.add)
                    self.tt("dve", Y, Y, X, ALU.mult, ["x2", "xh"], ["x2"])
                    self.act(Y, Y, AF.Tanh, ["x2"], ["x2"], scale=0.7978845608)
                    self.stt("dve", hT[:, 0:n_cmp], Y, 1.0, X, ALU.add, ALU.mult, ["x2", "xh"], ["hTc"])
                    if kv == 0:
                        ps, pk = self.nps()
                        self.mm(ps[:, 0:n_cmp], w2k[:, g, :], hT[:, 0:n_cmp], True, True, ["w2k", "hTc"], pk)
                        if g == 0:
                            self.act(KcT[:, 0:n_cmp], ps[:, 0:n_cmp], AF.Identity, [pk], ["KcT"], scale=0.5)
                        else:
                            self.stt("dve", KcT[:, 0:n_cmp], ps[:, 0:n_cmp], 0.5, KcT[:, 0:n_cmp], ALU.mult, ALU.add, [pk, "KcT"], ["KcT"])
                    else:
                        for i in range(NCT):
                            ps, pk = self.nps()
                            self.mm(ps[:, 0:64], hT[:, i * 128:(i + 1) * 128], w2v[:], True, True, ["w2v", "hTc"], pk)
                            self.act(Vca[:, i, g, 0:64], ps[:, 0:64], AF.Identity, [pk], ["Vca"], scale=0.5)
            for i in range(NCT):
                for g in range(2):
                    c.dma("pool", Vca[:, i, g, 65:193], W["c_ov"][i * 128:(i + 1) * 128, :], writes=["Vca"])
                    c.dma("pool", Vca[:, i, g, 64:65], W["c_ones"][:, 0:1], writes=["Vca"])
            c.barrier()
        QT = sb("QT", [128, 4, S], BF16)
        KT2 = sb("KT2", [128, 2, S], BF16)
        Va = sb("Va", [128, NT, 2, 2, 65], BF16)
        Eo = sb("Eo", [128, S], BF16)
        c.dma("pool", Eo[:], W["c_E"][:, :], writes=["Eo"])
        caus = sb("caus", [128, 4, 512], BF16)
        winb = sb("winb", [128, 8, 512], BF16)
        cmpb = sb("cmpb", [128, 5, 512], BF16)
        c.dma("pool", caus[:].rearrange("p a q -> p (a q)"), W["c_caus"][:, :], writes=["caus"])
        c.dma("pool", winb[:].rearrange("p a q -> p (a q)"), W["c_win"][:, :], writes=["winb"])
        c.dma("pool", cmpb[:].rearrange("p a q -> p (a q)"), W["c_cmpb"][:, :], writes=["cmpb"])
        c.op("pool", lambda e: e.memset(Va[:].rearrange("p t a g d -> p (t a g d)"), 1.0), writes=["Va"])
        with ExitStack() as st2:
            sb2 = lambda n, shp, dt=F32: self.sb(st2, n, shp, dt)
            ropeB = sb2("ropeB", [128, 16])
            pn = [sb2("pn", [128, 1280]) for _ in range(2)]
            qp = [sb2("qp", [128, 512]) for _ in range(2)]
            kp = [sb2("kpn", [128, 256]) for _ in range(2)]
            tB = [sb2("tB", [128, 8, 8]) for _ in range(2)]
            for t in range(NT):
                b = t % 2
                r0 = t * 128
                kn, kq, kk_ = "pn%d" % b, "qp%d" % b, "kpn%d" % b
                c.dma("sp", pn[b][:], p0[r0:r0 + 128, QC:QC + 1280], reads=["p0"], writes=[kn])
                c.dma("sp", ropeB[:], W["c_rope"][r0:r0 + 128, :], writes=["ropeB"])
                qsrc = pn[b][:, 0:512].rearrange("p (g j d) -> p g j d", g=2, j=4)
                qdst = qp[b][:].rearrange("p (j g d) -> p g j d", g=2, j=4)
                self.cp("pool", qdst, qsrc, [kn], [kq])
                self._rope(qsrc, qdst, 8, ropeB, tB[b], kn, kq, "ropeB", "tB%d" % b, four=True)
                for a in range(2):
                    o = 512 + 256 * (a + 1)
                    self.cp("pool", kp[b][:, a * 128:(a + 1) * 128], pn[b][:, o:o + 128], [kn], [kk_])
                    self._rope(pn[b][:, o:o + 128], kp[b][:, a * 128:(a + 1) * 128], 2, ropeB, tB[b], kn, kk_, "ropeB", "tB%d" % b)
                    self.cp("act", Va[:, t, a, :, 0:64], pn[b][:, o + 128:o + 256].rearrange("p (g d) -> p g d", g=2), [kn], ["Va"])
                self.transpose_in(QT[:, :, r0:r0 + 128], qp[b], 4, kq, "QT")
                self.transpose_in(KT2[:, :, r0:r0 + 128], kp[b], 2, kk_, "KT2")
            c.barrier()
        Qh = [[sb("Qh", [128, 512], BF16) for _ in range(2)] for _ in range(2)]
        for g in range(2):
            for k_ in range(2):
                c.op("pool", lambda e: e.memset(Qh[g][k_][:], 0.0), writes=["Qh%d_%d" % (g, k_)])
        qh_i = [0]
        qcur = [None, None]
        PT = [sb("PT", [128, 512], BF16) for _ in range(3)]
        MbT = [sb("MbT", [128, 512], BF16) for _ in range(2)]
        acc = [sb("acc", [128, 512]) for _ in range(4)]
        imp = [[sb("imp", [128, 128]) for _ in range(4)] for _ in range(2)]
        sig = [sb("sig", [128, 24]) for _ in range(4)]
        selF = [sb("selF", [128, 128]) for _ in range(4)]
        rz4 = [sb("rz", [128, 2]) for _ in range(4)]
        ot4 = [sb("oto", [128, 193]) for _ in range(4)]
        m8 = sb("m8", [128, 16])
        pri = sb("pri", [128, 128])
        pri2 = sb("pri2", [128, 128])
        mb = sb("mb", [128, 128])
        SPS = [(self.ps[i], "ps%d" % i) for i in range(4)]
        APS = [(self.ps[4 + i], "ps%d" % (4 + i)) for i in range(4)]
        sps_i = [0]
        pt_i = [0]

        pending = []

        def flush_pv():
            while pending:
                P, pkey, vaug, nv, subs = pending.pop(0)
                for (sub, first, last) in subs:
                    aps, apk = APS[sub]
                    self.mm(aps[:, 0:nv], P[:, sub * 128:(sub + 1) * 128], vaug, first, last, [pkey, "Va", "Vca"], apk)

        def unit(h, Q, kT, kcols, bias_terms, vaug, nv, subs, started):
            g = h // 4
            ps, pk = SPS[sps_i[0] % 4]
            sps_i[0] += 1
            nb = len(bias_terms)
            self.mm(ps[:, :], kT, qcur[0][:], True, nb == 0, ["KcT", "KT2", qcur[1]], pk)
            for bi, (lt, rt, rk) in enumerate(bias_terms):
                self.mm(ps[:, :], lt, rt, False, bi == nb - 1, rk, pk)
            P = PT[pt_i[0] % 3]
            pkey = "PT%d" % (pt_i[0] % 3)
            pt_i[0] += 1
            self.act(P[:], ps[:, :], AF.Exp, [pk], [pkey], scale=0.125)
            flush_pv()
            pending.append((P, pkey, vaug, nv, subs))

        def finish_branch(h, br, nv, g=None, hh=None):
            hs = slice(h * 64, (h + 1) * 64)
            for sub in range(4):
                aps, apk = APS[sub]
                self.cp("dve", ot4[sub][:, 0:nv], aps[:, 0:nv], [apk], ["oto%d" % sub])
            for sub in range(4):
                ot = ot4[sub]
                ko, kr = "oto%d" % sub, "rz%d" % sub
                rz = rz4[sub]
                self.ts("dve", rz[:, 0:1], ot[:, 64:65], 1e-30, ALU.max, [ko], [kr])
                c.op("dve", lambda e: e.reciprocal(out=rz[:, 0:1], in_=rz[:, 0:1]), reads=[kr], writes=[kr])
                self.tt("dve", rz[:, 1:2], rz[:, 0:1], sig[sub][:, br * 8 + h:br * 8 + h + 1], ALU.mult, [kr, "sig%d" % sub], [kr])
                if br == 0:
                    self.ts("dve", acc[sub][:, hs], ot[:, 0:64], rz[:, 1:2], ALU.mult, [ko, kr], ["acc%d" % sub])
                    if hh == 0:
                        self.ts("dve", imp[g][sub][:], ot[:, 65:193], rz[:, 0:1], ALU.mult, [ko, kr], ["imp%d%d" % (g, sub)])
                    else:
                        self.stt("dve", imp[g][sub][:], ot[:, 65:193], rz[:, 0:1], imp[g][sub][:], ALU.mult, ALU.add,
                                 [ko, kr, "imp%d%d" % (g, sub)], ["imp%d%d" % (g, sub)])
                else:
                    self.stt("dve", acc[sub][:, hs], ot[:, 0:64], rz[:, 1:2], acc[sub][:, hs], ALU.mult, ALU.add,
                             [ko, kr, "acc%d" % sub], ["acc%d" % sub])

        for Q in range(NQ):
            q0 = Q * 512
            for sub in range(4):
                r0 = q0 + sub * 128
                c.dma("sp", sig[sub][:], p0[r0:r0 + 128, GC0:GC0 + 24], reads=["p0"], writes=["sig%d" % sub])
                self.act(sig[sub][:], sig[sub][:], AF.Sigmoid, ["sig%d" % sub], ["sig%d" % sub])
                c.dma("sp", selF[sub][:], W["c_selF"][r0:r0 + 128, :], writes=["selF%d" % sub])
            for g in range(2):
                for hh in range(4):
                    h = g * 4 + hh
                    k_ = qh_i[0] % 2
                    qh_i[0] += 1
                    qcur[0], qcur[1] = Qh[g][k_], "Qh%d_%d" % (g, k_)
                    self.cp("pool", Qh[g][k_][g * 64:(g + 1) * 64, :], QT[g * 64:(g + 1) * 64, hh, q0:q0 + 512], ["QT"], [qcur[1]])
                    started = [False] * 4
                    for i in range(NCT):
                        dj = Q - 4 * i
                        if dj < 0:
                            continue
                        bt = [] if dj > 4 else [(identb[:], cmpb[:, dj, :], ["identb", "cmpb"])]
                        imax = min(NCT - 1, Q // 4)
                        unit(h, Q, KcT[:, i * 128:(i + 1) * 128], None, bt, Vca[:, i, g, :], 193,
                             [(s_, i == 0, i == imax) for s_ in range(4)], started)
                    flush_pv()
                    finish_branch(h, 0, 193, g, hh)
                psM, pkM = SPS[sps_i[0] % 4]
                sps_i[0] += 1
                for sub in range(4):
                    self.tt("dve", pri[:], imp[g][sub][:], selF[sub][:], ALU.add, ["imp%d%d" % (g, sub), "selF%d" % sub], ["pri"])
                    c.op("dve", lambda e: e.max(out=m8[:, 0:8], in_=pri[:]), reads=["pri"], writes=["m8"])
                    c.op("dve", lambda e: e.match_replace(out=pri2[:], in_to_replace=m8[:, 0:8], in_values=pri[:], imm_value=-1e9),
                         reads=["pri", "m8"], writes=["pri2"])
                    c.op("dve", lambda e: e.max(out=m8[:, 8:16], in_=pri2[:]), reads=["pri2"], writes=["m8"])
                    self.ts("dve", mb[:], pri[:], m8[:, 15:16], ALU.is_ge, ["pri", "m8"], ["mb"])
                    self.ts("dve", mb[:], mb[:], -1.0, ALU.add, ["mb"], ["mb"], s2=-NEG, op1=ALU.mult)
                    self.tr(psM[:, sub * 128:(sub + 1) * 128], mb[:], self.ident[:], ["mb", "ident"], pkM)
                self.cp("act", MbT[g][:], psM[:, :], [pkM], ["MbT%d" % g])
                for hh in range(4):
                    h = g * 4 + hh
                    k_ = qh_i[0] % 2
                    qh_i[0] += 1
                    qcur[0], qcur[1] = Qh[g][k_], "Qh%d_%d" % (g, k_)
                    self.cp("pool", Qh[g][k_][g * 64:(g + 1) * 64, :], QT[g * 64:(g + 1) * 64, hh, q0:q0 + 512], ["QT"], [qcur[1]])
                    started = [False] * 4
                    for kt in range(0, 4 * Q + 4):
                        d = kt - 4 * Q
                        bt = [(Eo[:, kt * 128:(kt + 1) * 128], MbT[g][:], ["Eo", "MbT%d" % g])]
                        if d >= 0:
                            bt.append((identb[:], caus[:, d, :], ["identb", "caus"]))
                        subs = [(s_, kt == 0, kt == 4 * Q + s_) for s_ in range(4) if s_ >= d]
                        unit(h, Q, KT2[:, 0, kt * 128:(kt + 1) * 128], None, bt, Va[:, kt, 0, g, :], 65, subs, started)
                    flush_pv()
                    finish_branch(h, 1, 65)
                    started = [False] * 4
                    for kt in range(max(0, 4 * Q - 4), 4 * Q + 4):
                        d = kt - 4 * Q
                        bt = [(identb[:], winb[:, d + 4, :], ["identb", "winb"])]
                        subs = [(s_, kt == max(0, 4 * Q + s_ - 4), kt == 4 * Q + s_) for s_ in range(4) if s_ - 4 <= d <= s_]
                        unit(h, Q, KT2[:, 1, kt * 128:(kt + 1) * 128], None, bt, Va[:, kt, 1, g, :], 65, subs, started)
                    flush_pv()
                    finish_branch(h, 2, 65)
            for sub in range(4):
                r0 = q0 + sub * 128
                c.dma("sp", oab[r0:r0 + 128, 512:1024], acc[sub][:], reads=["acc%d" % sub], writes=["oab"])
        c.barrier()


def _rope(self, src, dst, nh, rope, tmp, ksrc, kdst, krope, ktmp, four=False):
    if four:
        s4, d4 = src, dst
        x1, x2 = s4[:, :, :, 0:8], s4[:, :, :, 8:16]
        o1, o2 = d4[:, :, :, 0:8], d4[:, :, :, 8:16]
        cos = rope[:, 0:8].unsqueeze(1).unsqueeze(1).to_broadcast([128, 2, 4, 8])
        sin = rope[:, 8:16].unsqueeze(1).unsqueeze(1).to_broadcast([128, 2, 4, 8])
        tm = tmp[:].rearrange("p (g j) d -> p g j d", g=2)
    else:
        s3 = src.rearrange("p (h d) -> p h d", h=nh)
        d3 = dst.rearrange("p (h d) -> p h d", h=nh)
        x1, x2 = s3[:, :, 0:8], s3[:, :, 8:16]
        o1, o2 = d3[:, :, 0:8], d3[:, :, 8:16]
        cos = rope[:, 0:8].unsqueeze(1).to_broadcast([128, nh, 8])
        sin = rope[:, 8:16].unsqueeze(1).to_broadcast([128, nh, 8])
        tm = tmp[:, 0:nh, :]
    self.tt("dve", o1, x1, cos, ALU.mult, [ksrc, krope], [kdst])
    self.tt("dve", tm, x2, sin, ALU.mult, [ksrc, krope], [ktmp])
    self.tt("dve", o1, o1, tm, ALU.subtract, [kdst, ktmp], [kdst])
    self.tt("dve", o2, x2, cos, ALU.mult, [ksrc, krope], [kdst])
    self.tt("dve", tm, x1, sin, ALU.mult, [ksrc, krope], [ktmp])
    self.tt("dve", o2, o2, tm, ALU.add, [kdst, ktmp], [kdst])


B.stage_nsa = stage_nsa
B._rope = _rope


def ln_tile(self, z, zkey, lnp, out, okey, tl):
    s1, zc, sq = tl
    stats, mv, rstd, nb = s1[:, 0:12], s1[:, 12:14], s1[:, 14:15], s1[:, 15:16]
    for i in range(2):
        self.c.op("dve", lambda e: e.bn_stats(out=s1[:, i * 6:(i + 1) * 6], in_=z[:, i * 512:(i + 1) * 512]),
                  reads=[zkey], writes=["ln_st"])
    self.c.op("dve", lambda e: e.bn_aggr(out=mv, in_=stats), reads=["ln_st"], writes=["ln_mv"])
    self.rsqrt(rstd, mv[:, 1:2], LN_EPS, ["ln_mv"], ["ln_rs"])
    self.stt("dve", nb, mv[:, 0:1], -1.0, rstd, ALU.mult, ALU.mult, ["ln_mv", "ln_rs"], ["ln_nb"])
    self.act(zc[:], z, AF.Identity, [zkey, "ln_rs", "ln_nb"], ["ln_zc"], bias=nb, scale=rstd)
    self.tt("dve", zc[:], zc[:], lnp[:, 0:D], ALU.mult, ["ln_zc", "lnp"], ["ln_zc"])
    self.tt("dve", out, zc[:], lnp[:, D:2 * D], ALU.add, ["ln_zc", "lnp"], [okey])


def stage_mix(self, src, K, w_ap, resid, lnp_ap, dst):
    c = self.c
    with ExitStack() as st:
        sb = lambda n, shp, dt=F32: self.sb(st, n, shp, dt)
        wb = sb("wmix", [128, K // 128, D], BF16)
        self.load_w_fast(st, wb, w_ap, K, D, "wmix")
        lnp = sb("lnp", [128, 2 * D])
        c.dma("sp", lnp[:], lnp_ap[:, :], writes=["lnp"])
        tl = (sb("ln_s", [128, 16]), sb("ln_zc", [128, D]), None)
        xin = [sb("min", [128, K]) for _ in range(2)]
        xT = [sb("mxT", [128, K // 128, 128], BF16) for _ in range(2)]
        rs = [sb("mrs", [128, D]) for _ in range(2)]
        z = [sb("mz", [128, D]) for _ in range(2)]
        o = [sb("mo", [128, D]) for _ in range(2)]
        for t in range(self.NT):
            b = t % 2
            r0 = t * 128
            c.dma("sp", xin[b][:], src[r0:r0 + 128, :], reads=[src.tensor.name], writes=["min%d" % b])
            c.dma("sp", rs[b][:], resid[r0:r0 + 128, :], reads=[resid.tensor.name], writes=["mrs%d" % b])
            self.transpose_in(xT[b], xin[b], K // 128, "min%d" % b, "mxT%d" % b)
            for half in range(2):
                ps, pk = self.nps()
                for kc in range(K // 128):
                    self.mm(ps[:, :], xT[b][:, kc, :], wb[:, kc, half * 512:(half + 1) * 512], kc == 0, kc == K // 128 - 1,
                            ["mxT%d" % b, "wmix"], pk)
                self.stt("dve", z[b][:, half * 512:(half + 1) * 512], rs[b][:, half * 512:(half + 1) * 512], ALPHA, ps[:, :],
                         ALU.mult, ALU.add, [pk, "mrs%d" % b], ["mz%d" % b])
            self.ln_tile(z[b][:], "mz%d" % b, lnp, o[b][:], "mo%d" % b, tl)
            c.dma("sp", dst[r0:r0 + 128, :], o[b][:], reads=["mo%d" % b], writes=[dst.tensor.name])
        c.barrier()


def moe_cap(S):
    m = (S // 8) * 3 // 2
    return ((m + 511) // 512) * 512


def stage_moe(self, xin, W, layer, lnp_ap, dst, toklist, ybuf):
    c = self.c
    S, NT = self.S, self.NT
    CAP = moe_cap(S)
    NG = CAP // 512
    with ExitStack() as st:
        sb = lambda n, shp, dt=F32: self.sb(st, n, shp, dt)
        slotAB = sb("slotAB", [128, NT, 2], I32)
        wAB = sb("wAB", [128, NT, 2])
        tokid = sb("tokid", [128, NT, 16], I32)
        c.dma("sp", tokid[:].rearrange("p t r -> p (t r)"), W["c_tokid"][:, :], writes=["tokid"])
        c.dma("sp", toklist[:, :], W["c_tokinit"][:, :], reads=["toklist"], writes=["toklist"])
        with ExitStack() as st2:
            sb2 = lambda n, shp, dt=F32: self.sb(st2, n, shp, dt)
            rw = sb2("rw", [128, 8, 16])
            c.dma("sp", rw[:], W["router_w"].rearrange("(c p) e -> p c e", p=128), writes=["rw"])
            rb = sb2("rb", [128, 128])
            c.dma("sp", rb[:], W["c_rb"][:, :], writes=["rb"])
            ebase = sb2("ebase", [128, 128])
            c.dma("sp", ebase[:], W["c_ebase"][:, :], writes=["ebase"])
            SU = sb2("SU", [128, 128], BF16)
            ONES = sb2("ONESm", [128, 128], BF16)
            c.dma("pool", SU[:], W["c_su"][:, :], writes=["SU"])
            c.op("pool", lambda e: e.memset(ONES[:], 1.0), writes=["ONESm"])
            offs = sb2("offs", [128, 16])
            c.op("pool", lambda e: e.memset(offs[:], 0.0), writes=["offs"])
            TB = min(8, NT)
            TE = TB * 16
            xt = [sb2("rxt", [128, D]) for _ in range(2)]
            xT = [sb2("rxT", [128, 8, 128]) for _ in range(2)]
            aff = sb2("aff", [128, TE])
            s = sb2("s", [128, TE])
            s2 = sb2("s2", [128, TE])
            eq = sb2("eq", [128, TE])
            m1 = sb2("m1", [128, TB * 4])
            m2 = sb2("m2", [128, TB * 4])
            gs = sb2("gs", [128, TB * 4])
            gm = sb2("gm", [128, 2, TB])
            sel = sb2("sel", [128, TE])
            selb = sb2("selb", [128, TE], BF16)
            gate = sb2("gate", [128, TE])
            val = sb2("val", [128, TE])
            offT = sb2("offT", [128, TE])
            sl = sb2("sl", [128, 2, TB])
            g4 = lambda ap: ap.rearrange("p (a e) -> p a e", e=4)
            t16 = lambda ap: ap.rearrange("p (t e) -> p t e", e=16)
            bc4 = lambda ap: ap.unsqueeze(2).to_broadcast([128, TB * 4, 4])
            bc16 = lambda ap: ap.unsqueeze(2).to_broadcast([128, TB, 16])
            n = 0
            for tb in range(NT // TB):
                for i in range(TB):
                    t = tb * TB + i
                    b = n % 2
                    n += 1
                    r0 = t * 128
                    c.dma("sp", xt[b][:], xin[r0:r0 + 128, :], reads=[xin.tensor.name], writes=["rxt%d" % b])
                    self.transpose_in(xT[b], xt[b], 8, "rxt%d" % b, "rxT%d" % b)
                    psL, pkL = self.nps()
                    for kc in range(8):
                        self.mm(psL[:, 0:16], xT[b][:, kc, :], rw[:, kc, :], kc == 0, kc == 7, ["rxT%d" % b, "rw"], pkL)
                    self.act(aff[:, i * 16:(i + 1) * 16], psL[:, 0:16], AF.Sigmoid, [pkL], ["aff"])
                self.tt("dve", s[:], aff[:], rb[:, 0:TE], ALU.add, ["aff", "rb"], ["s"])
                self.red("dve", m1[:], g4(s[:]), ALU.max, ["s"], ["m1"])
                self.tt("dve", g4(eq[:]), g4(s[:]), bc4(m1[:]), ALU.is_ge, ["s", "m1"], ["eq"])
                self.stt("dve", s2[:], eq[:], -1e9, s[:], ALU.mult, ALU.add, ["eq", "s"], ["s2"])
                self.red("dve", m2[:], g4(s2[:]), ALU.max, ["s2"], ["m2"])
                self.tt("dve", gs[:], m1[:], m2[:], ALU.add, ["m1", "m2"], ["gs"])
                gs3 = gs[:].rearrange("p (t g) -> p t g", g=4)
                self.red("dve", gm[:, 0, :], gs3, ALU.max, ["gs"], ["gm"])
                self.tt("dve", gs3, gs3, gm[:, 0, :].unsqueeze(2).to_broadcast([128, TB, 4]), ALU.is_ge, ["gs", "gm"], ["gs"])
                self.tt("dve", g4(sel[:]), g4(s[:]), bc4(m2[:]), ALU.is_ge, ["s", "m2"], ["sel"])
                self.tt("dve", g4(sel[:]), g4(sel[:]), bc4(gs[:]), ALU.mult, ["sel", "gs"], ["sel"])
                self.tt("dve", gate[:], aff[:], sel[:], ALU.mult, ["aff", "sel"], ["gate"])
                self.red("dve", gm[:, 1, :], t16(gate[:]), ALU.add, ["gate"], ["gm"])
                c.op("dve", lambda e: e.reciprocal(out=gm[:, 1, :], in_=gm[:, 1, :]), reads=["gm"], writes=["gm"])
                self.tt("dve", t16(gate[:]), t16(gate[:]), bc16(gm[:, 1, :]), ALU.mult, ["gate", "gm"], ["gate"])
                self.cp("dve", selb[:], sel[:], ["sel"], ["selb"])
                psC, pkC = self.nps()
                for i in range(TB):
                    self.mm(psC[:, i * 16:(i + 1) * 16], SU[:], selb[:, i * 16:(i + 1) * 16], True, True, ["SU", "selb"], pkC)
                    self.mm(psC[:, 128 + i * 16:128 + (i + 1) * 16], ONES[:], selb[:, i * 16:(i + 1) * 16], True, True, ["ONESm", "selb"], pkC)
                self.cp("dve", offT[:, 0:16], offs[:], ["offs"], ["offT"])
                for i in range(1, TB):
                    self.tt("dve", offT[:, i * 16:(i + 1) * 16], offT[:, (i - 1) * 16:i * 16], psC[:, 128 + (i - 1) * 16:128 + i * 16],
                            ALU.add, [pkC, "offT"], ["offT"])
                self.tt("dve", offs[:], offT[:, (TB - 1) * 16:TB * 16], psC[:, 128 + (TB - 1) * 16:128 + TB * 16], ALU.add,
                        [pkC, "offT"], ["offs"])
                self.tt("dve", val[:], offT[:], psC[:, 0:TE], ALU.add, [pkC, "offT"], ["val"])
                self.ts("dve", val[:], val[:], float(CAP - 1), ALU.min, ["val"], ["val"])
                self.tt("dve", val[:], val[:], ebase[:, 0:TE], ALU.add, ["val", "ebase"], ["val"])
                self.tt("dve", val[:], val[:], sel[:], ALU.mult, ["val", "sel"], ["val"])
                self.ts("dve", val[:], val[:], -1.0, ALU.add, ["val"], ["val"])
                ts_ = slice(tb * TB, (tb + 1) * TB)
                for j in range(2):
                    self.red("dve", sl[:, j, :], t16(val[:]), ALU.max, ["val"], ["sl"])
                    self.tt("dve", t16(eq[:]), t16(val[:]), bc16(sl[:, j, :]), ALU.is_equal, ["val", "sl"], ["eq"])
                    self.tt("dve", s2[:], eq[:], gate[:], ALU.mult, ["eq", "gate"], ["s2"])
                    self.red("dve", wAB[:, ts_, j], t16(s2[:]), ALU.add, ["s2"], ["wAB"])
                    self.cp("dve", slotAB[:, ts_, j], sl[:, j, :], ["sl"], ["slotAB"])
                    if j == 0:
                        self.stt("dve", val[:], eq[:], -1e9, val[:], ALU.mult, ALU.add, ["eq", "val"], ["val"])
                if "dbg_aff" in self.dbg and tb == 0 and layer == 0:
                    for nm, tl_, w_ in (("dbg_aff", aff, TE), ("dbg_offT", offT, TE), ("dbg_gate", gate, TE), ("dbg_sel", sel, TE)):
                        dd = self.dscr(nm, [128, w_])
                        c.dma("sp", dd[:, :], tl_[:, 0:w_], reads=["aff", "offT", "gate", "sel"], writes=[nm])
                    dd = self.dscr("dbg_slotAB", [128, NT * 2], I32)
                    c.dma("sp", dd[:, :], slotAB[:].rearrange("p t j -> p (t j)"), reads=["slotAB"], writes=["dbg_slotAB"])
                    dd = self.dscr("dbg_wAB", [128, NT * 2])
                    c.dma("sp", dd[:, :], wAB[:].rearrange("p t j -> p (t j)"), reads=["wAB"], writes=["dbg_wAB"])
                    dd = self.dscr("dbg_sl", [128, 2 * TB])
                    c.dma("sp", dd[:, :], sl[:].rearrange("p a t -> p (a t)"), reads=["sl"], writes=["dbg_sl"])
                for i in range(TB):
                    t = tb * TB + i
                    for j in range(2):
                        c.dma("pool", toklist, tokid[:, t, :], reads=["tokid", "slotAB"], writes=["toklist"],
                              indirect=(bass.IndirectOffsetOnAxis(ap=slotAB[:, t, j:j + 1], axis=0), None))
            c.barrier()
        with ExitStack() as st2:
            sb2 = lambda n, shp, dt=F32: self.sb(st2, n, shp, dt)
            Wg = [sb2("Wg", [128, 8, D], BF16) for _ in range(2)]
            Wu = [sb2("Wu", [128, 8, D], BF16) for _ in range(2)]
            Wd = [sb2("Wd", [128, 8, D], BF16) for _ in range(2)]
            idx = [sb2("idx", [128, 16], I32) for _ in range(2)]
            X = [sb2("Xg", [128, D]) for _ in range(2)]
            xTg = [sb2("xTg", [128, 8, 512], BF16) for _ in range(2)]
            hs = [sb2("hs", [128, 512]) for _ in range(2)]
            hT = sb2("hTm", [128, 8, 512], BF16)
            ysb = [sb2("ysb", [128, D]) for _ in range(2)]
            n = 0
            gi = 0
            wstg = [sb2("wstg", [128, D]) for _ in range(8)]
            wsrc = [W["moe_w_gate"], W["moe_w_up"], W["moe_w_down"]]

            def chunk_dma(e, ci, si):
                m_, kc = ci // 8, ci % 8
                c.dma("sp", wstg[si][:], wsrc[m_][layer, e][kc * 128:(kc + 1) * 128, :], reads=[], writes=["mwstg%d" % si])

            def chunk_cast(e, ci, si):
                m_, kc = ci // 8, ci % 8
                dstw = (Wg, Wu, Wd)[m_][e % 2]
                self.cp("act" if ci % 2 == 0 else "dve", dstw[:, kc, :], wstg[si][:], ["mwstg%d" % si], ["W%d" % (e % 2)])

            for ci in range(24):
                chunk_dma(0, ci, ci % 8)
                chunk_cast(0, ci, ci % 8)
            per_slot = (24 + NG - 1) // NG
            for e in range(16):
                wbuf = e % 2
                kw = "W%d" % wbuf
                for grp in range(NG):
                    gb = gi % 2
                    gi += 1
                    nxt = [ci for ci in range(grp * per_slot, min(24, (grp + 1) * per_slot))] if e + 1 < 16 else []
                    if per_slot <= 8:
                        for k_, ci in enumerate(nxt):
                            chunk_dma(e + 1, ci, k_)
                    for i in range(4):
                        s0 = e * CAP + grp * 512 + i * 128
                        b = n % 2
                        n += 1
                        c.dma("pool", idx[b][:], toklist[s0:s0 + 128, :], reads=["toklist"], writes=["idx%d" % b])
                        c.dma("pool", X[b][:], xin, reads=[xin.tensor.name, "idx%d" % b], writes=["Xg%d" % b],
                              indirect=(None, bass.IndirectOffsetOnAxis(ap=idx[b][:, 0:1], axis=0)))
                        self.transpose_in(xTg[gb][:, :, i * 128:(i + 1) * 128], X[b], 8, "Xg%d" % b, "xTg%d" % gb)
                    for fc in range(8):
                        fs = slice(fc * 128, (fc + 1) * 128)
                        psG, pkG = self.nps()
                        for kc in range(8):
                            self.mm(psG[:, :], Wg[wbuf][:, kc, fs], xTg[gb][:, kc, :], kc == 0, kc == 7, [kw, "xTg%d" % gb], pkG)
                        psU, pkU = self.nps()
                        for kc in range(8):
                            self.mm(psU[:, :], Wu[wbuf][:, kc, fs], xTg[gb][:, kc, :], kc == 0, kc == 7, [kw, "xTg%d" % gb], pkU)
                        hb = fc % 2
                        self.act(hs[hb][:], psG[:, :], AF.Silu, [pkG], ["hs%d" % hb])
                        self.tt("dve", hT[:, fc, :], hs[hb][:], psU[:, :], ALU.mult, ["hs%d" % hb, pkU], ["hTm"])
                    for i in range(4):
                        s0 = e * CAP + grp * 512 + i * 128
                        yb = i % 2
                        for half in range(2):
                            ps, pk = self.nps()
                            for fc in range(8):
                                self.mm(ps[:, :], hT[:, fc, i * 128:(i + 1) * 128], Wd[wbuf][:, fc, half * 512:(half + 1) * 512],
                                        fc == 0, fc == 7, [kw, "hTm"], pk)
                            self.cp("act", ysb[yb][:, half * 512:(half + 1) * 512], ps[:, :], [pk], ["ysb%d" % yb])
                        c.dma("sp", ybuf[s0:s0 + 128, :], ysb[yb][:], reads=["ysb%d" % yb], writes=["ybuf"])
                    for k_, ci in enumerate(nxt):
                        if per_slot > 8:
                            chunk_dma(e + 1, ci, k_ % 8)
                        chunk_cast(e + 1, ci, k_ % 8)
            c.barrier()
        with ExitStack() as st2:
            sb2 = lambda n, shp, dt=F32: self.sb(st2, n, shp, dt)
            lnp = sb2("lnp", [128, 2 * D])
            c.dma("sp", lnp[:], lnp_ap[:, :], writes=["lnp"])
            tl = (sb2("ln_s", [128, 16]), sb2("ln_zc", [128, D]), None)
            xr = [sb2("cx", [128, D]) for _ in range(2)]
            yA = [sb2("cyA", [128, D]) for _ in range(2)]
            yB = [sb2("cyB", [128, D]) for _ in range(2)]
            z = [sb2("cz", [128, D]) for _ in range(2)]
            o = [sb2("co", [128, D]) for _ in range(2)]
            for t in range(NT):
                b = t % 2
                r0 = t * 128
                c.dma("sp", xr[b][:], xin[r0:r0 + 128, :], reads=[xin.tensor.name], writes=["cx%d" % b])
                c.dma("pool", yA[b][:], ybuf, reads=["ybuf", "slotAB"], writes=["cyA%d" % b],
                      indirect=(None, bass.IndirectOffsetOnAxis(ap=slotAB[:, t, 0:1], axis=0)))
                c.dma("pool", yB[b][:], ybuf, reads=["ybuf", "slotAB"], writes=["cyB%d" % b],
                      indirect=(None, bass.IndirectOffsetOnAxis(ap=slotAB[:, t, 1:2], axis=0)))
                self.ts("dve", z[b][:], xr[b][:], ALPHA, ALU.mult, ["cx%d" % b], ["cz%d" % b])
                self.stt("dve", z[b][:], yA[b][:], wAB[:, t, 0:1], z[b][:], ALU.mult, ALU.add, ["cyA%d" % b, "wAB", "cz%d" % b], ["cz%d" % b])
                self.stt("dve", z[b][:], yB[b][:], wAB[:, t, 1:2], z[b][:], ALU.mult, ALU.add, ["cyB%d" % b, "wAB", "cz%d" % b], ["cz%d" % b])
                self.ln_tile(z[b][:], "cz%d" % b, lnp, o[b][:], "co%d" % b, tl)
                c.dma("sp", dst[r0:r0 + 128, :], o[b][:], reads=["co%d" % b], writes=[dst.tensor.name])
            c.barrier()


B.ln_tile = ln_tile
B.stage_mix = stage_mix
B.stage_moe = stage_moe


def stage_ret(self, p1, ret, W):
    c = self.c
    NT = self.NT
    with ExitStack() as st:
        sb = lambda n, shp, dt=F32: self.sb(st, n, shp, dt)
        dec = sb("rtdec", [128, 8 * 128 + 24])
        c.dma("sp", dec[:], W["c_rtdec"][:, :], writes=["rtdec"])
        DT = lambda h: dec[:, h * 128:(h + 1) * 128]
        qd = dec[:, 1024:1032]
        kd = dec[:, 1032:1040]
        cd = dec[:, 1040:1048]
        gn = sb("rtgn", [128, 4096])
        c.dma("sp", gn[:], W["c_rtgn"][:, :], writes=["rtgn"])
        R = sb("R", [128, 8, 256])
        Rb = sb("Rb", [128, 8, 256], BF16)
        c.op("pool", lambda e: e.memset(R[:].rearrange("p h v -> p (h v)"), 0.0), writes=["R"])
        c.op("pool", lambda e: e.memset(Rb[:].rearrange("p h v -> p (h v)"), 0.0), writes=["Rb"])
        rope = sb("rtrope", [128, 256])
        Pq = [sb("Pq", [128, 2048]) for _ in range(2)]
        Vv = [sb("Vv", [128, 2048]) for _ in range(2)]
        Gg = [sb("Gg", [128, 2048]) for _ in range(2)]
        qk = sb("qkr", [128, 3, 1024])
        ktb = sb("ktb", [128, 1024], BF16)
        tmp = sb("rtmp", [128, 8, 64])
        T3 = sb("T3", [128, 24, 128], BF16)
        Vb = sb("Vb", [128, 2048], BF16)
        attm = sb("attm", [128, 1024], BF16)
        Os = sb("rOs", [128, 2048])
        sq = sb("rsq", [128, 2048])
        sg = sb("rsg", [128, 2048])
        st8 = sb("rst8", [128, 8])
        oo = sb("roo", [128, 2048])
        for t in range(NT):
            b = t % 2
            r0 = t * 128
            kp, kv, kg = "Pq%d" % b, "Vv%d" % b, "Gg%d" % b
            c.dma("sp", Pq[b][:], p1[r0:r0 + 128, 0:2048], reads=["p1"], writes=[kp])
            c.dma("sp", Vv[b][:], p1[r0:r0 + 128, 2048:4096], reads=["p1"], writes=[kv])
            c.dma("sp", Gg[b][:], p1[r0:r0 + 128, 4096:6144], reads=["p1"], writes=[kg])
            c.dma("sp", rope[:], W["c_rtrope"][r0:r0 + 128, :], writes=["rtrope"])
            for a in range(2):
                src = Pq[b][:, a * 1024:(a + 1) * 1024].rearrange("p (h d) -> p h d", h=8)
                dst = qk[:, a, :].rearrange("p (h d) -> p h d", h=8)
                cos = rope[:, a * 128:a * 128 + 64].unsqueeze(1).to_broadcast([128, 8, 64])
                sin = rope[:, a * 128 + 64:a * 128 + 128].unsqueeze(1).to_broadcast([128, 8, 64])
                x1, x2 = src[:, :, 0:64], src[:, :, 64:128]
                o1, o2 = dst[:, :, 0:64], dst[:, :, 64:128]
                kq = "qk%d" % a
                self.tt("dve", o1, x1, cos, ALU.mult, [kp, "rtrope"], [kq])
                self.tt("pool", tmp[:], x2, sin, ALU.mult, [kp, "rtrope"], ["rtmp"])
                self.tt("dve", o1, o1, tmp[:], ALU.subtract, [kq, "rtmp"], [kq])
                self.tt("dve", o2, x2, cos, ALU.mult, [kp, "rtrope"], [kq])
                self.tt("pool", tmp[:], x1, sin, ALU.mult, [kp, "rtrope"], ["rtmp"])
                self.tt("dve", o2, o2, tmp[:], ALU.add, [kq, "rtmp"], [kq])
            q3 = qk[:, 0, :].rearrange("p (h d) -> p h d", h=8)
            k3 = qk[:, 1, :].rearrange("p (h d) -> p h d", h=8)
            self.tt("pool", qk[:, 2, :].rearrange("p (h d) -> p h d", h=8), q3, qd.unsqueeze(2).to_broadcast([128, 8, 128]),
                    ALU.mult, ["qk0", "rtdec"], ["qk2"])
            self.tt("dve", ktb[:].rearrange("p (h d) -> p h d", h=8), k3, kd.unsqueeze(2).to_broadcast([128, 8, 128]),
                    ALU.mult, ["qk1", "rtdec"], ["ktb"])
            self.cp("act", Vb[:], Vv[b][:], [kv], ["Vb"])
            for a in range(3):
                self.transpose_in(T3, qk[:, a, :], 8, "qk%d" % a, "T3_%d" % a, ch0=8 * a)
            for hq in range(2):
                psA, pkA = self.nps()
                for hh in range(4):
                    h = hq * 4 + hh
                    self.mm(psA[:, hh * 128:(hh + 1) * 128], T3[:, 8 + h, :], T3[:, h, :], True, True, ["T3_0", "T3_1"], pkA)
                self.tt("dve", attm[:, hq * 512:(hq + 1) * 512], psA[:, :], dec[:, hq * 512:(hq + 1) * 512], ALU.mult,
                        [pkA, "rtdec"], ["attm%d" % hq])
            for hp in range(4):
                psO, pkO = self.nps()
                for hh in range(2):
                    h = hp * 2 + hh
                    vs = slice(h * 256, (h + 1) * 256)
                    self.mm(psO[:, hh * 256:(hh + 1) * 256], attm[:, h * 128:(h + 1) * 128], Vb[:, vs], True, False,
                            ["attm%d" % (h // 4), "Vb"], pkO)
                    self.mm(psO[:, hh * 256:(hh + 1) * 256], T3[:, 16 + h, :], Rb[:, h, :], False, True, ["T3_2", "Rb"], pkO)
                self.cp("act", Os[:, hp * 512:(hp + 1) * 512], psO[:, :], [pkO], ["rOs"])
            R2 = R[:].rearrange("p h v -> p (h v)")
            self.tt("pool", R[:], R[:], cd.unsqueeze(2).to_broadcast([128, 8, 256]), ALU.mult, ["R", "rtdec"], ["R"])
            for hp in range(4):
                psR, pkR = self.nps()
                for hh in range(2):
                    h = hp * 2 + hh
                    vs = slice(h * 256, (h + 1) * 256)
                    self.mm(psR[:, hh * 256:(hh + 1) * 256], ktb[:, h * 128:(h + 1) * 128], Vb[:, vs], True, True, ["ktb", "Vb"], pkR)
                self.tt("dve", R2[:, hp * 512:(hp + 1) * 512], R2[:, hp * 512:(hp + 1) * 512], psR[:, :], ALU.add, [pkR, "R"], ["R"])
            self.cp("act", Rb[:].rearrange("p h v -> p (h v)"), R2, ["R"], ["Rb"])
            O3 = Os[:].rearrange("p (h v) -> p h v", h=8)
            self.red("dve", st8[:], O3, ALU.add, ["rOs"], ["rst8"])
            self.ts("dve", st8[:], st8[:], 1.0 / 256, ALU.mult, ["rst8"], ["rst8"])
            self.tt("dve", O3, O3, st8[:].unsqueeze(2).to_broadcast([128, 8, 256]), ALU.subtract, ["rOs", "rst8"], ["rOs"])
            self.act(sq[:], Os[:], AF.Square, ["rOs"], ["rsq"])
            self.red("dve", st8[:], sq[:].rearrange("p (h v) -> p h v", h=8), ALU.add, ["rsq"], ["rst8"])
            self.rsqrt(st8[:], st8[:], 1e-5, ["rst8"], ["rst8"], scale=1.0 / 256)
            self.tt("dve", O3, O3, st8[:].unsqueeze(2).to_broadcast([128, 8, 256]), ALU.mult, ["rOs", "rst8"], ["rOs"])
            self.tt("dve", Os[:], Os[:], gn[:, 0:2048], ALU.mult, ["rOs", "rtgn"], ["rOs"])
            self.tt("pool", Os[:], Os[:], gn[:, 2048:4096], ALU.add, ["rOs", "rtgn"], ["rOs"])
            self.act(sg[:], Gg[b][:], AF.Silu, [kg], ["rsg"])
            self.tt("dve", oo[:], Os[:], sg[:], ALU.mult, ["rOs", "rsg"], ["roo"])
            c.dma("sp", ret[r0:r0 + 128, :], oo[:], reads=["roo"], writes=["ret"])
        c.barrier()


B.stage_ret = stage_ret


STAGES = ["proj0", "rwkv", "nsa", "mix0", "moe0", "proj1", "ret", "mix1", "moe1"]


def build(S, upto="moe1", dbg=()):
    b = B(S, dbg)
    n_st = STAGES.index(upto) + 1
    on = lambda s: STAGES.index(s) < n_st
    CAP = moe_cap(S)
    NSLOT = 16 * CAP
    ncp = ((((S - 32) // 16 + 1) + 127) // 128) * 128
    W = {}
    for name, shp, dt in [("c_ident", [128, 128], F32), ("c_tri", [128, 128], F32), ("c_mask4", [128, 512], F32),
                          ("c_maskL", [128, 128], F32), ("c_bd", [128, 128], F32),
                          ("c_rkv", [128, 13 * 512], F32), ("rk_w1", [512, 64], F32), ("rk_a1", [512, 64], F32),
                          ("rk_g1", [512, 128], F32), ("rk_w2", [64, 512], F32), ("rk_a2", [64, 512], F32),
                          ("rk_g2", [128, 512], F32),
                          ("ns_c_w1", [2, 2048, 128], F32), ("ns_c_w2", [2, 128, 64], F32), ("c_peT", [128, 64], F32),
                          ("c_ones", [128, 1], F32), ("c_rope", [S, 16], F32), ("c_selF", [S, 128], F32),
                          ("c_E", [128, S], F32), ("c_caus", [128, 4 * 512], F32), ("c_win", [128, 8 * 512], F32),
                          ("c_cmpb", [128, 5 * 512], F32), ("c_ov", [ncp, 128], F32),
                          ("ab_w_out", [D, D], F32), ("c_ln", [4, 128, 2 * D], F32),
                          ("router_w", [D, 16], F32), ("c_rb", [128, 128], F32), ("c_ebase", [128, 128], F32),
                          ("c_su", [128, 128], F32), ("c_tokid", [128, (S // 128) * 16], I32),
                          ("c_tokinit", [NSLOT + 1, 16], I32),
                          ("moe_w_gate", [2, 16, D, D], F32), ("moe_w_up", [2, 16, D, D], F32),
                          ("moe_w_down", [2, 16, D, D], F32),
                          ("rt_w_in", [D, 6144], F32), ("rt_w_out", [2048, D], F32), ("c_rtgn", [128, 2 * 2048], F32),
                          ("c_rtrope", [S, 256], F32), ("c_rtdec", [128, 8 * 128 + 24], F32)]:
        W[name] = b.din(name, shp, dt)
    x = b.din("x", [S, D])
    ab_w_in = b.din("ab_w_in", [D, 3352])
    p0 = b.dscr("p0", [S, 3352])
    oab = b.dscr("oab", [S, 1024])
    x1 = b.dscr("x1", [S + 1, D])
    x2 = b.dscr("x2", [S + 1, D])
    x3 = b.dscr("x3", [S + 1, D])
    p1 = b.dscr("p1", [S, 6144])
    ret = b.dscr("ret", [S, 2048])
    toklist = b.dscr("toklist", [NSLOT + 1, 16], I32)
    ybuf = b.dscr("ybuf", [NSLOT, D])
    out = b.nc.dram_tensor("out", [S, D], F32, kind="ExternalOutput").ap()
    with ExitStack() as st:
        b.load_consts(st)
        zrow = b.sb(st, "zrow", [1, D])
        b.c.op("pool", lambda e: e.memset(zrow[:], 0.0), writes=["zrow"])
        for xx in (x1, x3):
            b.c.dma("sp", xx[S:S + 1, :], zrow[:], reads=["zrow"], writes=[xx.tensor.name])
        b.stage_proj(x, ab_w_in, p0, D, 3352)
        if on("rwkv"):
            b.stage_rwkv(p0, oab, W)
        if on("nsa"):
            b.stage_nsa(p0, oab, W)
        if on("mix0"):
            b.stage_mix(oab, 1024, W["ab_w_out"], x, W["c_ln"][0], x1)
        if on("moe0"):
            b.stage_moe(x1, W, 0, W["c_ln"][1], x2, toklist, ybuf)
        if on("proj1"):
            b.stage_proj(x2, W["rt_w_in"], p1, D, 6144)
        if on("ret"):
            b.stage_ret(p1, ret, W)
        if on("mix1"):
            b.stage_mix(ret, 2048, W["rt_w_out"], x2, W["c_ln"][2], x3)
        if on("moe1"):
            b.stage_moe(x3, W, 1, W["c_ln"][3], out, toklist, ybuf)
        b.c.finish()
    return b


def consts(S):
    c = {}
    c["c_ident"] = np.eye(128, dtype=np.float32)
    i = np.arange(128)
    same = (i[:, None] // 64) == (i[None, :] // 64)
    strict = ((i[:, None] < i[None, :]) & same).astype(np.float32)
    incl = ((i[:, None] <= i[None, :]) & same).astype(np.float32)
    c["c_tri"] = incl
    c["c_mask4"] = np.concatenate([strict, incl, strict, incl], axis=1)
    c["c_bd"] = same.astype(np.float32)
    c["c_ones"] = np.ones((128, 1), np.float32)
    inv = 500000.0 ** (-np.arange(8, dtype=np.float32) / 8)
    ang = np.arange(S, dtype=np.float32)[:, None] * inv[None, :]
    c["c_rope"] = np.concatenate([np.cos(ang), np.sin(ang)], axis=1).astype(np.float32)
    tpos = np.arange(S)
    cur = tpos // 64
    jb = np.arange(128)
    F = np.zeros((S, 128), np.float32)
    F[jb[None, :] > cur[:, None]] = -10.0
    forced = (jb[None, :] == 0) | (jb[None, :] == cur[:, None]) | (jb[None, :] == cur[:, None] - 1)
    F[forced & (jb[None, :] <= cur[:, None])] = 10.0
    c["c_selF"] = F
    c["c_E"] = (np.arange(S)[None, :] // 64 == jb[:, None]).astype(np.float32)
    k = np.arange(128)[:, None]
    q = np.arange(512)[None, :]
    c["c_caus"] = np.concatenate([np.where(128 * d + k <= q, 0.0, NEG) for d in range(4)], axis=1).astype(np.float32)
    c["c_win"] = np.concatenate([np.where((128 * d + k <= q) & (128 * d + k > q - 512), 0.0, NEG) for d in range(-4, 4)],
                                axis=1).astype(np.float32)
    c["c_cmpb"] = np.concatenate([np.where(16 * k + 31 <= 512 * dj + q, 0.0, NEG) for dj in range(5)], axis=1).astype(np.float32)
    n_cmp = (S - 32) // 16 + 1
    ncp = ((n_cmp + 127) // 128) * 128
    cs = np.arange(ncp) * 16
    ss = np.arange(128) * 64
    ov = np.clip(np.minimum(cs[:, None] + 32, ss[None, :] + 64) - np.maximum(cs[:, None], ss[None, :]), 0, None).astype(np.float32) / 32
    ov[n_cmp:] = 0.0
    c["c_ov"] = ov
    CAP = moe_cap(S)
    c["c_ebase"] = np.ascontiguousarray(np.broadcast_to(np.tile((np.arange(16) * CAP + 1).astype(np.float32), 8)[None, :], (128, 128)))
    c["c_su"] = (i[:, None] < i[None, :]).astype(np.float32)
    NT = S // 128
    tok = (np.arange(NT)[None, :, None] * 128 + np.arange(128)[:, None, None] + np.zeros((1, 1, 16), np.int64))
    c["c_tokid"] = np.ascontiguousarray(tok.reshape(128, NT * 16)).astype(np.int32)
    inv = 10000.0 ** (-np.linspace(0.0, 1.0, 64, dtype=np.float32))
    ang = np.arange(S, dtype=np.float32)[:, None] * inv[None, :]
    cs_, sn_ = np.cos(ang), np.sin(ang)
    sc = 128.0 ** -0.5
    c["c_rtrope"] = np.concatenate([cs_, sn_, cs_ * sc, sn_ * sc], axis=1).astype(np.float32)
    log_g = np.log(1.0 - 2.0 ** (-5.0 - np.arange(8, dtype=np.float64)))
    ii = np.arange(128, dtype=np.float64)
    diff = ii[None, :] - ii[:, None]
    DTm = [np.where(diff >= 0, np.exp(np.maximum(diff, 0.0) * lg), 0.0) for lg in log_g]
    qd = np.exp((ii[:, None] + 1.0) * log_g[None, :])
    kd = np.exp((127.0 - ii[:, None]) * log_g[None, :])
    cd = np.broadcast_to(np.exp(128.0 * log_g)[None, :], (128, 8))
    c["c_rtdec"] = np.concatenate(DTm + [qd, kd, cd], axis=1).astype(np.float32)
    c["c_tokinit"] = np.full((16 * CAP + 1, 16), S, np.int32)
    c["c_maskL"] = np.ascontiguousarray(strict.T)
    return c


def derived(inputs):
    d = {}
    rk = np.concatenate([inputs["rk_mu"].reshape(-1), inputs["rk_w0"].reshape(-1), inputs["rk_a0"].reshape(-1),
                         inputs["rk_kk"].reshape(-1), inputs["rk_ka"].reshape(-1), inputs["rk_rk"].reshape(-1),
                         inputs["rk_ln"].reshape(-1)])
    pe = np.asarray(inputs["ns_pe"]).reshape(2, 32, 64)
    peT = np.transpose(pe, (2, 0, 1)).reshape(64, 64)
    d["c_peT"] = np.ascontiguousarray(np.concatenate([peT, peT], axis=0)).astype(np.float32)
    ln = np.asarray(inputs["ln"]).reshape(4, 2 * D)
    d["c_ln"] = np.ascontiguousarray(np.broadcast_to(ln[:, None, :], (4, 128, 2 * D))).astype(np.float32)
    d["c_rtgn"] = np.ascontiguousarray(np.broadcast_to(np.asarray(inputs["rt_gn"]).reshape(1, 4096), (128, 4096))).astype(np.float32)
    d["c_rb"] = np.ascontiguousarray(np.broadcast_to(np.tile(np.asarray(inputs["router_b"]).reshape(16), 8)[None, :], (128, 128))).astype(np.float32)
    d["c_rkv"] = np.ascontiguousarray(np.broadcast_to(rk[None, :], (128, rk.size))).astype(np.float32)
    return d


def make_inputs(b, inputs, bi, S):
    cs = consts(S)
    cs.update(derived(inputs))
    m = {}
    for name, ap in b.inp.items():
        if name in cs:
            m[name] = cs[name]
        elif name == "x":
            m[name] = np.ascontiguousarray(inputs["x"][bi, :S])
        else:
            a = np.asarray(inputs[name])
            m[name] = np.ascontiguousarray(a.reshape(ap.shape))
    return m


_BUILT = {}


def kernel(**inputs):
    S = 8192
    if S not in _BUILT:
        _BUILT[S] = build(S)
    b = _BUILT[S]
    shared = make_inputs(b, inputs, 0, S)
    in_maps = []
    for bi in range(8):
        m = dict(shared)
        m["x"] = np.ascontiguousarray(np.asarray(inputs["x"])[bi, :S]).astype(np.float32)
        in_maps.append(m)
    res = run_bass_kernel_spmd(b.nc, in_maps, core_ids=list(range(8)))
    return np.stack([np.asarray(r["out"]) for r in res.results], axis=0).astype(np.float32)
```

```python
import numpy as np
import ml_dtypes
from contextlib import ExitStack
import concourse.bass as bass
import concourse.mybir as mybir
from concourse.bass_utils import run_bass_kernel_spmd

F32 = mybir.dt.float32
BF16 = mybir.dt.bfloat16
I32 = mybir.dt.int32
U32 = mybir.dt.uint32
AF = mybir.ActivationFunctionType
ALU = mybir.AluOpType
AX = mybir.AxisListType

D = 1024
ALPHA = (2.0 * 2) ** 0.25
LN_EPS = 1e-5
NEG = -30000.0


class Ctx:
    NDMA = 10

    def __init__(self, nc):
        self.nc = nc
        self.eng = {"pe": nc.tensor, "dve": nc.vector, "act": nc.scalar, "pool": nc.gpsimd, "sp": nc.sync}
        self.sem = {}
        self.cnt = {}
        for e in self.eng:
            self.sem["e_" + e] = nc.alloc_semaphore("sem_e_" + e)
            self.cnt["e_" + e] = 0
        self.dma_pool = {}
        for q in ("sp", "pool", "act"):
            names = []
            for i in range(self.NDMA):
                n = "d_%s_%d" % (q, i)
                self.sem[n] = nc.alloc_semaphore("sem_" + n)
                self.cnt[n] = 0
                names.append(n)
            self.dma_pool[q] = [names, 0]
        self.known = {e: {} for e in self.eng}
        self.last_w = {}
        self.readers = {}
        self.n_inst = 0
        self.n_wait = 0

    def _wait(self, e, semname, val):
        kn = self.known[e]
        if kn.get(semname, 0) >= val:
            return
        self.eng[e].wait_ge(self.sem[semname], val)
        kn[semname] = val
        self.n_wait += 1

    def _deps(self, e, reads, writes, is_dma=False):
        own = "e_" + e if not is_dma else None
        need = {}

        def add(ev, raw):
            s, v = ev
            if s == own and e == "pe":
                return
            if need.get(s, 0) < v:
                need[s] = v

        for k in reads:
            ev = self.last_w.get(k)
            if ev is not None:
                add(ev, True)
        for k in writes:
            ev = self.last_w.get(k)
            if ev is not None:
                add(ev, False)
            for s, v in self.readers.get(k, {}).items():
                add((s, v), False)
        for s, v in need.items():
            self._wait(e, s, v)

    def _commit(self, ev, reads, writes):
        s, v = ev
        for k in writes:
            self.last_w[k] = ev
            self.readers[k] = {}
        for k in reads:
            if k in writes:
                continue
            r = self.readers.setdefault(k, {})
            if r.get(s, 0) < v:
                r[s] = v

    def op(self, e, fn, reads=(), writes=()):
        reads = list(reads)
        writes = list(writes)
        self._deps(e, reads, writes)
        ins = fn(self.eng[e])
        s = "e_" + e
        self.cnt[s] += 1
        ins.then_inc(self.sem[s], 1)
        self._commit((s, self.cnt[s]), reads, writes)
        self.n_inst += 1
        return ins

    def dma(self, q, out, in_, reads=(), writes=(), indirect=None, **kw):
        reads = list(reads)
        writes = list(writes)
        self._deps(q, reads, writes, is_dma=True)
        names, i = self.dma_pool[q]
        s = names[i % len(names)]
        self.dma_pool[q][1] = i + 1
        if self.cnt[s] > 0:
            self._wait(q, s, self.cnt[s])
        if indirect is None:
            ins = self.eng[q].dma_start(out=out, in_=in_, **kw)
        else:
            ins = self.eng[q].indirect_dma_start(out, indirect[0], in_, indirect[1], **kw)
        self.cnt[s] += 16
        ins.then_inc(self.sem[s], 16)
        self._commit((s, self.cnt[s]), reads, writes)
        self.n_inst += 1
        return ins

    def barrier(self):
        for e in self.eng:
            for s, c in self.cnt.items():
                if c > 0:
                    self._wait(e, s, c)

    def finish(self):
        for s, c in self.cnt.items():
            if c > 0:
                self._wait("sp", s, c)


class B:
    def __init__(self, S, dbg=()):
        self.S = S
        self.NT = S // 128
        self.dbg = set(dbg)
        nc = self.nc = bass.Bass("TRN2", target_bir_lowering=False)
        self.c = Ctx(nc)
        self.inp = {}
        self.ps = [nc.alloc_psum_tensor("psb%d" % i, [128, 512], F32) for i in range(8)]
        self.ps_i = 0
        self._uid = 0
        import os
        self.cut = int(os.environ['CUT']) if 'CUT' in os.environ else None

    def din(self, name, shape, dt=F32):
        t = self.nc.dram_tensor(name, list(shape), dt, kind="ExternalInput").ap()
        self.inp[name] = t
        return t

    def dscr(self, name, shape, dt=F32):
        kind = "ExternalOutput" if name in self.dbg else "Internal"
        return self.nc.dram_tensor(name, list(shape), dt, kind=kind).ap()

    def sb(self, st, name, shape, dt=F32):
        self._uid += 1
        return st.enter_context(self.nc.sbuf_tensor("%s_%d" % (name, self._uid), list(shape), dt))

    def nps(self):
        i = self.ps_i % 8
        self.ps_i += 1
        return self.ps[i], "ps%d" % i

    def mm(self, out, lhsT, rhs, start, stop, reads, pk):
        return self.c.op("pe", lambda e: e.matmul(out, lhsT, rhs, start=start, stop=stop), reads=reads, writes=[pk])

    def tr(self, out, in_, ident, reads, pk):
        return self.c.op("pe", lambda e: e.transpose(out, in_, ident), reads=reads, writes=[pk])

    def cp(self, eng, out, in_, reads, writes):
        if eng == "act":
            return self.c.op("act", lambda e: e.copy(out=out, in_=in_), reads=reads, writes=writes)
        return self.c.op(eng, lambda e: e.tensor_copy(out=out, in_=in_), reads=reads, writes=writes)

    def act(self, out, in_, func, reads, writes, bias=0.0, scale=1.0, accum_out=None):
        kw = {}
        if accum_out is not None:
            kw["accum_out"] = accum_out
        return self.c.op("act", lambda e: e.activation(out=out, in_=in_, func=func, bias=bias, scale=scale, **kw),
                         reads=reads, writes=writes)

    def tt(self, eng, out, in0, in1, op, reads, writes):
        return self.c.op(eng, lambda e: e.tensor_tensor(out=out, in0=in0, in1=in1, op=op), reads=reads, writes=writes)

    def ts(self, eng, out, in0, s1, op0, reads, writes, s2=None, op1=None):
        if op0 in (ALU.pow, ALU.divide) or op1 in (ALU.pow, ALU.divide):
            eng = "pool"
        if op1 is None:
            return self.c.op(eng, lambda e: e.tensor_scalar(out=out, in0=in0, scalar1=s1, scalar2=None, op0=op0),
                             reads=reads, writes=writes)
        return self.c.op(eng, lambda e: e.tensor_scalar(out=out, in0=in0, scalar1=s1, scalar2=s2, op0=op0, op1=op1),
                         reads=reads, writes=writes)

    def rsqrt(self, out, in_, eps, reads, writes, scale=1.0):
        self.act(out, in_, AF.Sqrt, reads, writes, bias=eps, scale=scale)
        return self.c.op("dve", lambda e: e.reciprocal(out=out, in_=out), reads=writes, writes=writes)

    def stt(self, eng, out, in0, scalar, in1, op0, op1, reads, writes):
        eng = "dve"
        return self.c.op(eng, lambda e: e.scalar_tensor_tensor(out=out, in0=in0, scalar=scalar, in1=in1, op0=op0, op1=op1),
                         reads=reads, writes=writes)

    def red(self, eng, out, in_, op, reads, writes, axis=AX.X):
        return self.c.op(eng, lambda e: e.tensor_reduce(out=out, in_=in_, axis=axis, op=op), reads=reads, writes=writes)

    def load_consts(self, st):
        self.ident = self.sb(st, "ident", [128, 128], F32)
        self.c.dma("sp", self.ident[:], self.inp["c_ident"][:, :], reads=[], writes=["ident"])

    def load_w(self, dst, w_ap, K, N, key, q="pool"):
        for kc in range(K // 128):
            for n0 in range(0, N, 2048):
                n1 = min(N, n0 + 2048)
                self.c.dma(q, dst[:, kc, n0:n1], w_ap[kc * 128:(kc + 1) * 128, n0:n1], reads=[], writes=[key])

    def load_w_fast(self, st, dst, w_ap, K, N, key):
        stg = [self.sb(st, "wstg", [128, 1024]) for _ in range(4)]
        i = 0
        for kc in range(K // 128):
            for n0 in range(0, N, 1024):
                n1 = min(N, n0 + 1024)
                s_ = stg[i % 4]
                sk = "wstg%d_%s" % (i % 4, key)
                self.c.dma("sp", s_[:, 0:n1 - n0], w_ap[kc * 128:(kc + 1) * 128, n0:n1], reads=[], writes=[sk])
                self.cp("act" if i % 2 == 0 else "dve", dst[:, kc, n0:n1], s_[:, 0:n1 - n0], [sk], [key])
                i += 1

    def transpose_in(self, xT, xin, nch, rkey, wkey, col0=0, ch0=0):
        j = 0
        k = 0
        while j < nch:
            g = min(4, nch - j)
            ps, pk = self.nps()
            for i in range(g):
                self.tr(ps[:, i * 128:(i + 1) * 128], xin[:, col0 + (j + i) * 128: col0 + (j + i + 1) * 128],
                        self.ident[:], [rkey, "ident"], pk)
            eng = "act" if k % 2 == 0 else "dve"
            self.cp(eng, xT[:, ch0 + j:ch0 + j + g, :], ps[:, 0:g * 128].rearrange("p (g t) -> p g t", g=g), [pk], [wkey])
            j += g
            k += 1

    def layer_norm(self, st_tiles, z, zkey, gam, bet, out, okey):
        stats, mv, rstd = st_tiles
        nc = self.nc
        c = self.c
        for i in range(2):
            c.op("dve", lambda e: e.bn_stats(out=stats[:, i, :], in_=z[:, i * 512:(i + 1) * 512]), reads=[zkey], writes=["ln_stats"])
        c.op("dve", lambda e: e.bn_aggr(out=mv[:], in_=stats[:]), reads=["ln_stats"], writes=["ln_mv"])
        self.rsqrt(rstd[:], mv[:, 1:2], LN_EPS, ["ln_mv"], ["ln_rstd"])
        self.ts("dve", out, z, mv[:, 0:1], ALU.subtract, [zkey, "ln_mv", "ln_rstd"], [okey], s2=rstd[:, 0:1], op1=ALU.mult)
        self.tt("pool", out, out, gam, ALU.mult, [okey, "lnp"], [okey])
        self.tt("pool", out, out, bet, ALU.add, [okey, "lnp"], [okey])

    def stage_proj(self, src, w_ap, dst, K, N):
        with ExitStack() as st:
            wb = self.sb(st, "wproj", [128, K // 128, N], BF16)
            self.load_w_fast(st, wb, w_ap, K, N, "wproj")
            xin = [self.sb(st, "xin", [128, K], F32) for _ in range(2)]
            xT = [self.sb(st, "xT", [128, K // 128, 128], BF16) for _ in range(2)]
            ot = [self.sb(st, "ot", [128, N], F32) for _ in range(2)]
            for t in range(self.NT):
                b = t % 2
                self.c.dma("sp", xin[b][:], src[t * 128:(t + 1) * 128, :], reads=[src.tensor.name], writes=["xin%d" % b])
                self.transpose_in(xT[b], xin[b], K // 128, "xin%d" % b, "xT%d" % b)
                k = 0
                for n0 in range(0, N, 512):
                    w = min(512, N - n0)
                    ps, pk = self.nps()
                    for kc in range(K // 128):
                        self.mm(ps[:, 0:w], xT[b][:, kc, :], wb[:, kc, n0:n0 + w], kc == 0, kc == K // 128 - 1,
                                ["xT%d" % b, "wproj"], pk)
                    self.cp("act" if k % 2 == 0 else "dve", ot[b][:, n0:n0 + w], ps[:, 0:w], [pk], ["ot%d" % b])
                    k += 1
                self.c.dma("sp", dst[t * 128:(t + 1) * 128, :], ot[b][:], reads=["ot%d" % b], writes=[dst.tensor.name])
            self.c.barrier()


RK_C = 0.606531


def stage_rwkv(self, p0, oab, W):
    c = self.c
    NT = self.NT
    with ExitStack() as st:
        sb = lambda n, shp, dt=F32: self.sb(st, n, shp, dt)
        pv = sb("rkv", [128, 13 * 512])
        c.dma("sp", pv[:], W["c_rkv"][:, :], writes=["rkv"])
        MU = lambda i: pv[:, i * 512:(i + 1) * 512]
        W0, A0, KK_, KA_, RKk, LNG, LNB = [pv[:, (6 + i) * 512:(7 + i) * 512] for i in range(7)]
        trib = sb("trib", [128, 128], BF16)
        c.dma("pool", trib[:], W["c_tri"][:, :], writes=["trib"])
        mask4 = sb("mask4", [128, 512])
        c.dma("sp", mask4[:], W["c_mask4"][:, :], writes=["mask4"])
        maskL = sb("maskL", [128, 128])
        c.dma("sp", maskL[:], W["c_maskL"][:, :], writes=["maskL"])
        identb = sb("identb", [128, 128], BF16)
        c.dma("pool", identb[:], W["c_ident"][:, :], writes=["identb"])
        w1 = sb("w1", [128, 4, 64], BF16)
        a1 = sb("a1", [128, 4, 64], BF16)
        g1 = sb("g1", [128, 4, 128], BF16)
        self.load_w(w1, W["rk_w1"], 512, 64, "w1")
        self.load_w(a1, W["rk_a1"], 512, 64, "a1")
        self.load_w(g1, W["rk_g1"], 512, 128, "g1")
        w2 = sb("w2", [64, 512], BF16)
        a2 = sb("a2", [64, 512], BF16)
        g2 = sb("g2", [128, 512], BF16)
        c.dma("pool", w2[:], W["rk_w2"][:, :], writes=["w2"])
        c.dma("pool", a2[:], W["rk_a2"][:, :], writes=["a2"])
        c.dma("pool", g2[:], W["rk_g2"][:, :], writes=["g2"])
        H = sb("H", [128, 4, 128])
        Hb = sb("Hb", [128, 4, 128], BF16)
        bd = sb("bd", [128, 128])
        c.dma("sp", bd[:], W["c_bd"][:, :], writes=["bd"])
        c.op("dve", lambda e: e.memset(H[:], 0.0), writes=["H"])
        c.op("dve", lambda e: e.memset(Hb[:], 0.0), writes=["Hb"])
        SINGLE = {"Pmm", "swh", "P", "Ps", "X", "xT", "hT", "sw", "a", "kk", "kp", "tmp", "ss", "cs", "e", "T", "BT", "KT", "Q"}

        def two(n, shp, dt=F32):
            return [sb(n, shp, dt) for _ in range(2)]

        def one(n, shp, dt=F32):
            x = sb(n, shp, dt)
            return [x, x]
        P_ = one("P", [128, 2048])
        Ps_ = one("Ps", [128, 2048])
        X6_ = one("X6", [128, 6, 512])
        xT_ = one("xT3", [128, 12, 128], BF16)
        hT_ = one("hT", [128, 384], BF16)
        sw_ = one("sw", [128, 512])
        swh = sb("swh", [128, 2, 512], BF16)
        a_ = one("a", [128, 512])
        g_ = two("g", [128, 512])
        kk_ = one("kk", [128, 512])
        kp_ = one("kp", [128, 512])
        tmp_ = one("tmp", [128, 512])
        tmp2_ = two("tq", [128, 512])
        ss_ = one("ss", [128, 8])
        bon_ = two("bon", [128, 512])
        sq_ = two("sq", [128, 8])
        bdg_ = [[sb("bdg", [128, 4, 128]) for _ in range(2)] for _ in range(2)]
        cs_ = one("cs", [128, 512])
        e_ = one("e3", [128, 3, 512])
        T4_ = one("T4", [128, 4, 512])
        Bt_ = two("Bt", [128, 512], BF16)
        Kt_ = two("Kt", [128, 512], BF16)
        Vt_ = two("Vt", [128, 512], BF16)
        ART_ = two("ART", [128, 4, 256], BF16)
        BT_ = one("BT", [128, 4, 128], BF16)
        KT_ = one("KT", [128, 4, 128], BF16)
        ET_ = two("ET", [128, 4, 128])
        G_ = two("G", [128, 8, 512], BF16)
        Wm_ = two("Wm", [128, 8, 128], BF16)
        Pm_ = one("Pm", [128, 8, 128], BF16)
        Qm_ = one("Qm", [128, 8, 128], BF16)
        Xs_ = two("Xs", [128, 512], BF16)
        Ub_ = [[sb("Ub", [128, 512], BF16) for _ in range(2)] for _ in range(2)]
        Vm_ = [[sb("Vm", [128, 512], BF16) for _ in range(2)] for _ in range(2)]
        for bb in range(2):
            c.op("pool", lambda e: e.memset(Xs_[bb][:], 0.0), writes=["Xs%d" % bb])
            for cc in range(2):
                c.op("pool", lambda e: e.memset(Ub_[bb][cc][:], 0.0), writes=["Ub%d_%d" % (cc, bb)])
                c.op("pool", lambda e: e.memset(Vm_[bb][cc][:], 0.0), writes=["Vm%d%d" % (cc, bb)])
        Os_ = two("Os", [128, 512])
        oo_ = two("oo", [128, 512])
        for t in range(NT if self.cut is None else 1):
            b = t % 2
            K = lambda n: n if n.rstrip("0123456789_") in SINGLE else "%s%d" % (n, b)
            P, Ps, X6, xT, hT = P_[b], Ps_[b], X6_[b], xT_[b], hT_[b]
            sw, a, g, kk, kp, tmp, tmp2, ss, bon, cs, e3, T4 = sw_[b], a_[b], g_[b], kk_[b], kp_[b], tmp_[b], tmp2_[b], ss_[b], bon_[b], cs_[b], e_[b], T4_[b]
            Bt, Kt, Vt, ART, BT, KT, ET, G, Wm, Pm, Qm, Xs, Ub, Os, oo = Bt_[b], Kt_[b], Vt_[b], ART_[b], BT_[b], KT_[b], ET_[b], G_[b], Wm_[b], Pm_[b], Qm_[b], Xs_[b], Ub_[b], Os_[b], oo_[b]
            sq = sq_[b]
            bdg = bdg_[b]
            Vm = Vm_[b]
            r0 = t * 128
            c.dma("sp", P[:], p0[r0:r0 + 128, 0:2048], reads=["p0"], writes=[K("Pmm")])
            if t == 0:
                c.op("pool", lambda e: e.memset(Ps[0:1, :], 0.0), writes=[K("Ps")])
                c.dma("sp", Ps[1:128, :], p0[0:127, 0:2048], reads=["p0"], writes=[K("Ps")])
            else:
                c.dma("sp", Ps[:], p0[r0 - 1:r0 + 127, 0:2048], reads=["p0"], writes=[K("Ps")])
            self.tt("dve", Ps[:], Ps[:], P[:], ALU.subtract, [K("Ps"), K("Pmm")], [K("Ps")])
            srcs = [0, 1, 2, 3, 3, 3]
            for i in range(6):
                eng = "dve" if i % 2 == 0 else "pool"
                sc = srcs[i] * 512
                self.tt(eng, X6[:, i, :], Ps[:, sc:sc + 512], MU(i), ALU.mult, [K("Ps"), "rkv"], [K("X6_%d" % i)])
                self.tt(eng, X6[:, i, :], X6[:, i, :], P[:, sc:sc + 512], ALU.add, [K("X6_%d" % i), K("Pmm")], [K("X6_%d" % i)])
            if self.cut == 1:
                return
            r, k, v = X6[:, 0, :], X6[:, 1, :], X6[:, 2, :]
            for i in range(3):
                self.transpose_in(xT, X6[:, 3 + i, :], 4, K("X6_%d" % (3 + i)), K("xT3"), ch0=4 * i)
            ps, pk = self.nps()
            for kc in range(4):
                self.mm(ps[0:64, 0:128], w1[:, kc, :], xT[:, kc, :], kc == 0, kc == 3, ["w1", K("xT3")], pk)
            for kc in range(4):
                self.mm(ps[0:64, 128:256], a1[:, kc, :], xT[:, 4 + kc, :], kc == 0, kc == 3, ["a1", K("xT3")], pk)
            for kc in range(4):
                self.mm(ps[:, 256:384], g1[:, kc, :], xT[:, 8 + kc, :], kc == 0, kc == 3, ["g1", K("xT3")], pk)
            self.act(hT[0:64, 0:128], ps[0:64, 0:128], AF.Tanh, [pk], [K("hT")])
            self.act(hT[0:64, 128:256], ps[0:64, 128:256], AF.Identity, [pk], [K("hT")])
            self.act(hT[:, 256:384], ps[:, 256:384], AF.Sigmoid, [pk], [K("hT")])
            ps, pk = self.nps()
            self.mm(ps[:, :], hT[0:64, 0:128], w2[:, :], True, True, [K("hT"), "w2"], pk)
            self.tt("dve", sw[:], ps[:, :], W0, ALU.add, [pk, "rkv"], [K("sw")])
            self.act(sw[:], sw[:], AF.Sigmoid, [K("sw")], [K("sw")])
            ps, pk = self.nps()
            self.mm(ps[:, :], hT[0:64, 128:256], a2[:, :], True, True, [K("hT"), "a2"], pk)
            self.tt("dve", a[:], ps[:, :], A0, ALU.add, [pk, "rkv"], [K("a")])
            self.act(a[:], a[:], AF.Sigmoid, [K("a")], [K("a")])
            ps, pk = self.nps()
            self.mm(ps[:, :], hT[:, 256:384], g2[:, :], True, True, [K("hT"), "g2"], pk)
            self.cp("act", g[:], ps[:, :], [pk], [K("g")])
            if self.cut == 2:
                return
            self.tt("pool", kk[:], k, KK_, ALU.mult, [K("X6_1"), "rkv"], [K("kk")])
            self.act(tmp[:], kk[:], AF.Square, [K("kk")], [K("tmp")])
            self.red("dve", ss[:], tmp[:].rearrange("p (h n) -> p h n", h=8), ALU.add, [K("tmp")], [K("ss")])
            self.rsqrt(ss[:], ss[:], 1e-24, [K("ss")], [K("ss")])
            kk3 = kk[:].rearrange("p (h n) -> p h n", h=8)
            self.tt("dve", kk3, kk3, ss[:].unsqueeze(2).to_broadcast([128, 8, 64]), ALU.mult, [K("kk"), K("ss")], [K("kk")])
            self.stt("pool", tmp[:], a[:], -1.0, KA_, ALU.add, ALU.mult, [K("a"), "rkv", K("tmp")], [K("tmp")])
            self.stt("pool", kp[:], tmp[:], 1.0, k, ALU.add, ALU.mult, [K("tmp"), K("X6_1")], [K("kp")])
            self.tt("dve", tmp[:], r, kp[:], ALU.mult, [K("X6_0"), K("kp")], [K("tmp")])
            self.tt("dve", tmp[:], tmp[:], RKk, ALU.mult, [K("tmp"), "rkv"], [K("tmp")])
            self.red("dve", ss[:], tmp[:].rearrange("p (h n) -> p h n", h=8), ALU.add, [K("tmp")], [K("ss")])
            self.tt("dve", bon[:].rearrange("p (h n) -> p h n", h=8), v.rearrange("p (h n) -> p h n", h=8),
                    ss[:].unsqueeze(2).to_broadcast([128, 8, 64]), ALU.mult, [K("X6_2"), K("ss")], [K("bon")])
            self.cp("act", Vt[:], v, [K("X6_2")], [K("Vt")])
            if self.cut == 3:
                return
            self.cp("act", swh[:, 0, :], sw[:], [K("sw")], [K("swh")])
            self.tt("pool", tmp[:], sw[:], swh[:, 0, :], ALU.subtract, [K("sw"), K("swh"), K("tmp")], [K("tmp")])
            self.cp("act", swh[:, 1, :], tmp[:], [K("tmp")], [K("swh")])
            ps, pk = self.nps()
            self.mm(ps[:, :], trib[:], swh[:, 0, :], True, False, ["trib", K("swh")], pk)
            self.mm(ps[:, :], trib[:], swh[:, 1, :], False, True, ["trib", K("swh")], pk)
            self.cp("dve", cs[:], ps[:, :], [pk], [K("cs")])
            self.act(e3[:, 0, :], cs[:], AF.Exp, [K("cs")], [K("e3")], scale=-RK_C)
            self.act(e3[:, 2, :], cs[:], AF.Exp, [K("cs")], [K("e3")], scale=RK_C)
            self.tt("dve", cs[:], cs[:], sw[:], ALU.subtract, [K("cs"), K("sw")], [K("cs")])
            self.act(e3[:, 1, :], cs[:], AF.Exp, [K("cs")], [K("e3")], scale=-RK_C)
            self.tt("dve", T4[:, 0, :], kk[:], e3[:, 1, :], ALU.mult, [K("kk"), K("e3")], [K("T4")])
            self.tt("pool", T4[:, 1, :], r, e3[:, 0, :], ALU.mult, [K("X6_0"), K("e3")], [K("T4")])
            self.tt("dve", tmp[:], kk[:], a[:], ALU.mult, [K("kk"), K("a"), K("tmp")], [K("tmp")])
            self.tt("dve", T4[:, 2, :], tmp[:], e3[:, 2, :], ALU.mult, [K("tmp"), K("e3")], [K("T4")])
            self.tt("pool", T4[:, 3, :], kp[:], e3[:, 2, :], ALU.mult, [K("kp"), K("e3")], [K("T4")])
            self.cp("act", Bt[:], T4[:, 2, :], [K("T4")], [K("Bt")])
            self.cp("act", Kt[:], T4[:, 3, :], [K("T4")], [K("Kt")])
            if self.cut == 4:
                d4 = self.dscr("dbgT4", [128, 2048])
                d3 = self.dscr("dbge3", [128, 1536])
                dsw = self.dscr("dbgsw", [128, 512])
                c.dma("sp", d4[:, :], T4[:].rearrange("p i n -> p (i n)"), reads=[K("T4")], writes=["dbgT4"])
                c.dma("sp", d3[:, :], e3[:].rearrange("p i n -> p (i n)"), reads=[K("e3")], writes=["dbge3"])
                c.dma("sp", dsw[:, :], sw[:], reads=[K("sw")], writes=["dbgsw"])
                return
            ART4 = ART[:].rearrange("p j (a t) -> p j a t", a=2)
            self.transpose_in(ART4[:, :, 0, :], T4[:, 0, :], 4, K("T4"), K("ART"))
            self.transpose_in(ART4[:, :, 1, :], T4[:, 1, :], 4, K("T4"), K("ART"))
            self.transpose_in(BT, T4[:, 2, :], 4, K("T4"), K("BT"))
            self.transpose_in(KT, T4[:, 3, :], 4, K("T4"), K("KT"))
            if self.cut in (45, 46):
                return
            self.transpose_in(ET, e3[:, 0, :], 4, K("e3"), K("ET"))
            if self.cut == 5:
                return
            for h in range(8):
                j, po = h // 2, (h % 2) * 64
                ps, pk = self.nps()
                self.mm(ps[:, 0:256], BT[po:po + 64, j, :], ART[po:po + 64, j, :], True, True, [K("BT"), K("ART")], pk)
                self.mm(ps[:, 256:512], KT[po:po + 64, j, :], ART[po:po + 64, j, :], True, True, [K("KT"), K("ART")], pk)
                self.tt("dve", G[:, h, :], ps[:, :], mask4[:], ALU.mult, [pk, "mask4"], [K("G%d" % h)])
            for par in range(2):
                ps, pk = self.nps()
                for hh in range(4):
                    h = 2 * hh + par
                    j, po = h // 2, par * 64
                    self.mm(ps[:, hh * 128:(hh + 1) * 128], ART[po:po + 64, j, 0:128], BT[po:po + 64, j, :], True, True,
                            [K("BT"), K("ART")], pk)
                self.tt("dve", Qm[:, par:8:2, :], ps[:, :].rearrange("p (h t) -> p h t", h=4),
                        maskL[:].unsqueeze(1).to_broadcast([128, 4, 128]), ALU.mult, [pk, "maskL"], [K("Q")])
            for h in range(8):
                self.tt("pool", Wm[:, h, :], identb[:], G[:, h, 0:128], ALU.subtract, ["identb", K("G%d" % h)], [K("W")])
            Pg = [None, None]
            for lvl in range(1, 6):
                for hq in range(2):
                    hsl = slice(hq * 4, (hq + 1) * 4)
                    psq, pkq = self.nps()
                    psp, pkp = (self.nps() if lvl < 5 else (None, None))
                    for hh in range(4):
                        h = hq * 4 + hh
                        Pc = G[:, h, 0:128] if Pg[hq] is None else Pm[:, h, :]
                        pkey = K("G%d" % h) if Pg[hq] is None else K("Pmm")
                        csl = slice(hh * 128, (hh + 1) * 128)
                        self.mm(psq[:, csl], Pc, Qm[:, h, :], True, True, [pkey, K("Q")], pkq)
                        if lvl < 5:
                            self.mm(psp[:, csl], Qm[:, h, :], Pc, True, True, [pkey, K("Q")], pkp)
                    self.cp("act", Qm[:, hsl, :], psq[:, :].rearrange("p (h t) -> p h t", h=4), [pkq], [K("Q")])
                    if lvl < 5:
                        self.cp("dve", Pm[:, hsl, :], psp[:, :].rearrange("p (h t) -> p h t", h=4), [pkp], [K("Pmm")])
                        Pg[hq] = 1
                for hq in range(2):
                    hsl = slice(hq * 4, (hq + 1) * 4)
                    psw, pkw = self.nps()
                    for hh in range(4):
                        h = hq * 4 + hh
                        self.mm(psw[:, hh * 128:(hh + 1) * 128], Qm[:, h, :], Wm[:, h, :], True, True, [K("Q"), K("W")], pkw)
                    self.tt("dve", Wm[:, hsl, :], Wm[:, hsl, :], psw[:, :].rearrange("p (h t) -> p h t", h=4), ALU.add,
                            [pkw, K("W")], [K("W")])
            if self.cut == 6:
                return
            for cc in range(2):
                self.tt("pool", bdg[cc][:], bd[:].unsqueeze(1).to_broadcast([128, 4, 128]),
                        ET[:, :, cc * 64 + 63:cc * 64 + 64].to_broadcast([128, 4, 128]), ALU.mult, ["bd", K("ET")], [K("bdg%d" % cc)])
            self.cp("act", Vm[0][0:64, :], v[0:64, :], [K("X6_2")], [K("Vm0")])
            self.cp("act", Vm[1][64:128, :], v[64:128, :], [K("X6_2")], [K("Vm1")])
            for cc in range(2):
                q0 = cc * 64
                rs = slice(q0, q0 + 64)
                Ubc, Vtc = Ub[cc], Vm[cc]
                ku, kv = K("Ub%d_" % cc), K("Vm%d" % cc)
                psX, pkX = self.nps()
                for j in range(4):
                    self.mm(psX[:, j * 128:(j + 1) * 128], ART[:, j, 0:128], Hb[:, j, :], True, False, [K("ART"), "Hb"], pkX)
                    for h in (2 * j, 2 * j + 1):
                        hs = slice(h * 64, h * 64 + 64)
                        self.mm(psX[:, hs], G[:, h, 256:384], Vt[:, hs], False, h == 2 * j + 1, [K("G%d" % h), K("Vt")], pkX)
                self.ts("dve", Xs[rs, :], psX[rs, :], -1.0, ALU.mult, [pkX], [K("Xs")])
                psU, pkU = self.nps()
                for h in range(8):
                    hs = slice(h * 64, h * 64 + 64)
                    self.mm(psU[:, hs], Wm[:, h, :], Xs[:, hs], True, True, [K("W"), K("Xs")], pkU)
                self.cp("act", Ubc[rs, :], psU[rs, :], [pkU], [ku])
                psO, pkO = self.nps()
                for j in range(4):
                    self.mm(psO[:, j * 128:(j + 1) * 128], ART[:, j, 128:256], Hb[:, j, :], True, False, [K("ART"), "Hb"], pkO)
                    for h in (2 * j, 2 * j + 1):
                        hs = slice(h * 64, h * 64 + 64)
                        self.mm(psO[:, hs], G[:, h, 128:256], Ubc[:, hs], False, False, [K("G%d" % h), ku], pkO)
                        self.mm(psO[:, hs], G[:, h, 384:512], Vt[:, hs], False, h == 2 * j + 1, [K("G%d" % h), K("Vt")], pkO)
                self.cp("act", Os[rs, :], psO[rs, :], [pkO], [K("Os")])
                psH, pkH = self.nps()
                for j in range(4):
                    js = slice(j * 128, (j + 1) * 128)
                    self.mm(psH[:, js], Bt[:, js], Ubc[:, js], True, False, [K("Bt"), ku], pkH)
                    self.mm(psH[:, js], Kt[:, js], Vtc[:, js], False, True, [K("Kt"), kv], pkH)
                H2 = H[:].rearrange("p j v -> p (j v)")
                self.tt("dve", H2, H2, psH[:, :], ALU.add, [pkH, "H"], ["H"])
                self.tt("dve", H[:], H[:], bdg[cc][:], ALU.mult, ["H", K("bdg%d" % cc)], ["H"])
                self.cp("act", Hb[:].rearrange("p j v -> p (j v)"), H2, ["H"], ["Hb"])
            O3 = Os[:].rearrange("p (h n) -> p h n", h=8)
            self.red("dve", sq[:], O3, ALU.add, [K("Os")], [K("sq")])
            self.ts("dve", sq[:], sq[:], 1.0 / 64, ALU.mult, [K("sq")], [K("sq")])
            self.tt("dve", O3, O3, sq[:].unsqueeze(2).to_broadcast([128, 8, 64]), ALU.subtract, [K("Os"), K("sq")], [K("Os")])
            self.act(tmp2[:], Os[:], AF.Square, [K("Os")], [K("tq")])
            self.red("dve", sq[:], tmp2[:].rearrange("p (h n) -> p h n", h=8), ALU.add, [K("tq")], [K("sq")])
            self.rsqrt(sq[:], sq[:], 64e-5, [K("sq")], [K("sq")], scale=1.0 / 64)
            self.tt("dve", O3, O3, sq[:].unsqueeze(2).to_broadcast([128, 8, 64]), ALU.mult, [K("Os"), K("sq")], [K("Os")])
            self.tt("pool", Os[:], Os[:], LNG, ALU.mult, [K("Os"), "rkv"], [K("Os")])
            self.tt("pool", Os[:], Os[:], LNB, ALU.add, [K("Os"), "rkv"], [K("Os")])
            self.tt("dve", Os[:], Os[:], bon[:], ALU.add, [K("Os"), K("bon")], [K("Os")])
            self.tt("dve", oo[:], Os[:], g[:], ALU.mult, [K("Os"), K("g")], [K("oo")])
            c.dma("sp", oab[r0:r0 + 128, 0:512], oo[:], reads=[K("oo")], writes=["oab"])
        c.barrier()


B.stage_rwkv = stage_rwkv


def stage_nsa(self, p0, oab, W):
    c = self.c
    S, NT = self.S, self.NT
    n_cmp = (S - 32) // 16 + 1
    NCT = (n_cmp + 127) // 128
    NCP = NCT * 128
    NQ = S // 512
    QC, KC0, GC0 = 2048, 2560, 3328
    with ExitStack() as st:
        sb = lambda n, shp, dt=F32: self.sb(st, n, shp, dt)
        identb = sb("identb", [128, 128], BF16)
        c.dma("pool", identb[:], W["c_ident"][:, :], writes=["identb"])
        KcT = sb("KcT", [128, NCP], BF16)
        Vca = sb("Vca", [128, NCT, 2, 193], BF16)
        c.op("pool", lambda e: e.memset(KcT[:], 0.0), writes=["KcT"])
        c.op("pool", lambda e: e.memset(Vca[:], 0.0), writes=["Vca"])
        with ExitStack() as st2:
            sb2 = lambda n, shp, dt=F32: self.sb(st2, n, shp, dt)
            kvT = sb2("kvT", [128, 2, S], BF16)
            W1p = sb2("W1p", [128, 2, 2, 32, 128], BF16)
            c.op("pool", lambda e: e.memset(W1p[:].rearrange("p a g l h -> p (a g l h)"), 0.0), writes=["W1p"])
            for kv in range(2):
                for g in range(2):
                    c.dma("pool", W1p[g * 64:(g + 1) * 64, kv, g, :, :],
                          W["ns_c_w1"][kv].rearrange("(l d) h -> d l h", d=64), writes=["W1p"])
            w2k = sb2("w2k", [128, 2, 128], BF16)
            c.op("pool", lambda e: e.memset(w2k[:].rearrange("p g n -> p (g n)"), 0.0), writes=["w2k"])
            for g in range(2):
                c.dma("pool", w2k[:, g, g * 64:(g + 1) * 64], W["ns_c_w2"][0], writes=["w2k"])
            w2v = sb2("w2v", [128, 64], BF16)
            c.dma("pool", w2v[:], W["ns_c_w2"][1], writes=["w2v"])
            peT = sb2("peT", [128, 2, 32], BF16)
            c.dma("pool", peT[:].rearrange("p a l -> p (a l)"), W["c_peT"][:, :], writes=["peT"])
            ropeA = sb2("ropeA", [128, 16])
            pa = [sb2("pa", [128, 256]) for _ in range(2)]
            pr = [sb2("pra", [128, 256]) for _ in range(2)]
            tA = [sb2("tA", [128, 2, 8]) for _ in range(2)]
            for t in range(NT):
                b = t % 2
                r0 = t * 128
                c.dma("sp", pa[b][:], p0[r0:r0 + 128, KC0:KC0 + 256], reads=["p0"], writes=["pa%d" % b])
                c.dma("sp", ropeA[:], W["c_rope"][r0:r0 + 128, :], writes=["ropeA"])
                self.cp("pool", pr[b][:], pa[b][:], ["pa%d" % b], ["pra%d" % b])
                self._rope(pa[b][:, 0:128], pr[b][:, 0:128], 2, ropeA, tA[b], "pa%d" % b, "pra%d" % b, "ropeA", "tA%d" % b)
                self.transpose_in(kvT[:, :, r0:r0 + 128], pr[b], 2, "pra%d" % b, "kvT")
            hT = sb2("hTc", [128, NCP], BF16)
            c.op("pool", lambda e: e.memset(hT[:], 0.0), writes=["hTc"])
            bia = sb2("bia", [128, 1])
            xh = sb2("xh", [128, 512])
            x2 = sb2("x2", [128, 512])
            for kv in range(2):
                for g in range(2):
                    ps, pk = self.nps()
                    for l in range(32):
                        self.mm(ps[:, 0:1], W1p[:, kv, g, l, :], peT[:, kv, l:l + 1], l == 0, l == 31, ["W1p", "peT"], pk)
                    self.cp("dve", bia[:], ps[:, 0:1], [pk], ["bia"])
                    ps, pk = self.nps()
                    for l in range(32):
                        self.mm(ps[:, 0:n_cmp], W1p[:, kv, g, l, :], kvT[:, kv, l:l + 16 * (n_cmp - 1) + 1:16], l == 0, l == 31,
                                ["W1p", "kvT"], pk)
                    X = xh[:, 0:n_cmp]
                    Y = x2[:, 0:n_cmp]
                    self.act(X, ps[:, 0:n_cmp], AF.Identity, [pk, "bia"], ["xh"], bias=bia[:, 0:1])
                    self.tt("dve", Y, X, X, ALU.mult, ["xh"], ["x2"])
                    self.ts("dve", Y, Y, 0.044715, ALU.mult, ["x2"], ["x2"], s2=1.0, op1=ALU.add)
                    self.tt("dve", Y, Y, X, ALU.mult, ["x2", "xh"], ["x2"])
                    self.act(Y, Y, AF.Tanh, ["x2"], ["x2"], scale=0.7978845608)
                    self.stt("dve", hT[:, 0:n_cmp], Y, 1.0, X, ALU.add, ALU.mult, ["x2", "xh"], ["hTc"])
                    if kv == 0:
                        ps, pk = self.nps()
                        self.mm(ps[:, 0:n_cmp], w2k[:, g, :], hT[:, 0:n_cmp], True, True, ["w2k", "hTc"], pk)
                        if g == 0:
                            self.act(KcT[:, 0:n_cmp], ps[:, 0:n_cmp], AF.Identity, [pk], ["KcT"], scale=0.5)
                        else:
                            self.stt("dve", KcT[:, 0:n_cmp], ps[:, 0:n_cmp], 0.5, KcT[:, 0:n_cmp], ALU.mult, ALU.add, [pk, "KcT"], ["KcT"])
                    else:
                        for i in range(NCT):
                            ps, pk = self.nps()
                            self.mm(ps[:, 0:64], hT[:, i * 128:(i + 1) * 128], w2v[:], True, True, ["w2v", "hTc"], pk)
                            self.act(Vca[:, i, g, 0:64], ps[:, 0:64], AF.Identity, [pk], ["Vca"], scale=0.5)
            for i in range(NCT):
                for g in range(2):
                    c.dma("pool", Vca[:, i, g, 65:193], W["c_ov"][i * 128:(i + 1) * 128, :], writes=["Vca"])
                    c.dma("pool", Vca[:, i, g, 64:65], W["c_ones"][:, 0:1], writes=["Vca"])
            c.barrier()
        QT = sb("QT", [128, 4, S], BF16)
        KT2 = sb("KT2", [128, 2, S], BF16)
        Va = sb("Va", [128, NT, 2, 2, 65], BF16)
        Eo = sb("Eo", [128, S], BF16)
        c.dma("pool", Eo[:], W["c_E"][:, :], writes=["Eo"])
        caus = sb("caus", [128, 4, 512], BF16)
        winb = sb("winb", [128, 8, 512], BF16)
        cmpb = sb("cmpb", [128, 5, 512], BF16)
        c.dma("pool", caus[:].rearrange("p a q -> p (a q)"), W["c_caus"][:, :], writes=["caus"])
        c.dma("pool", winb[:].rearrange("p a q -> p (a q)"), W["c_win"][:, :], writes=["winb"])
        c.dma("pool", cmpb[:].rearrange("p a q -> p (a q)"), W["c_cmpb"][:, :], writes=["cmpb"])
        c.op("pool", lambda e: e.memset(Va[:].rearrange("p t a g d -> p (t a g d)"), 1.0), writes=["Va"])
        with ExitStack() as st2:
            sb2 = lambda n, shp, dt=F32: self.sb(st2, n, shp, dt)
            ropeB = sb2("ropeB", [128, 16])
            pn = [sb2("pn", [128, 1280]) for _ in range(2)]
            qp = [sb2("qp", [128, 512]) for _ in range(2)]
            kp = [sb2("kpn", [128, 256]) for _ in range(2)]
            tB = [sb2("tB", [128, 8, 8]) for _ in range(2)]
            for t in range(NT):
                b = t % 2
                r0 = t * 128
                kn, kq, kk_ = "pn%d" % b, "qp%d" % b, "kpn%d" % b
                c.dma("sp", pn[b][:], p0[r0:r0 + 128, QC:QC + 1280], reads=["p0"], writes=[kn])
                c.dma("sp", ropeB[:], W["c_rope"][r0:r0 + 128, :], writes=["ropeB"])
                qsrc = pn[b][:, 0:512].rearrange("p (g j d) -> p g j d", g=2, j=4)
                qdst = qp[b][:].rearrange("p (j g d) -> p g j d", g=2, j=4)
                self.cp("pool", qdst, qsrc, [kn], [kq])
                self._rope(qsrc, qdst, 8, ropeB, tB[b], kn, kq, "ropeB", "tB%d" % b, four=True)
                for a in range(2):
                    o = 512 + 256 * (a + 1)
                    self.cp("pool", kp[b][:, a * 128:(a + 1) * 128], pn[b][:, o:o + 128], [kn], [kk_])
                    self._rope(pn[b][:, o:o + 128], kp[b][:, a * 128:(a + 1) * 128], 2, ropeB, tB[b], kn, kk_, "ropeB", "tB%d" % b)
                    self.cp("act", Va[:, t, a, :, 0:64], pn[b][:, o + 128:o + 256].rearrange("p (g d) -> p g d", g=2), [kn], ["Va"])
                self.transpose_in(QT[:, :, r0:r0 + 128], qp[b], 4, kq, "QT")
                self.transpose_in(KT2[:, :, r0:r0 + 128], kp[b], 2, kk_, "KT2")
            c.barrier()
        Qh = [[sb("Qh", [128, 512], BF16) for _ in range(2)] for _ in range(2)]
        for g in range(2):
            for k_ in range(2):
                c.op("pool", lambda e: e.memset(Qh[g][k_][:], 0.0), writes=["Qh%d_%d" % (g, k_)])
        qh_i = [0]
        qcur = [None, None]
        PT = [sb("PT", [128, 512], BF16) for _ in range(3)]
        MbT = [sb("MbT", [128, 512], BF16) for _ in range(2)]
        acc = [sb("acc", [128, 512]) for _ in range(4)]
        imp = [[sb("imp", [128, 128]) for _ in range(4)] for _ in range(2)]
        sig = [sb("sig", [128, 24]) for _ in range(4)]
        selF = [sb("selF", [128, 128]) for _ in range(4)]
        rz4 = [sb("rz", [128, 2]) for _ in range(4)]
        ot4 = [sb("oto", [128, 193]) for _ in range(4)]
        m8 = sb("m8", [128, 16])
        pri = sb("pri", [128, 128])
        pri2 = sb("pri2", [128, 128])
        mb = sb("mb", [128, 128])
        SPS = [(self.ps[i], "ps%d" % i) for i in range(4)]
        APS = [(self.ps[4 + i], "ps%d" % (4 + i)) for i in range(4)]
        sps_i = [0]
        pt_i = [0]

        pending = []

        def flush_pv():
            while pending:
                P, pkey, vaug, nv, subs = pending.pop(0)
                for (sub, first, last) in subs:
                    aps, apk = APS[sub]
                    self.mm(aps[:, 0:nv], P[:, sub * 128:(sub + 1) * 128], vaug, first, last, [pkey, "Va", "Vca"], apk)

        def unit(h, Q, kT, kcols, bias_terms, vaug, nv, subs, started):
            g = h // 4
            ps, pk = SPS[sps_i[0] % 4]
            sps_i[0] += 1
            nb = len(bias_terms)
            self.mm(ps[:, :], kT, qcur[0][:], True, nb == 0, ["KcT", "KT2", qcur[1]], pk)
            for bi, (lt, rt, rk) in enumerate(bias_terms):
                self.mm(ps[:, :], lt, rt, False, bi == nb - 1, rk, pk)
            P = PT[pt_i[0] % 3]
            pkey = "PT%d" % (pt_i[0] % 3)
            pt_i[0] += 1
            self.act(P[:], ps[:, :], AF.Exp, [pk], [pkey], scale=0.125)
            flush_pv()
            pending.append((P, pkey, vaug, nv, subs))

        def finish_branch(h, br, nv, g=None, hh=None):
            hs = slice(h * 64, (h + 1) * 64)
            for sub in range(4):
                aps, apk = APS[sub]
                self.cp("dve", ot4[sub][:, 0:nv], aps[:, 0:nv], [apk], ["oto%d" % sub])
            for sub in range(4):
                ot = ot4[sub]
                ko, kr = "oto%d" % sub, "rz%d" % sub
                rz = rz4[sub]
                self.ts("dve", rz[:, 0:1], ot[:, 64:65], 1e-30, ALU.max, [ko], [kr])
                c.op("dve", lambda e: e.reciprocal(out=rz[:, 0:1], in_=rz[:, 0:1]), reads=[kr], writes=[kr])
                self.tt("dve", rz[:, 1:2], rz[:, 0:1], sig[sub][:, br * 8 + h:br * 8 + h + 1], ALU.mult, [kr, "sig%d" % sub], [kr])
                if br == 0:
                    self.ts("dve", acc[sub][:, hs], ot[:, 0:64], rz[:, 1:2], ALU.mult, [ko, kr], ["acc%d" % sub])
                    if hh == 0:
                        self.ts("dve", imp[g][sub][:], ot[:, 65:193], rz[:, 0:1], ALU.mult, [ko, kr], ["imp%d%d" % (g, sub)])
                    else:
                        self.stt("dve", imp[g][sub][:], ot[:, 65:193], rz[:, 0:1], imp[g][sub][:], ALU.mult, ALU.add,
                                 [ko, kr, "imp%d%d" % (g, sub)], ["imp%d%d" % (g, sub)])
                else:
                    self.stt("dve", acc[sub][:, hs], ot[:, 0:64], rz[:, 1:2], acc[sub][:, hs], ALU.mult, ALU.add,
                             [ko, kr, "acc%d" % sub], ["acc%d" % sub])

        for Q in range(NQ):
            q0 = Q * 512
            for sub in range(4):
                r0 = q0 + sub * 128
                c.dma("sp", sig[sub][:], p0[r0:r0 + 128, GC0:GC0 + 24], reads=["p0"], writes=["sig%d" % sub])
                self.act(sig[sub][:], sig[sub][:], AF.Sigmoid, ["sig%d" % sub], ["sig%d" % sub])
                c.dma("sp", selF[sub][:], W["c_selF"][r0:r0 + 128, :], writes=["selF%d" % sub])
            for g in range(2):
                for hh in range(4):
                    h = g * 4 + hh
                    k_ = qh_i[0] % 2
                    qh_i[0] += 1
                    qcur[0], qcur[1] = Qh[g][k_], "Qh%d_%d" % (g, k_)
                    self.cp("pool", Qh[g][k_][g * 64:(g + 1) * 64, :], QT[g * 64:(g + 1) * 64, hh, q0:q0 + 512], ["QT"], [qcur[1]])
                    started = [False] * 4
                    for i in range(NCT):
                        dj = Q - 4 * i
                        if dj < 0:
                            continue
                        bt = [] if dj > 4 else [(identb[:], cmpb[:, dj, :], ["identb", "cmpb"])]
                        imax = min(NCT - 1, Q // 4)
                        unit(h, Q, KcT[:, i * 128:(i + 1) * 128], None, bt, Vca[:, i, g, :], 193,
                             [(s_, i == 0, i == imax) for s_ in range(4)], started)
                    flush_pv()
                    finish_branch(h, 0, 193, g, hh)
                psM, pkM = SPS[sps_i[0] % 4]
                sps_i[0] += 1
                for sub in range(4):
                    self.tt("dve", pri[:], imp[g][sub][:], selF[sub][:], ALU.add, ["imp%d%d" % (g, sub), "selF%d" % sub], ["pri"])
                    c.op("dve", lambda e: e.max(out=m8[:, 0:8], in_=pri[:]), reads=["pri"], writes=["m8"])
                    c.op("dve", lambda e: e.match_replace(out=pri2[:], in_to_replace=m8[:, 0:8], in_values=pri[:], imm_value=-1e9),
                         reads=["pri", "m8"], writes=["pri2"])
                    c.op("dve", lambda e: e.max(out=m8[:, 8:16], in_=pri2[:]), reads=["pri2"], writes=["m8"])
                    self.ts("dve", mb[:], pri[:], m8[:, 15:16], ALU.is_ge, ["pri", "m8"], ["mb"])
                    self.ts("dve", mb[:], mb[:], -1.0, ALU.add, ["mb"], ["mb"], s2=-NEG, op1=ALU.mult)
                    self.tr(psM[:, sub * 128:(sub + 1) * 128], mb[:], self.ident[:], ["mb", "ident"], pkM)
                self.cp("act", MbT[g][:], psM[:, :], [pkM], ["MbT%d" % g])
                for hh in range(4):
                    h = g * 4 + hh
                    k_ = qh_i[0] % 2
                    qh_i[0] += 1
                    qcur[0], qcur[1] = Qh[g][k_], "Qh%d_%d" % (g, k_)
                    self.cp("pool", Qh[g][k_][g * 64:(g + 1) * 64, :], QT[g * 64:(g + 1) * 64, hh, q0:q0 + 512], ["QT"], [qcur[1]])
                    started = [False] * 4
                    for kt in range(0, 4 * Q + 4):
                        d = kt - 4 * Q
                        bt = [(Eo[:, kt * 128:(kt + 1) * 128], MbT[g][:], ["Eo", "MbT%d" % g])]
                        if d >= 0:
                            bt.append((identb[:], caus[:, d, :], ["identb", "caus"]))
                        subs = [(s_, kt == 0, kt == 4 * Q + s_) for s_ in range(4) if s_ >= d]
                        unit(h, Q, KT2[:, 0, kt * 128:(kt + 1) * 128], None, bt, Va[:, kt, 0, g, :], 65, subs, started)
                    flush_pv()
                    finish_branch(h, 1, 65)
                    started = [False] * 4
                    for kt in range(max(0, 4 * Q - 4), 4 * Q + 4):
                        d = kt - 4 * Q
                        bt = [(identb[:], winb[:, d + 4, :], ["identb", "winb"])]
                        subs = [(s_, kt == max(0, 4 * Q + s_ - 4), kt == 4 * Q + s_) for s_ in range(4) if s_ - 4 <= d <= s_]
                        unit(h, Q, KT2[:, 1, kt * 128:(kt + 1) * 128], None, bt, Va[:, kt, 1, g, :], 65, subs, started)
                    flush_pv()
                    finish_branch(h, 2, 65)
            for sub in range(4):
                r0 = q0 + sub * 128
                c.dma("sp", oab[r0:r0 + 128, 512:1024], acc[sub][:], reads=["acc%d" % sub], writes=["oab"])
        c.barrier()


def _rope(self, src, dst, nh, rope, tmp, ksrc, kdst, krope, ktmp, four=False):
    if four:
        s4, d4 = src, dst
        x1, x2 = s4[:, :, :, 0:8], s4[:, :, :, 8:16]
        o1, o2 = d4[:, :, :, 0:8], d4[:, :, :, 8:16]
        cos = rope[:, 0:8].unsqueeze(1).unsqueeze(1).to_broadcast([128, 2, 4, 8])
        sin = rope[:, 8:16].unsqueeze(1).unsqueeze(1).to_broadcast([128, 2, 4, 8])
        tm = tmp[:].rearrange("p (g j) d -> p g j d", g=2)
    else:
        s3 = src.rearrange("p (h d) -> p h d", h=nh)
        d3 = dst.rearrange("p (h d) -> p h d", h=nh)
        x1, x2 = s3[:, :, 0:8], s3[:, :, 8:16]
        o1, o2 = d3[:, :, 0:8], d3[:, :, 8:16]
        cos = rope[:, 0:8].unsqueeze(1).to_broadcast([128, nh, 8])
        sin = rope[:, 8:16].unsqueeze(1).to_broadcast([128, nh, 8])
        tm = tmp[:, 0:nh, :]
    self.tt("dve", o1, x1, cos, ALU.mult, [ksrc, krope], [kdst])
    self.tt("dve", tm, x2, sin, ALU.mult, [ksrc, krope], [ktmp])
    self.tt("dve", o1, o1, tm, ALU.subtract, [kdst, ktmp], [kdst])
    self.tt("dve", o2, x2, cos, ALU.mult, [ksrc, krope], [kdst])
    self.tt("dve", tm, x1, sin, ALU.mult, [ksrc, krope], [ktmp])
    self.tt("dve", o2, o2, tm, ALU.add, [kdst, ktmp], [kdst])


B.stage_nsa = stage_nsa
B._rope = _rope


def ln_tile(self, z, zkey, lnp, out, okey, tl):
    s1, zc, sq = tl
    stats, mv, rstd, nb = s1[:, 0:12], s1[:, 12:14], s1[:, 14:15], s1[:, 15:16]
    for i in range(2):
        self.c.op("dve", lambda e: e.bn_stats(out=s1[:, i * 6:(i + 1) * 6], in_=z[:, i * 512:(i + 1) * 512]),
                  reads=[zkey], writes=["ln_st"])
    self.c.op("dve", lambda e: e.bn_aggr(out=mv, in_=stats), reads=["ln_st"], writes=["ln_mv"])
    self.rsqrt(rstd, mv[:, 1:2], LN_EPS, ["ln_mv"], ["ln_rs"])
    self.stt("dve", nb, mv[:, 0:1], -1.0, rstd, ALU.mult, ALU.mult, ["ln_mv", "ln_rs"], ["ln_nb"])
    self.act(zc[:], z, AF.Identity, [zkey, "ln_rs", "ln_nb"], ["ln_zc"], bias=nb, scale=rstd)
    self.tt("dve", zc[:], zc[:], lnp[:, 0:D], ALU.mult, ["ln_zc", "lnp"], ["ln_zc"])
    self.tt("dve", out, zc[:], lnp[:, D:2 * D], ALU.add, ["ln_zc", "lnp"], [okey])


def stage_mix(self, src, K, w_ap, resid, lnp_ap, dst):
    c = self.c
    with ExitStack() as st:
        sb = lambda n, shp, dt=F32: self.sb(st, n, shp, dt)
        wb = sb("wmix", [128, K // 128, D], BF16)
        self.load_w_fast(st, wb, w_ap, K, D, "wmix")
        lnp = sb("lnp", [128, 2 * D])
        c.dma("sp", lnp[:], lnp_ap[:, :], writes=["lnp"])
        tl = (sb("ln_s", [128, 16]), sb("ln_zc", [128, D]), None)
        xin = [sb("min", [128, K]) for _ in range(2)]
        xT = [sb("mxT", [128, K // 128, 128], BF16) for _ in range(2)]
        rs = [sb("mrs", [128, D]) for _ in range(2)]
        z = [sb("mz", [128, D]) for _ in range(2)]
        o = [sb("mo", [128, D]) for _ in range(2)]
        def epilogue(pend):
            b, r0, banks = pend
            for half, (ps, pk) in enumerate(banks):
                self.stt("dve", z[b][:, half * 512:(half + 1) * 512], rs[b][:, half * 512:(half + 1) * 512], ALPHA, ps[:, :],
                         ALU.mult, ALU.add, [pk, "mrs%d" % b], ["mz%d" % b])
            self.ln_tile(z[b][:], "mz%d" % b, lnp, o[b][:], "mo%d" % b, tl)
            c.dma("sp", dst[r0:r0 + 128, :], o[b][:], reads=["mo%d" % b], writes=[dst.tensor.name])

        pend = None
        for t in range(self.NT):
            b = t % 2
            r0 = t * 128
            c.dma("sp", xin[b][:], src[r0:r0 + 128, :], reads=[src.tensor.name], writes=["min%d" % b])
            c.dma("sp", rs[b][:], resid[r0:r0 + 128, :], reads=[resid.tensor.name], writes=["mrs%d" % b])
            self.transpose_in(xT[b], xin[b], K // 128, "min%d" % b, "mxT%d" % b)
            banks = []
            for half in range(2):
                ps, pk = self.nps()
                for kc in range(K // 128):
                    self.mm(ps[:, :], xT[b][:, kc, :], wb[:, kc, half * 512:(half + 1) * 512], kc == 0, kc == K // 128 - 1,
                            ["mxT%d" % b, "wmix"], pk)
                banks.append((ps, pk))
            if pend is not None:
                epilogue(pend)
            pend = (b, r0, banks)
        epilogue(pend)
        c.barrier()


def moe_cap(S):
    m = (S // 8) * 3 // 2
    return ((m + 511) // 512) * 512


def stage_moe(self, xin, W, layer, lnp_ap, dst, toklist, ybuf):
    c = self.c
    S, NT = self.S, self.NT
    CAP = moe_cap(S)
    NG = CAP // 512
    with ExitStack() as st:
        sb = lambda n, shp, dt=F32: self.sb(st, n, shp, dt)
        slotAB = sb("slotAB", [128, NT, 2], I32)
        wAB = sb("wAB", [128, NT, 2])
        tokid = sb("tokid", [128, NT, 16], I32)
        c.dma("sp", tokid[:].rearrange("p t r -> p (t r)"), W["c_tokid"][:, :], writes=["tokid"])
        c.dma("sp", toklist[:, :], W["c_tokinit"][:, :], reads=["toklist"], writes=["toklist"])
        with ExitStack() as st2:
            sb2 = lambda n, shp, dt=F32: self.sb(st2, n, shp, dt)
            rw = sb2("rw", [128, 8, 16])
            c.dma("sp", rw[:], W["router_w"].rearrange("(c p) e -> p c e", p=128), writes=["rw"])
            rb = sb2("rb", [128, 128])
            c.dma("sp", rb[:], W["c_rb"][:, :], writes=["rb"])
            ebase = sb2("ebase", [128, 128])
            c.dma("sp", ebase[:], W["c_ebase"][:, :], writes=["ebase"])
            SU = sb2("SU", [128, 128], BF16)
            ONES = sb2("ONESm", [128, 128], BF16)
            c.dma("pool", SU[:], W["c_su"][:, :], writes=["SU"])
            c.op("pool", lambda e: e.memset(ONES[:], 1.0), writes=["ONESm"])
            offs = sb2("offs", [128, 16])
            c.op("pool", lambda e: e.memset(offs[:], 0.0), writes=["offs"])
            TB = min(8, NT)
            TE = TB * 16
            xt = [sb2("rxt", [128, D]) for _ in range(2)]
            xT = [sb2("rxT", [128, 8, 128]) for _ in range(2)]
            aff = sb2("aff", [128, TE])
            s = sb2("s", [128, TE])
            s2 = sb2("s2", [128, TE])
            eq = sb2("eq", [128, TE])
            m1 = sb2("m1", [128, TB * 4])
            m2 = sb2("m2", [128, TB * 4])
            gs = sb2("gs", [128, TB * 4])
            gm = sb2("gm", [128, 2, TB])
            sel = sb2("sel", [128, TE])
            selb = sb2("selb", [128, TE], BF16)
            gate = sb2("gate", [128, TE])
            val = sb2("val", [128, TE])
            offT = sb2("offT", [128, TE])
            sl = sb2("sl", [128, 2, TB])
            g4 = lambda ap: ap.rearrange("p (a e) -> p a e", e=4)
            t16 = lambda ap: ap.rearrange("p (t e) -> p t e", e=16)
            bc4 = lambda ap: ap.unsqueeze(2).to_broadcast([128, TB * 4, 4])
            bc16 = lambda ap: ap.unsqueeze(2).to_broadcast([128, TB, 16])
            n = 0
            for tb in range(NT // TB):
                for i in range(TB):
                    t = tb * TB + i
                    b = n % 2
                    n += 1
                    r0 = t * 128
                    c.dma("sp", xt[b][:], xin[r0:r0 + 128, :], reads=[xin.tensor.name], writes=["rxt%d" % b])
                    self.transpose_in(xT[b], xt[b], 8, "rxt%d" % b, "rxT%d" % b)
                    psL, pkL = self.nps()
                    for kc in range(8):
                        self.mm(psL[:, 0:16], xT[b][:, kc, :], rw[:, kc, :], kc == 0, kc == 7, ["rxT%d" % b, "rw"], pkL)
                    self.act(aff[:, i * 16:(i + 1) * 16], psL[:, 0:16], AF.Sigmoid, [pkL], ["aff"])
                self.tt("dve", s[:], aff[:], rb[:, 0:TE], ALU.add, ["aff", "rb"], ["s"])
                self.red("dve", m1[:], g4(s[:]), ALU.max, ["s"], ["m1"])
                self.tt("dve", g4(eq[:]), g4(s[:]), bc4(m1[:]), ALU.is_ge, ["s", "m1"], ["eq"])
                self.stt("dve", s2[:], eq[:], -1e9, s[:], ALU.mult, ALU.add, ["eq", "s"], ["s2"])
                self.red("dve", m2[:], g4(s2[:]), ALU.max, ["s2"], ["m2"])
                self.tt("dve", gs[:], m1[:], m2[:], ALU.add, ["m1", "m2"], ["gs"])
                gs3 = gs[:].rearrange("p (t g) -> p t g", g=4)
                self.red("dve", gm[:, 0, :], gs3, ALU.max, ["gs"], ["gm"])
                self.tt("dve", gs3, gs3, gm[:, 0, :].unsqueeze(2).to_broadcast([128, TB, 4]), ALU.is_ge, ["gs", "gm"], ["gs"])
                self.tt("dve", g4(sel[:]), g4(s[:]), bc4(m2[:]), ALU.is_ge, ["s", "m2"], ["sel"])
                self.tt("dve", g4(sel[:]), g4(sel[:]), bc4(gs[:]), ALU.mult, ["sel", "gs"], ["sel"])
                self.tt("dve", gate[:], aff[:], sel[:], ALU.mult, ["aff", "sel"], ["gate"])
                self.red("dve", gm[:, 1, :], t16(gate[:]), ALU.add, ["gate"], ["gm"])
                c.op("dve", lambda e: e.reciprocal(out=gm[:, 1, :], in_=gm[:, 1, :]), reads=["gm"], writes=["gm"])
                self.tt("dve", t16(gate[:]), t16(gate[:]), bc16(gm[:, 1, :]), ALU.mult, ["gate", "gm"], ["gate"])
                self.cp("dve", selb[:], sel[:], ["sel"], ["selb"])
                psC, pkC = self.nps()
                for i in range(TB):
                    self.mm(psC[:, i * 16:(i + 1) * 16], SU[:], selb[:, i * 16:(i + 1) * 16], True, True, ["SU", "selb"], pkC)
                    self.mm(psC[:, 128 + i * 16:128 + (i + 1) * 16], ONES[:], selb[:, i * 16:(i + 1) * 16], True, True, ["ONESm", "selb"], pkC)
                self.cp("dve", offT[:, 0:16], offs[:], ["offs"], ["offT"])
                for i in range(1, TB):
                    self.tt("dve", offT[:, i * 16:(i + 1) * 16], offT[:, (i - 1) * 16:i * 16], psC[:, 128 + (i - 1) * 16:128 + i * 16],
                            ALU.add, [pkC, "offT"], ["offT"])
                self.tt("dve", offs[:], offT[:, (TB - 1) * 16:TB * 16], psC[:, 128 + (TB - 1) * 16:128 + TB * 16], ALU.add,
                        [pkC, "offT"], ["offs"])
                self.tt("dve", val[:], offT[:], psC[:, 0:TE], ALU.add, [pkC, "offT"], ["val"])
                self.ts("dve", val[:], val[:], float(CAP - 1), ALU.min, ["val"], ["val"])
                self.tt("dve", val[:], val[:], ebase[:, 0:TE], ALU.add, ["val", "ebase"], ["val"])
                self.tt("dve", val[:], val[:], sel[:], ALU.mult, ["val", "sel"], ["val"])
                self.ts("dve", val[:], val[:], -1.0, ALU.add, ["val"], ["val"])
                ts_ = slice(tb * TB, (tb + 1) * TB)
                for j in range(2):
                    self.red("dve", sl[:, j, :], t16(val[:]), ALU.max, ["val"], ["sl"])
                    self.tt("dve", t16(eq[:]), t16(val[:]), bc16(sl[:, j, :]), ALU.is_equal, ["val", "sl"], ["eq"])
                    self.tt("dve", s2[:], eq[:], gate[:], ALU.mult, ["eq", "gate"], ["s2"])
                    self.red("dve", wAB[:, ts_, j], t16(s2[:]), ALU.add, ["s2"], ["wAB"])
                    self.cp("dve", slotAB[:, ts_, j], sl[:, j, :], ["sl"], ["slotAB"])
                    if j == 0:
                        self.stt("dve", val[:], eq[:], -1e9, val[:], ALU.mult, ALU.add, ["eq", "val"], ["val"])
                if "dbg_aff" in self.dbg and tb == 0 and layer == 0:
                    for nm, tl_, w_ in (("dbg_aff", aff, TE), ("dbg_offT", offT, TE), ("dbg_gate", gate, TE), ("dbg_sel", sel, TE)):
                        dd = self.dscr(nm, [128, w_])
                        c.dma("sp", dd[:, :], tl_[:, 0:w_], reads=["aff", "offT", "gate", "sel"], writes=[nm])
                    dd = self.dscr("dbg_slotAB", [128, NT * 2], I32)
                    c.dma("sp", dd[:, :], slotAB[:].rearrange("p t j -> p (t j)"), reads=["slotAB"], writes=["dbg_slotAB"])
                    dd = self.dscr("dbg_wAB", [128, NT * 2])
                    c.dma("sp", dd[:, :], wAB[:].rearrange("p t j -> p (t j)"), reads=["wAB"], writes=["dbg_wAB"])
                    dd = self.dscr("dbg_sl", [128, 2 * TB])
                    c.dma("sp", dd[:, :], sl[:].rearrange("p a t -> p (a t)"), reads=["sl"], writes=["dbg_sl"])
                for i in range(TB):
                    t = tb * TB + i
                    for j in range(2):
                        c.dma("pool", toklist, tokid[:, t, :], reads=["tokid", "slotAB"], writes=["toklist"],
                              indirect=(bass.IndirectOffsetOnAxis(ap=slotAB[:, t, j:j + 1], axis=0), None))
            c.barrier()
        with ExitStack() as st2:
            sb2 = lambda n, shp, dt=F32: self.sb(st2, n, shp, dt)
            Wg = [sb2("Wg", [128, 8, D], BF16) for _ in range(2)]
            Wu = [sb2("Wu", [128, 8, D], BF16) for _ in range(2)]
            Wd = [sb2("Wd", [128, 8, D], BF16) for _ in range(2)]
            idx = [sb2("idx", [128, 16], I32) for _ in range(2)]
            X = [sb2("Xg", [128, D]) for _ in range(2)]
            xTg = [sb2("xTg", [128, 8, 512], BF16) for _ in range(2)]
            hs = [sb2("hs", [128, 512]) for _ in range(2)]
            hT = sb2("hTm", [128, 8, 512], BF16)
            ysb = [sb2("ysb", [128, D]) for _ in range(2)]
            n = 0
            gi = 0
            wstg = [sb2("wstg", [128, D]) for _ in range(8)]
            wsrc = [W["moe_w_gate"], W["moe_w_up"], W["moe_w_down"]]

            def chunk_dma(e, ci, si):
                m_, kc = ci // 8, ci % 8
                c.dma("sp", wstg[si][:], wsrc[m_][layer, e][kc * 128:(kc + 1) * 128, :], reads=[], writes=["mwstg%d" % si])

            def chunk_cast(e, ci, si):
                m_, kc = ci // 8, ci % 8
                dstw = (Wg, Wu, Wd)[m_][e % 2]
                self.cp("act" if ci % 2 == 0 else "dve", dstw[:, kc, :], wstg[si][:], ["mwstg%d" % si], ["W%d" % (e % 2)])

            for ci in range(24):
                chunk_dma(0, ci, ci % 8)
                chunk_cast(0, ci, ci % 8)
            per_slot = (24 + NG - 1) // NG
            for e in range(16):
                wbuf = e % 2
                kw = "W%d" % wbuf
                for grp in range(NG):
                    gb = gi % 2
                    gi += 1
                    nxt = [ci for ci in range(grp * per_slot, min(24, (grp + 1) * per_slot))] if e + 1 < 16 else []
                    if per_slot <= 8:
                        for k_, ci in enumerate(nxt):
                            chunk_dma(e + 1, ci, k_)
                    for i in range(4):
                        s0 = e * CAP + grp * 512 + i * 128
                        b = n % 2
                        n += 1
                        c.dma("pool", idx[b][:], toklist[s0:s0 + 128, :], reads=["toklist"], writes=["idx%d" % b])
                        c.dma("pool", X[b][:], xin, reads=[xin.tensor.name, "idx%d" % b], writes=["Xg%d" % b],
                              indirect=(None, bass.IndirectOffsetOnAxis(ap=idx[b][:, 0:1], axis=0)))
                        self.transpose_in(xTg[gb][:, :, i * 128:(i + 1) * 128], X[b], 8, "Xg%d" % b, "xTg%d" % gb)
                    for fc in range(8):
                        fs = slice(fc * 128, (fc + 1) * 128)
                        psG, pkG = self.nps()
                        for kc in range(8):
                            self.mm(psG[:, :], Wg[wbuf][:, kc, fs], xTg[gb][:, kc, :], kc == 0, kc == 7, [kw, "xTg%d" % gb], pkG)
                        psU, pkU = self.nps()
                        for kc in range(8):
                            self.mm(psU[:, :], Wu[wbuf][:, kc, fs], xTg[gb][:, kc, :], kc == 0, kc == 7, [kw, "xTg%d" % gb], pkU)
                        hb = fc % 2
                        self.act(hs[hb][:], psG[:, :], AF.Silu, [pkG], ["hs%d" % hb])
                        self.tt("dve", hT[:, fc, :], hs[hb][:], psU[:, :], ALU.mult, ["hs%d" % hb, pkU], ["hTm"])
                    for i in range(4):
                        s0 = e * CAP + grp * 512 + i * 128
                        yb = i % 2
                        for half in range(2):
                            ps, pk = self.nps()
                            for fc in range(8):
                                self.mm(ps[:, :], hT[:, fc, i * 128:(i + 1) * 128], Wd[wbuf][:, fc, half * 512:(half + 1) * 512],
                                        fc == 0, fc == 7, [kw, "hTm"], pk)
                            self.cp("act", ysb[yb][:, half * 512:(half + 1) * 512], ps[:, :], [pk], ["ysb%d" % yb])
                        c.dma("sp", ybuf[s0:s0 + 128, :], ysb[yb][:], reads=["ysb%d" % yb], writes=["ybuf"])
                    for k_, ci in enumerate(nxt):
                        if per_slot > 8:
                            chunk_dma(e + 1, ci, k_ % 8)
                        chunk_cast(e + 1, ci, k_ % 8)
            c.barrier()
        with ExitStack() as st2:
            sb2 = lambda n, shp, dt=F32: self.sb(st2, n, shp, dt)
            lnp = sb2("lnp", [128, 2 * D])
            c.dma("sp", lnp[:], lnp_ap[:, :], writes=["lnp"])
            tl = (sb2("ln_s", [128, 16]), sb2("ln_zc", [128, D]), None)
            xr = [sb2("cx", [128, D]) for _ in range(2)]
            yA = [sb2("cyA", [128, D]) for _ in range(2)]
            yB = [sb2("cyB", [128, D]) for _ in range(2)]
            z = [sb2("cz", [128, D]) for _ in range(2)]
            o = [sb2("co", [128, D]) for _ in range(2)]
            for t in range(NT):
                b = t % 2
                r0 = t * 128
                c.dma("sp", xr[b][:], xin[r0:r0 + 128, :], reads=[xin.tensor.name], writes=["cx%d" % b])
                c.dma("pool", yA[b][:], ybuf, reads=["ybuf", "slotAB"], writes=["cyA%d" % b],
                      indirect=(None, bass.IndirectOffsetOnAxis(ap=slotAB[:, t, 0:1], axis=0)))
                c.dma("pool", yB[b][:], ybuf, reads=["ybuf", "slotAB"], writes=["cyB%d" % b],
                      indirect=(None, bass.IndirectOffsetOnAxis(ap=slotAB[:, t, 1:2], axis=0)))
                self.ts("dve", z[b][:], xr[b][:], ALPHA, ALU.mult, ["cx%d" % b], ["cz%d" % b])
                self.stt("dve", z[b][:], yA[b][:], wAB[:, t, 0:1], z[b][:], ALU.mult, ALU.add, ["cyA%d" % b, "wAB", "cz%d" % b], ["cz%d" % b])
                self.stt("dve", z[b][:], yB[b][:], wAB[:, t, 1:2], z[b][:], ALU.mult, ALU.add, ["cyB%d" % b, "wAB", "cz%d" % b], ["cz%d" % b])
                self.ln_tile(z[b][:], "cz%d" % b, lnp, o[b][:], "co%d" % b, tl)
                c.dma("sp", dst[r0:r0 + 128, :], o[b][:], reads=["co%d" % b], writes=[dst.tensor.name])
            c.barrier()


B.ln_tile = ln_tile
B.stage_mix = stage_mix
B.stage_moe = stage_moe


def stage_ret(self, p1, ret, W):
    c = self.c
    NT = self.NT
    with ExitStack() as st:
        sb = lambda n, shp, dt=F32: self.sb(st, n, shp, dt)
        dec = sb("rtdec", [128, 8 * 128 + 24])
        c.dma("sp", dec[:], W["c_rtdec"][:, :], writes=["rtdec"])
        DT = lambda h: dec[:, h * 128:(h + 1) * 128]
        qd = dec[:, 1024:1032]
        kd = dec[:, 1032:1040]
        cd = dec[:, 1040:1048]
        gn = sb("rtgn", [128, 4096])
        c.dma("sp", gn[:], W["c_rtgn"][:, :], writes=["rtgn"])
        R = sb("R", [128, 8, 256])
        Rb = sb("Rb", [128, 8, 256], BF16)
        c.op("pool", lambda e: e.memset(R[:].rearrange("p h v -> p (h v)"), 0.0), writes=["R"])
        c.op("pool", lambda e: e.memset(Rb[:].rearrange("p h v -> p (h v)"), 0.0), writes=["Rb"])
        rope = sb("rtrope", [128, 256])
        Pq = [sb("Pq", [128, 2048]) for _ in range(2)]
        Vv = [sb("Vv", [128, 2048]) for _ in range(2)]
        Gg = [sb("Gg", [128, 2048]) for _ in range(2)]
        qk = sb("qkr", [128, 3, 1024])
        ktb = sb("ktb", [128, 1024], BF16)
        tmp = sb("rtmp", [128, 8, 64])
        T3 = sb("T3", [128, 24, 128], BF16)
        Vb = sb("Vb", [128, 2048], BF16)
        attm = sb("attm", [128, 1024], BF16)
        Os_ = [sb("rOs", [128, 2048]) for _ in range(2)]
        sq = sb("rsq", [128, 2048])
        sg = sb("rsg", [128, 2048])
        st8 = sb("rst8", [128, 8])
        oo = sb("roo", [128, 2048])
        def tail(t, b):
            r0 = t * 128
            kg = "Gg%d" % b
            Os = Os_[b]
            ko = "rOs%d" % b
            O3 = Os[:].rearrange("p (h v) -> p h v", h=8)
            self.red("dve", st8[:], O3, ALU.add, [ko], ["rst8"])
            self.ts("dve", st8[:], st8[:], 1.0 / 256, ALU.mult, ["rst8"], ["rst8"])
            self.tt("dve", O3, O3, st8[:].unsqueeze(2).to_broadcast([128, 8, 256]), ALU.subtract, [ko, "rst8"], [ko])
            self.act(sq[:], Os[:], AF.Square, [ko], ["rsq"])
            self.red("dve", st8[:], sq[:].rearrange("p (h v) -> p h v", h=8), ALU.add, ["rsq"], ["rst8"])
            self.rsqrt(st8[:], st8[:], 1e-5, ["rst8"], ["rst8"], scale=1.0 / 256)
            self.tt("dve", O3, O3, st8[:].unsqueeze(2).to_broadcast([128, 8, 256]), ALU.mult, [ko, "rst8"], [ko])
            self.tt("dve", Os[:], Os[:], gn[:, 0:2048], ALU.mult, [ko, "rtgn"], [ko])
            self.tt("pool", Os[:], Os[:], gn[:, 2048:4096], ALU.add, [ko, "rtgn"], [ko])
            self.act(sg[:], Gg[b][:], AF.Silu, [kg], ["rsg"])
            self.tt("dve", oo[:], Os[:], sg[:], ALU.mult, [ko, "rsg"], ["roo"])
            c.dma("sp", ret[r0:r0 + 128, :], oo[:], reads=["roo"], writes=["ret"])
        for t in range(NT):
            b = t % 2
            r0 = t * 128
            kp, kv, kg = "Pq%d" % b, "Vv%d" % b, "Gg%d" % b
            c.dma("sp", Pq[b][:], p1[r0:r0 + 128, 0:2048], reads=["p1"], writes=[kp])
            c.dma("sp", Vv[b][:], p1[r0:r0 + 128, 2048:4096], reads=["p1"], writes=[kv])
            c.dma("sp", Gg[b][:], p1[r0:r0 + 128, 4096:6144], reads=["p1"], writes=[kg])
            c.dma("sp", rope[:], W["c_rtrope"][r0:r0 + 128, :], writes=["rtrope"])
            for a in range(2):
                src = Pq[b][:, a * 1024:(a + 1) * 1024].rearrange("p (h d) -> p h d", h=8)
                dst = qk[:, a, :].rearrange("p (h d) -> p h d", h=8)
                cos = rope[:, a * 128:a * 128 + 64].unsqueeze(1).to_broadcast([128, 8, 64])
                sin = rope[:, a * 128 + 64:a * 128 + 128].unsqueeze(1).to_broadcast([128, 8, 64])
                x1, x2 = src[:, :, 0:64], src[:, :, 64:128]
                o1, o2 = dst[:, :, 0:64], dst[:, :, 64:128]
                kq = "qk%d" % a
                self.tt("dve", o1, x1, cos, ALU.mult, [kp, "rtrope"], [kq])
                self.tt("pool", tmp[:], x2, sin, ALU.mult, [kp, "rtrope"], ["rtmp"])
                self.tt("dve", o1, o1, tmp[:], ALU.subtract, [kq, "rtmp"], [kq])
                self.tt("dve", o2, x2, cos, ALU.mult, [kp, "rtrope"], [kq])
                self.tt("pool", tmp[:], x1, sin, ALU.mult, [kp, "rtrope"], ["rtmp"])
                self.tt("dve", o2, o2, tmp[:], ALU.add, [kq, "rtmp"], [kq])
            q3 = qk[:, 0, :].rearrange("p (h d) -> p h d", h=8)
            k3 = qk[:, 1, :].rearrange("p (h d) -> p h d", h=8)
            self.tt("pool", qk[:, 2, :].rearrange("p (h d) -> p h d", h=8), q3, qd.unsqueeze(2).to_broadcast([128, 8, 128]),
                    ALU.mult, ["qk0", "rtdec"], ["qk2"])
            self.tt("dve", ktb[:].rearrange("p (h d) -> p h d", h=8), k3, kd.unsqueeze(2).to_broadcast([128, 8, 128]),
                    ALU.mult, ["qk1", "rtdec"], ["ktb"])
            self.cp("act", Vb[:], Vv[b][:], [kv], ["Vb"])
            for a in range(3):
                self.transpose_in(T3, qk[:, a, :], 8, "qk%d" % a, "T3_%d" % a, ch0=8 * a)
            for hq in range(2):
                psA, pkA = self.nps()
                for hh in range(4):
                    h = hq * 4 + hh
                    self.mm(psA[:, hh * 128:(hh + 1) * 128], T3[:, 8 + h, :], T3[:, h, :], True, True, ["T3_0", "T3_1"], pkA)
                self.tt("dve", attm[:, hq * 512:(hq + 1) * 512], psA[:, :], dec[:, hq * 512:(hq + 1) * 512], ALU.mult,
                        [pkA, "rtdec"], ["attm%d" % hq])
            for hp in range(4):
                psO, pkO = self.nps()
                for hh in range(2):
                    h = hp * 2 + hh
                    vs = slice(h * 256, (h + 1) * 256)
                    self.mm(psO[:, hh * 256:(hh + 1) * 256], attm[:, h * 128:(h + 1) * 128], Vb[:, vs], True, False,
                            ["attm%d" % (h // 4), "Vb"], pkO)
                    self.mm(psO[:, hh * 256:(hh + 1) * 256], T3[:, 16 + h, :], Rb[:, h, :], False, True, ["T3_2", "Rb"], pkO)
                self.cp("act", Os_[b][:, hp * 512:(hp + 1) * 512], psO[:, :], [pkO], ["rOs%d" % b])
            R2 = R[:].rearrange("p h v -> p (h v)")
            self.tt("pool", R[:], R[:], cd.unsqueeze(2).to_broadcast([128, 8, 256]), ALU.mult, ["R", "rtdec"], ["R"])
            for hp in range(4):
                psR, pkR = self.nps()
                for hh in range(2):
                    h = hp * 2 + hh
                    vs = slice(h * 256, (h + 1) * 256)
                    self.mm(psR[:, hh * 256:(hh + 1) * 256], ktb[:, h * 128:(h + 1) * 128], Vb[:, vs], True, True, ["ktb", "Vb"], pkR)
                self.tt("dve", R2[:, hp * 512:(hp + 1) * 512], R2[:, hp * 512:(hp + 1) * 512], psR[:, :], ALU.add, [pkR, "R"], ["R"])
            self.cp("act", Rb[:].rearrange("p h v -> p (h v)"), R2, ["R"], ["Rb"])
            if t > 0:
                tail(t - 1, 1 - b)
        tail(NT - 1, (NT - 1) % 2)
        c.barrier()


B.stage_ret = stage_ret


STAGES = ["proj0", "rwkv", "nsa", "mix0", "moe0", "proj1", "ret", "mix1", "moe1"]


def build(S, upto="moe1", dbg=()):
    b = B(S, dbg)
    n_st = STAGES.index(upto) + 1
    on = lambda s: STAGES.index(s) < n_st
    CAP = moe_cap(S)
    NSLOT = 16 * CAP
    ncp = ((((S - 32) // 16 + 1) + 127) // 128) * 128
    W = {}
    for name, shp, dt in [("c_ident", [128, 128], F32), ("c_tri", [128, 128], F32), ("c_mask4", [128, 512], F32),
                          ("c_maskL", [128, 128], F32), ("c_bd", [128, 128], F32),
                          ("c_rkv", [128, 13 * 512], F32), ("rk_w1", [512, 64], F32), ("rk_a1", [512, 64], F32),
                          ("rk_g1", [512, 128], F32), ("rk_w2", [64, 512], F32), ("rk_a2", [64, 512], F32),
                          ("rk_g2", [128, 512], F32),
                          ("ns_c_w1", [2, 2048, 128], F32), ("ns_c_w2", [2, 128, 64], F32), ("c_peT", [128, 64], F32),
                          ("c_ones", [128, 1], F32), ("c_rope", [S, 16], F32), ("c_selF", [S, 128], F32),
                          ("c_E", [128, S], F32), ("c_caus", [128, 4 * 512], F32), ("c_win", [128, 8 * 512], F32),
                          ("c_cmpb", [128, 5 * 512], F32), ("c_ov", [ncp, 128], F32),
                          ("ab_w_out", [D, D], F32), ("c_ln", [4, 128, 2 * D], F32),
                          ("router_w", [D, 16], F32), ("c_rb", [128, 128], F32), ("c_ebase", [128, 128], F32),
                          ("c_su", [128, 128], F32), ("c_tokid", [128, (S // 128) * 16], I32),
                          ("c_tokinit", [NSLOT + 1, 16], I32),
                          ("moe_w_gate", [2, 16, D, D], F32), ("moe_w_up", [2, 16, D, D], F32),
                          ("moe_w_down", [2, 16, D, D], F32),
                          ("rt_w_in", [D, 6144], F32), ("rt_w_out", [2048, D], F32), ("c_rtgn", [128, 2 * 2048], F32),
                          ("c_rtrope", [S, 256], F32), ("c_rtdec", [128, 8 * 128 + 24], F32)]:
        W[name] = b.din(name, shp, dt)
    x = b.din("x", [S, D])
    ab_w_in = b.din("ab_w_in", [D, 3352])
    p0 = b.dscr("p0", [S, 3352])
    oab = b.dscr("oab", [S, 1024])
    x1 = b.dscr("x1", [S + 1, D])
    x2 = b.dscr("x2", [S + 1, D])
    x3 = b.dscr("x3", [S + 1, D])
    p1 = b.dscr("p1", [S, 6144])
    ret = b.dscr("ret", [S, 2048])
    toklist = b.dscr("toklist", [NSLOT + 1, 16], I32)
    ybuf = b.dscr("ybuf", [NSLOT, D])
    out = b.nc.dram_tensor("out", [S, D], F32, kind="ExternalOutput").ap()
    with ExitStack() as st:
        b.load_consts(st)
        zrow = b.sb(st, "zrow", [1, D])
        b.c.op("pool", lambda e: e.memset(zrow[:], 0.0), writes=["zrow"])
        for xx in (x1, x3):
            b.c.dma("sp", xx[S:S + 1, :], zrow[:], reads=["zrow"], writes=[xx.tensor.name])
        b.stage_proj(x, ab_w_in, p0, D, 3352)
        if on("rwkv"):
            b.stage_rwkv(p0, oab, W)
        if on("nsa"):
            b.stage_nsa(p0, oab, W)
        if on("mix0"):
            b.stage_mix(oab, 1024, W["ab_w_out"], x, W["c_ln"][0], x1)
        if on("moe0"):
            b.stage_moe(x1, W, 0, W["c_ln"][1], x2, toklist, ybuf)
        if on("proj1"):
            b.stage_proj(x2, W["rt_w_in"], p1, D, 6144)
        if on("ret"):
            b.stage_ret(p1, ret, W)
        if on("mix1"):
            b.stage_mix(ret, 2048, W["rt_w_out"], x2, W["c_ln"][2], x3)
        if on("moe1"):
            b.stage_moe(x3, W, 1, W["c_ln"][3], out, toklist, ybuf)
        b.c.finish()
    return b


def consts(S):
    c = {}
    c["c_ident"] = np.eye(128, dtype=np.float32)
    i = np.arange(128)
    same = (i[:, None] // 64) == (i[None, :] // 64)
    strict = ((i[:, None] < i[None, :]) & same).astype(np.float32)
    incl = ((i[:, None] <= i[None, :]) & same).astype(np.float32)
    c["c_tri"] = incl
    c["c_mask4"] = np.concatenate([strict, incl, strict, incl], axis=1)
    c["c_bd"] = same.astype(np.float32)
    c["c_ones"] = np.ones((128, 1), np.float32)
    inv = 500000.0 ** (-np.arange(8, dtype=np.float32) / 8)
    ang = np.arange(S, dtype=np.float32)[:, None] * inv[None, :]
    c["c_rope"] = np.concatenate([np.cos(ang), np.sin(ang)], axis=1).astype(np.float32)
    tpos = np.arange(S)
    cur = tpos // 64
    jb = np.arange(128)
    F = np.zeros((S, 128), np.float32)
    F[jb[None, :] > cur[:, None]] = -10.0
    forced = (jb[None, :] == 0) | (jb[None, :] == cur[:, None]) | (jb[None, :] == cur[:, None] - 1)
    F[forced & (jb[None, :] <= cur[:, None])] = 10.0
    c["c_selF"] = F
    c["c_E"] = (np.arange(S)[None, :] // 64 == jb[:, None]).astype(np.float32)
    k = np.arange(128)[:, None]
    q = np.arange(512)[None, :]
    c["c_caus"] = np.concatenate([np.where(128 * d + k <= q, 0.0, NEG) for d in range(4)], axis=1).astype(np.float32)
    c["c_win"] = np.concatenate([np.where((128 * d + k <= q) & (128 * d + k > q - 512), 0.0, NEG) for d in range(-4, 4)],
                                axis=1).astype(np.float32)
    c["c_cmpb"] = np.concatenate([np.where(16 * k + 31 <= 512 * dj + q, 0.0, NEG) for dj in range(5)], axis=1).astype(np.float32)
    n_cmp = (S - 32) // 16 + 1
    ncp = ((n_cmp + 127) // 128) * 128
    cs = np.arange(ncp) * 16
    ss = np.arange(128) * 64
    ov = np.clip(np.minimum(cs[:, None] + 32, ss[None, :] + 64) - np.maximum(cs[:, None], ss[None, :]), 0, None).astype(np.float32) / 32
    ov[n_cmp:] = 0.0
    c["c_ov"] = ov
    CAP = moe_cap(S)
    c["c_ebase"] = np.ascontiguousarray(np.broadcast_to(np.tile((np.arange(16) * CAP + 1).astype(np.float32), 8)[None, :], (128, 128)))
    c["c_su"] = (i[:, None] < i[None, :]).astype(np.float32)
    NT = S // 128
    tok = (np.arange(NT)[None, :, None] * 128 + np.arange(128)[:, None, None] + np.zeros((1, 1, 16), np.int64))
    c["c_tokid"] = np.ascontiguousarray(tok.reshape(128, NT * 16)).astype(np.int32)
    inv = 10000.0 ** (-np.linspace(0.0, 1.0, 64, dtype=np.float32))
    ang = np.arange(S, dtype=np.float32)[:, None] * inv[None, :]
    cs_, sn_ = np.cos(ang), np.sin(ang)
    sc = 128.0 ** -0.5
    c["c_rtrope"] = np.concatenate([cs_, sn_, cs_ * sc, sn_ * sc], axis=1).astype(np.float32)
    log_g = np.log(1.0 - 2.0 ** (-5.0 - np.arange(8, dtype=np.float64)))
    ii = np.arange(128, dtype=np.float64)
    diff = ii[None, :] - ii[:, None]
    DTm = [np.where(diff >= 0, np.exp(np.maximum(diff, 0.0) * lg), 0.0) for lg in log_g]
    qd = np.exp((ii[:, None] + 1.0) * log_g[None, :])
    kd = np.exp((127.0 - ii[:, None]) * log_g[None, :])
    cd = np.broadcast_to(np.exp(128.0 * log_g)[None, :], (128, 8))
    c["c_rtdec"] = np.concatenate(DTm + [qd, kd, cd], axis=1).astype(np.float32)
    c["c_tokinit"] = np.full((16 * CAP + 1, 16), S, np.int32)
    c["c_maskL"] = np.ascontiguousarray(strict.T)
    return c


def derived(inputs):
    d = {}
    rk = np.concatenate([inputs["rk_mu"].reshape(-1), inputs["rk_w0"].reshape(-1), inputs["rk_a0"].reshape(-1),
                         inputs["rk_kk"].reshape(-1), inputs["rk_ka"].reshape(-1), inputs["rk_rk"].reshape(-1),
                         inputs["rk_ln"].reshape(-1)])
    pe = np.asarray(inputs["ns_pe"]).reshape(2, 32, 64)
    peT = np.transpose(pe, (2, 0, 1)).reshape(64, 64)
    d["c_peT"] = np.ascontiguousarray(np.concatenate([peT, peT], axis=0)).astype(np.float32)
    ln = np.asarray(inputs["ln"]).reshape(4, 2 * D)
    d["c_ln"] = np.ascontiguousarray(np.broadcast_to(ln[:, None, :], (4, 128, 2 * D))).astype(np.float32)
    d["c_rtgn"] = np.ascontiguousarray(np.broadcast_to(np.asarray(inputs["rt_gn"]).reshape(1, 4096), (128, 4096))).astype(np.float32)
    d["c_rb"] = np.ascontiguousarray(np.broadcast_to(np.tile(np.asarray(inputs["router_b"]).reshape(16), 8)[None, :], (128, 128))).astype(np.float32)
    d["c_rkv"] = np.ascontiguousarray(np.broadcast_to(rk[None, :], (128, rk.size))).astype(np.float32)
    return d


def make_inputs(b, inputs, bi, S):
    cs = consts(S)
    cs.update(derived(inputs))
    m = {}
    for name, ap in b.inp.items():
        if name in cs:
            m[name] = cs[name]
        elif name == "x":
            m[name] = np.ascontiguousarray(inputs["x"][bi, :S])
        else:
            a = np.asarray(inputs[name])
            m[name] = np.ascontiguousarray(a.reshape(ap.shape))
    return m


_BUILT = {}


def kernel(**inputs):
    S = 8192
    if S not in _BUILT:
        _BUILT[S] = build(S)
    b = _BUILT[S]
    shared = make_inputs(b, inputs, 0, S)
    in_maps = []
    for bi in range(8):
        m = dict(shared)
        m["x"] = np.ascontiguousarray(np.asarray(inputs["x"])[bi, :S]).astype(np.float32)
        in_maps.append(m)
    res = run_bass_kernel_spmd(b.nc, in_maps, core_ids=list(range(8)))
    return np.stack([np.asarray(r["out"]) for r in res.results], axis=0).astype(np.float32)
```

```python
import numpy as np
import ml_dtypes
from contextlib import ExitStack
import concourse.bass as bass
import concourse.mybir as mybir
from concourse.bass_utils import run_bass_kernel_spmd

F32 = mybir.dt.float32
BF16 = mybir.dt.bfloat16
I32 = mybir.dt.int32
U32 = mybir.dt.uint32
AF = mybir.ActivationFunctionType
ALU = mybir.AluOpType
AX = mybir.AxisListType

D = 1024
ALPHA = (2.0 * 2) ** 0.25
LN_EPS = 1e-5
NEG = -30000.0


class Ctx:
    NDMA = 10

    def __init__(self, nc):
        self.nc = nc
        self.eng = {"pe": nc.tensor, "dve": nc.vector, "act": nc.scalar, "pool": nc.gpsimd, "sp": nc.sync}
        self.sem = {}
        self.cnt = {}
        for e in self.eng:
            self.sem["e_" + e] = nc.alloc_semaphore("sem_e_" + e)
            self.cnt["e_" + e] = 0
        self.dma_pool = {}
        for q in ("sp", "pool", "act"):
            names = []
            for i in range(self.NDMA):
                n = "d_%s_%d" % (q, i)
                self.sem[n] = nc.alloc_semaphore("sem_" + n)
                self.cnt[n] = 0
                names.append(n)
            self.dma_pool[q] = [names, 0]
        self.known = {e: {} for e in self.eng}
        self.last_w = {}
        self.readers = {}
        self.n_inst = 0
        self.n_wait = 0

    def _wait(self, e, semname, val):
        kn = self.known[e]
        if kn.get(semname, 0) >= val:
            return
        self.eng[e].wait_ge(self.sem[semname], val)
        kn[semname] = val
        self.n_wait += 1

    def _deps(self, e, reads, writes, is_dma=False):
        own = "e_" + e if not is_dma else None
        need = {}

        def add(ev, raw):
            s, v = ev
            if s == own and e == "pe":
                return
            if need.get(s, 0) < v:
                need[s] = v

        for k in reads:
            ev = self.last_w.get(k)
            if ev is not None:
                add(ev, True)
        for k in writes:
            ev = self.last_w.get(k)
            if ev is not None:
                add(ev, False)
            for s, v in self.readers.get(k, {}).items():
                add((s, v), False)
        for s, v in need.items():
            self._wait(e, s, v)

    def _commit(self, ev, reads, writes):
        s, v = ev
        for k in writes:
            self.last_w[k] = ev
            self.readers[k] = {}
        for k in reads:
            if k in writes:
                continue
            r = self.readers.setdefault(k, {})
            if r.get(s, 0) < v:
                r[s] = v

    def op(self, e, fn, reads=(), writes=()):
        reads = list(reads)
        writes = list(writes)
        self._deps(e, reads, writes)
        ins = fn(self.eng[e])
        s = "e_" + e
        self.cnt[s] += 1
        ins.then_inc(self.sem[s], 1)
        self._commit((s, self.cnt[s]), reads, writes)
        self.n_inst += 1
        return ins

    def dma(self, q, out, in_, reads=(), writes=(), indirect=None, **kw):
        reads = list(reads)
        writes = list(writes)
        self._deps(q, reads, writes, is_dma=True)
        names, i = self.dma_pool[q]
        s = names[i % len(names)]
        self.dma_pool[q][1] = i + 1
        if self.cnt[s] > 0:
            self._wait(q, s, self.cnt[s])
        if indirect is None:
            ins = self.eng[q].dma_start(out=out, in_=in_, **kw)
        else:
            ins = self.eng[q].indirect_dma_start(out, indirect[0], in_, indirect[1], **kw)
        self.cnt[s] += 16
        ins.then_inc(self.sem[s], 16)
        self._commit((s, self.cnt[s]), reads, writes)
        self.n_inst += 1
        return ins

    def barrier(self):
        for e in self.eng:
            for s, c in self.cnt.items():
                if c > 0:
                    self._wait(e, s, c)

    def finish(self):
        for s, c in self.cnt.items():
            if c > 0:
                self._wait("sp", s, c)


class B:
    def __init__(self, S, dbg=()):
        self.S = S
        self.NT = S // 128
        self.dbg = set(dbg)
        nc = self.nc = bass.Bass("TRN2", target_bir_lowering=False)
        self.c = Ctx(nc)
        self.inp = {}
        self.ps = [nc.alloc_psum_tensor("psb%d" % i, [128, 512], F32) for i in range(8)]
        self.ps_i = 0
        self._uid = 0
        import os
        self.cut = int(os.environ['CUT']) if 'CUT' in os.environ else None

    def din(self, name, shape, dt=F32):
        t = self.nc.dram_tensor(name, list(shape), dt, kind="ExternalInput").ap()
        self.inp[name] = t
        return t

    def dscr(self, name, shape, dt=F32):
        kind = "ExternalOutput" if name in self.dbg else "Internal"
        return self.nc.dram_tensor(name, list(shape), dt, kind=kind).ap()

    def sb(self, st, name, shape, dt=F32):
        self._uid += 1
        return st.enter_context(self.nc.sbuf_tensor("%s_%d" % (name, self._uid), list(shape), dt))

    def nps(self):
        i = self.ps_i % 8
        self.ps_i += 1
        return self.ps[i], "ps%d" % i

    def mm(self, out, lhsT, rhs, start, stop, reads, pk):
        return self.c.op("pe", lambda e: e.matmul(out, lhsT, rhs, start=start, stop=stop), reads=reads, writes=[pk])

    def tr(self, out, in_, ident, reads, pk):
        return self.c.op("pe", lambda e: e.transpose(out, in_, ident), reads=reads, writes=[pk])

    def cp(self, eng, out, in_, reads, writes):
        if eng == "act":
            return self.c.op("act", lambda e: e.copy(out=out, in_=in_), reads=reads, writes=writes)
        return self.c.op(eng, lambda e: e.tensor_copy(out=out, in_=in_), reads=reads, writes=writes)

    def act(self, out, in_, func, reads, writes, bias=0.0, scale=1.0, accum_out=None):
        kw = {}
        if accum_out is not None:
            kw["accum_out"] = accum_out
        return self.c.op("act", lambda e: e.activation(out=out, in_=in_, func=func, bias=bias, scale=scale, **kw),
                         reads=reads, writes=writes)

    def tt(self, eng, out, in0, in1, op, reads, writes):
        return self.c.op(eng, lambda e: e.tensor_tensor(out=out, in0=in0, in1=in1, op=op), reads=reads, writes=writes)

    def ts(self, eng, out, in0, s1, op0, reads, writes, s2=None, op1=None):
        if op0 in (ALU.pow, ALU.divide) or op1 in (ALU.pow, ALU.divide):
            eng = "pool"
        if op1 is None:
            return self.c.op(eng, lambda e: e.tensor_scalar(out=out, in0=in0, scalar1=s1, scalar2=None, op0=op0),
                             reads=reads, writes=writes)
        return self.c.op(eng, lambda e: e.tensor_scalar(out=out, in0=in0, scalar1=s1, scalar2=s2, op0=op0, op1=op1),
                         reads=reads, writes=writes)

    def rsqrt(self, out, in_, eps, reads, writes, scale=1.0):
        self.act(out, in_, AF.Sqrt, reads, writes, bias=eps, scale=scale)
        return self.c.op("dve", lambda e: e.reciprocal(out=out, in_=out), reads=writes, writes=writes)

    def stt(self, eng, out, in0, scalar, in1, op0, op1, reads, writes):
        eng = "dve"
        return self.c.op(eng, lambda e: e.scalar_tensor_tensor(out=out, in0=in0, scalar=scalar, in1=in1, op0=op0, op1=op1),
                         reads=reads, writes=writes)

    def red(self, eng, out, in_, op, reads, writes, axis=AX.X):
        return self.c.op(eng, lambda e: e.tensor_reduce(out=out, in_=in_, axis=axis, op=op), reads=reads, writes=writes)

    def load_consts(self, st):
        self.ident = self.sb(st, "ident", [128, 128], F32)
        self.c.dma("sp", self.ident[:], self.inp["c_ident"][:, :], reads=[], writes=["ident"])

    def load_w(self, dst, w_ap, K, N, key, q="pool"):
        for kc in range(K // 128):
            for n0 in range(0, N, 2048):
                n1 = min(N, n0 + 2048)
                self.c.dma(q, dst[:, kc, n0:n1], w_ap[kc * 128:(kc + 1) * 128, n0:n1], reads=[], writes=[key])

    def load_w_fast(self, st, dst, w_ap, K, N, key):
        stg = [self.sb(st, "wstg", [128, 1024]) for _ in range(4)]
        i = 0
        for kc in range(K // 128):
            for n0 in range(0, N, 1024):
                n1 = min(N, n0 + 1024)
                s_ = stg[i % 4]
                sk = "wstg%d_%s" % (i % 4, key)
                self.c.dma("sp", s_[:, 0:n1 - n0], w_ap[kc * 128:(kc + 1) * 128, n0:n1], reads=[], writes=[sk])
                self.cp("act" if i % 2 == 0 else "dve", dst[:, kc, n0:n1], s_[:, 0:n1 - n0], [sk], [key])
                i += 1

    def transpose_in(self, xT, xin, nch, rkey, wkey, col0=0, ch0=0):
        j = 0
        k = 0
        while j < nch:
            g = min(4, nch - j)
            ps, pk = self.nps()
            for i in range(g):
                self.tr(ps[:, i * 128:(i + 1) * 128], xin[:, col0 + (j + i) * 128: col0 + (j + i + 1) * 128],
                        self.ident[:], [rkey, "ident"], pk)
            eng = "act" if k % 2 == 0 else "dve"
            self.cp(eng, xT[:, ch0 + j:ch0 + j + g, :], ps[:, 0:g * 128].rearrange("p (g t) -> p g t", g=g), [pk], [wkey])
            j += g
            k += 1

    def layer_norm(self, st_tiles, z, zkey, gam, bet, out, okey):
        stats, mv, rstd = st_tiles
        nc = self.nc
        c = self.c
        for i in range(2):
            c.op("dve", lambda e: e.bn_stats(out=stats[:, i, :], in_=z[:, i * 512:(i + 1) * 512]), reads=[zkey], writes=["ln_stats"])
        c.op("dve", lambda e: e.bn_aggr(out=mv[:], in_=stats[:]), reads=["ln_stats"], writes=["ln_mv"])
        self.rsqrt(rstd[:], mv[:, 1:2], LN_EPS, ["ln_mv"], ["ln_rstd"])
        self.ts("dve", out, z, mv[:, 0:1], ALU.subtract, [zkey, "ln_mv", "ln_rstd"], [okey], s2=rstd[:, 0:1], op1=ALU.mult)
        self.tt("pool", out, out, gam, ALU.mult, [okey, "lnp"], [okey])
        self.tt("pool", out, out, bet, ALU.add, [okey, "lnp"], [okey])

    def stage_proj(self, src, w_ap, dst, K, N):
        with ExitStack() as st:
            wb = self.sb(st, "wproj", [128, K // 128, N], BF16)
            self.load_w_fast(st, wb, w_ap, K, N, "wproj")
            xin = [self.sb(st, "xin", [128, K], F32) for _ in range(2)]
            xT = [self.sb(st, "xT", [128, K // 128, 128], BF16) for _ in range(2)]
            ot = [self.sb(st, "ot", [128, N], F32) for _ in range(2)]
            for t in range(self.NT):
                b = t % 2
                self.c.dma("sp", xin[b][:], src[t * 128:(t + 1) * 128, :], reads=[src.tensor.name], writes=["xin%d" % b])
                self.transpose_in(xT[b], xin[b], K // 128, "xin%d" % b, "xT%d" % b)
                k = 0
                for n0 in range(0, N, 512):
                    w = min(512, N - n0)
                    ps, pk = self.nps()
                    for kc in range(K // 128):
                        self.mm(ps[:, 0:w], xT[b][:, kc, :], wb[:, kc, n0:n0 + w], kc == 0, kc == K // 128 - 1,
                                ["xT%d" % b, "wproj"], pk)
                    self.cp("act" if k % 2 == 0 else "dve", ot[b][:, n0:n0 + w], ps[:, 0:w], [pk], ["ot%d" % b])
                    k += 1
                self.c.dma("sp", dst[t * 128:(t + 1) * 128, :], ot[b][:], reads=["ot%d" % b], writes=[dst.tensor.name])
            self.c.barrier()


RK_C = 0.606531


def stage_rwkv(self, p0, oab, W):
    c = self.c
    NT = self.NT
    with ExitStack() as st:
        sb = lambda n, shp, dt=F32: self.sb(st, n, shp, dt)
        pv = sb("rkv", [128, 13 * 512])
        c.dma("sp", pv[:], W["c_rkv"][:, :], writes=["rkv"])
        MU = lambda i: pv[:, i * 512:(i + 1) * 512]
        W0, A0, KK_, KA_, RKk, LNG, LNB = [pv[:, (6 + i) * 512:(7 + i) * 512] for i in range(7)]
        trib = sb("trib", [128, 128], BF16)
        c.dma("pool", trib[:], W["c_tri"][:, :], writes=["trib"])
        mask4 = sb("mask4", [128, 512])
        c.dma("sp", mask4[:], W["c_mask4"][:, :], writes=["mask4"])
        maskL = sb("maskL", [128, 128])
        c.dma("sp", maskL[:], W["c_maskL"][:, :], writes=["maskL"])
        identb = sb("identb", [128, 128], BF16)
        c.dma("pool", identb[:], W["c_ident"][:, :], writes=["identb"])
        w1 = sb("w1", [128, 4, 64], BF16)
        a1 = sb("a1", [128, 4, 64], BF16)
        g1 = sb("g1", [128, 4, 128], BF16)
        self.load_w(w1, W["rk_w1"], 512, 64, "w1")
        self.load_w(a1, W["rk_a1"], 512, 64, "a1")
        self.load_w(g1, W["rk_g1"], 512, 128, "g1")
        w2 = sb("w2", [64, 512], BF16)
        a2 = sb("a2", [64, 512], BF16)
        g2 = sb("g2", [128, 512], BF16)
        c.dma("pool", w2[:], W["rk_w2"][:, :], writes=["w2"])
        c.dma("pool", a2[:], W["rk_a2"][:, :], writes=["a2"])
        c.dma("pool", g2[:], W["rk_g2"][:, :], writes=["g2"])
        H = sb("H", [128, 4, 128])
        Hb = sb("Hb", [128, 4, 128], BF16)
        bd = sb("bd", [128, 128])
        c.dma("sp", bd[:], W["c_bd"][:, :], writes=["bd"])
        c.op("dve", lambda e: e.memset(H[:], 0.0), writes=["H"])
        c.op("dve", lambda e: e.memset(Hb[:], 0.0), writes=["Hb"])
        SINGLE = {"Pmm", "swh", "P", "Ps", "X", "xT", "hT", "sw", "a", "kk", "kp", "tmp", "ss", "cs", "e", "T", "BT", "KT", "Q"}

        def two(n, shp, dt=F32):
            return [sb(n, shp, dt) for _ in range(2)]

        def one(n, shp, dt=F32):
            x = sb(n, shp, dt)
            return [x, x]
        P_ = one("P", [128, 2048])
        Ps_ = one("Ps", [128, 2048])
        X6_ = one("X6", [128, 6, 512])
        xT_ = one("xT3", [128, 12, 128], BF16)
        hT_ = one("hT", [128, 384], BF16)
        sw_ = one("sw", [128, 512])
        swh = sb("swh", [128, 2, 512], BF16)
        a_ = one("a", [128, 512])
        g_ = two("g", [128, 512])
        kk_ = one("kk", [128, 512])
        kp_ = one("kp", [128, 512])
        tmp_ = one("tmp", [128, 512])
        tmp2_ = two("tq", [128, 512])
        ss_ = one("ss", [128, 8])
        bon_ = two("bon", [128, 512])
        sq_ = two("sq", [128, 8])
        bdg_ = [[sb("bdg", [128, 4, 128]) for _ in range(2)] for _ in range(2)]
        cs_ = one("cs", [128, 512])
        e_ = one("e3", [128, 3, 512])
        T4_ = one("T4", [128, 4, 512])
        Bt_ = two("Bt", [128, 512], BF16)
        Kt_ = two("Kt", [128, 512], BF16)
        Vt_ = two("Vt", [128, 512], BF16)
        ART_ = two("ART", [128, 4, 256], BF16)
        BT_ = one("BT", [128, 4, 128], BF16)
        KT_ = one("KT", [128, 4, 128], BF16)
        ET_ = two("ET", [128, 4, 128])
        G_ = two("G", [128, 8, 512], BF16)
        Wm_ = two("Wm", [128, 8, 128], BF16)
        Pm_ = one("Pm", [128, 8, 128], BF16)
        Qm_ = one("Qm", [128, 8, 128], BF16)
        Xs_ = two("Xs", [128, 512], BF16)
        Ub_ = [[sb("Ub", [128, 512], BF16) for _ in range(2)] for _ in range(2)]
        Vm_ = [[sb("Vm", [128, 512], BF16) for _ in range(2)] for _ in range(2)]
        for bb in range(2):
            c.op("pool", lambda e: e.memset(Xs_[bb][:], 0.0), writes=["Xs%d" % bb])
            for cc in range(2):
                c.op("pool", lambda e: e.memset(Ub_[bb][cc][:], 0.0), writes=["Ub%d_%d" % (cc, bb)])
                c.op("pool", lambda e: e.memset(Vm_[bb][cc][:], 0.0), writes=["Vm%d%d" % (cc, bb)])
        Os_ = two("Os", [128, 512])
        oo_ = two("oo", [128, 512])
        pend_post = [None]
        for t in range(NT if self.cut is None else 1):
            b = t % 2
            K = lambda n: n if n.rstrip("0123456789_") in SINGLE else "%s%d" % (n, b)
            P, Ps, X6, xT, hT = P_[b], Ps_[b], X6_[b], xT_[b], hT_[b]
            sw, a, g, kk, kp, tmp, tmp2, ss, bon, cs, e3, T4 = sw_[b], a_[b], g_[b], kk_[b], kp_[b], tmp_[b], tmp2_[b], ss_[b], bon_[b], cs_[b], e_[b], T4_[b]
            Bt, Kt, Vt, ART, BT, KT, ET, G, Wm, Pm, Qm, Xs, Ub, Os, oo = Bt_[b], Kt_[b], Vt_[b], ART_[b], BT_[b], KT_[b], ET_[b], G_[b], Wm_[b], Pm_[b], Qm_[b], Xs_[b], Ub_[b], Os_[b], oo_[b]
            sq = sq_[b]
            bdg = bdg_[b]
            Vm = Vm_[b]
            r0 = t * 128
            c.dma("sp", P[:], p0[r0:r0 + 128, 0:2048], reads=["p0"], writes=[K("Pmm")])
            if t == 0:
                c.op("pool", lambda e: e.memset(Ps[0:1, :], 0.0), writes=[K("Ps")])
                c.dma("sp", Ps[1:128, :], p0[0:127, 0:2048], reads=["p0"], writes=[K("Ps")])
            else:
                c.dma("sp", Ps[:], p0[r0 - 1:r0 + 127, 0:2048], reads=["p0"], writes=[K("Ps")])
            self.tt("dve", Ps[:], Ps[:], P[:], ALU.subtract, [K("Ps"), K("Pmm")], [K("Ps")])
            srcs = [0, 1, 2, 3, 3, 3]
            for i in range(6):
                eng = "dve" if i % 2 == 0 else "pool"
                sc = srcs[i] * 512
                self.tt(eng, X6[:, i, :], Ps[:, sc:sc + 512], MU(i), ALU.mult, [K("Ps"), "rkv"], [K("X6_%d" % i)])
                self.tt(eng, X6[:, i, :], X6[:, i, :], P[:, sc:sc + 512], ALU.add, [K("X6_%d" % i), K("Pmm")], [K("X6_%d" % i)])
            if self.cut == 1:
                return
            r, k, v = X6[:, 0, :], X6[:, 1, :], X6[:, 2, :]
            for i in range(3):
                self.transpose_in(xT, X6[:, 3 + i, :], 4, K("X6_%d" % (3 + i)), K("xT3"), ch0=4 * i)
            ps, pk = self.nps()
            for kc in range(4):
                self.mm(ps[0:64, 0:128], w1[:, kc, :], xT[:, kc, :], kc == 0, kc == 3, ["w1", K("xT3")], pk)
            for kc in range(4):
                self.mm(ps[0:64, 128:256], a1[:, kc, :], xT[:, 4 + kc, :], kc == 0, kc == 3, ["a1", K("xT3")], pk)
            for kc in range(4):
                self.mm(ps[:, 256:384], g1[:, kc, :], xT[:, 8 + kc, :], kc == 0, kc == 3, ["g1", K("xT3")], pk)
            self.act(hT[0:64, 0:128], ps[0:64, 0:128], AF.Tanh, [pk], [K("hT")])
            self.act(hT[0:64, 128:256], ps[0:64, 128:256], AF.Identity, [pk], [K("hT")])
            self.act(hT[:, 256:384], ps[:, 256:384], AF.Sigmoid, [pk], [K("hT")])
            ps, pk = self.nps()
            self.mm(ps[:, :], hT[0:64, 0:128], w2[:, :], True, True, [K("hT"), "w2"], pk)
            self.tt("dve", sw[:], ps[:, :], W0, ALU.add, [pk, "rkv"], [K("sw")])
            self.act(sw[:], sw[:], AF.Sigmoid, [K("sw")], [K("sw")])
            ps, pk = self.nps()
            self.mm(ps[:, :], hT[0:64, 128:256], a2[:, :], True, True, [K("hT"), "a2"], pk)
            self.tt("dve", a[:], ps[:, :], A0, ALU.add, [pk, "rkv"], [K("a")])
            self.act(a[:], a[:], AF.Sigmoid, [K("a")], [K("a")])
            ps, pk = self.nps()
            self.mm(ps[:, :], hT[:, 256:384], g2[:, :], True, True, [K("hT"), "g2"], pk)
            self.cp("act", g[:], ps[:, :], [pk], [K("g")])
            if self.cut == 2:
                return
            self.tt("pool", kk[:], k, KK_, ALU.mult, [K("X6_1"), "rkv"], [K("kk")])
            self.act(tmp[:], kk[:], AF.Square, [K("kk")], [K("tmp")])
            self.red("dve", ss[:], tmp[:].rearrange("p (h n) -> p h n", h=8), ALU.add, [K("tmp")], [K("ss")])
            self.rsqrt(ss[:], ss[:], 1e-24, [K("ss")], [K("ss")])
            kk3 = kk[:].rearrange("p (h n) -> p h n", h=8)
            self.tt("dve", kk3, kk3, ss[:].unsqueeze(2).to_broadcast([128, 8, 64]), ALU.mult, [K("kk"), K("ss")], [K("kk")])
            self.stt("pool", tmp[:], a[:], -1.0, KA_, ALU.add, ALU.mult, [K("a"), "rkv", K("tmp")], [K("tmp")])
            self.stt("pool", kp[:], tmp[:], 1.0, k, ALU.add, ALU.mult, [K("tmp"), K("X6_1")], [K("kp")])
            self.tt("dve", tmp[:], r, kp[:], ALU.mult, [K("X6_0"), K("kp")], [K("tmp")])
            self.tt("dve", tmp[:], tmp[:], RKk, ALU.mult, [K("tmp"), "rkv"], [K("tmp")])
            self.red("dve", ss[:], tmp[:].rearrange("p (h n) -> p h n", h=8), ALU.add, [K("tmp")], [K("ss")])
            self.tt("dve", bon[:].rearrange("p (h n) -> p h n", h=8), v.rearrange("p (h n) -> p h n", h=8),
                    ss[:].unsqueeze(2).to_broadcast([128, 8, 64]), ALU.mult, [K("X6_2"), K("ss")], [K("bon")])
            self.cp("act", Vt[:], v, [K("X6_2")], [K("Vt")])
            if self.cut == 3:
                return
            self.cp("act", swh[:, 0, :], sw[:], [K("sw")], [K("swh")])
            self.tt("pool", tmp[:], sw[:], swh[:, 0, :], ALU.subtract, [K("sw"), K("swh"), K("tmp")], [K("tmp")])
            self.cp("act", swh[:, 1, :], tmp[:], [K("tmp")], [K("swh")])
            ps, pk = self.nps()
            self.mm(ps[:, :], trib[:], swh[:, 0, :], True, False, ["trib", K("swh")], pk)
            self.mm(ps[:, :], trib[:], swh[:, 1, :], False, True, ["trib", K("swh")], pk)
            self.cp("dve", cs[:], ps[:, :], [pk], [K("cs")])
            self.act(e3[:, 0, :], cs[:], AF.Exp, [K("cs")], [K("e3")], scale=-RK_C)
            self.act(e3[:, 2, :], cs[:], AF.Exp, [K("cs")], [K("e3")], scale=RK_C)
            self.tt("dve", cs[:], cs[:], sw[:], ALU.subtract, [K("cs"), K("sw")], [K("cs")])
            self.act(e3[:, 1, :], cs[:], AF.Exp, [K("cs")], [K("e3")], scale=-RK_C)
            self.tt("dve", T4[:, 0, :], kk[:], e3[:, 1, :], ALU.mult, [K("kk"), K("e3")], [K("T4")])
            self.tt("pool", T4[:, 1, :], r, e3[:, 0, :], ALU.mult, [K("X6_0"), K("e3")], [K("T4")])
            self.tt("dve", tmp[:], kk[:], a[:], ALU.mult, [K("kk"), K("a"), K("tmp")], [K("tmp")])
            self.tt("dve", T4[:, 2, :], tmp[:], e3[:, 2, :], ALU.mult, [K("tmp"), K("e3")], [K("T4")])
            self.tt("pool", T4[:, 3, :], kp[:], e3[:, 2, :], ALU.mult, [K("kp"), K("e3")], [K("T4")])
            self.cp("act", Bt[:], T4[:, 2, :], [K("T4")], [K("Bt")])
            self.cp("act", Kt[:], T4[:, 3, :], [K("T4")], [K("Kt")])
            if self.cut == 4:
                d4 = self.dscr("dbgT4", [128, 2048])
                d3 = self.dscr("dbge3", [128, 1536])
                dsw = self.dscr("dbgsw", [128, 512])
                c.dma("sp", d4[:, :], T4[:].rearrange("p i n -> p (i n)"), reads=[K("T4")], writes=["dbgT4"])
                c.dma("sp", d3[:, :], e3[:].rearrange("p i n -> p (i n)"), reads=[K("e3")], writes=["dbge3"])
                c.dma("sp", dsw[:, :], sw[:], reads=[K("sw")], writes=["dbgsw"])
                return
            ART4 = ART[:].rearrange("p j (a t) -> p j a t", a=2)
            self.transpose_in(ART4[:, :, 0, :], T4[:, 0, :], 4, K("T4"), K("ART"))
            self.transpose_in(ART4[:, :, 1, :], T4[:, 1, :], 4, K("T4"), K("ART"))
            self.transpose_in(BT, T4[:, 2, :], 4, K("T4"), K("BT"))
            self.transpose_in(KT, T4[:, 3, :], 4, K("T4"), K("KT"))
            if self.cut in (45, 46):
                return
            self.transpose_in(ET, e3[:, 0, :], 4, K("e3"), K("ET"))
            if self.cut == 5:
                return
            for h in range(8):
                j, po = h // 2, (h % 2) * 64
                ps, pk = self.nps()
                self.mm(ps[:, 0:256], BT[po:po + 64, j, :], ART[po:po + 64, j, :], True, True, [K("BT"), K("ART")], pk)
                self.mm(ps[:, 256:512], KT[po:po + 64, j, :], ART[po:po + 64, j, :], True, True, [K("KT"), K("ART")], pk)
                self.tt("dve", G[:, h, :], ps[:, :], mask4[:], ALU.mult, [pk, "mask4"], [K("G%d" % h)])
            for par in range(2):
                ps, pk = self.nps()
                for hh in range(4):
                    h = 2 * hh + par
                    j, po = h // 2, par * 64
                    self.mm(ps[:, hh * 128:(hh + 1) * 128], ART[po:po + 64, j, 0:128], BT[po:po + 64, j, :], True, True,
                            [K("BT"), K("ART")], pk)
                self.tt("dve", Qm[:, par:8:2, :], ps[:, :].rearrange("p (h t) -> p h t", h=4),
                        maskL[:].unsqueeze(1).to_broadcast([128, 4, 128]), ALU.mult, [pk, "maskL"], [K("Q")])
            for h in range(8):
                self.tt("pool", Wm[:, h, :], identb[:], G[:, h, 0:128], ALU.subtract, ["identb", K("G%d" % h)], [K("W")])
            Pg = [None, None]
            for lvl in range(1, 6):
                for hq in range(2):
                    hsl = slice(hq * 4, (hq + 1) * 4)
                    psq, pkq = self.nps()
                    psp, pkp = (self.nps() if lvl < 5 else (None, None))
                    for hh in range(4):
                        h = hq * 4 + hh
                        Pc = G[:, h, 0:128] if Pg[hq] is None else Pm[:, h, :]
                        pkey = K("G%d" % h) if Pg[hq] is None else K("Pmm")
                        csl = slice(hh * 128, (hh + 1) * 128)
                        self.mm(psq[:, csl], Pc, Qm[:, h, :], True, True, [pkey, K("Q")], pkq)
                        if lvl < 5:
                            self.mm(psp[:, csl], Qm[:, h, :], Pc, True, True, [pkey, K("Q")], pkp)
                    self.cp("act", Qm[:, hsl, :], psq[:, :].rearrange("p (h t) -> p h t", h=4), [pkq], [K("Q")])
                    if lvl < 5:
                        self.cp("dve", Pm[:, hsl, :], psp[:, :].rearrange("p (h t) -> p h t", h=4), [pkp], [K("Pmm")])
                        Pg[hq] = 1
                for hq in range(2):
                    hsl = slice(hq * 4, (hq + 1) * 4)
                    psw, pkw = self.nps()
                    for hh in range(4):
                        h = hq * 4 + hh
                        self.mm(psw[:, hh * 128:(hh + 1) * 128], Qm[:, h, :], Wm[:, h, :], True, True, [K("Q"), K("W")], pkw)
                    self.tt("dve", Wm[:, hsl, :], Wm[:, hsl, :], psw[:, :].rearrange("p (h t) -> p h t", h=4), ALU.add,
                            [pkw, K("W")], [K("W")])
            if self.cut == 6:
                return
            if pend_post[0] is not None:
                pend_post[0]()
                pend_post[0] = None
            for cc in range(2):
                self.tt("pool", bdg[cc][:], bd[:].unsqueeze(1).to_broadcast([128, 4, 128]),
                        ET[:, :, cc * 64 + 63:cc * 64 + 64].to_broadcast([128, 4, 128]), ALU.mult, ["bd", K("ET")], [K("bdg%d" % cc)])
            self.cp("act", Vm[0][0:64, :], v[0:64, :], [K("X6_2")], [K("Vm0")])
            self.cp("act", Vm[1][64:128, :], v[64:128, :], [K("X6_2")], [K("Vm1")])
            for cc in range(2):
                q0 = cc * 64
                rs = slice(q0, q0 + 64)
                Ubc, Vtc = Ub[cc], Vm[cc]
                ku, kv = K("Ub%d_" % cc), K("Vm%d" % cc)
                psX, pkX = self.nps()
                for j in range(4):
                    self.mm(psX[:, j * 128:(j + 1) * 128], ART[:, j, 0:128], Hb[:, j, :], True, False, [K("ART"), "Hb"], pkX)
                    for h in (2 * j, 2 * j + 1):
                        hs = slice(h * 64, h * 64 + 64)
                        self.mm(psX[:, hs], G[:, h, 256:384], Vt[:, hs], False, h == 2 * j + 1, [K("G%d" % h), K("Vt")], pkX)
                self.ts("dve", Xs[rs, :], psX[rs, :], -1.0, ALU.mult, [pkX], [K("Xs")])
                psU, pkU = self.nps()
                for h in range(8):
                    hs = slice(h * 64, h * 64 + 64)
                    self.mm(psU[:, hs], Wm[:, h, :], Xs[:, hs], True, True, [K("W"), K("Xs")], pkU)
                self.cp("act", Ubc[rs, :], psU[rs, :], [pkU], [ku])
                psO, pkO = self.nps()
                for j in range(4):
                    self.mm(psO[:, j * 128:(j + 1) * 128], ART[:, j, 128:256], Hb[:, j, :], True, False, [K("ART"), "Hb"], pkO)
                    for h in (2 * j, 2 * j + 1):
                        hs = slice(h * 64, h * 64 + 64)
                        self.mm(psO[:, hs], G[:, h, 128:256], Ubc[:, hs], False, False, [K("G%d" % h), ku], pkO)
                        self.mm(psO[:, hs], G[:, h, 384:512], Vt[:, hs], False, h == 2 * j + 1, [K("G%d" % h), K("Vt")], pkO)
                self.cp("act", Os[rs, :], psO[rs, :], [pkO], [K("Os")])
                psH, pkH = self.nps()
                for j in range(4):
                    js = slice(j * 128, (j + 1) * 128)
                    self.mm(psH[:, js], Bt[:, js], Ubc[:, js], True, False, [K("Bt"), ku], pkH)
                    self.mm(psH[:, js], Kt[:, js], Vtc[:, js], False, True, [K("Kt"), kv], pkH)
                H2 = H[:].rearrange("p j v -> p (j v)")
                self.tt("dve", H2, H2, psH[:, :], ALU.add, [pkH, "H"], ["H"])
                self.tt("dve", H[:], H[:], bdg[cc][:], ALU.mult, ["H", K("bdg%d" % cc)], ["H"])
                self.cp("act", Hb[:].rearrange("p j v -> p (j v)"), H2, ["H"], ["Hb"])
            def post(b=b, r0=r0, Os=Os, tmp2=tmp2, sq=sq, bon=bon, g=g, oo=oo):
                K = lambda n: n if n.rstrip("0123456789_") in SINGLE else "%s%d" % (n, b)
                O3 = Os[:].rearrange("p (h n) -> p h n", h=8)
                self.red("dve", sq[:], O3, ALU.add, [K("Os")], [K("sq")])
                self.ts("dve", sq[:], sq[:], 1.0 / 64, ALU.mult, [K("sq")], [K("sq")])
                self.tt("dve", O3, O3, sq[:].unsqueeze(2).to_broadcast([128, 8, 64]), ALU.subtract, [K("Os"), K("sq")], [K("Os")])
                self.act(tmp2[:], Os[:], AF.Square, [K("Os")], [K("tq")])
                self.red("dve", sq[:], tmp2[:].rearrange("p (h n) -> p h n", h=8), ALU.add, [K("tq")], [K("sq")])
                self.rsqrt(sq[:], sq[:], 64e-5, [K("sq")], [K("sq")], scale=1.0 / 64)
                self.tt("dve", O3, O3, sq[:].unsqueeze(2).to_broadcast([128, 8, 64]), ALU.mult, [K("Os"), K("sq")], [K("Os")])
                self.tt("pool", Os[:], Os[:], LNG, ALU.mult, [K("Os"), "rkv"], [K("Os")])
                self.tt("pool", Os[:], Os[:], LNB, ALU.add, [K("Os"), "rkv"], [K("Os")])
                self.tt("dve", Os[:], Os[:], bon[:], ALU.add, [K("Os"), K("bon")], [K("Os")])
                self.tt("dve", oo[:], Os[:], g[:], ALU.mult, [K("Os"), K("g")], [K("oo")])
                c.dma("sp", oab[r0:r0 + 128, 0:512], oo[:], reads=[K("oo")], writes=["oab"])
            pend_post[0] = post
        pend_post[0]()
        c.barrier()


B.stage_rwkv = stage_rwkv


def stage_nsa(self, p0, oab, W):
    c = self.c
    S, NT = self.S, self.NT
    n_cmp = (S - 32) // 16 + 1
    NCT = (n_cmp + 127) // 128
    NCP = NCT * 128
    NQ = S // 512
    QC, KC0, GC0 = 2048, 2560, 3328
    with ExitStack() as st:
        sb = lambda n, shp, dt=F32: self.sb(st, n, shp, dt)
        identb = sb("identb", [128, 128], BF16)
        c.dma("pool", identb[:], W["c_ident"][:, :], writes=["identb"])
        KcT = sb("KcT", [128, NCP], BF16)
        Vca = sb("Vca", [128, NCT, 2, 193], BF16)
        c.op("pool", lambda e: e.memset(KcT[:], 0.0), writes=["KcT"])
        c.op("pool", lambda e: e.memset(Vca[:], 0.0), writes=["Vca"])
        with ExitStack() as st2:
            sb2 = lambda n, shp, dt=F32: self.sb(st2, n, shp, dt)
            kvT = sb2("kvT", [128, 2, S], BF16)
            W1p = sb2("W1p", [128, 2, 2, 32, 128], BF16)
            c.op("pool", lambda e: e.memset(W1p[:].rearrange("p a g l h -> p (a g l h)"), 0.0), writes=["W1p"])
            for kv in range(2):
                for g in range(2):
                    c.dma("pool", W1p[g * 64:(g + 1) * 64, kv, g, :, :],
                          W["ns_c_w1"][kv].rearrange("(l d) h -> d l h", d=64), writes=["W1p"])
            w2k = sb2("w2k", [128, 2, 128], BF16)
            c.op("pool", lambda e: e.memset(w2k[:].rearrange("p g n -> p (g n)"), 0.0), writes=["w2k"])
            for g in range(2):
                c.dma("pool", w2k[:, g, g * 64:(g + 1) * 64], W["ns_c_w2"][0], writes=["w2k"])
            w2v = sb2("w2v", [128, 64], BF16)
            c.dma("pool", w2v[:], W["ns_c_w2"][1], writes=["w2v"])
            peT = sb2("peT", [128, 2, 32], BF16)
            c.dma("pool", peT[:].rearrange("p a l -> p (a l)"), W["c_peT"][:, :], writes=["peT"])
            ropeA = sb2("ropeA", [128, 16])
            pa = [sb2("pa", [128, 256]) for _ in range(2)]
            pr = [sb2("pra", [128, 256]) for _ in range(2)]
            tA = [sb2("tA", [128, 2, 8]) for _ in range(2)]
            for t in range(NT):
                b = t % 2
                r0 = t * 128
                c.dma("sp", pa[b][:], p0[r0:r0 + 128, KC0:KC0 + 256], reads=["p0"], writes=["pa%d" % b])
                c.dma("sp", ropeA[:], W["c_rope"][r0:r0 + 128, :], writes=["ropeA"])
                self.cp("pool", pr[b][:], pa[b][:], ["pa%d" % b], ["pra%d" % b])
                self._rope(pa[b][:, 0:128], pr[b][:, 0:128], 2, ropeA, tA[b], "pa%d" % b, "pra%d" % b, "ropeA", "tA%d" % b)
                self.transpose_in(kvT[:, :, r0:r0 + 128], pr[b], 2, "pra%d" % b, "kvT")
            hT = sb2("hTc", [128, NCP], BF16)
            c.op("pool", lambda e: e.memset(hT[:], 0.0), writes=["hTc"])
            bia = sb2("bia", [128, 1])
            xh = sb2("xh", [128, 512])
            x2 = sb2("x2", [128, 512])
            for kv in range(2):
                for g in range(2):
                    ps, pk = self.nps()
                    for l in range(32):
                        self.mm(ps[:, 0:1], W1p[:, kv, g, l, :], peT[:, kv, l:l + 1], l == 0, l == 31, ["W1p", "peT"], pk)
                    self.cp("dve", bia[:], ps[:, 0:1], [pk], ["bia"])
                    ps, pk = self.nps()
                    for l in range(32):
                        self.mm(ps[:, 0:n_cmp], W1p[:, kv, g, l, :], kvT[:, kv, l:l + 16 * (n_cmp - 1) + 1:16], l == 0, l == 31,
                                ["W1p", "kvT"], pk)
                    X = xh[:, 0:n_cmp]
                    Y = x2[:, 0:n_cmp]
                    self.act(X, ps[:, 0:n_cmp], AF.Identity, [pk, "bia"], ["xh"], bias=bia[:, 0:1])
                    self.tt("dve", Y, X, X, ALU.mult, ["xh"], ["x2"])
                    self.ts("dve", Y, Y, 0.044715, ALU.mult, ["x2"], ["x2"], s2=1.0, op1=ALU.add)
                    self.tt("dve", Y, Y, X, ALU.mult, ["x2", "xh"], ["x2"])
                    self.act(Y, Y, AF.Tanh, ["x2"], ["x2"], scale=0.7978845608)
                    self.stt("dve", hT[:, 0:n_cmp], Y, 1.0, X, ALU.add, ALU.mult, ["x2", "xh"], ["hTc"])
                    if kv == 0:
                        ps, pk = self.nps()
                        self.mm(ps[:, 0:n_cmp], w2k[:, g, :], hT[:, 0:n_cmp], True, True, ["w2k", "hTc"], pk)
                        if g == 0:
                            self.act(KcT[:, 0:n_cmp], ps[:, 0:n_cmp], AF.Identity, [pk], ["KcT"], scale=0.5)
                        else:
                            self.stt("dve", KcT[:, 0:n_cmp], ps[:, 0:n_cmp], 0.5, KcT[:, 0:n_cmp], ALU.mult, ALU.add, [pk, "KcT"], ["KcT"])
                    else:
                        for i in range(NCT):
                            ps, pk = self.nps()
                            self.mm(ps[:, 0:64], hT[:, i * 128:(i + 1) * 128], w2v[:], True, True, ["w2v", "hTc"], pk)
                            self.act(Vca[:, i, g, 0:64], ps[:, 0:64], AF.Identity, [pk], ["Vca"], scale=0.5)
            for i in range(NCT):
                for g in range(2):
                    c.dma("pool", Vca[:, i, g, 65:193], W["c_ov"][i * 128:(i + 1) * 128, :], writes=["Vca"])
                    c.dma("pool", Vca[:, i, g, 64:65], W["c_ones"][:, 0:1], writes=["Vca"])
            c.barrier()
        QT = sb("QT", [128, 4, S], BF16)
        KT2 = sb("KT2", [128, 2, S], BF16)
        Va = sb("Va", [128, NT, 2, 2, 65], BF16)
        Eo = sb("Eo", [128, S], BF16)
        c.dma("pool", Eo[:], W["c_E"][:, :], writes=["Eo"])
        caus = sb("caus", [128, 4, 512], BF16)
        winb = sb("winb", [128, 8, 512], BF16)
        cmpb = sb("cmpb", [128, 5, 512], BF16)
        c.dma("pool", caus[:].rearrange("p a q -> p (a q)"), W["c_caus"][:, :], writes=["caus"])
        c.dma("pool", winb[:].rearrange("p a q -> p (a q)"), W["c_win"][:, :], writes=["winb"])
        c.dma("pool", cmpb[:].rearrange("p a q -> p (a q)"), W["c_cmpb"][:, :], writes=["cmpb"])
        c.op("pool", lambda e: e.memset(Va[:].rearrange("p t a g d -> p (t a g d)"), 1.0), writes=["Va"])
        with ExitStack() as st2:
            sb2 = lambda n, shp, dt=F32: self.sb(st2, n, shp, dt)
            ropeB = sb2("ropeB", [128, 16])
            pn = [sb2("pn", [128, 1280]) for _ in range(2)]
            qp = [sb2("qp", [128, 512]) for _ in range(2)]
            kp = [sb2("kpn", [128, 256]) for _ in range(2)]
            tB = [sb2("tB", [128, 8, 8]) for _ in range(2)]
            for t in range(NT):
                b = t % 2
                r0 = t * 128
                kn, kq, kk_ = "pn%d" % b, "qp%d" % b, "kpn%d" % b
                c.dma("sp", pn[b][:], p0[r0:r0 + 128, QC:QC + 1280], reads=["p0"], writes=[kn])
                c.dma("sp", ropeB[:], W["c_rope"][r0:r0 + 128, :], writes=["ropeB"])
                qsrc = pn[b][:, 0:512].rearrange("p (g j d) -> p g j d", g=2, j=4)
                qdst = qp[b][:].rearrange("p (j g d) -> p g j d", g=2, j=4)
                self.cp("pool", qdst, qsrc, [kn], [kq])
                self._rope(qsrc, qdst, 8, ropeB, tB[b], kn, kq, "ropeB", "tB%d" % b, four=True)
                for a in range(2):
                    o = 512 + 256 * (a + 1)
                    self.cp("pool", kp[b][:, a * 128:(a + 1) * 128], pn[b][:, o:o + 128], [kn], [kk_])
                    self._rope(pn[b][:, o:o + 128], kp[b][:, a * 128:(a + 1) * 128], 2, ropeB, tB[b], kn, kk_, "ropeB", "tB%d" % b)
                    self.cp("act", Va[:, t, a, :, 0:64], pn[b][:, o + 128:o + 256].rearrange("p (g d) -> p g d", g=2), [kn], ["Va"])
                self.transpose_in(QT[:, :, r0:r0 + 128], qp[b], 4, kq, "QT")
                self.transpose_in(KT2[:, :, r0:r0 + 128], kp[b], 2, kk_, "KT2")
            c.barrier()
        Qh = [[sb("Qh", [128, 512], BF16) for _ in range(2)] for _ in range(2)]
        for g in range(2):
            for k_ in range(2):
                c.op("pool", lambda e: e.memset(Qh[g][k_][:], 0.0), writes=["Qh%d_%d" % (g, k_)])
        qh_i = [0]
        qcur = [None, None]
        PT = [sb("PT", [128, 512], BF16) for _ in range(3)]
        MbT = [sb("MbT", [128, 512], BF16) for _ in range(2)]
        acc = [sb("acc", [128, 512]) for _ in range(4)]
        imp = [[sb("imp", [128, 128]) for _ in range(4)] for _ in range(2)]
        sig = [sb("sig", [128, 24]) for _ in range(4)]
        selF = [sb("selF", [128, 128]) for _ in range(4)]
        rz4 = [sb("rz", [128, 2]) for _ in range(4)]
        ot4 = [sb("oto", [128, 193]) for _ in range(4)]
        m8 = sb("m8", [128, 16])
        pri = sb("pri", [128, 128])
        pri2 = sb("pri2", [128, 128])
        mb = sb("mb", [128, 128])
        SPS = [(self.ps[i], "ps%d" % i) for i in range(4)]
        APS = [(self.ps[4 + i], "ps%d" % (4 + i)) for i in range(4)]
        sps_i = [0]
        pt_i = [0]

        pending = []

        def flush_pv():
            while pending:
                P, pkey, vaug, nv, subs = pending.pop(0)
                for (sub, first, last) in subs:
                    aps, apk = APS[sub]
                    self.mm(aps[:, 0:nv], P[:, sub * 128:(sub + 1) * 128], vaug, first, last, [pkey, "Va", "Vca"], apk)

        def unit(h, Q, kT, kcols, bias_terms, vaug, nv, subs, started):
            g = h // 4
            ps, pk = SPS[sps_i[0] % 4]
            sps_i[0] += 1
            nb = len(bias_terms)
            self.mm(ps[:, :], kT, qcur[0][:], True, nb == 0, ["KcT", "KT2", qcur[1]], pk)
            for bi, (lt, rt, rk) in enumerate(bias_terms):
                self.mm(ps[:, :], lt, rt, False, bi == nb - 1, rk, pk)
            P = PT[pt_i[0] % 3]
            pkey = "PT%d" % (pt_i[0] % 3)
            pt_i[0] += 1
            self.act(P[:], ps[:, :], AF.Exp, [pk], [pkey], scale=0.125)
            flush_pv()
            pending.append((P, pkey, vaug, nv, subs))

        def finish_branch(h, br, nv, g=None, hh=None):
            hs = slice(h * 64, (h + 1) * 64)
            for sub in range(4):
                aps, apk = APS[sub]
                self.cp("dve", ot4[sub][:, 0:nv], aps[:, 0:nv], [apk], ["oto%d" % sub])
            for sub in range(4):
                ot = ot4[sub]
                ko, kr = "oto%d" % sub, "rz%d" % sub
                rz = rz4[sub]
                self.ts("dve", rz[:, 0:1], ot[:, 64:65], 1e-30, ALU.max, [ko], [kr])
                c.op("dve", lambda e: e.reciprocal(out=rz[:, 0:1], in_=rz[:, 0:1]), reads=[kr], writes=[kr])
                self.tt("dve", rz[:, 1:2], rz[:, 0:1], sig[sub][:, br * 8 + h:br * 8 + h + 1], ALU.mult, [kr, "sig%d" % sub], [kr])
                if br == 0:
                    self.ts("dve", acc[sub][:, hs], ot[:, 0:64], rz[:, 1:2], ALU.mult, [ko, kr], ["acc%d" % sub])
                    if hh == 0:
                        self.ts("dve", imp[g][sub][:], ot[:, 65:193], rz[:, 0:1], ALU.mult, [ko, kr], ["imp%d%d" % (g, sub)])
                    else:
                        self.stt("dve", imp[g][sub][:], ot[:, 65:193], rz[:, 0:1], imp[g][sub][:], ALU.mult, ALU.add,
                                 [ko, kr, "imp%d%d" % (g, sub)], ["imp%d%d" % (g, sub)])
                else:
                    self.stt("dve", acc[sub][:, hs], ot[:, 0:64], rz[:, 1:2], acc[sub][:, hs], ALU.mult, ALU.add,
                             [ko, kr, "acc%d" % sub], ["acc%d" % sub])

        for Q in range(NQ):
            q0 = Q * 512
            for sub in range(4):
                r0 = q0 + sub * 128
                c.dma("sp", sig[sub][:], p0[r0:r0 + 128, GC0:GC0 + 24], reads=["p0"], writes=["sig%d" % sub])
                self.act(sig[sub][:], sig[sub][:], AF.Sigmoid, ["sig%d" % sub], ["sig%d" % sub])
                c.dma("sp", selF[sub][:], W["c_selF"][r0:r0 + 128, :], writes=["selF%d" % sub])
            for g in range(2):
                for hh in range(4):
                    h = g * 4 + hh
                    k_ = qh_i[0] % 2
                    qh_i[0] += 1
                    qcur[0], qcur[1] = Qh[g][k_], "Qh%d_%d" % (g, k_)
                    self.cp("pool", Qh[g][k_][g * 64:(g + 1) * 64, :], QT[g * 64:(g + 1) * 64, hh, q0:q0 + 512], ["QT"], [qcur[1]])
                    started = [False] * 4
                    for i in range(NCT):
                        dj = Q - 4 * i
                        if dj < 0:
                            continue
                        bt = [] if dj > 4 else [(identb[:], cmpb[:, dj, :], ["identb", "cmpb"])]
                        imax = min(NCT - 1, Q // 4)
                        unit(h, Q, KcT[:, i * 128:(i + 1) * 128], None, bt, Vca[:, i, g, :], 193,
                             [(s_, i == 0, i == imax) for s_ in range(4)], started)
                    flush_pv()
                    finish_branch(h, 0, 193, g, hh)
                psM, pkM = SPS[sps_i[0] % 4]
                sps_i[0] += 1
                for sub in range(4):
                    self.tt("dve", pri[:], imp[g][sub][:], selF[sub][:], ALU.add, ["imp%d%d" % (g, sub), "selF%d" % sub], ["pri"])
                    c.op("dve", lambda e: e.max(out=m8[:, 0:8], in_=pri[:]), reads=["pri"], writes=["m8"])
                    c.op("dve", lambda e: e.match_replace(out=pri2[:], in_to_replace=m8[:, 0:8], in_values=pri[:], imm_value=-1e9),
                         reads=["pri", "m8"], writes=["pri2"])
                    c.op("dve", lambda e: e.max(out=m8[:, 8:16], in_=pri2[:]), reads=["pri2"], writes=["m8"])
                    self.ts("dve", mb[:], pri[:], m8[:, 15:16], ALU.is_ge, ["pri", "m8"], ["mb"])
                    self.ts("dve", mb[:], mb[:], -1.0, ALU.add, ["mb"], ["mb"], s2=-NEG, op1=ALU.mult)
                    self.tr(psM[:, sub * 128:(sub + 1) * 128], mb[:], self.ident[:], ["mb", "ident"], pkM)
                self.cp("act", MbT[g][:], psM[:, :], [pkM], ["MbT%d" % g])
                for hh in range(4):
                    h = g * 4 + hh
                    k_ = qh_i[0] % 2
                    qh_i[0] += 1
                    qcur[0], qcur[1] = Qh[g][k_], "Qh%d_%d" % (g, k_)
                    self.cp("pool", Qh[g][k_][g * 64:(g + 1) * 64, :], QT[g * 64:(g + 1) * 64, hh, q0:q0 + 512], ["QT"], [qcur[1]])
                    started = [False] * 4
                    for kt in range(0, 4 * Q + 4):
                        d = kt - 4 * Q
                        bt = [(Eo[:, kt * 128:(kt + 1) * 128], MbT[g][:], ["Eo", "MbT%d" % g])]
                        if d >= 0:
                            bt.append((identb[:], caus[:, d, :], ["identb", "caus"]))
                        subs = [(s_, kt == 0, kt == 4 * Q + s_) for s_ in range(4) if s_ >= d]
                        unit(h, Q, KT2[:, 0, kt * 128:(kt + 1) * 128], None, bt, Va[:, kt, 0, g, :], 65, subs, started)
                    flush_pv()
                    finish_branch(h, 1, 65)
                    started = [False] * 4
                    for kt in range(max(0, 4 * Q - 4), 4 * Q + 4):
                        d = kt - 4 * Q
                        bt = [(identb[:], winb[:, d + 4, :], ["identb", "winb"])]
                        subs = [(s_, kt == max(0, 4 * Q + s_ - 4), kt == 4 * Q + s_) for s_ in range(4) if s_ - 4 <= d <= s_]
                        unit(h, Q, KT2[:, 1, kt * 128:(kt + 1) * 128], None, bt, Va[:, kt, 1, g, :], 65, subs, started)
                    flush_pv()
                    finish_branch(h, 2, 65)
            for sub in range(4):
                r0 = q0 + sub * 128
                c.dma("sp", oab[r0:r0 + 128, 512:1024], acc[sub][:], reads=["acc%d" % sub], writes=["oab"])
        c.barrier()


def _rope(self, src, dst, nh, rope, tmp, ksrc, kdst, krope, ktmp, four=False):
    if four:
        s4, d4 = src, dst
        x1, x2 = s4[:, :, :, 0:8], s4[:, :, :, 8:16]
        o1, o2 = d4[:, :, :, 0:8], d4[:, :, :, 8:16]
        cos = rope[:, 0:8].unsqueeze(1).unsqueeze(1).to_broadcast([128, 2, 4, 8])
        sin = rope[:, 8:16].unsqueeze(1).unsqueeze(1).to_broadcast([128, 2, 4, 8])
        tm = tmp[:].rearrange("p (g j) d -> p g j d", g=2)
    else:
        s3 = src.rearrange("p (h d) -> p h d", h=nh)
        d3 = dst.rearrange("p (h d) -> p h d", h=nh)
        x1, x2 = s3[:, :, 0:8], s3[:, :, 8:16]
        o1, o2 = d3[:, :, 0:8], d3[:, :, 8:16]
        cos = rope[:, 0:8].unsqueeze(1).to_broadcast([128, nh, 8])
        sin = rope[:, 8:16].unsqueeze(1).to_broadcast([128, nh, 8])
        tm = tmp[:, 0:nh, :]
    self.tt("dve", o1, x1, cos, ALU.mult, [ksrc, krope], [kdst])
    self.tt("dve", tm, x2, sin, ALU.mult, [ksrc, krope], [ktmp])
    self.tt("dve", o1, o1, tm, ALU.subtract, [kdst, ktmp], [kdst])
    self.tt("dve", o2, x2, cos, ALU.mult, [ksrc, krope], [kdst])
    self.tt("dve", tm, x1, sin, ALU.mult, [ksrc, krope], [ktmp])
    self.tt("dve", o2, o2, tm, ALU.add, [kdst, ktmp], [kdst])


B.stage_nsa = stage_nsa
B._rope = _rope


def ln_tile(self, z, zkey, lnp, out, okey, tl):
    s1, zc, sq = tl
    stats, mv, rstd, nb = s1[:, 0:12], s1[:, 12:14], s1[:, 14:15], s1[:, 15:16]
    for i in range(2):
        self.c.op("dve", lambda e: e.bn_stats(out=s1[:, i * 6:(i + 1) * 6], in_=z[:, i * 512:(i + 1) * 512]),
                  reads=[zkey], writes=["ln_st"])
    self.c.op("dve", lambda e: e.bn_aggr(out=mv, in_=stats), reads=["ln_st"], writes=["ln_mv"])
    self.rsqrt(rstd, mv[:, 1:2], LN_EPS, ["ln_mv"], ["ln_rs"])
    self.stt("dve", nb, mv[:, 0:1], -1.0, rstd, ALU.mult, ALU.mult, ["ln_mv", "ln_rs"], ["ln_nb"])
    self.act(zc[:], z, AF.Identity, [zkey, "ln_rs", "ln_nb"], ["ln_zc"], bias=nb, scale=rstd)
    self.tt("dve", zc[:], zc[:], lnp[:, 0:D], ALU.mult, ["ln_zc", "lnp"], ["ln_zc"])
    self.tt("dve", out, zc[:], lnp[:, D:2 * D], ALU.add, ["ln_zc", "lnp"], [okey])


def stage_mix(self, src, K, w_ap, resid, lnp_ap, dst):
    c = self.c
    with ExitStack() as st:
        sb = lambda n, shp, dt=F32: self.sb(st, n, shp, dt)
        wb = sb("wmix", [128, K // 128, D], BF16)
        self.load_w_fast(st, wb, w_ap, K, D, "wmix")
        lnp = sb("lnp", [128, 2 * D])
        c.dma("sp", lnp[:], lnp_ap[:, :], writes=["lnp"])
        tl = (sb("ln_s", [128, 16]), sb("ln_zc", [128, D]), None)
        xin = [sb("min", [128, K]) for _ in range(2)]
        xT = [sb("mxT", [128, K // 128, 128], BF16) for _ in range(2)]
        rs = [sb("mrs", [128, D]) for _ in range(2)]
        z = [sb("mz", [128, D]) for _ in range(2)]
        o = [sb("mo", [128, D]) for _ in range(2)]
        def epilogue(pend):
            b, r0, banks = pend
            for half, (ps, pk) in enumerate(banks):
                self.stt("dve", z[b][:, half * 512:(half + 1) * 512], rs[b][:, half * 512:(half + 1) * 512], ALPHA, ps[:, :],
                         ALU.mult, ALU.add, [pk, "mrs%d" % b], ["mz%d" % b])
            self.ln_tile(z[b][:], "mz%d" % b, lnp, o[b][:], "mo%d" % b, tl)
            c.dma("sp", dst[r0:r0 + 128, :], o[b][:], reads=["mo%d" % b], writes=[dst.tensor.name])

        pend = None
        for t in range(self.NT):
            b = t % 2
            r0 = t * 128
            c.dma("sp", xin[b][:], src[r0:r0 + 128, :], reads=[src.tensor.name], writes=["min%d" % b])
            c.dma("sp", rs[b][:], resid[r0:r0 + 128, :], reads=[resid.tensor.name], writes=["mrs%d" % b])
            self.transpose_in(xT[b], xin[b], K // 128, "min%d" % b, "mxT%d" % b)
            banks = []
            for half in range(2):
                ps, pk = self.nps()
                for kc in range(K // 128):
                    self.mm(ps[:, :], xT[b][:, kc, :], wb[:, kc, half * 512:(half + 1) * 512], kc == 0, kc == K // 128 - 1,
                            ["mxT%d" % b, "wmix"], pk)
                banks.append((ps, pk))
            if pend is not None:
                epilogue(pend)
            pend = (b, r0, banks)
        epilogue(pend)
        c.barrier()


def moe_cap(S):
    m = (S // 8) * 3 // 2
    return ((m + 511) // 512) * 512


def stage_moe(self, xin, W, layer, lnp_ap, dst, toklist, ybuf):
    c = self.c
    S, NT = self.S, self.NT
    CAP = moe_cap(S)
    NG = CAP // 512
    with ExitStack() as st:
        sb = lambda n, shp, dt=F32: self.sb(st, n, shp, dt)
        slotAB = sb("slotAB", [128, NT, 2], I32)
        wAB = sb("wAB", [128, NT, 2])
        tokid = sb("tokid", [128, NT, 16], I32)
        c.dma("sp", tokid[:].rearrange("p t r -> p (t r)"), W["c_tokid"][:, :], writes=["tokid"])
        c.dma("sp", toklist[:, :], W["c_tokinit"][:, :], reads=["toklist"], writes=["toklist"])
        with ExitStack() as st2:
            sb2 = lambda n, shp, dt=F32: self.sb(st2, n, shp, dt)
            rw = sb2("rw", [128, 8, 16])
            c.dma("sp", rw[:], W["router_w"].rearrange("(c p) e -> p c e", p=128), writes=["rw"])
            rb = sb2("rb", [128, 128])
            c.dma("sp", rb[:], W["c_rb"][:, :], writes=["rb"])
            ebase = sb2("ebase", [128, 128])
            c.dma("sp", ebase[:], W["c_ebase"][:, :], writes=["ebase"])
            SU = sb2("SU", [128, 128], BF16)
            ONES = sb2("ONESm", [128, 128], BF16)
            c.dma("pool", SU[:], W["c_su"][:, :], writes=["SU"])
            c.op("pool", lambda e: e.memset(ONES[:], 1.0), writes=["ONESm"])
            offs = sb2("offs", [128, 16])
            c.op("pool", lambda e: e.memset(offs[:], 0.0), writes=["offs"])
            TB = min(8, NT)
            TE = TB * 16
            xt = [sb2("rxt", [128, D]) for _ in range(2)]
            xT = [sb2("rxT", [128, 8, 128]) for _ in range(2)]
            aff = sb2("aff", [128, TE])
            s = sb2("s", [128, TE])
            s2 = sb2("s2", [128, TE])
            eq = sb2("eq", [128, TE])
            m1 = sb2("m1", [128, TB * 4])
            m2 = sb2("m2", [128, TB * 4])
            gs = sb2("gs", [128, TB * 4])
            gm = sb2("gm", [128, 2, TB])
            sel = sb2("sel", [128, TE])
            selb = sb2("selb", [128, TE], BF16)
            gate = sb2("gate", [128, TE])
            val = sb2("val", [128, TE])
            offT = sb2("offT", [128, TE])
            sl = sb2("sl", [128, 2, TB])
            g4 = lambda ap: ap.rearrange("p (a e) -> p a e", e=4)
            t16 = lambda ap: ap.rearrange("p (t e) -> p t e", e=16)
            bc4 = lambda ap: ap.unsqueeze(2).to_broadcast([128, TB * 4, 4])
            bc16 = lambda ap: ap.unsqueeze(2).to_broadcast([128, TB, 16])
            n = 0
            for tb in range(NT // TB):
                for i in range(TB):
                    t = tb * TB + i
                    b = n % 2
                    n += 1
                    r0 = t * 128
                    c.dma("sp", xt[b][:], xin[r0:r0 + 128, :], reads=[xin.tensor.name], writes=["rxt%d" % b])
                    self.transpose_in(xT[b], xt[b], 8, "rxt%d" % b, "rxT%d" % b)
                    psL, pkL = self.nps()
                    for kc in range(8):
                        self.mm(psL[:, 0:16], xT[b][:, kc, :], rw[:, kc, :], kc == 0, kc == 7, ["rxT%d" % b, "rw"], pkL)
                    self.act(aff[:, i * 16:(i + 1) * 16], psL[:, 0:16], AF.Sigmoid, [pkL], ["aff"])
                self.tt("dve", s[:], aff[:], rb[:, 0:TE], ALU.add, ["aff", "rb"], ["s"])
                self.red("dve", m1[:], g4(s[:]), ALU.max, ["s"], ["m1"])
                self.tt("dve", g4(eq[:]), g4(s[:]), bc4(m1[:]), ALU.is_ge, ["s", "m1"], ["eq"])
                self.stt("dve", s2[:], eq[:], -1e9, s[:], ALU.mult, ALU.add, ["eq", "s"], ["s2"])
                self.red("dve", m2[:], g4(s2[:]), ALU.max, ["s2"], ["m2"])
                self.tt("dve", gs[:], m1[:], m2[:], ALU.add, ["m1", "m2"], ["gs"])
                gs3 = gs[:].rearrange("p (t g) -> p t g", g=4)
                self.red("dve", gm[:, 0, :], gs3, ALU.max, ["gs"], ["gm"])
                self.tt("dve", gs3, gs3, gm[:, 0, :].unsqueeze(2).to_broadcast([128, TB, 4]), ALU.is_ge, ["gs", "gm"], ["gs"])
                self.tt("dve", g4(sel[:]), g4(s[:]), bc4(m2[:]), ALU.is_ge, ["s", "m2"], ["sel"])
                self.tt("dve", g4(sel[:]), g4(sel[:]), bc4(gs[:]), ALU.mult, ["sel", "gs"], ["sel"])
                self.tt("dve", gate[:], aff[:], sel[:], ALU.mult, ["aff", "sel"], ["gate"])
                self.red("dve", gm[:, 1, :], t16(gate[:]), ALU.add, ["gate"], ["gm"])
                c.op("dve", lambda e: e.reciprocal(out=gm[:, 1, :], in_=gm[:, 1, :]), reads=["gm"], writes=["gm"])
                self.tt("dve", t16(gate[:]), t16(gate[:]), bc16(gm[:, 1, :]), ALU.mult, ["gate", "gm"], ["gate"])
                self.cp("dve", selb[:], sel[:], ["sel"], ["selb"])
                psC, pkC = self.nps()
                for i in range(TB):
                    self.mm(psC[:, i * 16:(i + 1) * 16], SU[:], selb[:, i * 16:(i + 1) * 16], True, True, ["SU", "selb"], pkC)
                    self.mm(psC[:, 128 + i * 16:128 + (i + 1) * 16], ONES[:], selb[:, i * 16:(i + 1) * 16], True, True, ["ONESm", "selb"], pkC)
                self.cp("dve", offT[:, 0:16], offs[:], ["offs"], ["offT"])
                for i in range(1, TB):
                    self.tt("dve", offT[:, i * 16:(i + 1) * 16], offT[:, (i - 1) * 16:i * 16], psC[:, 128 + (i - 1) * 16:128 + i * 16],
                            ALU.add, [pkC, "offT"], ["offT"])
                self.tt("dve", offs[:], offT[:, (TB - 1) * 16:TB * 16], psC[:, 128 + (TB - 1) * 16:128 + TB * 16], ALU.add,
                        [pkC, "offT"], ["offs"])
                self.tt("dve", val[:], offT[:], psC[:, 0:TE], ALU.add, [pkC, "offT"], ["val"])
                self.ts("dve", val[:], val[:], float(CAP - 1), ALU.min, ["val"], ["val"])
                self.tt("dve", val[:], val[:], ebase[:, 0:TE], ALU.add, ["val", "ebase"], ["val"])
                self.tt("dve", val[:], val[:], sel[:], ALU.mult, ["val", "sel"], ["val"])
                self.ts("dve", val[:], val[:], -1.0, ALU.add, ["val"], ["val"])
                ts_ = slice(tb * TB, (tb + 1) * TB)
                for j in range(2):
                    self.red("dve", sl[:, j, :], t16(val[:]), ALU.max, ["val"], ["sl"])
                    self.tt("dve", t16(eq[:]), t16(val[:]), bc16(sl[:, j, :]), ALU.is_equal, ["val", "sl"], ["eq"])
                    self.tt("dve", s2[:], eq[:], gate[:], ALU.mult, ["eq", "gate"], ["s2"])
                    self.red("dve", wAB[:, ts_, j], t16(s2[:]), ALU.add, ["s2"], ["wAB"])
                    self.cp("dve", slotAB[:, ts_, j], sl[:, j, :], ["sl"], ["slotAB"])
                    if j == 0:
                        self.stt("dve", val[:], eq[:], -1e9, val[:], ALU.mult, ALU.add, ["eq", "val"], ["val"])
                if "dbg_aff" in self.dbg and tb == 0 and layer == 0:
                    for nm, tl_, w_ in (("dbg_aff", aff, TE), ("dbg_offT", offT, TE), ("dbg_gate", gate, TE), ("dbg_sel", sel, TE)):
                        dd = self.dscr(nm, [128, w_])
                        c.dma("sp", dd[:, :], tl_[:, 0:w_], reads=["aff", "offT", "gate", "sel"], writes=[nm])
                    dd = self.dscr("dbg_slotAB", [128, NT * 2], I32)
                    c.dma("sp", dd[:, :], slotAB[:].rearrange("p t j -> p (t j)"), reads=["slotAB"], writes=["dbg_slotAB"])
                    dd = self.dscr("dbg_wAB", [128, NT * 2])
                    c.dma("sp", dd[:, :], wAB[:].rearrange("p t j -> p (t j)"), reads=["wAB"], writes=["dbg_wAB"])
                    dd = self.dscr("dbg_sl", [128, 2 * TB])
                    c.dma("sp", dd[:, :], sl[:].rearrange("p a t -> p (a t)"), reads=["sl"], writes=["dbg_sl"])
                for i in range(TB):
                    t = tb * TB + i
                    for j in range(2):
                        c.dma("pool", toklist, tokid[:, t, :], reads=["tokid", "slotAB"], writes=["toklist"],
                              indirect=(bass.IndirectOffsetOnAxis(ap=slotAB[:, t, j:j + 1], axis=0), None))
            c.barrier()
        with ExitStack() as st2:
            sb2 = lambda n, shp, dt=F32: self.sb(st2, n, shp, dt)
            Wg = [sb2("Wg", [128, 8, D], BF16) for _ in range(2)]
            Wu = [sb2("Wu", [128, 8, D], BF16) for _ in range(2)]
            Wd = [sb2("Wd", [128, 8, D], BF16) for _ in range(2)]
            idx = [sb2("idx", [128, 16], I32) for _ in range(2)]
            X = [sb2("Xg", [128, D]) for _ in range(2)]
            xTg = [sb2("xTg", [128, 8, 512], BF16) for _ in range(2)]
            hs = [sb2("hs", [128, 512]) for _ in range(2)]
            hT = sb2("hTm", [128, 8, 512], BF16)
            ysb = [sb2("ysb", [128, D]) for _ in range(2)]
            n = 0
            gi = 0
            wstg = [sb2("wstg", [128, D]) for _ in range(8)]
            wsrc = [W["moe_w_gate"], W["moe_w_up"], W["moe_w_down"]]

            def chunk_dma(e, ci, si):
                m_, kc = ci // 8, ci % 8
                c.dma("sp", wstg[si][:], wsrc[m_][layer, e][kc * 128:(kc + 1) * 128, :], reads=[], writes=["mwstg%d" % si])

            def chunk_cast(e, ci, si):
                m_, kc = ci // 8, ci % 8
                dstw = (Wg, Wu, Wd)[m_][e % 2]
                self.cp("act" if ci % 2 == 0 else "dve", dstw[:, kc, :], wstg[si][:], ["mwstg%d" % si], ["W%d" % (e % 2)])

            for ci in range(24):
                chunk_dma(0, ci, ci % 8)
                chunk_cast(0, ci, ci % 8)
            per_slot = (24 + NG - 1) // NG
            for e in range(16):
                wbuf = e % 2
                kw = "W%d" % wbuf
                for grp in range(NG):
                    gb = gi % 2
                    gi += 1
                    nxt = [ci for ci in range(grp * per_slot, min(24, (grp + 1) * per_slot))] if e + 1 < 16 else []
                    if per_slot <= 8:
                        for k_, ci in enumerate(nxt):
                            chunk_dma(e + 1, ci, k_)
                    for i in range(4):
                        s0 = e * CAP + grp * 512 + i * 128
                        b = n % 2
                        n += 1
                        c.dma("pool", idx[b][:], toklist[s0:s0 + 128, :], reads=["toklist"], writes=["idx%d" % b])
                        c.dma("pool", X[b][:], xin, reads=[xin.tensor.name, "idx%d" % b], writes=["Xg%d" % b],
                              indirect=(None, bass.IndirectOffsetOnAxis(ap=idx[b][:, 0:1], axis=0)))
                        self.transpose_in(xTg[gb][:, :, i * 128:(i + 1) * 128], X[b], 8, "Xg%d" % b, "xTg%d" % gb)
                    for fc in range(8):
                        fs = slice(fc * 128, (fc + 1) * 128)
                        psG, pkG = self.nps()
                        for kc in range(8):
                            self.mm(psG[:, :], Wg[wbuf][:, kc, fs], xTg[gb][:, kc, :], kc == 0, kc == 7, [kw, "xTg%d" % gb], pkG)
                        psU, pkU = self.nps()
                        for kc in range(8):
                            self.mm(psU[:, :], Wu[wbuf][:, kc, fs], xTg[gb][:, kc, :], kc == 0, kc == 7, [kw, "xTg%d" % gb], pkU)
                        hb = fc % 2
                        self.act(hs[hb][:], psG[:, :], AF.Silu, [pkG], ["hs%d" % hb])
                        self.tt("dve", hT[:, fc, :], hs[hb][:], psU[:, :], ALU.mult, ["hs%d" % hb, pkU], ["hTm"])
                    for i in range(4):
                        s0 = e * CAP + grp * 512 + i * 128
                        yb = i % 2
                        for half in range(2):
                            ps, pk = self.nps()
                            for fc in range(8):
                                self.mm(ps[:, :], hT[:, fc, i * 128:(i + 1) * 128], Wd[wbuf][:, fc, half * 512:(half + 1) * 512],
                                        fc == 0, fc == 7, [kw, "hTm"], pk)
                            self.cp("act", ysb[yb][:, half * 512:(half + 1) * 512], ps[:, :], [pk], ["ysb%d" % yb])
                        c.dma("sp", ybuf[s0:s0 + 128, :], ysb[yb][:], reads=["ysb%d" % yb], writes=["ybuf"])
                    for k_, ci in enumerate(nxt):
                        if per_slot > 8:
                            chunk_dma(e + 1, ci, k_ % 8)
                        chunk_cast(e + 1, ci, k_ % 8)
            c.barrier()
        with ExitStack() as st2:
            sb2 = lambda n, shp, dt=F32: self.sb(st2, n, shp, dt)
            lnp = sb2("lnp", [128, 2 * D])
            c.dma("sp", lnp[:], lnp_ap[:, :], writes=["lnp"])
            tl = (sb2("ln_s", [128, 16]), sb2("ln_zc", [128, D]), None)
            xr = [sb2("cx", [128, D]) for _ in range(2)]
            yA = [sb2("cyA", [128, D]) for _ in range(2)]
            yB = [sb2("cyB", [128, D]) for _ in range(2)]
            z = [sb2("cz", [128, D]) for _ in range(2)]
            o = [sb2("co", [128, D]) for _ in range(2)]
            for t in range(NT):
                b = t % 2
                r0 = t * 128
                c.dma("sp", xr[b][:], xin[r0:r0 + 128, :], reads=[xin.tensor.name], writes=["cx%d" % b])
                c.dma("pool", yA[b][:], ybuf, reads=["ybuf", "slotAB"], writes=["cyA%d" % b],
                      indirect=(None, bass.IndirectOffsetOnAxis(ap=slotAB[:, t, 0:1], axis=0)))
                c.dma("pool", yB[b][:], ybuf, reads=["ybuf", "slotAB"], writes=["cyB%d" % b],
                      indirect=(None, bass.IndirectOffsetOnAxis(ap=slotAB[:, t, 1:2], axis=0)))
                self.ts("dve", z[b][:], xr[b][:], ALPHA, ALU.mult, ["cx%d" % b], ["cz%d" % b])
                self.stt("dve", z[b][:], yA[b][:], wAB[:, t, 0:1], z[b][:], ALU.mult, ALU.add, ["cyA%d" % b, "wAB", "cz%d" % b], ["cz%d" % b])
                self.stt("dve", z[b][:], yB[b][:], wAB[:, t, 1:2], z[b][:], ALU.mult, ALU.add, ["cyB%d" % b, "wAB", "cz%d" % b], ["cz%d" % b])
                self.ln_tile(z[b][:], "cz%d" % b, lnp, o[b][:], "co%d" % b, tl)
                c.dma("sp", dst[r0:r0 + 128, :], o[b][:], reads=["co%d" % b], writes=[dst.tensor.name])
            c.barrier()


B.ln_tile = ln_tile
B.stage_mix = stage_mix
B.stage_moe = stage_moe


def stage_ret(self, p1, ret, W):
    c = self.c
    NT = self.NT
    with ExitStack() as st:
        sb = lambda n, shp, dt=F32: self.sb(st, n, shp, dt)
        dec = sb("rtdec", [128, 8 * 128 + 24])
        c.dma("sp", dec[:], W["c_rtdec"][:, :], writes=["rtdec"])
        DT = lambda h: dec[:, h * 128:(h + 1) * 128]
        qd = dec[:, 1024:1032]
        kd = dec[:, 1032:1040]
        cd = dec[:, 1040:1048]
        gn = sb("rtgn", [128, 4096])
        c.dma("sp", gn[:], W["c_rtgn"][:, :], writes=["rtgn"])
        R = sb("R", [128, 8, 256])
        Rb = sb("Rb", [128, 8, 256], BF16)
        c.op("pool", lambda e: e.memset(R[:].rearrange("p h v -> p (h v)"), 0.0), writes=["R"])
        c.op("pool", lambda e: e.memset(Rb[:].rearrange("p h v -> p (h v)"), 0.0), writes=["Rb"])
        rope = sb("rtrope", [128, 256])
        Pq = [sb("Pq", [128, 2048]) for _ in range(2)]
        Vv = [sb("Vv", [128, 2048]) for _ in range(2)]
        Gg = [sb("Gg", [128, 2048]) for _ in range(2)]
        qk = sb("qkr", [128, 3, 1024])
        ktb = sb("ktb", [128, 1024], BF16)
        tmp = sb("rtmp", [128, 8, 64])
        T3 = sb("T3", [128, 24, 128], BF16)
        Vb = sb("Vb", [128, 2048], BF16)
        attm = sb("attm", [128, 1024], BF16)
        Os_ = [sb("rOs", [128, 2048]) for _ in range(2)]
        sq = sb("rsq", [128, 2048])
        sg = sb("rsg", [128, 2048])
        st8 = sb("rst8", [128, 8])
        oo = sb("roo", [128, 2048])
        def tail(t, b):
            r0 = t * 128
            kg = "Gg%d" % b
            Os = Os_[b]
            ko = "rOs%d" % b
            O3 = Os[:].rearrange("p (h v) -> p h v", h=8)
            self.red("dve", st8[:], O3, ALU.add, [ko], ["rst8"])
            self.ts("dve", st8[:], st8[:], 1.0 / 256, ALU.mult, ["rst8"], ["rst8"])
            self.tt("dve", O3, O3, st8[:].unsqueeze(2).to_broadcast([128, 8, 256]), ALU.subtract, [ko, "rst8"], [ko])
            self.act(sq[:], Os[:], AF.Square, [ko], ["rsq"])
            self.red("dve", st8[:], sq[:].rearrange("p (h v) -> p h v", h=8), ALU.add, ["rsq"], ["rst8"])
            self.rsqrt(st8[:], st8[:], 1e-5, ["rst8"], ["rst8"], scale=1.0 / 256)
            self.tt("dve", O3, O3, st8[:].unsqueeze(2).to_broadcast([128, 8, 256]), ALU.mult, [ko, "rst8"], [ko])
            self.tt("dve", Os[:], Os[:], gn[:, 0:2048], ALU.mult, [ko, "rtgn"], [ko])
            self.tt("pool", Os[:], Os[:], gn[:, 2048:4096], ALU.add, [ko, "rtgn"], [ko])
            self.act(sg[:], Gg[b][:], AF.Silu, [kg], ["rsg"])
            self.tt("dve", oo[:], Os[:], sg[:], ALU.mult, [ko, "rsg"], ["roo"])
            c.dma("sp", ret[r0:r0 + 128, :], oo[:], reads=["roo"], writes=["ret"])
        for t in range(NT):
            b = t % 2
            r0 = t * 128
            kp, kv, kg = "Pq%d" % b, "Vv%d" % b, "Gg%d" % b
            c.dma("sp", Pq[b][:], p1[r0:r0 + 128, 0:2048], reads=["p1"], writes=[kp])
            c.dma("sp", Vv[b][:], p1[r0:r0 + 128, 2048:4096], reads=["p1"], writes=[kv])
            c.dma("sp", Gg[b][:], p1[r0:r0 + 128, 4096:6144], reads=["p1"], writes=[kg])
            c.dma("sp", rope[:], W["c_rtrope"][r0:r0 + 128, :], writes=["rtrope"])
            for a in range(2):
                src = Pq[b][:, a * 1024:(a + 1) * 1024].rearrange("p (h d) -> p h d", h=8)
                dst = qk[:, a, :].rearrange("p (h d) -> p h d", h=8)
                cos = rope[:, a * 128:a * 128 + 64].unsqueeze(1).to_broadcast([128, 8, 64])
                sin = rope[:, a * 128 + 64:a * 128 + 128].unsqueeze(1).to_broadcast([128, 8, 64])
                x1, x2 = src[:, :, 0:64], src[:, :, 64:128]
                o1, o2 = dst[:, :, 0:64], dst[:, :, 64:128]
                kq = "qk%d" % a
                self.tt("dve", o1, x1, cos, ALU.mult, [kp, "rtrope"], [kq])
                self.tt("pool", tmp[:], x2, sin, ALU.mult, [kp, "rtrope"], ["rtmp"])
                self.tt("dve", o1, o1, tmp[:], ALU.subtract, [kq, "rtmp"], [kq])
                self.tt("dve", o2, x2, cos, ALU.mult, [kp, "rtrope"], [kq])
                self.tt("pool", tmp[:], x1, sin, ALU.mult, [kp, "rtrope"], ["rtmp"])
                self.tt("dve", o2, o2, tmp[:], ALU.add, [kq, "rtmp"], [kq])
            q3 = qk[:, 0, :].rearrange("p (h d) -> p h d", h=8)
            k3 = qk[:, 1, :].rearrange("p (h d) -> p h d", h=8)
            self.tt("pool", qk[:, 2, :].rearrange("p (h d) -> p h d", h=8), q3, qd.unsqueeze(2).to_broadcast([128, 8, 128]),
                    ALU.mult, ["qk0", "rtdec"], ["qk2"])
            self.tt("dve", ktb[:].rearrange("p (h d) -> p h d", h=8), k3, kd.unsqueeze(2).to_broadcast([128, 8, 128]),
                    ALU.mult, ["qk1", "rtdec"], ["ktb"])
            self.cp("act", Vb[:], Vv[b][:], [kv], ["Vb"])
            for a in range(3):
                self.transpose_in(T3, qk[:, a, :], 8, "qk%d" % a, "T3_%d" % a, ch0=8 * a)
            for hq in range(2):
                psA, pkA = self.nps()
                for hh in range(4):
                    h = hq * 4 + hh
                    self.mm(psA[:, hh * 128:(hh + 1) * 128], T3[:, 8 + h, :], T3[:, h, :], True, True, ["T3_0", "T3_1"], pkA)
                self.tt("dve", attm[:, hq * 512:(hq + 1) * 512], psA[:, :], dec[:, hq * 512:(hq + 1) * 512], ALU.mult,
                        [pkA, "rtdec"], ["attm%d" % hq])
            for hp in range(4):
                psO, pkO = self.nps()
                for hh in range(2):
                    h = hp * 2 + hh
                    vs = slice(h * 256, (h + 1) * 256)
                    self.mm(psO[:, hh * 256:(hh + 1) * 256], attm[:, h * 128:(h + 1) * 128], Vb[:, vs], True, False,
                            ["attm%d" % (h // 4), "Vb"], pkO)
                    self.mm(psO[:, hh * 256:(hh + 1) * 256], T3[:, 16 + h, :], Rb[:, h, :], False, True, ["T3_2", "Rb"], pkO)
                self.cp("act", Os_[b][:, hp * 512:(hp + 1) * 512], psO[:, :], [pkO], ["rOs%d" % b])
            R2 = R[:].rearrange("p h v -> p (h v)")
            self.tt("pool", R[:], R[:], cd.unsqueeze(2).to_broadcast([128, 8, 256]), ALU.mult, ["R", "rtdec"], ["R"])
            for hp in range(4):
                psR, pkR = self.nps()
                for hh in range(2):
                    h = hp * 2 + hh
                    vs = slice(h * 256, (h + 1) * 256)
                    self.mm(psR[:, hh * 256:(hh + 1) * 256], ktb[:, h * 128:(h + 1) * 128], Vb[:, vs], True, True, ["ktb", "Vb"], pkR)
                self.tt("dve", R2[:, hp * 512:(hp + 1) * 512], R2[:, hp * 512:(hp + 1) * 512], psR[:, :], ALU.add, [pkR, "R"], ["R"])
            self.cp("act", Rb[:].rearrange("p h v -> p (h v)"), R2, ["R"], ["Rb"])
            if t > 0:
                tail(t - 1, 1 - b)
        tail(NT - 1, (NT - 1) % 2)
        c.barrier()


B.stage_ret = stage_ret


STAGES = ["proj0", "rwkv", "nsa", "mix0", "moe0", "proj1", "ret", "mix1", "moe1"]


def build(S, upto="moe1", dbg=()):
    b = B(S, dbg)
    n_st = STAGES.index(upto) + 1
    on = lambda s: STAGES.index(s) < n_st
    CAP = moe_cap(S)
    NSLOT = 16 * CAP
    ncp = ((((S - 32) // 16 + 1) + 127) // 128) * 128
    W = {}
    for name, shp, dt in [("c_ident", [128, 128], F32), ("c_tri", [128, 128], F32), ("c_mask4", [128, 512], F32),
                          ("c_maskL", [128, 128], F32), ("c_bd", [128, 128], F32),
                          ("c_rkv", [128, 13 * 512], F32), ("rk_w1", [512, 64], F32), ("rk_a1", [512, 64], F32),
                          ("rk_g1", [512, 128], F32), ("rk_w2", [64, 512], F32), ("rk_a2", [64, 512], F32),
                          ("rk_g2", [128, 512], F32),
                          ("ns_c_w1", [2, 2048, 128], F32), ("ns_c_w2", [2, 128, 64], F32), ("c_peT", [128, 64], F32),
                          ("c_ones", [128, 1], F32), ("c_rope", [S, 16], F32), ("c_selF", [S, 128], F32),
                          ("c_E", [128, S], F32), ("c_caus", [128, 4 * 512], F32), ("c_win", [128, 8 * 512], F32),
                          ("c_cmpb", [128, 5 * 512], F32), ("c_ov", [ncp, 128], F32),
                          ("ab_w_out", [D, D], F32), ("c_ln", [4, 128, 2 * D], F32),
                          ("router_w", [D, 16], F32), ("c_rb", [128, 128], F32), ("c_ebase", [128, 128], F32),
                          ("c_su", [128, 128], F32), ("c_tokid", [128, (S // 128) * 16], I32),
                          ("c_tokinit", [NSLOT + 1, 16], I32),
                          ("moe_w_gate", [2, 16, D, D], F32), ("moe_w_up", [2, 16, D, D], F32),
                          ("moe_w_down", [2, 16, D, D], F32),
                          ("rt_w_in", [D, 6144], F32), ("rt_w_out", [2048, D], F32), ("c_rtgn", [128, 2 * 2048], F32),
                          ("c_rtrope", [S, 256], F32), ("c_rtdec", [128, 8 * 128 + 24], F32)]:
        W[name] = b.din(name, shp, dt)
    x = b.din("x", [S, D])
    ab_w_in = b.din("ab_w_in", [D, 3352])
    p0 = b.dscr("p0", [S, 3352])
    oab = b.dscr("oab", [S, 1024])
    x1 = b.dscr("x1", [S + 1, D])
    x2 = b.dscr("x2", [S + 1, D])
    x3 = b.dscr("x3", [S + 1, D])
    p1 = b.dscr("p1", [S, 6144])
    ret = b.dscr("ret", [S, 2048])
    toklist = b.dscr("toklist", [NSLOT + 1, 16], I32)
    ybuf = b.dscr("ybuf", [NSLOT, D])
    out = b.nc.dram_tensor("out", [S, D], F32, kind="ExternalOutput").ap()
    with ExitStack() as st:
        b.load_consts(st)
        zrow = b.sb(st, "zrow", [1, D])
        b.c.op("pool", lambda e: e.memset(zrow[:], 0.0), writes=["zrow"])
        for xx in (x1, x3):
            b.c.dma("sp", xx[S:S + 1, :], zrow[:], reads=["zrow"], writes=[xx.tensor.name])
        b.stage_proj(x, ab_w_in, p0, D, 3352)
        if on("rwkv"):
            b.stage_rwkv(p0, oab, W)
        if on("nsa"):
            b.stage_nsa(p0, oab, W)
        if on("mix0"):
            b.stage_mix(oab, 1024, W["ab_w_out"], x, W["c_ln"][0], x1)
        if on("moe0"):
            b.stage_moe(x1, W, 0, W["c_ln"][1], x2, toklist, ybuf)
        if on("proj1"):
            b.stage_proj(x2, W["rt_w_in"], p1, D, 6144)
        if on("ret"):
            b.stage_ret(p1, ret, W)
        if on("mix1"):
            b.stage_mix(ret, 2048, W["rt_w_out"], x2, W["c_ln"][2], x3)
        if on("moe1"):
            b.stage_moe(x3, W, 1, W["c_ln"][3], out, toklist, ybuf)
        b.c.finish()
    return b


def consts(S):
    c = {}
    c["c_ident"] = np.eye(128, dtype=np.float32)
    i = np.arange(128)
    same = (i[:, None] // 64) == (i[None, :] // 64)
    strict = ((i[:, None] < i[None, :]) & same).astype(np.float32)
    incl = ((i[:, None] <= i[None, :]) & same).astype(np.float32)
    c["c_tri"] = incl
    c["c_mask4"] = np.concatenate([strict, incl, strict, incl], axis=1)
    c["c_bd"] = same.astype(np.float32)
    c["c_ones"] = np.ones((128, 1), np.float32)
    inv = 500000.0 ** (-np.arange(8, dtype=np.float32) / 8)
    ang = np.arange(S, dtype=np.float32)[:, None] * inv[None, :]
    c["c_rope"] = np.concatenate([np.cos(ang), np.sin(ang)], axis=1).astype(np.float32)
    tpos = np.arange(S)
    cur = tpos // 64
    jb = np.arange(128)
    F = np.zeros((S, 128), np.float32)
    F[jb[None, :] > cur[:, None]] = -10.0
    forced = (jb[None, :] == 0) | (jb[None, :] == cur[:, None]) | (jb[None, :] == cur[:, None] - 1)
    F[forced & (jb[None, :] <= cur[:, None])] = 10.0
    c["c_selF"] = F
    c["c_E"] = (np.arange(S)[None, :] // 64 == jb[:, None]).astype(np.float32)
    k = np.arange(128)[:, None]
    q = np.arange(512)[None, :]
    c["c_caus"] = np.concatenate([np.where(128 * d + k <= q, 0.0, NEG) for d in range(4)], axis=1).astype(np.float32)
    c["c_win"] = np.concatenate([np.where((128 * d + k <= q) & (128 * d + k > q - 512), 0.0, NEG) for d in range(-4, 4)],
                                axis=1).astype(np.float32)
    c["c_cmpb"] = np.concatenate([np.where(16 * k + 31 <= 512 * dj + q, 0.0, NEG) for dj in range(5)], axis=1).astype(np.float32)
    n_cmp = (S - 32) // 16 + 1
    ncp = ((n_cmp + 127) // 128) * 128
    cs = np.arange(ncp) * 16
    ss = np.arange(128) * 64
    ov = np.clip(np.minimum(cs[:, None] + 32, ss[None, :] + 64) - np.maximum(cs[:, None], ss[None, :]), 0, None).astype(np.float32) / 32
    ov[n_cmp:] = 0.0
    c["c_ov"] = ov
    CAP = moe_cap(S)
    c["c_ebase"] = np.ascontiguousarray(np.broadcast_to(np.tile((np.arange(16) * CAP + 1).astype(np.float32), 8)[None, :], (128, 128)))
    c["c_su"] = (i[:, None] < i[None, :]).astype(np.float32)
    NT = S // 128
    tok = (np.arange(NT)[None, :, None] * 128 + np.arange(128)[:, None, None] + np.zeros((1, 1, 16), np.int64))
    c["c_tokid"] = np.ascontiguousarray(tok.reshape(128, NT * 16)).astype(np.int32)
    inv = 10000.0 ** (-np.linspace(0.0, 1.0, 64, dtype=np.float32))
    ang = np.arange(S, dtype=np.float32)[:, None] * inv[None, :]
    cs_, sn_ = np.cos(ang), np.sin(ang)
    sc = 128.0 ** -0.5
    c["c_rtrope"] = np.concatenate([cs_, sn_, cs_ * sc, sn_ * sc], axis=1).astype(np.float32)
    log_g = np.log(1.0 - 2.0 ** (-5.0 - np.arange(8, dtype=np.float64)))
    ii = np.arange(128, dtype=np.float64)
    diff = ii[None, :] - ii[:, None]
    DTm = [np.where(diff >= 0, np.exp(np.maximum(diff, 0.0) * lg), 0.0) for lg in log_g]
    qd = np.exp((ii[:, None] + 1.0) * log_g[None, :])
    kd = np.exp((127.0 - ii[:, None]) * log_g[None, :])
    cd = np.broadcast_to(np.exp(128.0 * log_g)[None, :], (128, 8))
    c["c_rtdec"] = np.concatenate(DTm + [qd, kd, cd], axis=1).astype(np.float32)
    c["c_tokinit"] = np.full((16 * CAP + 1, 16), S, np.int32)
    c["c_maskL"] = np.ascontiguousarray(strict.T)
    return c


def derived(inputs):
    d = {}
    rk = np.concatenate([inputs["rk_mu"].reshape(-1), inputs["rk_w0"].reshape(-1), inputs["rk_a0"].reshape(-1),
                         inputs["rk_kk"].reshape(-1), inputs["rk_ka"].reshape(-1), inputs["rk_rk"].reshape(-1),
                         inputs["rk_ln"].reshape(-1)])
    pe = np.asarray(inputs["ns_pe"]).reshape(2, 32, 64)
    peT = np.transpose(pe, (2, 0, 1)).reshape(64, 64)
    d["c_peT"] = np.ascontiguousarray(np.concatenate([peT, peT], axis=0)).astype(np.float32)
    ln = np.asarray(inputs["ln"]).reshape(4, 2 * D)
    d["c_ln"] = np.ascontiguousarray(np.broadcast_to(ln[:, None, :], (4, 128, 2 * D))).astype(np.float32)
    d["c_rtgn"] = np.ascontiguousarray(np.broadcast_to(np.asarray(inputs["rt_gn"]).reshape(1, 4096), (128, 4096))).astype(np.float32)
    d["c_rb"] = np.ascontiguousarray(np.broadcast_to(np.tile(np.asarray(inputs["router_b"]).reshape(16), 8)[None, :], (128, 128))).astype(np.float32)
    d["c_rkv"] = np.ascontiguousarray(np.broadcast_to(rk[None, :], (128, rk.size))).astype(np.float32)
    return d


def make_inputs(b, inputs, bi, S):
    cs = consts(S)
    cs.update(derived(inputs))
    m = {}
    for name, ap in b.inp.items():
        if name in cs:
            m[name] = cs[name]
        elif name == "x":
            m[name] = np.ascontiguousarray(inputs["x"][bi, :S])
        else:
            a = np.asarray(inputs[name])
            m[name] = np.ascontiguousarray(a.reshape(ap.shape))
    return m


_BUILT = {}


def kernel(**inputs):
    S = 8192
    if S not in _BUILT:
        _BUILT[S] = build(S)
    b = _BUILT[S]
    shared = make_inputs(b, inputs, 0, S)
    in_maps = []
    for bi in range(8):
        m = dict(shared)
        m["x"] = np.ascontiguousarray(np.asarray(inputs["x"])[bi, :S]).astype(np.float32)
        in_maps.append(m)
    res = run_bass_kernel_spmd(b.nc, in_maps, core_ids=list(range(8)))
    return np.stack([np.asarray(r["out"]) for r in res.results], axis=0).astype(np.float32)
```

```python
import numpy as np
import ml_dtypes
from contextlib import ExitStack
import concourse.bass as bass
import concourse.mybir as mybir
from concourse.bass_utils import run_bass_kernel_spmd

F32 = mybir.dt.float32
BF16 = mybir.dt.bfloat16
I32 = mybir.dt.int32
U32 = mybir.dt.uint32
AF = mybir.ActivationFunctionType
ALU = mybir.AluOpType
AX = mybir.AxisListType

D = 1024
ALPHA = (2.0 * 2) ** 0.25
LN_EPS = 1e-5
NEG = -30000.0


class Ctx:
    NDMA = 10

    def __init__(self, nc):
        self.nc = nc
        self.eng = {"pe": nc.tensor, "dve": nc.vector, "act": nc.scalar, "pool": nc.gpsimd, "sp": nc.sync}
        self.sem = {}
        self.cnt = {}
        for e in self.eng:
            self.sem["e_" + e] = nc.alloc_semaphore("sem_e_" + e)
            self.cnt["e_" + e] = 0
        self.dma_pool = {}
        for q in ("sp", "pool", "act"):
            names = []
            for i in range(self.NDMA):
                n = "d_%s_%d" % (q, i)
                self.sem[n] = nc.alloc_semaphore("sem_" + n)
                self.cnt[n] = 0
                names.append(n)
            self.dma_pool[q] = [names, 0]
        self.known = {e: {} for e in self.eng}
        self.last_w = {}
        self.readers = {}
        self.n_inst = 0
        self.n_wait = 0

    def _wait(self, e, semname, val):
        kn = self.known[e]
        if kn.get(semname, 0) >= val:
            return
        self.eng[e].wait_ge(self.sem[semname], val)
        kn[semname] = val
        self.n_wait += 1

    def _deps(self, e, reads, writes, is_dma=False):
        own = "e_" + e if not is_dma else None
        need = {}

        def add(ev, raw):
            s, v = ev
            if s == own and e == "pe":
                return
            if need.get(s, 0) < v:
                need[s] = v

        for k in reads:
            ev = self.last_w.get(k)
            if ev is not None:
                add(ev, True)
        for k in writes:
            ev = self.last_w.get(k)
            if ev is not None:
                add(ev, False)
            for s, v in self.readers.get(k, {}).items():
                add((s, v), False)
        for s, v in need.items():
            self._wait(e, s, v)

    def _commit(self, ev, reads, writes):
        s, v = ev
        for k in writes:
            self.last_w[k] = ev
            self.readers[k] = {}
        for k in reads:
            if k in writes:
                continue
            r = self.readers.setdefault(k, {})
            if r.get(s, 0) < v:
                r[s] = v

    def op(self, e, fn, reads=(), writes=()):
        reads = list(reads)
        writes = list(writes)
        self._deps(e, reads, writes)
        ins = fn(self.eng[e])
        s = "e_" + e
        self.cnt[s] += 1
        ins.then_inc(self.sem[s], 1)
        self._commit((s, self.cnt[s]), reads, writes)
        self.n_inst += 1
        return ins

    def dma(self, q, out, in_, reads=(), writes=(), indirect=None, **kw):
        reads = list(reads)
        writes = list(writes)
        self._deps(q, reads, writes, is_dma=True)
        names, i = self.dma_pool[q]
        s = names[i % len(names)]
        self.dma_pool[q][1] = i + 1
        if self.cnt[s] > 0:
            self._wait(q, s, self.cnt[s])
        if indirect is None:
            ins = self.eng[q].dma_start(out=out, in_=in_, **kw)
        else:
            ins = self.eng[q].indirect_dma_start(out, indirect[0], in_, indirect[1], **kw)
        self.cnt[s] += 16
        ins.then_inc(self.sem[s], 16)
        self._commit((s, self.cnt[s]), reads, writes)
        self.n_inst += 1
        return ins

    def barrier(self):
        for e in self.eng:
            for s, c in self.cnt.items():
                if c > 0:
                    self._wait(e, s, c)

    def finish(self):
        for s, c in self.cnt.items():
            if c > 0:
                self._wait("sp", s, c)


class B:
    def __init__(self, S, dbg=()):
        self.S = S
        self.NT = S // 128
        self.dbg = set(dbg)
        nc = self.nc = bass.Bass("TRN2", target_bir_lowering=False)
        self.c = Ctx(nc)
        self.inp = {}
        self.ps = [nc.alloc_psum_tensor("psb%d" % i, [128, 512], F32) for i in range(8)]
        self.ps_i = 0
        self._uid = 0
        import os
        self.cut = int(os.environ['CUT']) if 'CUT' in os.environ else None

    def din(self, name, shape, dt=F32):
        t = self.nc.dram_tensor(name, list(shape), dt, kind="ExternalInput").ap()
        self.inp[name] = t
        return t

    def dscr(self, name, shape, dt=F32):
        kind = "ExternalOutput" if name in self.dbg else "Internal"
        return self.nc.dram_tensor(name, list(shape), dt, kind=kind).ap()

    def sb(self, st, name, shape, dt=F32):
        self._uid += 1
        return st.enter_context(self.nc.sbuf_tensor("%s_%d" % (name, self._uid), list(shape), dt))

    def nps(self):
        i = self.ps_i % 8
        self.ps_i += 1
        return self.ps[i], "ps%d" % i

    def mm(self, out, lhsT, rhs, start, stop, reads, pk):
        return self.c.op("pe", lambda e: e.matmul(out, lhsT, rhs, start=start, stop=stop), reads=reads, writes=[pk])

    def tr(self, out, in_, ident, reads, pk):
        return self.c.op("pe", lambda e: e.transpose(out, in_, ident), reads=reads, writes=[pk])

    def cp(self, eng, out, in_, reads, writes):
        if eng == "act":
            return self.c.op("act", lambda e: e.copy(out=out, in_=in_), reads=reads, writes=writes)
        return self.c.op(eng, lambda e: e.tensor_copy(out=out, in_=in_), reads=reads, writes=writes)

    def act(self, out, in_, func, reads, writes, bias=0.0, scale=1.0, accum_out=None):
        kw = {}
        if accum_out is not None:
            kw["accum_out"] = accum_out
        return self.c.op("act", lambda e: e.activation(out=out, in_=in_, func=func, bias=bias, scale=scale, **kw),
                         reads=reads, writes=writes)

    def tt(self, eng, out, in0, in1, op, reads, writes):
        return self.c.op(eng, lambda e: e.tensor_tensor(out=out, in0=in0, in1=in1, op=op), reads=reads, writes=writes)

    def ts(self, eng, out, in0, s1, op0, reads, writes, s2=None, op1=None):
        if op0 in (ALU.pow, ALU.divide) or op1 in (ALU.pow, ALU.divide):
            eng = "pool"
        if op1 is None:
            return self.c.op(eng, lambda e: e.tensor_scalar(out=out, in0=in0, scalar1=s1, scalar2=None, op0=op0),
                             reads=reads, writes=writes)
        return self.c.op(eng, lambda e: e.tensor_scalar(out=out, in0=in0, scalar1=s1, scalar2=s2, op0=op0, op1=op1),
                         reads=reads, writes=writes)

    def rsqrt(self, out, in_, eps, reads, writes, scale=1.0):
        self.act(out, in_, AF.Sqrt, reads, writes, bias=eps, scale=scale)
        return self.c.op("dve", lambda e: e.reciprocal(out=out, in_=out), reads=writes, writes=writes)

    def stt(self, eng, out, in0, scalar, in1, op0, op1, reads, writes):
        eng = "dve"
        return self.c.op(eng, lambda e: e.scalar_tensor_tensor(out=out, in0=in0, scalar=scalar, in1=in1, op0=op0, op1=op1),
                         reads=reads, writes=writes)

    def red(self, eng, out, in_, op, reads, writes, axis=AX.X):
        return self.c.op(eng, lambda e: e.tensor_reduce(out=out, in_=in_, axis=axis, op=op), reads=reads, writes=writes)

    def load_consts(self, st):
        self.ident = self.sb(st, "ident", [128, 128], F32)
        self.c.dma("sp", self.ident[:], self.inp["c_ident"][:, :], reads=[], writes=["ident"])

    def load_w(self, dst, w_ap, K, N, key, q="pool"):
        for kc in range(K // 128):
            for n0 in range(0, N, 2048):
                n1 = min(N, n0 + 2048)
                self.c.dma(q, dst[:, kc, n0:n1], w_ap[kc * 128:(kc + 1) * 128, n0:n1], reads=[], writes=[key])

    def load_w_fast(self, st, dst, w_ap, K, N, key):
        stg = [self.sb(st, "wstg", [128, 1024]) for _ in range(4)]
        i = 0
        for kc in range(K // 128):
            for n0 in range(0, N, 1024):
                n1 = min(N, n0 + 1024)
                s_ = stg[i % 4]
                sk = "wstg%d_%s" % (i % 4, key)
                self.c.dma("sp", s_[:, 0:n1 - n0], w_ap[kc * 128:(kc + 1) * 128, n0:n1], reads=[], writes=[sk])
                self.cp("act" if i % 2 == 0 else "dve", dst[:, kc, n0:n1], s_[:, 0:n1 - n0], [sk], [key])
                i += 1

    def transpose_in(self, xT, xin, nch, rkey, wkey, col0=0, ch0=0):
        j = 0
        k = 0
        while j < nch:
            g = min(4, nch - j)
            ps, pk = self.nps()
            for i in range(g):
                self.tr(ps[:, i * 128:(i + 1) * 128], xin[:, col0 + (j + i) * 128: col0 + (j + i + 1) * 128],
                        self.ident[:], [rkey, "ident"], pk)
            eng = "act" if k % 2 == 0 else "dve"
            self.cp(eng, xT[:, ch0 + j:ch0 + j + g, :], ps[:, 0:g * 128].rearrange("p (g t) -> p g t", g=g), [pk], [wkey])
            j += g
            k += 1

    def layer_norm(self, st_tiles, z, zkey, gam, bet, out, okey):
        stats, mv, rstd = st_tiles
        nc = self.nc
        c = self.c
        for i in range(2):
            c.op("dve", lambda e: e.bn_stats(out=stats[:, i, :], in_=z[:, i * 512:(i + 1) * 512]), reads=[zkey], writes=["ln_stats"])
        c.op("dve", lambda e: e.bn_aggr(out=mv[:], in_=stats[:]), reads=["ln_stats"], writes=["ln_mv"])
        self.rsqrt(rstd[:], mv[:, 1:2], LN_EPS, ["ln_mv"], ["ln_rstd"])
        self.ts("dve", out, z, mv[:, 0:1], ALU.subtract, [zkey, "ln_mv", "ln_rstd"], [okey], s2=rstd[:, 0:1], op1=ALU.mult)
        self.tt("pool", out, out, gam, ALU.mult, [okey, "lnp"], [okey])
        self.tt("pool", out, out, bet, ALU.add, [okey, "lnp"], [okey])

    def stage_proj(self, src, w_ap, dst, K, N):
        with ExitStack() as st:
            wb = self.sb(st, "wproj", [128, K // 128, N], BF16)
            self.load_w_fast(st, wb, w_ap, K, N, "wproj")
            xin = [self.sb(st, "xin", [128, K], F32) for _ in range(2)]
            xT = [self.sb(st, "xT", [128, K // 128, 128], BF16) for _ in range(2)]
            ot = [self.sb(st, "ot", [128, N], F32) for _ in range(2)]
            for t in range(self.NT):
                b = t % 2
                self.c.dma("sp", xin[b][:], src[t * 128:(t + 1) * 128, :], reads=[src.tensor.name], writes=["xin%d" % b])
                self.transpose_in(xT[b], xin[b], K // 128, "xin%d" % b, "xT%d" % b)
                k = 0
                for n0 in range(0, N, 512):
                    w = min(512, N - n0)
                    ps, pk = self.nps()
                    for kc in range(K // 128):
                        self.mm(ps[:, 0:w], xT[b][:, kc, :], wb[:, kc, n0:n0 + w], kc == 0, kc == K // 128 - 1,
                                ["xT%d" % b, "wproj"], pk)
                    self.cp("act" if k % 2 == 0 else "dve", ot[b][:, n0:n0 + w], ps[:, 0:w], [pk], ["ot%d" % b])
                    k += 1
                self.c.dma("sp", dst[t * 128:(t + 1) * 128, :], ot[b][:], reads=["ot%d" % b], writes=[dst.tensor.name])
            self.c.barrier()


RK_C = 0.606531


def stage_rwkv(self, p0, oab, W):
    c = self.c
    NT = self.NT
    with ExitStack() as st:
        sb = lambda n, shp, dt=F32: self.sb(st, n, shp, dt)
        pv = sb("rkv", [128, 13 * 512])
        c.dma("sp", pv[:], W["c_rkv"][:, :], writes=["rkv"])
        MU = lambda i: pv[:, i * 512:(i + 1) * 512]
        W0, A0, KK_, KA_, RKk, LNG, LNB = [pv[:, (6 + i) * 512:(7 + i) * 512] for i in range(7)]
        trib = sb("trib", [128, 128], BF16)
        c.dma("pool", trib[:], W["c_tri"][:, :], writes=["trib"])
        mask4 = sb("mask4", [128, 512])
        c.dma("sp", mask4[:], W["c_mask4"][:, :], writes=["mask4"])
        maskL = sb("maskL", [128, 128])
        c.dma("sp", maskL[:], W["c_maskL"][:, :], writes=["maskL"])
        identb = sb("identb", [128, 128], BF16)
        c.dma("pool", identb[:], W["c_ident"][:, :], writes=["identb"])
        w1 = sb("w1", [128, 4, 64], BF16)
        a1 = sb("a1", [128, 4, 64], BF16)
        g1 = sb("g1", [128, 4, 128], BF16)
        self.load_w(w1, W["rk_w1"], 512, 64, "w1")
        self.load_w(a1, W["rk_a1"], 512, 64, "a1")
        self.load_w(g1, W["rk_g1"], 512, 128, "g1")
        w2 = sb("w2", [64, 512], BF16)
        a2 = sb("a2", [64, 512], BF16)
        g2 = sb("g2", [128, 512], BF16)
        c.dma("pool", w2[:], W["rk_w2"][:, :], writes=["w2"])
        c.dma("pool", a2[:], W["rk_a2"][:, :], writes=["a2"])
        c.dma("pool", g2[:], W["rk_g2"][:, :], writes=["g2"])
        H = sb("H", [128, 4, 128])
        Hb = sb("Hb", [128, 4, 128], BF16)
        bd = sb("bd", [128, 128])
        c.dma("sp", bd[:], W["c_bd"][:, :], writes=["bd"])
        c.op("dve", lambda e: e.memset(H[:], 0.0), writes=["H"])
        c.op("dve", lambda e: e.memset(Hb[:], 0.0), writes=["Hb"])
        SINGLE = {"Pmm", "swh", "P", "Ps", "X", "xT", "hT", "sw", "a", "kk", "kp", "tmp", "ss", "cs", "e", "T", "BT", "KT", "Q"}

        def two(n, shp, dt=F32):
            return [sb(n, shp, dt) for _ in range(2)]

        def one(n, shp, dt=F32):
            x = sb(n, shp, dt)
            return [x, x]
        P_ = one("P", [128, 2048])
        Ps_ = one("Ps", [128, 2048])
        X6_ = one("X6", [128, 6, 512])
        xT_ = one("xT3", [128, 12, 128], BF16)
        hT_ = one("hT", [128, 384], BF16)
        sw_ = one("sw", [128, 512])
        swh = sb("swh", [128, 2, 512], BF16)
        a_ = one("a", [128, 512])
        g_ = two("g", [128, 512])
        kk_ = one("kk", [128, 512])
        kp_ = one("kp", [128, 512])
        tmp_ = one("tmp", [128, 512])
        tmp2_ = two("tq", [128, 512])
        ss_ = one("ss", [128, 8])
        bon_ = two("bon", [128, 512])
        sq_ = two("sq", [128, 8])
        bdg_ = [[sb("bdg", [128, 4, 128]) for _ in range(2)] for _ in range(2)]
        cs_ = one("cs", [128, 512])
        e_ = one("e3", [128, 3, 512])
        T4_ = one("T4", [128, 4, 512])
        Bt_ = two("Bt", [128, 512], BF16)
        Kt_ = two("Kt", [128, 512], BF16)
        Vt_ = two("Vt", [128, 512], BF16)
        ART_ = two("ART", [128, 4, 256], BF16)
        BT_ = one("BT", [128, 4, 128], BF16)
        KT_ = one("KT", [128, 4, 128], BF16)
        ET_ = two("ET", [128, 4, 128])
        G_ = two("G", [128, 8, 512], BF16)
        Wm_ = two("Wm", [128, 8, 128], BF16)
        Pm_ = one("Pm", [128, 8, 128], BF16)
        Qm_ = one("Qm", [128, 8, 128], BF16)
        Xs_ = two("Xs", [128, 512], BF16)
        Ub_ = [[sb("Ub", [128, 512], BF16) for _ in range(2)] for _ in range(2)]
        Vm_ = [[sb("Vm", [128, 512], BF16) for _ in range(2)] for _ in range(2)]
        for bb in range(2):
            c.op("pool", lambda e: e.memset(Xs_[bb][:], 0.0), writes=["Xs%d" % bb])
            for cc in range(2):
                c.op("pool", lambda e: e.memset(Ub_[bb][cc][:], 0.0), writes=["Ub%d_%d" % (cc, bb)])
                c.op("pool", lambda e: e.memset(Vm_[bb][cc][:], 0.0), writes=["Vm%d%d" % (cc, bb)])
        Os_ = two("Os", [128, 512])
        oo_ = two("oo", [128, 512])
        pend_post = [None]
        for t in range(NT if self.cut is None else 1):
            b = t % 2
            K = lambda n: n if n.rstrip("0123456789_") in SINGLE else "%s%d" % (n, b)
            P, Ps, X6, xT, hT = P_[b], Ps_[b], X6_[b], xT_[b], hT_[b]
            sw, a, g, kk, kp, tmp, tmp2, ss, bon, cs, e3, T4 = sw_[b], a_[b], g_[b], kk_[b], kp_[b], tmp_[b], tmp2_[b], ss_[b], bon_[b], cs_[b], e_[b], T4_[b]
            Bt, Kt, Vt, ART, BT, KT, ET, G, Wm, Pm, Qm, Xs, Ub, Os, oo = Bt_[b], Kt_[b], Vt_[b], ART_[b], BT_[b], KT_[b], ET_[b], G_[b], Wm_[b], Pm_[b], Qm_[b], Xs_[b], Ub_[b], Os_[b], oo_[b]
            sq = sq_[b]
            bdg = bdg_[b]
            Vm = Vm_[b]
            r0 = t * 128
            c.dma("sp", P[:], p0[r0:r0 + 128, 0:2048], reads=["p0"], writes=[K("Pmm")])
            if t == 0:
                c.op("pool", lambda e: e.memset(Ps[0:1, :], 0.0), writes=[K("Ps")])
                c.dma("sp", Ps[1:128, :], p0[0:127, 0:2048], reads=["p0"], writes=[K("Ps")])
            else:
                c.dma("sp", Ps[:], p0[r0 - 1:r0 + 127, 0:2048], reads=["p0"], writes=[K("Ps")])
            self.tt("dve", Ps[:], Ps[:], P[:], ALU.subtract, [K("Ps"), K("Pmm")], [K("Ps")])
            srcs = [0, 1, 2, 3, 3, 3]
            for i in range(6):
                eng = "dve" if i % 2 == 0 else "pool"
                sc = srcs[i] * 512
                self.tt(eng, X6[:, i, :], Ps[:, sc:sc + 512], MU(i), ALU.mult, [K("Ps"), "rkv"], [K("X6_%d" % i)])
                self.tt(eng, X6[:, i, :], X6[:, i, :], P[:, sc:sc + 512], ALU.add, [K("X6_%d" % i), K("Pmm")], [K("X6_%d" % i)])
            if self.cut == 1:
                return
            r, k, v = X6[:, 0, :], X6[:, 1, :], X6[:, 2, :]
            for i in range(3):
                self.transpose_in(xT, X6[:, 3 + i, :], 4, K("X6_%d" % (3 + i)), K("xT3"), ch0=4 * i)
            ps, pk = self.nps()
            for kc in range(4):
                self.mm(ps[0:64, 0:128], w1[:, kc, :], xT[:, kc, :], kc == 0, kc == 3, ["w1", K("xT3")], pk)
            for kc in range(4):
                self.mm(ps[0:64, 128:256], a1[:, kc, :], xT[:, 4 + kc, :], kc == 0, kc == 3, ["a1", K("xT3")], pk)
            for kc in range(4):
                self.mm(ps[:, 256:384], g1[:, kc, :], xT[:, 8 + kc, :], kc == 0, kc == 3, ["g1", K("xT3")], pk)
            self.act(hT[0:64, 0:128], ps[0:64, 0:128], AF.Tanh, [pk], [K("hT")])
            self.act(hT[0:64, 128:256], ps[0:64, 128:256], AF.Identity, [pk], [K("hT")])
            self.act(hT[:, 256:384], ps[:, 256:384], AF.Sigmoid, [pk], [K("hT")])
            ps, pk = self.nps()
            self.mm(ps[:, :], hT[0:64, 0:128], w2[:, :], True, True, [K("hT"), "w2"], pk)
            self.tt("dve", sw[:], ps[:, :], W0, ALU.add, [pk, "rkv"], [K("sw")])
            self.act(sw[:], sw[:], AF.Sigmoid, [K("sw")], [K("sw")])
            ps, pk = self.nps()
            self.mm(ps[:, :], hT[0:64, 128:256], a2[:, :], True, True, [K("hT"), "a2"], pk)
            self.tt("dve", a[:], ps[:, :], A0, ALU.add, [pk, "rkv"], [K("a")])
            self.act(a[:], a[:], AF.Sigmoid, [K("a")], [K("a")])
            ps, pk = self.nps()
            self.mm(ps[:, :], hT[:, 256:384], g2[:, :], True, True, [K("hT"), "g2"], pk)
            self.cp("act", g[:], ps[:, :], [pk], [K("g")])
            if self.cut == 2:
                return
            self.tt("pool", kk[:], k, KK_, ALU.mult, [K("X6_1"), "rkv"], [K("kk")])
            self.act(tmp[:], kk[:], AF.Square, [K("kk")], [K("tmp")])
            self.red("dve", ss[:], tmp[:].rearrange("p (h n) -> p h n", h=8), ALU.add, [K("tmp")], [K("ss")])
            self.rsqrt(ss[:], ss[:], 1e-24, [K("ss")], [K("ss")])
            kk3 = kk[:].rearrange("p (h n) -> p h n", h=8)
            self.tt("dve", kk3, kk3, ss[:].unsqueeze(2).to_broadcast([128, 8, 64]), ALU.mult, [K("kk"), K("ss")], [K("kk")])
            self.stt("pool", tmp[:], a[:], -1.0, KA_, ALU.add, ALU.mult, [K("a"), "rkv", K("tmp")], [K("tmp")])
            self.stt("pool", kp[:], tmp[:], 1.0, k, ALU.add, ALU.mult, [K("tmp"), K("X6_1")], [K("kp")])
            self.tt("dve", tmp[:], r, kp[:], ALU.mult, [K("X6_0"), K("kp")], [K("tmp")])
            self.tt("dve", tmp[:], tmp[:], RKk, ALU.mult, [K("tmp"), "rkv"], [K("tmp")])
            self.red("dve", ss[:], tmp[:].rearrange("p (h n) -> p h n", h=8), ALU.add, [K("tmp")], [K("ss")])
            self.tt("dve", bon[:].rearrange("p (h n) -> p h n", h=8), v.rearrange("p (h n) -> p h n", h=8),
                    ss[:].unsqueeze(2).to_broadcast([128, 8, 64]), ALU.mult, [K("X6_2"), K("ss")], [K("bon")])
            self.cp("act", Vt[:], v, [K("X6_2")], [K("Vt")])
            if self.cut == 3:
                return
            self.cp("act", swh[:, 0, :], sw[:], [K("sw")], [K("swh")])
            self.tt("pool", tmp[:], sw[:], swh[:, 0, :], ALU.subtract, [K("sw"), K("swh"), K("tmp")], [K("tmp")])
            self.cp("act", swh[:, 1, :], tmp[:], [K("tmp")], [K("swh")])
            ps, pk = self.nps()
            self.mm(ps[:, :], trib[:], swh[:, 0, :], True, False, ["trib", K("swh")], pk)
            self.mm(ps[:, :], trib[:], swh[:, 1, :], False, True, ["trib", K("swh")], pk)
            self.cp("dve", cs[:], ps[:, :], [pk], [K("cs")])
            self.act(e3[:, 0, :], cs[:], AF.Exp, [K("cs")], [K("e3")], scale=-RK_C)
            self.act(e3[:, 2, :], cs[:], AF.Exp, [K("cs")], [K("e3")], scale=RK_C)
            self.tt("dve", cs[:], cs[:], sw[:], ALU.subtract, [K("cs"), K("sw")], [K("cs")])
            self.act(e3[:, 1, :], cs[:], AF.Exp, [K("cs")], [K("e3")], scale=-RK_C)
            self.tt("dve", T4[:, 0, :], kk[:], e3[:, 1, :], ALU.mult, [K("kk"), K("e3")], [K("T4")])
            self.tt("pool", T4[:, 1, :], r, e3[:, 0, :], ALU.mult, [K("X6_0"), K("e3")], [K("T4")])
            self.tt("dve", tmp[:], kk[:], a[:], ALU.mult, [K("kk"), K("a"), K("tmp")], [K("tmp")])
            self.tt("dve", T4[:, 2, :], tmp[:], e3[:, 2, :], ALU.mult, [K("tmp"), K("e3")], [K("T4")])
            self.tt("pool", T4[:, 3, :], kp[:], e3[:, 2, :], ALU.mult, [K("kp"), K("e3")], [K("T4")])
            self.cp("act", Bt[:], T4[:, 2, :], [K("T4")], [K("Bt")])
            self.cp("act", Kt[:], T4[:, 3, :], [K("T4")], [K("Kt")])
            if self.cut == 4:
                d4 = self.dscr("dbgT4", [128, 2048])
                d3 = self.dscr("dbge3", [128, 1536])
                dsw = self.dscr("dbgsw", [128, 512])
                c.dma("sp", d4[:, :], T4[:].rearrange("p i n -> p (i n)"), reads=[K("T4")], writes=["dbgT4"])
                c.dma("sp", d3[:, :], e3[:].rearrange("p i n -> p (i n)"), reads=[K("e3")], writes=["dbge3"])
                c.dma("sp", dsw[:, :], sw[:], reads=[K("sw")], writes=["dbgsw"])
                return
            ART4 = ART[:].rearrange("p j (a t) -> p j a t", a=2)
            self.transpose_in(ART4[:, :, 0, :], T4[:, 0, :], 4, K("T4"), K("ART"))
            self.transpose_in(ART4[:, :, 1, :], T4[:, 1, :], 4, K("T4"), K("ART"))
            self.transpose_in(BT, T4[:, 2, :], 4, K("T4"), K("BT"))
            self.transpose_in(KT, T4[:, 3, :], 4, K("T4"), K("KT"))
            if self.cut in (45, 46):
                return
            self.transpose_in(ET, e3[:, 0, :], 4, K("e3"), K("ET"))
            if self.cut == 5:
                return
            for h in range(8):
                j, po = h // 2, (h % 2) * 64
                ps, pk = self.nps()
                self.mm(ps[:, 0:256], BT[po:po + 64, j, :], ART[po:po + 64, j, :], True, True, [K("BT"), K("ART")], pk)
                self.mm(ps[:, 256:512], KT[po:po + 64, j, :], ART[po:po + 64, j, :], True, True, [K("KT"), K("ART")], pk)
                self.tt("dve", G[:, h, :], ps[:, :], mask4[:], ALU.mult, [pk, "mask4"], [K("G%d" % h)])
            for par in range(2):
                ps, pk = self.nps()
                for hh in range(4):
                    h = 2 * hh + par
                    j, po = h // 2, par * 64
                    self.mm(ps[:, hh * 128:(hh + 1) * 128], ART[po:po + 64, j, 0:128], BT[po:po + 64, j, :], True, True,
                            [K("BT"), K("ART")], pk)
                self.tt("dve", Qm[:, par:8:2, :], ps[:, :].rearrange("p (h t) -> p h t", h=4),
                        maskL[:].unsqueeze(1).to_broadcast([128, 4, 128]), ALU.mult, [pk, "maskL"], [K("Q")])
            for h in range(8):
                self.tt("pool", Wm[:, h, :], identb[:], G[:, h, 0:128], ALU.subtract, ["identb", K("G%d" % h)], [K("W")])
            Pg = [None, None]
            for lvl in range(1, 6):
                for hq in range(2):
                    hsl = slice(hq * 4, (hq + 1) * 4)
                    psq, pkq = self.nps()
                    psp, pkp = (self.nps() if lvl < 5 else (None, None))
                    for hh in range(4):
                        h = hq * 4 + hh
                        Pc = G[:, h, 0:128] if Pg[hq] is None else Pm[:, h, :]
                        pkey = K("G%d" % h) if Pg[hq] is None else K("Pmm")
                        csl = slice(hh * 128, (hh + 1) * 128)
                        self.mm(psq[:, csl], Pc, Qm[:, h, :], True, True, [pkey, K("Q")], pkq)
                        if lvl < 5:
                            self.mm(psp[:, csl], Qm[:, h, :], Pc, True, True, [pkey, K("Q")], pkp)
                    self.cp("act", Qm[:, hsl, :], psq[:, :].rearrange("p (h t) -> p h t", h=4), [pkq], [K("Q")])
                    if lvl < 5:
                        self.cp("dve", Pm[:, hsl, :], psp[:, :].rearrange("p (h t) -> p h t", h=4), [pkp], [K("Pmm")])
                        Pg[hq] = 1
                for hq in range(2):
                    hsl = slice(hq * 4, (hq + 1) * 4)
                    psw, pkw = self.nps()
                    for hh in range(4):
                        h = hq * 4 + hh
                        self.mm(psw[:, hh * 128:(hh + 1) * 128], Qm[:, h, :], Wm[:, h, :], True, True, [K("Q"), K("W")], pkw)
                    self.tt("dve", Wm[:, hsl, :], Wm[:, hsl, :], psw[:, :].rearrange("p (h t) -> p h t", h=4), ALU.add,
                            [pkw, K("W")], [K("W")])
            if self.cut == 6:
                return
            if pend_post[0] is not None:
                pend_post[0]()
                pend_post[0] = None
            for cc in range(2):
                self.tt("pool", bdg[cc][:], bd[:].unsqueeze(1).to_broadcast([128, 4, 128]),
                        ET[:, :, cc * 64 + 63:cc * 64 + 64].to_broadcast([128, 4, 128]), ALU.mult, ["bd", K("ET")], [K("bdg%d" % cc)])
            self.cp("act", Vm[0][0:64, :], v[0:64, :], [K("X6_2")], [K("Vm0")])
            self.cp("act", Vm[1][64:128, :], v[64:128, :], [K("X6_2")], [K("Vm1")])
            for cc in range(2):
                q0 = cc * 64
                rs = slice(q0, q0 + 64)
                Ubc, Vtc = Ub[cc], Vm[cc]
                ku, kv = K("Ub%d_" % cc), K("Vm%d" % cc)
                psX, pkX = self.nps()
                for j in range(4):
                    self.mm(psX[:, j * 128:(j + 1) * 128], ART[:, j, 0:128], Hb[:, j, :], True, False, [K("ART"), "Hb"], pkX)
                    for h in (2 * j, 2 * j + 1):
                        hs = slice(h * 64, h * 64 + 64)
                        self.mm(psX[:, hs], G[:, h, 256:384], Vt[:, hs], False, h == 2 * j + 1, [K("G%d" % h), K("Vt")], pkX)
                self.ts("dve", Xs[rs, :], psX[rs, :], -1.0, ALU.mult, [pkX], [K("Xs")])
                psU, pkU = self.nps()
                for h in range(8):
                    hs = slice(h * 64, h * 64 + 64)
                    self.mm(psU[:, hs], Wm[:, h, :], Xs[:, hs], True, True, [K("W"), K("Xs")], pkU)
                self.cp("act", Ubc[rs, :], psU[rs, :], [pkU], [ku])
                psO, pkO = self.nps()
                for j in range(4):
                    self.mm(psO[:, j * 128:(j + 1) * 128], ART[:, j, 128:256], Hb[:, j, :], True, False, [K("ART"), "Hb"], pkO)
                    for h in (2 * j, 2 * j + 1):
                        hs = slice(h * 64, h * 64 + 64)
                        self.mm(psO[:, hs], G[:, h, 128:256], Ubc[:, hs], False, False, [K("G%d" % h), ku], pkO)
                        self.mm(psO[:, hs], G[:, h, 384:512], Vt[:, hs], False, h == 2 * j + 1, [K("G%d" % h), K("Vt")], pkO)
                self.cp("act", Os[rs, :], psO[rs, :], [pkO], [K("Os")])
                psH, pkH = self.nps()
                for j in range(4):
                    js = slice(j * 128, (j + 1) * 128)
                    self.mm(psH[:, js], Bt[:, js], Ubc[:, js], True, False, [K("Bt"), ku], pkH)
                    self.mm(psH[:, js], Kt[:, js], Vtc[:, js], False, True, [K("Kt"), kv], pkH)
                H2 = H[:].rearrange("p j v -> p (j v)")
                self.tt("dve", H2, H2, psH[:, :], ALU.add, [pkH, "H"], ["H"])
                self.tt("dve", H[:], H[:], bdg[cc][:], ALU.mult, ["H", K("bdg%d" % cc)], ["H"])
                self.cp("act", Hb[:].rearrange("p j v -> p (j v)"), H2, ["H"], ["Hb"])
            def post(b=b, r0=r0, Os=Os, tmp2=tmp2, sq=sq, bon=bon, g=g, oo=oo):
                K = lambda n: n if n.rstrip("0123456789_") in SINGLE else "%s%d" % (n, b)
                O3 = Os[:].rearrange("p (h n) -> p h n", h=8)
                self.red("dve", sq[:], O3, ALU.add, [K("Os")], [K("sq")])
                self.ts("dve", sq[:], sq[:], 1.0 / 64, ALU.mult, [K("sq")], [K("sq")])
                self.tt("dve", O3, O3, sq[:].unsqueeze(2).to_broadcast([128, 8, 64]), ALU.subtract, [K("Os"), K("sq")], [K("Os")])
                self.act(tmp2[:], Os[:], AF.Square, [K("Os")], [K("tq")])
                self.red("dve", sq[:], tmp2[:].rearrange("p (h n) -> p h n", h=8), ALU.add, [K("tq")], [K("sq")])
                self.rsqrt(sq[:], sq[:], 64e-5, [K("sq")], [K("sq")], scale=1.0 / 64)
                self.tt("dve", O3, O3, sq[:].unsqueeze(2).to_broadcast([128, 8, 64]), ALU.mult, [K("Os"), K("sq")], [K("Os")])
                self.tt("pool", Os[:], Os[:], LNG, ALU.mult, [K("Os"), "rkv"], [K("Os")])
                self.tt("pool", Os[:], Os[:], LNB, ALU.add, [K("Os"), "rkv"], [K("Os")])
                self.tt("dve", Os[:], Os[:], bon[:], ALU.add, [K("Os"), K("bon")], [K("Os")])
                self.tt("dve", oo[:], Os[:], g[:], ALU.mult, [K("Os"), K("g")], [K("oo")])
                c.dma("sp", oab[r0:r0 + 128, 0:512], oo[:], reads=[K("oo")], writes=["oab"])
            pend_post[0] = post
        pend_post[0]()
        c.barrier()


B.stage_rwkv = stage_rwkv


def stage_nsa(self, p0, oab, W):
    c = self.c
    S, NT = self.S, self.NT
    n_cmp = (S - 32) // 16 + 1
    NCT = (n_cmp + 127) // 128
    NCP = NCT * 128
    NQ = S // 512
    QC, KC0, GC0 = 2048, 2560, 3328
    with ExitStack() as st:
        sb = lambda n, shp, dt=F32: self.sb(st, n, shp, dt)
        identb = sb("identb", [128, 128], BF16)
        c.dma("pool", identb[:], W["c_ident"][:, :], writes=["identb"])
        KcT = sb("KcT", [128, NCP], BF16)
        Vca = sb("Vca", [128, NCT, 2, 193], BF16)
        c.op("pool", lambda e: e.memset(KcT[:], 0.0), writes=["KcT"])
        c.op("pool", lambda e: e.memset(Vca[:], 0.0), writes=["Vca"])
        with ExitStack() as st2:
            sb2 = lambda n, shp, dt=F32: self.sb(st2, n, shp, dt)
            kvT = sb2("kvT", [128, 2, S], BF16)
            W1p = sb2("W1p", [128, 2, 2, 32, 128], BF16)
            c.op("pool", lambda e: e.memset(W1p[:].rearrange("p a g l h -> p (a g l h)"), 0.0), writes=["W1p"])
            for kv in range(2):
                for g in range(2):
                    c.dma("pool", W1p[g * 64:(g + 1) * 64, kv, g, :, :],
                          W["ns_c_w1"][kv].rearrange("(l d) h -> d l h", d=64), writes=["W1p"])
            w2k = sb2("w2k", [128, 2, 128], BF16)
            c.op("pool", lambda e: e.memset(w2k[:].rearrange("p g n -> p (g n)"), 0.0), writes=["w2k"])
            for g in range(2):
                c.dma("pool", w2k[:, g, g * 64:(g + 1) * 64], W["ns_c_w2"][0], writes=["w2k"])
            w2v = sb2("w2v", [128, 64], BF16)
            c.dma("pool", w2v[:], W["ns_c_w2"][1], writes=["w2v"])
            peT = sb2("peT", [128, 2, 32], BF16)
            c.dma("pool", peT[:].rearrange("p a l -> p (a l)"), W["c_peT"][:, :], writes=["peT"])
            ropeA = sb2("ropeA", [128, 16])
            pa = [sb2("pa", [128, 256]) for _ in range(2)]
            pr = [sb2("pra", [128, 256]) for _ in range(2)]
            tA = [sb2("tA", [128, 2, 8]) for _ in range(2)]
            for t in range(NT):
                b = t % 2
                r0 = t * 128
                c.dma("sp", pa[b][:], p0[r0:r0 + 128, KC0:KC0 + 256], reads=["p0"], writes=["pa%d" % b])
                c.dma("sp", ropeA[:], W["c_rope"][r0:r0 + 128, :], writes=["ropeA"])
                self.cp("pool", pr[b][:], pa[b][:], ["pa%d" % b], ["pra%d" % b])
                self._rope(pa[b][:, 0:128], pr[b][:, 0:128], 2, ropeA, tA[b], "pa%d" % b, "pra%d" % b, "ropeA", "tA%d" % b)
                self.transpose_in(kvT[:, :, r0:r0 + 128], pr[b], 2, "pra%d" % b, "kvT")
            hT = sb2("hTc", [128, NCP], BF16)
            c.op("pool", lambda e: e.memset(hT[:], 0.0), writes=["hTc"])
            bia = sb2("bia", [128, 1])
            xh = sb2("xh", [128, 512])
            x2 = sb2("x2", [128, 512])
            for kv in range(2):
                for g in range(2):
                    ps, pk = self.nps()
                    for l in range(32):
                        self.mm(ps[:, 0:1], W1p[:, kv, g, l, :], peT[:, kv, l:l + 1], l == 0, l == 31, ["W1p", "peT"], pk)
                    self.cp("dve", bia[:], ps[:, 0:1], [pk], ["bia"])
                    ps, pk = self.nps()
                    for l in range(32):
                        self.mm(ps[:, 0:n_cmp], W1p[:, kv, g, l, :], kvT[:, kv, l:l + 16 * (n_cmp - 1) + 1:16], l == 0, l == 31,
                                ["W1p", "kvT"], pk)
                    X = xh[:, 0:n_cmp]
                    Y = x2[:, 0:n_cmp]
                    self.act(X, ps[:, 0:n_cmp], AF.Identity, [pk, "bia"], ["xh"], bias=bia[:, 0:1])
                    self.tt("dve", Y, X, X, ALU.mult, ["xh"], ["x2"])
                    self.ts("dve", Y, Y, 0.044715, ALU.mult, ["x2"], ["x2"], s2=1.0, op1=ALU.add)
                    self.tt("dve", Y, Y, X, ALU.mult, ["x2", "xh"], ["x2"])
                    self.act(Y, Y, AF.Tanh, ["x2"], ["x2"], scale=0.7978845608)
                    self.stt("dve", hT[:, 0:n_cmp], Y, 1.0, X, ALU.add, ALU.mult, ["x2", "xh"], ["hTc"])
                    if kv == 0:
                        ps, pk = self.nps()
                        self.mm(ps[:, 0:n_cmp], w2k[:, g, :], hT[:, 0:n_cmp], True, True, ["w2k", "hTc"], pk)
                        if g == 0:
                            self.act(KcT[:, 0:n_cmp], ps[:, 0:n_cmp], AF.Identity, [pk], ["KcT"], scale=0.5)
                        else:
                            self.stt("dve", KcT[:, 0:n_cmp], ps[:, 0:n_cmp], 0.5, KcT[:, 0:n_cmp], ALU.mult, ALU.add, [pk, "KcT"], ["KcT"])
                    else:
                        for i in range(NCT):
                            ps, pk = self.nps()
                            self.mm(ps[:, 0:64], hT[:, i * 128:(i + 1) * 128], w2v[:], True, True, ["w2v", "hTc"], pk)
                            self.act(Vca[:, i, g, 0:64], ps[:, 0:64], AF.Identity, [pk], ["Vca"], scale=0.5)
            for i in range(NCT):
                for g in range(2):
                    c.dma("pool", Vca[:, i, g, 65:193], W["c_ov"][i * 128:(i + 1) * 128, :], writes=["Vca"])
                    c.dma("pool", Vca[:, i, g, 64:65], W["c_ones"][:, 0:1], writes=["Vca"])
            c.barrier()
        QT = sb("QT", [128, 4, S], BF16)
        KT2 = sb("KT2", [128, 2, S], BF16)
        Va = sb("Va", [128, NT, 2, 2, 65], BF16)
        Eo = sb("Eo", [128, S], BF16)
        c.dma("pool", Eo[:], W["c_E"][:, :], writes=["Eo"])
        caus = sb("caus", [128, 4, 512], BF16)
        winb = sb("winb", [128, 8, 512], BF16)
        cmpb = sb("cmpb", [128, 5, 512], BF16)
        c.dma("pool", caus[:].rearrange("p a q -> p (a q)"), W["c_caus"][:, :], writes=["caus"])
        c.dma("pool", winb[:].rearrange("p a q -> p (a q)"), W["c_win"][:, :], writes=["winb"])
        c.dma("pool", cmpb[:].rearrange("p a q -> p (a q)"), W["c_cmpb"][:, :], writes=["cmpb"])
        c.op("pool", lambda e: e.memset(Va[:].rearrange("p t a g d -> p (t a g d)"), 1.0), writes=["Va"])
        with ExitStack() as st2:
            sb2 = lambda n, shp, dt=F32: self.sb(st2, n, shp, dt)
            ropeB = sb2("ropeB", [128, 16])
            pn = [sb2("pn", [128, 1280]) for _ in range(2)]
            qp = [sb2("qp", [128, 512]) for _ in range(2)]
            kp = [sb2("kpn", [128, 256]) for _ in range(2)]
            tB = [sb2("tB", [128, 8, 8]) for _ in range(2)]
            for t in range(NT):
                b = t % 2
                r0 = t * 128
                kn, kq, kk_ = "pn%d" % b, "qp%d" % b, "kpn%d" % b
                c.dma("sp", pn[b][:], p0[r0:r0 + 128, QC:QC + 1280], reads=["p0"], writes=[kn])
                c.dma("sp", ropeB[:], W["c_rope"][r0:r0 + 128, :], writes=["ropeB"])
                qsrc = pn[b][:, 0:512].rearrange("p (g j d) -> p g j d", g=2, j=4)
                qdst = qp[b][:].rearrange("p (j g d) -> p g j d", g=2, j=4)
                self.cp("pool", qdst, qsrc, [kn], [kq])
                self._rope(qsrc, qdst, 8, ropeB, tB[b], kn, kq, "ropeB", "tB%d" % b, four=True)
                for a in range(2):
                    o = 512 + 256 * (a + 1)
                    self.cp("pool", kp[b][:, a * 128:(a + 1) * 128], pn[b][:, o:o + 128], [kn], [kk_])
                    self._rope(pn[b][:, o:o + 128], kp[b][:, a * 128:(a + 1) * 128], 2, ropeB, tB[b], kn, kk_, "ropeB", "tB%d" % b)
                    self.cp("act", Va[:, t, a, :, 0:64], pn[b][:, o + 128:o + 256].rearrange("p (g d) -> p g d", g=2), [kn], ["Va"])
                self.transpose_in(QT[:, :, r0:r0 + 128], qp[b], 4, kq, "QT")
                self.transpose_in(KT2[:, :, r0:r0 + 128], kp[b], 2, kk_, "KT2")
            c.barrier()
        Qh = [[sb("Qh", [128, 512], BF16) for _ in range(2)] for _ in range(2)]
        for g in range(2):
            for k_ in range(2):
                c.op("pool", lambda e: e.memset(Qh[g][k_][:], 0.0), writes=["Qh%d_%d" % (g, k_)])
        qh_i = [0]
        qcur = [None, None]
        PT = [sb("PT", [128, 512], BF16) for _ in range(3)]
        MbT = [sb("MbT", [128, 512], BF16) for _ in range(2)]
        acc = [sb("acc", [128, 512]) for _ in range(4)]
        imp = [[sb("imp", [128, 128]) for _ in range(4)] for _ in range(2)]
        sig = [sb("sig", [128, 24]) for _ in range(4)]
        selF = [sb("selF", [128, 128]) for _ in range(4)]
        rz4 = [sb("rz", [128, 2]) for _ in range(4)]
        ot4 = [sb("oto", [128, 193]) for _ in range(4)]
        m8 = sb("m8", [128, 16])
        pri = sb("pri", [128, 128])
        pri2 = sb("pri2", [128, 128])
        mb = sb("mb", [128, 128])
        SPS = [(self.ps[i], "ps%d" % i) for i in range(4)]
        APS = [(self.ps[4 + i], "ps%d" % (4 + i)) for i in range(4)]
        sps_i = [0]
        pt_i = [0]

        pending = []

        def flush_pv():
            while pending:
                P, pkey, vaug, nv, subs = pending.pop(0)
                for (sub, first, last) in subs:
                    aps, apk = APS[sub]
                    self.mm(aps[:, 0:nv], P[:, sub * 128:(sub + 1) * 128], vaug, first, last, [pkey, "Va", "Vca"], apk)

        def unit(h, Q, kT, kcols, bias_terms, vaug, nv, subs, started):
            g = h // 4
            ps, pk = SPS[sps_i[0] % 4]
            sps_i[0] += 1
            nb = len(bias_terms)
            self.mm(ps[:, :], kT, qcur[0][:], True, nb == 0, ["KcT", "KT2", qcur[1]], pk)
            for bi, (lt, rt, rk) in enumerate(bias_terms):
                self.mm(ps[:, :], lt, rt, False, bi == nb - 1, rk, pk)
            P = PT[pt_i[0] % 3]
            pkey = "PT%d" % (pt_i[0] % 3)
            pt_i[0] += 1
            self.act(P[:], ps[:, :], AF.Exp, [pk], [pkey], scale=0.125)
            flush_pv()
            pending.append((P, pkey, vaug, nv, subs))

        def finish_branch(h, br, nv, g=None, hh=None):
            hs = slice(h * 64, (h + 1) * 64)
            for sub in range(4):
                aps, apk = APS[sub]
                self.cp("dve", ot4[sub][:, 0:nv], aps[:, 0:nv], [apk], ["oto%d" % sub])
            for sub in range(4):
                ot = ot4[sub]
                ko, kr = "oto%d" % sub, "rz%d" % sub
                rz = rz4[sub]
                self.ts("dve", rz[:, 0:1], ot[:, 64:65], 1e-30, ALU.max, [ko], [kr])
                c.op("dve", lambda e: e.reciprocal(out=rz[:, 0:1], in_=rz[:, 0:1]), reads=[kr], writes=[kr])
                self.tt("dve", rz[:, 1:2], rz[:, 0:1], sig[sub][:, br * 8 + h:br * 8 + h + 1], ALU.mult, [kr, "sig%d" % sub], [kr])
                if br == 0:
                    self.ts("dve", acc[sub][:, hs], ot[:, 0:64], rz[:, 1:2], ALU.mult, [ko, kr], ["acc%d" % sub])
                    if hh == 0:
                        self.ts("dve", imp[g][sub][:], ot[:, 65:193], rz[:, 0:1], ALU.mult, [ko, kr], ["imp%d%d" % (g, sub)])
                    else:
                        self.stt("dve", imp[g][sub][:], ot[:, 65:193], rz[:, 0:1], imp[g][sub][:], ALU.mult, ALU.add,
                                 [ko, kr, "imp%d%d" % (g, sub)], ["imp%d%d" % (g, sub)])
                else:
                    self.stt("dve", acc[sub][:, hs], ot[:, 0:64], rz[:, 1:2], acc[sub][:, hs], ALU.mult, ALU.add,
                             [ko, kr, "acc%d" % sub], ["acc%d" % sub])

        for Q in range(NQ):
            q0 = Q * 512
            for sub in range(4):
                r0 = q0 + sub * 128
                c.dma("sp", sig[sub][:], p0[r0:r0 + 128, GC0:GC0 + 24], reads=["p0"], writes=["sig%d" % sub])
                self.act(sig[sub][:], sig[sub][:], AF.Sigmoid, ["sig%d" % sub], ["sig%d" % sub])
                c.dma("sp", selF[sub][:], W["c_selF"][r0:r0 + 128, :], writes=["selF%d" % sub])
            for g in range(2):
                for hh in range(4):
                    h = g * 4 + hh
                    k_ = qh_i[0] % 2
                    qh_i[0] += 1
                    qcur[0], qcur[1] = Qh[g][k_], "Qh%d_%d" % (g, k_)
                    self.cp("pool", Qh[g][k_][g * 64:(g + 1) * 64, :], QT[g * 64:(g + 1) * 64, hh, q0:q0 + 512], ["QT"], [qcur[1]])
                    started = [False] * 4
                    for i in range(NCT):
                        dj = Q - 4 * i
                        if dj < 0:
                            continue
                        bt = [] if dj > 4 else [(identb[:], cmpb[:, dj, :], ["identb", "cmpb"])]
                        imax = min(NCT - 1, Q // 4)
                        unit(h, Q, KcT[:, i * 128:(i + 1) * 128], None, bt, Vca[:, i, g, :], 193,
                             [(s_, i == 0, i == imax) for s_ in range(4)], started)
                    flush_pv()
                    finish_branch(h, 0, 193, g, hh)
                psM, pkM = SPS[sps_i[0] % 4]
                sps_i[0] += 1
                for sub in range(4):
                    self.tt("dve", pri[:], imp[g][sub][:], selF[sub][:], ALU.add, ["imp%d%d" % (g, sub), "selF%d" % sub], ["pri"])
                    c.op("dve", lambda e: e.max(out=m8[:, 0:8], in_=pri[:]), reads=["pri"], writes=["m8"])
                    c.op("dve", lambda e: e.match_replace(out=pri2[:], in_to_replace=m8[:, 0:8], in_values=pri[:], imm_value=-1e9),
                         reads=["pri", "m8"], writes=["pri2"])
                    c.op("dve", lambda e: e.max(out=m8[:, 8:16], in_=pri2[:]), reads=["pri2"], writes=["m8"])
                    self.ts("dve", mb[:], pri[:], m8[:, 15:16], ALU.is_ge, ["pri", "m8"], ["mb"])
                    self.ts("dve", mb[:], mb[:], -1.0, ALU.add, ["mb"], ["mb"], s2=-NEG, op1=ALU.mult)
                    self.tr(psM[:, sub * 128:(sub + 1) * 128], mb[:], self.ident[:], ["mb", "ident"], pkM)
                self.cp("act", MbT[g][:], psM[:, :], [pkM], ["MbT%d" % g])
                for hh in range(4):
                    h = g * 4 + hh
                    k_ = qh_i[0] % 2
                    qh_i[0] += 1
                    qcur[0], qcur[1] = Qh[g][k_], "Qh%d_%d" % (g, k_)
                    self.cp("pool", Qh[g][k_][g * 64:(g + 1) * 64, :], QT[g * 64:(g + 1) * 64, hh, q0:q0 + 512], ["QT"], [qcur[1]])
                    started = [False] * 4
                    for kt in range(0, 4 * Q + 4):
                        d = kt - 4 * Q
                        bt = [(Eo[:, kt * 128:(kt + 1) * 128], MbT[g][:], ["Eo", "MbT%d" % g])]
                        if d >= 0:
                            bt.append((identb[:], caus[:, d, :], ["identb", "caus"]))
                        subs = [(s_, kt == 0, kt == 4 * Q + s_) for s_ in range(4) if s_ >= d]
                        unit(h, Q, KT2[:, 0, kt * 128:(kt + 1) * 128], None, bt, Va[:, kt, 0, g, :], 65, subs, started)
                    flush_pv()
                    finish_branch(h, 1, 65)
                    started = [False] * 4
                    for kt in range(max(0, 4 * Q - 4), 4 * Q + 4):
                        d = kt - 4 * Q
                        bt = [(identb[:], winb[:, d + 4, :], ["identb", "winb"])]
                        subs = [(s_, kt == max(0, 4 * Q + s_ - 4), kt == 4 * Q + s_) for s_ in range(4) if s_ - 4 <= d <= s_]
                        unit(h, Q, KT2[:, 1, kt * 128:(kt + 1) * 128], None, bt, Va[:, kt, 1, g, :], 65, subs, started)
                    flush_pv()
                    finish_branch(h, 2, 65)
            for sub in range(4):
                r0 = q0 + sub * 128
                c.dma("sp", oab[r0:r0 + 128, 512:1024], acc[sub][:], reads=["acc%d" % sub], writes=["oab"])
        c.barrier()


def _rope(self, src, dst, nh, rope, tmp, ksrc, kdst, krope, ktmp, four=False):
    if four:
        s4, d4 = src, dst
        x1, x2 = s4[:, :, :, 0:8], s4[:, :, :, 8:16]
        o1, o2 = d4[:, :, :, 0:8], d4[:, :, :, 8:16]
        cos = rope[:, 0:8].unsqueeze(1).unsqueeze(1).to_broadcast([128, 2, 4, 8])
        sin = rope[:, 8:16].unsqueeze(1).unsqueeze(1).to_broadcast([128, 2, 4, 8])
        tm = tmp[:].rearrange("p (g j) d -> p g j d", g=2)
    else:
        s3 = src.rearrange("p (h d) -> p h d", h=nh)
        d3 = dst.rearrange("p (h d) -> p h d", h=nh)
        x1, x2 = s3[:, :, 0:8], s3[:, :, 8:16]
        o1, o2 = d3[:, :, 0:8], d3[:, :, 8:16]
        cos = rope[:, 0:8].unsqueeze(1).to_broadcast([128, nh, 8])
        sin = rope[:, 8:16].unsqueeze(1).to_broadcast([128, nh, 8])
        tm = tmp[:, 0:nh, :]
    self.tt("dve", o1, x1, cos, ALU.mult, [ksrc, krope], [kdst])
    self.tt("dve", tm, x2, sin, ALU.mult, [ksrc, krope], [ktmp])
    self.tt("dve", o1, o1, tm, ALU.subtract, [kdst, ktmp], [kdst])
    self.tt("dve", o2, x2, cos, ALU.mult, [ksrc, krope], [kdst])
    self.tt("dve", tm, x1, sin, ALU.mult, [ksrc, krope], [ktmp])
    self.tt("dve", o2, o2, tm, ALU.add, [kdst, ktmp], [kdst])


B.stage_nsa = stage_nsa
B._rope = _rope


def ln_tile(self, z, zkey, lnp, out, okey, tl):
    s1, zc, sq = tl
    stats, mv, rstd, nb = s1[:, 0:12], s1[:, 12:14], s1[:, 14:15], s1[:, 15:16]
    for i in range(2):
        self.c.op("dve", lambda e: e.bn_stats(out=s1[:, i * 6:(i + 1) * 6], in_=z[:, i * 512:(i + 1) * 512]),
                  reads=[zkey], writes=["ln_st"])
    self.c.op("dve", lambda e: e.bn_aggr(out=mv, in_=stats), reads=["ln_st"], writes=["ln_mv"])
    self.rsqrt(rstd, mv[:, 1:2], LN_EPS, ["ln_mv"], ["ln_rs"])
    self.stt("dve", nb, mv[:, 0:1], -1.0, rstd, ALU.mult, ALU.mult, ["ln_mv", "ln_rs"], ["ln_nb"])
    self.act(zc[:], z, AF.Identity, [zkey, "ln_rs", "ln_nb"], ["ln_zc"], bias=nb, scale=rstd)
    self.tt("dve", zc[:], zc[:], lnp[:, 0:D], ALU.mult, ["ln_zc", "lnp"], ["ln_zc"])
    self.tt("dve", out, zc[:], lnp[:, D:2 * D], ALU.add, ["ln_zc", "lnp"], [okey])


def stage_mix(self, src, K, w_ap, resid, lnp_ap, dst):
    c = self.c
    with ExitStack() as st:
        sb = lambda n, shp, dt=F32: self.sb(st, n, shp, dt)
        wb = sb("wmix", [128, K // 128, D], BF16)
        self.load_w_fast(st, wb, w_ap, K, D, "wmix")
        lnp = sb("lnp", [128, 2 * D])
        c.dma("sp", lnp[:], lnp_ap[:, :], writes=["lnp"])
        tl = (sb("ln_s", [128, 16]), sb("ln_zc", [128, D]), None)
        xin = [sb("min", [128, K]) for _ in range(2)]
        xT = [sb("mxT", [128, K // 128, 128], BF16) for _ in range(2)]
        rs = [sb("mrs", [128, D]) for _ in range(2)]
        z = [sb("mz", [128, D]) for _ in range(2)]
        o = [sb("mo", [128, D]) for _ in range(2)]
        def epilogue(pend):
            b, r0, banks = pend
            for half, (ps, pk) in enumerate(banks):
                self.stt("dve", z[b][:, half * 512:(half + 1) * 512], rs[b][:, half * 512:(half + 1) * 512], ALPHA, ps[:, :],
                         ALU.mult, ALU.add, [pk, "mrs%d" % b], ["mz%d" % b])
            self.ln_tile(z[b][:], "mz%d" % b, lnp, o[b][:], "mo%d" % b, tl)
            c.dma("sp", dst[r0:r0 + 128, :], o[b][:], reads=["mo%d" % b], writes=[dst.tensor.name])

        pend = None
        for t in range(self.NT):
            b = t % 2
            r0 = t * 128
            c.dma("sp", xin[b][:], src[r0:r0 + 128, :], reads=[src.tensor.name], writes=["min%d" % b])
            c.dma("sp", rs[b][:], resid[r0:r0 + 128, :], reads=[resid.tensor.name], writes=["mrs%d" % b])
            self.transpose_in(xT[b], xin[b], K // 128, "min%d" % b, "mxT%d" % b)
            banks = []
            for half in range(2):
                ps, pk = self.nps()
                for kc in range(K // 128):
                    self.mm(ps[:, :], xT[b][:, kc, :], wb[:, kc, half * 512:(half + 1) * 512], kc == 0, kc == K // 128 - 1,
                            ["mxT%d" % b, "wmix"], pk)
                banks.append((ps, pk))
            if pend is not None:
                epilogue(pend)
            pend = (b, r0, banks)
        epilogue(pend)
        c.barrier()


def moe_cap(S):
    m = (S // 8) * 3 // 2
    return ((m + 511) // 512) * 512


def stage_moe(self, xin, W, layer, lnp_ap, dst, toklist, ybuf):
    c = self.c
    S, NT = self.S, self.NT
    CAP = moe_cap(S)
    NG = CAP // 512
    with ExitStack() as st:
        sb = lambda n, shp, dt=F32: self.sb(st, n, shp, dt)
        slotAB = sb("slotAB", [128, NT, 2], I32)
        wAB = sb("wAB", [128, NT, 2])
        tokid = sb("tokid", [128, NT, 16], I32)
        c.dma("sp", tokid[:].rearrange("p t r -> p (t r)"), W["c_tokid"][:, :], writes=["tokid"])
        c.dma("sp", toklist[:, :], W["c_tokinit"][:, :], reads=["toklist"], writes=["toklist"])
        with ExitStack() as st2:
            sb2 = lambda n, shp, dt=F32: self.sb(st2, n, shp, dt)
            rw = sb2("rw", [128, 8, 16])
            c.dma("sp", rw[:], W["router_w"].rearrange("(c p) e -> p c e", p=128), writes=["rw"])
            rb = sb2("rb", [128, 128])
            c.dma("sp", rb[:], W["c_rb"][:, :], writes=["rb"])
            ebase = sb2("ebase", [128, 128])
            c.dma("sp", ebase[:], W["c_ebase"][:, :], writes=["ebase"])
            SU = sb2("SU", [128, 128], BF16)
            ONES = sb2("ONESm", [128, 128], BF16)
            c.dma("pool", SU[:], W["c_su"][:, :], writes=["SU"])
            c.op("pool", lambda e: e.memset(ONES[:], 1.0), writes=["ONESm"])
            offs = sb2("offs", [128, 16])
            c.op("pool", lambda e: e.memset(offs[:], 0.0), writes=["offs"])
            TB = min(8, NT)
            TE = TB * 16
            xt = [sb2("rxt", [128, D]) for _ in range(2)]
            xT = [sb2("rxT", [128, 8, 128]) for _ in range(2)]
            aff = sb2("aff", [128, TE])
            s = sb2("s", [128, TE])
            s2 = sb2("s2", [128, TE])
            eq = sb2("eq", [128, TE])
            m1 = sb2("m1", [128, TB * 4])
            m2 = sb2("m2", [128, TB * 4])
            gs = sb2("gs", [128, TB * 4])
            gm = sb2("gm", [128, 2, TB])
            sel = sb2("sel", [128, TE])
            selb = sb2("selb", [128, TE], BF16)
            gate = sb2("gate", [128, TE])
            val = sb2("val", [128, TE])
            offT = sb2("offT", [128, TE])
            sl = sb2("sl", [128, 2, TB])
            g4 = lambda ap: ap.rearrange("p (a e) -> p a e", e=4)
            t16 = lambda ap: ap.rearrange("p (t e) -> p t e", e=16)
            bc4 = lambda ap: ap.unsqueeze(2).to_broadcast([128, TB * 4, 4])
            bc16 = lambda ap: ap.unsqueeze(2).to_broadcast([128, TB, 16])
            n = 0
            for tb in range(NT // TB):
                for i in range(TB):
                    t = tb * TB + i
                    b = n % 2
                    n += 1
                    r0 = t * 128
                    c.dma("sp", xt[b][:], xin[r0:r0 + 128, :], reads=[xin.tensor.name], writes=["rxt%d" % b])
                    self.transpose_in(xT[b], xt[b], 8, "rxt%d" % b, "rxT%d" % b)
                    psL, pkL = self.nps()
                    for kc in range(8):
                        self.mm(psL[:, 0:16], xT[b][:, kc, :], rw[:, kc, :], kc == 0, kc == 7, ["rxT%d" % b, "rw"], pkL)
                    self.act(aff[:, i * 16:(i + 1) * 16], psL[:, 0:16], AF.Sigmoid, [pkL], ["aff"])
                self.tt("dve", s[:], aff[:], rb[:, 0:TE], ALU.add, ["aff", "rb"], ["s"])
                self.red("dve", m1[:], g4(s[:]), ALU.max, ["s"], ["m1"])
                self.tt("dve", g4(eq[:]), g4(s[:]), bc4(m1[:]), ALU.is_ge, ["s", "m1"], ["eq"])
                self.stt("dve", s2[:], eq[:], -1e9, s[:], ALU.mult, ALU.add, ["eq", "s"], ["s2"])
                self.red("dve", m2[:], g4(s2[:]), ALU.max, ["s2"], ["m2"])
                self.tt("dve", gs[:], m1[:], m2[:], ALU.add, ["m1", "m2"], ["gs"])
                gs3 = gs[:].rearrange("p (t g) -> p t g", g=4)
                self.red("dve", gm[:, 0, :], gs3, ALU.max, ["gs"], ["gm"])
                self.tt("dve", gs3, gs3, gm[:, 0, :].unsqueeze(2).to_broadcast([128, TB, 4]), ALU.is_ge, ["gs", "gm"], ["gs"])
                self.tt("dve", g4(sel[:]), g4(s[:]), bc4(m2[:]), ALU.is_ge, ["s", "m2"], ["sel"])
                self.tt("dve", g4(sel[:]), g4(sel[:]), bc4(gs[:]), ALU.mult, ["sel", "gs"], ["sel"])
                self.tt("dve", gate[:], aff[:], sel[:], ALU.mult, ["aff", "sel"], ["gate"])
                self.red("dve", gm[:, 1, :], t16(gate[:]), ALU.add, ["gate"], ["gm"])
                c.op("dve", lambda e: e.reciprocal(out=gm[:, 1, :], in_=gm[:, 1, :]), reads=["gm"], writes=["gm"])
                self.tt("dve", t16(gate[:]), t16(gate[:]), bc16(gm[:, 1, :]), ALU.mult, ["gate", "gm"], ["gate"])
                self.cp("dve", selb[:], sel[:], ["sel"], ["selb"])
                psC, pkC = self.nps()
                for i in range(TB):
                    self.mm(psC[:, i * 16:(i + 1) * 16], SU[:], selb[:, i * 16:(i + 1) * 16], True, True, ["SU", "selb"], pkC)
                    self.mm(psC[:, 128 + i * 16:128 + (i + 1) * 16], ONES[:], selb[:, i * 16:(i + 1) * 16], True, True, ["ONESm", "selb"], pkC)
                self.cp("dve", offT[:, 0:16], offs[:], ["offs"], ["offT"])
                for i in range(1, TB):
                    self.tt("dve", offT[:, i * 16:(i + 1) * 16], offT[:, (i - 1) * 16:i * 16], psC[:, 128 + (i - 1) * 16:128 + i * 16],
                            ALU.add, [pkC, "offT"], ["offT"])
                self.tt("dve", offs[:], offT[:, (TB - 1) * 16:TB * 16], psC[:, 128 + (TB - 1) * 16:128 + TB * 16], ALU.add,
                        [pkC, "offT"], ["offs"])
                self.tt("dve", val[:], offT[:], psC[:, 0:TE], ALU.add, [pkC, "offT"], ["val"])
                self.ts("dve", val[:], val[:], float(CAP - 1), ALU.min, ["val"], ["val"])
                self.tt("dve", val[:], val[:], ebase[:, 0:TE], ALU.add, ["val", "ebase"], ["val"])
                self.tt("dve", val[:], val[:], sel[:], ALU.mult, ["val", "sel"], ["val"])
                self.ts("dve", val[:], val[:], -1.0, ALU.add, ["val"], ["val"])
                ts_ = slice(tb * TB, (tb + 1) * TB)
                for j in range(2):
                    self.red("dve", sl[:, j, :], t16(val[:]), ALU.max, ["val"], ["sl"])
                    self.tt("dve", t16(eq[:]), t16(val[:]), bc16(sl[:, j, :]), ALU.is_equal, ["val", "sl"], ["eq"])
                    self.tt("dve", s2[:], eq[:], gate[:], ALU.mult, ["eq", "gate"], ["s2"])
                    self.red("dve", wAB[:, ts_, j], t16(s2[:]), ALU.add, ["s2"], ["wAB"])
                    self.cp("dve", slotAB[:, ts_, j], sl[:, j, :], ["sl"], ["slotAB"])
                    if j == 0:
                        self.stt("dve", val[:], eq[:], -1e9, val[:], ALU.mult, ALU.add, ["eq", "val"], ["val"])
                if "dbg_aff" in self.dbg and tb == 0 and layer == 0:
                    for nm, tl_, w_ in (("dbg_aff", aff, TE), ("dbg_offT", offT, TE), ("dbg_gate", gate, TE), ("dbg_sel", sel, TE)):
                        dd = self.dscr(nm, [128, w_])
                        c.dma("sp", dd[:, :], tl_[:, 0:w_], reads=["aff", "offT", "gate", "sel"], writes=[nm])
                    dd = self.dscr("dbg_slotAB", [128, NT * 2], I32)
                    c.dma("sp", dd[:, :], slotAB[:].rearrange("p t j -> p (t j)"), reads=["slotAB"], writes=["dbg_slotAB"])
                    dd = self.dscr("dbg_wAB", [128, NT * 2])
                    c.dma("sp", dd[:, :], wAB[:].rearrange("p t j -> p (t j)"), reads=["wAB"], writes=["dbg_wAB"])
                    dd = self.dscr("dbg_sl", [128, 2 * TB])
                    c.dma("sp", dd[:, :], sl[:].rearrange("p a t -> p (a t)"), reads=["sl"], writes=["dbg_sl"])
                for i in range(TB):
                    t = tb * TB + i
                    for j in range(2):
                        c.dma("pool", toklist, tokid[:, t, :], reads=["tokid", "slotAB"], writes=["toklist"],
                              indirect=(bass.IndirectOffsetOnAxis(ap=slotAB[:, t, j:j + 1], axis=0), None))
            c.barrier()
        with ExitStack() as st2:
            sb2 = lambda n, shp, dt=F32: self.sb(st2, n, shp, dt)
            Wg = [sb2("Wg", [128, 8, D], BF16) for _ in range(2)]
            Wu = [sb2("Wu", [128, 8, D], BF16) for _ in range(2)]
            Wd = [sb2("Wd", [128, 8, D], BF16) for _ in range(2)]
            idx = [sb2("idx", [128, 16], I32) for _ in range(4)]
            X = [sb2("Xg", [128, D]) for _ in range(4)]
            xTg = [sb2("xTg", [128, 8, 512], BF16) for _ in range(2)]
            hs = [sb2("hs", [128, 512]) for _ in range(2)]
            hT = sb2("hTm", [128, 8, 512], BF16)
            ysb = [sb2("ysb", [128, D]) for _ in range(2)]
            n = 0
            gi = 0
            wstg = [sb2("wstg", [128, D]) for _ in range(8)]
            wsrc = [W["moe_w_gate"], W["moe_w_up"], W["moe_w_down"]]

            def chunk_dma(e, ci, si):
                m_, kc = ci // 8, ci % 8
                c.dma("sp", wstg[si][:], wsrc[m_][layer, e][kc * 128:(kc + 1) * 128, :], reads=[], writes=["mwstg%d" % si])

            def chunk_cast(e, ci, si):
                m_, kc = ci // 8, ci % 8
                dstw = (Wg, Wu, Wd)[m_][e % 2]
                self.cp("act" if ci % 2 == 0 else "dve", dstw[:, kc, :], wstg[si][:], ["mwstg%d" % si], ["W%d" % (e % 2)])

            for ci in range(24):
                chunk_dma(0, ci, ci % 8)
                chunk_cast(0, ci, ci % 8)
            per_slot = (24 + NG - 1) // NG
            for e in range(16):
                wbuf = e % 2
                kw = "W%d" % wbuf
                for grp in range(NG):
                    gb = gi % 2
                    gi += 1
                    nxt = [ci for ci in range(grp * per_slot, min(24, (grp + 1) * per_slot))] if e + 1 < 16 else []
                    if per_slot <= 8:
                        for k_, ci in enumerate(nxt):
                            chunk_dma(e + 1, ci, k_)
                    for i in range(4):
                        s0 = e * CAP + grp * 512 + i * 128
                        b = n % 4
                        n += 1
                        c.dma("pool", idx[b][:], toklist[s0:s0 + 128, :], reads=["toklist"], writes=["idx%d" % b])
                        c.dma("pool", X[b][:], xin, reads=[xin.tensor.name, "idx%d" % b], writes=["Xg%d" % b],
                              indirect=(None, bass.IndirectOffsetOnAxis(ap=idx[b][:, 0:1], axis=0)))
                        self.transpose_in(xTg[gb][:, :, i * 128:(i + 1) * 128], X[b], 8, "Xg%d" % b, "xTg%d" % gb)
                    for fc in range(8):
                        fs = slice(fc * 128, (fc + 1) * 128)
                        psG, pkG = self.nps()
                        for kc in range(8):
                            self.mm(psG[:, :], Wg[wbuf][:, kc, fs], xTg[gb][:, kc, :], kc == 0, kc == 7, [kw, "xTg%d" % gb], pkG)
                        psU, pkU = self.nps()
                        for kc in range(8):
                            self.mm(psU[:, :], Wu[wbuf][:, kc, fs], xTg[gb][:, kc, :], kc == 0, kc == 7, [kw, "xTg%d" % gb], pkU)
                        hb = fc % 2
                        self.act(hs[hb][:], psG[:, :], AF.Silu, [pkG], ["hs%d" % hb])
                        self.tt("dve", hT[:, fc, :], hs[hb][:], psU[:, :], ALU.mult, ["hs%d" % hb, pkU], ["hTm"])
                    for i in range(4):
                        s0 = e * CAP + grp * 512 + i * 128
                        yb = i % 2
                        for half in range(2):
                            ps, pk = self.nps()
                            for fc in range(8):
                                self.mm(ps[:, :], hT[:, fc, i * 128:(i + 1) * 128], Wd[wbuf][:, fc, half * 512:(half + 1) * 512],
                                        fc == 0, fc == 7, [kw, "hTm"], pk)
                            self.cp("act", ysb[yb][:, half * 512:(half + 1) * 512], ps[:, :], [pk], ["ysb%d" % yb])
                        c.dma("sp", ybuf[s0:s0 + 128, :], ysb[yb][:], reads=["ysb%d" % yb], writes=["ybuf"])
                    for k_, ci in enumerate(nxt):
                        if per_slot > 8:
                            chunk_dma(e + 1, ci, k_ % 8)
                        chunk_cast(e + 1, ci, k_ % 8)
            c.barrier()
        with ExitStack() as st2:
            sb2 = lambda n, shp, dt=F32: self.sb(st2, n, shp, dt)
            lnp = sb2("lnp", [128, 2 * D])
            c.dma("sp", lnp[:], lnp_ap[:, :], writes=["lnp"])
            tl = (sb2("ln_s", [128, 16]), sb2("ln_zc", [128, D]), None)
            xr = [sb2("cx", [128, D]) for _ in range(2)]
            yA = [sb2("cyA", [128, D]) for _ in range(2)]
            yB = [sb2("cyB", [128, D]) for _ in range(2)]
            z = [sb2("cz", [128, D]) for _ in range(2)]
            o = [sb2("co", [128, D]) for _ in range(2)]
            for t in range(NT):
                b = t % 2
                r0 = t * 128
                c.dma("sp", xr[b][:], xin[r0:r0 + 128, :], reads=[xin.tensor.name], writes=["cx%d" % b])
                c.dma("pool", yA[b][:], ybuf, reads=["ybuf", "slotAB"], writes=["cyA%d" % b],
                      indirect=(None, bass.IndirectOffsetOnAxis(ap=slotAB[:, t, 0:1], axis=0)))
                c.dma("pool", yB[b][:], ybuf, reads=["ybuf", "slotAB"], writes=["cyB%d" % b],
                      indirect=(None, bass.IndirectOffsetOnAxis(ap=slotAB[:, t, 1:2], axis=0)))
                self.ts("dve", z[b][:], xr[b][:], ALPHA, ALU.mult, ["cx%d" % b], ["cz%d" % b])
                self.stt("dve", z[b][:], yA[b][:], wAB[:, t, 0:1], z[b][:], ALU.mult, ALU.add, ["cyA%d" % b, "wAB", "cz%d" % b], ["cz%d" % b])
                self.stt("dve", z[b][:], yB[b][:], wAB[:, t, 1:2], z[b][:], ALU.mult, ALU.add, ["cyB%d" % b, "wAB", "cz%d" % b], ["cz%d" % b])
                self.ln_tile(z[b][:], "cz%d" % b, lnp, o[b][:], "co%d" % b, tl)
                c.dma("sp", dst[r0:r0 + 128, :], o[b][:], reads=["co%d" % b], writes=[dst.tensor.name])
            c.barrier()


B.ln_tile = ln_tile
B.stage_mix = stage_mix
B.stage_moe = stage_moe


def stage_ret(self, p1, ret, W):
    c = self.c
    NT = self.NT
    with ExitStack() as st:
        sb = lambda n, shp, dt=F32: self.sb(st, n, shp, dt)
        dec = sb("rtdec", [128, 8 * 128 + 24])
        c.dma("sp", dec[:], W["c_rtdec"][:, :], writes=["rtdec"])
        DT = lambda h: dec[:, h * 128:(h + 1) * 128]
        qd = dec[:, 1024:1032]
        kd = dec[:, 1032:1040]
        cd = dec[:, 1040:1048]
        gn = sb("rtgn", [128, 4096])
        c.dma("sp", gn[:], W["c_rtgn"][:, :], writes=["rtgn"])
        R = sb("R", [128, 8, 256])
        Rb = sb("Rb", [128, 8, 256], BF16)
        c.op("pool", lambda e: e.memset(R[:].rearrange("p h v -> p (h v)"), 0.0), writes=["R"])
        c.op("pool", lambda e: e.memset(Rb[:].rearrange("p h v -> p (h v)"), 0.0), writes=["Rb"])
        rope = sb("rtrope", [128, 256])
        Pq = [sb("Pq", [128, 2048]) for _ in range(2)]
        Vv = [sb("Vv", [128, 2048]) for _ in range(2)]
        Gg = [sb("Gg", [128, 2048]) for _ in range(2)]
        qk = sb("qkr", [128, 3, 1024])
        ktb = sb("ktb", [128, 1024], BF16)
        tmp = sb("rtmp", [128, 8, 64])
        T3 = sb("T3", [128, 24, 128], BF16)
        Vb = sb("Vb", [128, 2048], BF16)
        attm = sb("attm", [128, 1024], BF16)
        Os_ = [sb("rOs", [128, 2048]) for _ in range(2)]
        sq = sb("rsq", [128, 2048])
        sg = sb("rsg", [128, 2048])
        st8 = sb("rst8", [128, 8])
        oo = sb("roo", [128, 2048])
        def tail(t, b):
            r0 = t * 128
            kg = "Gg%d" % b
            Os = Os_[b]
            ko = "rOs%d" % b
            O3 = Os[:].rearrange("p (h v) -> p h v", h=8)
            self.red("dve", st8[:], O3, ALU.add, [ko], ["rst8"])
            self.ts("dve", st8[:], st8[:], 1.0 / 256, ALU.mult, ["rst8"], ["rst8"])
            self.tt("dve", O3, O3, st8[:].unsqueeze(2).to_broadcast([128, 8, 256]), ALU.subtract, [ko, "rst8"], [ko])
            self.act(sq[:], Os[:], AF.Square, [ko], ["rsq"])
            self.red("dve", st8[:], sq[:].rearrange("p (h v) -> p h v", h=8), ALU.add, ["rsq"], ["rst8"])
            self.rsqrt(st8[:], st8[:], 1e-5, ["rst8"], ["rst8"], scale=1.0 / 256)
            self.tt("dve", O3, O3, st8[:].unsqueeze(2).to_broadcast([128, 8, 256]), ALU.mult, [ko, "rst8"], [ko])
            self.tt("dve", Os[:], Os[:], gn[:, 0:2048], ALU.mult, [ko, "rtgn"], [ko])
            self.tt("pool", Os[:], Os[:], gn[:, 2048:4096], ALU.add, [ko, "rtgn"], [ko])
            self.act(sg[:], Gg[b][:], AF.Silu, [kg], ["rsg"])
            self.tt("dve", oo[:], Os[:], sg[:], ALU.mult, [ko, "rsg"], ["roo"])
            c.dma("sp", ret[r0:r0 + 128, :], oo[:], reads=["roo"], writes=["ret"])
        for t in range(NT):
            b = t % 2
            r0 = t * 128
            kp, kv, kg = "Pq%d" % b, "Vv%d" % b, "Gg%d" % b
            c.dma("sp", Pq[b][:], p1[r0:r0 + 128, 0:2048], reads=["p1"], writes=[kp])
            c.dma("sp", Vv[b][:], p1[r0:r0 + 128, 2048:4096], reads=["p1"], writes=[kv])
            c.dma("sp", Gg[b][:], p1[r0:r0 + 128, 4096:6144], reads=["p1"], writes=[kg])
            c.dma("sp", rope[:], W["c_rtrope"][r0:r0 + 128, :], writes=["rtrope"])
            for a in range(2):
                src = Pq[b][:, a * 1024:(a + 1) * 1024].rearrange("p (h d) -> p h d", h=8)
                dst = qk[:, a, :].rearrange("p (h d) -> p h d", h=8)
                cos = rope[:, a * 128:a * 128 + 64].unsqueeze(1).to_broadcast([128, 8, 64])
                sin = rope[:, a * 128 + 64:a * 128 + 128].unsqueeze(1).to_broadcast([128, 8, 64])
                x1, x2 = src[:, :, 0:64], src[:, :, 64:128]
                o1, o2 = dst[:, :, 0:64], dst[:, :, 64:128]
                kq = "qk%d" % a
                self.tt("dve", o1, x1, cos, ALU.mult, [kp, "rtrope"], [kq])
                self.tt("pool", tmp[:], x2, sin, ALU.mult, [kp, "rtrope"], ["rtmp"])
                self.tt("dve", o1, o1, tmp[:], ALU.subtract, [kq, "rtmp"], [kq])
                self.tt("dve", o2, x2, cos, ALU.mult, [kp, "rtrope"], [kq])
                self.tt("pool", tmp[:], x1, sin, ALU.mult, [kp, "rtrope"], ["rtmp"])
                self.tt("dve", o2, o2, tmp[:], ALU.add, [kq, "rtmp"], [kq])
            q3 = qk[:, 0, :].rearrange("p (h d) -> p h d", h=8)
            k3 = qk[:, 1, :].rearrange("p (h d) -> p h d", h=8)
            self.tt("pool", qk[:, 2, :].rearrange("p (h d) -> p h d", h=8), q3, qd.unsqueeze(2).to_broadcast([128, 8, 128]),
                    ALU.mult, ["qk0", "rtdec"], ["qk2"])
            self.tt("dve", ktb[:].rearrange("p (h d) -> p h d", h=8), k3, kd.unsqueeze(2).to_broadcast([128, 8, 128]),
                    ALU.mult, ["qk1", "rtdec"], ["ktb"])
            self.cp("act", Vb[:], Vv[b][:], [kv], ["Vb"])
            for a in range(3):
                self.transpose_in(T3, qk[:, a, :], 8, "qk%d" % a, "T3_%d" % a, ch0=8 * a)
            for hq in range(2):
                psA, pkA = self.nps()
                for hh in range(4):
                    h = hq * 4 + hh
                    self.mm(psA[:, hh * 128:(hh + 1) * 128], T3[:, 8 + h, :], T3[:, h, :], True, True, ["T3_0", "T3_1"], pkA)
                self.tt("dve", attm[:, hq * 512:(hq + 1) * 512], psA[:, :], dec[:, hq * 512:(hq + 1) * 512], ALU.mult,
                        [pkA, "rtdec"], ["attm%d" % hq])
            for hp in range(4):
                psO, pkO = self.nps()
                for hh in range(2):
                    h = hp * 2 + hh
                    vs = slice(h * 256, (h + 1) * 256)
                    self.mm(psO[:, hh * 256:(hh + 1) * 256], attm[:, h * 128:(h + 1) * 128], Vb[:, vs], True, False,
                            ["attm%d" % (h // 4), "Vb"], pkO)
                    self.mm(psO[:, hh * 256:(hh + 1) * 256], T3[:, 16 + h, :], Rb[:, h, :], False, True, ["T3_2", "Rb"], pkO)
                self.cp("act", Os_[b][:, hp * 512:(hp + 1) * 512], psO[:, :], [pkO], ["rOs%d" % b])
            R2 = R[:].rearrange("p h v -> p (h v)")
            self.tt("pool", R[:], R[:], cd.unsqueeze(2).to_broadcast([128, 8, 256]), ALU.mult, ["R", "rtdec"], ["R"])
            for hp in range(4):
                psR, pkR = self.nps()
                for hh in range(2):
                    h = hp * 2 + hh
                    vs = slice(h * 256, (h + 1) * 256)
                    self.mm(psR[:, hh * 256:(hh + 1) * 256], ktb[:, h * 128:(h + 1) * 128], Vb[:, vs], True, True, ["ktb", "Vb"], pkR)
                self.tt("dve", R2[:, hp * 512:(hp + 1) * 512], R2[:, hp * 512:(hp + 1) * 512], psR[:, :], ALU.add, [pkR, "R"], ["R"])
            self.cp("act", Rb[:].rearrange("p h v -> p (h v)"), R2, ["R"], ["Rb"])
            if t > 0:
                tail(t - 1, 1 - b)
        tail(NT - 1, (NT - 1) % 2)
        c.barrier()


B.stage_ret = stage_ret


STAGES = ["proj0", "rwkv", "nsa", "mix0", "moe0", "proj1", "ret", "mix1", "moe1"]


def build(S, upto="moe1", dbg=()):
    b = B(S, dbg)
    n_st = STAGES.index(upto) + 1
    on = lambda s: STAGES.index(s) < n_st
    CAP = moe_cap(S)
    NSLOT = 16 * CAP
    ncp = ((((S - 32) // 16 + 1) + 127) // 128) * 128
    W = {}
    for name, shp, dt in [("c_ident", [128, 128], F32), ("c_tri", [128, 128], F32), ("c_mask4", [128, 512], F32),
                          ("c_maskL", [128, 128], F32), ("c_bd", [128, 128], F32),
                          ("c_rkv", [128, 13 * 512], F32), ("rk_w1", [512, 64], F32), ("rk_a1", [512, 64], F32),
                          ("rk_g1", [512, 128], F32), ("rk_w2", [64, 512], F32), ("rk_a2", [64, 512], F32),
                          ("rk_g2", [128, 512], F32),
                          ("ns_c_w1", [2, 2048, 128], F32), ("ns_c_w2", [2, 128, 64], F32), ("c_peT", [128, 64], F32),
                          ("c_ones", [128, 1], F32), ("c_rope", [S, 16], F32), ("c_selF", [S, 128], F32),
                          ("c_E", [128, S], F32), ("c_caus", [128, 4 * 512], F32), ("c_win", [128, 8 * 512], F32),
                          ("c_cmpb", [128, 5 * 512], F32), ("c_ov", [ncp, 128], F32),
                          ("ab_w_out", [D, D], F32), ("c_ln", [4, 128, 2 * D], F32),
                          ("router_w", [D, 16], F32), ("c_rb", [128, 128], F32), ("c_ebase", [128, 128], F32),
                          ("c_su", [128, 128], F32), ("c_tokid", [128, (S // 128) * 16], I32),
                          ("c_tokinit", [NSLOT + 1, 16], I32),
                          ("moe_w_gate", [2, 16, D, D], F32), ("moe_w_up", [2, 16, D, D], F32),
                          ("moe_w_down", [2, 16, D, D], F32),
                          ("rt_w_in", [D, 6144], F32), ("rt_w_out", [2048, D], F32), ("c_rtgn", [128, 2 * 2048], F32),
                          ("c_rtrope", [S, 256], F32), ("c_rtdec", [128, 8 * 128 + 24], F32)]:
        W[name] = b.din(name, shp, dt)
    x = b.din("x", [S, D])
    ab_w_in = b.din("ab_w_in", [D, 3352])
    p0 = b.dscr("p0", [S, 3352])
    oab = b.dscr("oab", [S, 1024])
    x1 = b.dscr("x1", [S + 1, D])
    x2 = b.dscr("x2", [S + 1, D])
    x3 = b.dscr("x3", [S + 1, D])
    p1 = b.dscr("p1", [S, 6144])
    ret = b.dscr("ret", [S, 2048])
    toklist = b.dscr("toklist", [NSLOT + 1, 16], I32)
    ybuf = b.dscr("ybuf", [NSLOT, D])
    out = b.nc.dram_tensor("out", [S, D], F32, kind="ExternalOutput").ap()
    with ExitStack() as st:
        b.load_consts(st)
        zrow = b.sb(st, "zrow", [1, D])
        b.c.op("pool", lambda e: e.memset(zrow[:], 0.0), writes=["zrow"])
        for xx in (x1, x3):
            b.c.dma("sp", xx[S:S + 1, :], zrow[:], reads=["zrow"], writes=[xx.tensor.name])
        b.stage_proj(x, ab_w_in, p0, D, 3352)
        if on("rwkv"):
            b.stage_rwkv(p0, oab, W)
        if on("nsa"):
            b.stage_nsa(p0, oab, W)
        if on("mix0"):
            b.stage_mix(oab, 1024, W["ab_w_out"], x, W["c_ln"][0], x1)
        if on("moe0"):
            b.stage_moe(x1, W, 0, W["c_ln"][1], x2, toklist, ybuf)
        if on("proj1"):
            b.stage_proj(x2, W["rt_w_in"], p1, D, 6144)
        if on("ret"):
            b.stage_ret(p1, ret, W)
        if on("mix1"):
            b.stage_mix(ret, 2048, W["rt_w_out"], x2, W["c_ln"][2], x3)
        if on("moe1"):
            b.stage_moe(x3, W, 1, W["c_ln"][3], out, toklist, ybuf)
        b.c.finish()
    return b


def consts(S):
    c = {}
    c["c_ident"] = np.eye(128, dtype=np.float32)
    i = np.arange(128)
    same = (i[:, None] // 64) == (i[None, :] // 64)
    strict = ((i[:, None] < i[None, :]) & same).astype(np.float32)
    incl = ((i[:, None] <= i[None, :]) & same).astype(np.float32)
    c["c_tri"] = incl
    c["c_mask4"] = np.concatenate([strict, incl, strict, incl], axis=1)
    c["c_bd"] = same.astype(np.float32)
    c["c_ones"] = np.ones((128, 1), np.float32)
    inv = 500000.0 ** (-np.arange(8, dtype=np.float32) / 8)
    ang = np.arange(S, dtype=np.float32)[:, None] * inv[None, :]
    c["c_rope"] = np.concatenate([np.cos(ang), np.sin(ang)], axis=1).astype(np.float32)
    tpos = np.arange(S)
    cur = tpos // 64
    jb = np.arange(128)
    F = np.zeros((S, 128), np.float32)
    F[jb[None, :] > cur[:, None]] = -10.0
    forced = (jb[None, :] == 0) | (jb[None, :] == cur[:, None]) | (jb[None, :] == cur[:, None] - 1)
    F[forced & (jb[None, :] <= cur[:, None])] = 10.0
    c["c_selF"] = F
    c["c_E"] = (np.arange(S)[None, :] // 64 == jb[:, None]).astype(np.float32)
    k = np.arange(128)[:, None]
    q = np.arange(512)[None, :]
    c["c_caus"] = np.concatenate([np.where(128 * d + k <= q, 0.0, NEG) for d in range(4)], axis=1).astype(np.float32)
    c["c_win"] = np.concatenate([np.where((128 * d + k <= q) & (128 * d + k > q - 512), 0.0, NEG) for d in range(-4, 4)],
                                axis=1).astype(np.float32)
    c["c_cmpb"] = np.concatenate([np.where(16 * k + 31 <= 512 * dj + q, 0.0, NEG) for dj in range(5)], axis=1).astype(np.float32)
    n_cmp = (S - 32) // 16 + 1
    ncp = ((n_cmp + 127) // 128) * 128
    cs = np.arange(ncp) * 16
    ss = np.arange(128) * 64
    ov = np.clip(np.minimum(cs[:, None] + 32, ss[None, :] + 64) - np.maximum(cs[:, None], ss[None, :]), 0, None).astype(np.float32) / 32
    ov[n_cmp:] = 0.0
    c["c_ov"] = ov
    CAP = moe_cap(S)
    c["c_ebase"] = np.ascontiguousarray(np.broadcast_to(np.tile((np.arange(16) * CAP + 1).astype(np.float32), 8)[None, :], (128, 128)))
    c["c_su"] = (i[:, None] < i[None, :]).astype(np.float32)
    NT = S // 128
    tok = (np.arange(NT)[None, :, None] * 128 + np.arange(128)[:, None, None] + np.zeros((1, 1, 16), np.int64))
    c["c_tokid"] = np.ascontiguousarray(tok.reshape(128, NT * 16)).astype(np.int32)
    inv = 10000.0 ** (-np.linspace(0.0, 1.0, 64, dtype=np.float32))
    ang = np.arange(S, dtype=np.float32)[:, None] * inv[None, :]
    cs_, sn_ = np.cos(ang), np.sin(ang)
    sc = 128.0 ** -0.5
    c["c_rtrope"] = np.concatenate([cs_, sn_, cs_ * sc, sn_ * sc], axis=1).astype(np.float32)
    log_g = np.log(1.0 - 2.0 ** (-5.0 - np.arange(8, dtype=np.float64)))
    ii = np.arange(128, dtype=np.float64)
    diff = ii[None, :] - ii[:, None]
    DTm = [np.where(diff >= 0, np.exp(np.maximum(diff, 0.0) * lg), 0.0) for lg in log_g]
    qd = np.exp((ii[:, None] + 1.0) * log_g[None, :])
    kd = np.exp((127.0 - ii[:, None]) * log_g[None, :])
    cd = np.broadcast_to(np.exp(128.0 * log_g)[None, :], (128, 8))
    c["c_rtdec"] = np.concatenate(DTm + [qd, kd, cd], axis=1).astype(np.float32)
    c["c_tokinit"] = np.full((16 * CAP + 1, 16), S, np.int32)
    c["c_maskL"] = np.ascontiguousarray(strict.T)
    return c


def derived(inputs):
    d = {}
    rk = np.concatenate([inputs["rk_mu"].reshape(-1), inputs["rk_w0"].reshape(-1), inputs["rk_a0"].reshape(-1),
                         inputs["rk_kk"].reshape(-1), inputs["rk_ka"].reshape(-1), inputs["rk_rk"].reshape(-1),
                         inputs["rk_ln"].reshape(-1)])
    pe = np.asarray(inputs["ns_pe"]).reshape(2, 32, 64)
    peT = np.transpose(pe, (2, 0, 1)).reshape(64, 64)
    d["c_peT"] = np.ascontiguousarray(np.concatenate([peT, peT], axis=0)).astype(np.float32)
    ln = np.asarray(inputs["ln"]).reshape(4, 2 * D)
    d["c_ln"] = np.ascontiguousarray(np.broadcast_to(ln[:, None, :], (4, 128, 2 * D))).astype(np.float32)
    d["c_rtgn"] = np.ascontiguousarray(np.broadcast_to(np.asarray(inputs["rt_gn"]).reshape(1, 4096), (128, 4096))).astype(np.float32)
    d["c_rb"] = np.ascontiguousarray(np.broadcast_to(np.tile(np.asarray(inputs["router_b"]).reshape(16), 8)[None, :], (128, 128))).astype(np.float32)
    d["c_rkv"] = np.ascontiguousarray(np.broadcast_to(rk[None, :], (128, rk.size))).astype(np.float32)
    return d


def make_inputs(b, inputs, bi, S):
    cs = consts(S)
    cs.update(derived(inputs))
    m = {}
    for name, ap in b.inp.items():
        if name in cs:
            m[name] = cs[name]
        elif name == "x":
            m[name] = np.ascontiguousarray(inputs["x"][bi, :S])
        else:
            a = np.asarray(inputs[name])
            m[name] = np.ascontiguousarray(a.reshape(ap.shape))
    return m


_BUILT = {}


def kernel(**inputs):
    S = 8192
    if S not in _BUILT:
        _BUILT[S] = build(S)
    b = _BUILT[S]
    shared = make_inputs(b, inputs, 0, S)
    in_maps = []
    for bi in range(8):
        m = dict(shared)
        m["x"] = np.ascontiguousarray(np.asarray(inputs["x"])[bi, :S]).astype(np.float32)
        in_maps.append(m)
    res = run_bass_kernel_spmd(b.nc, in_maps, core_ids=list(range(8)))
    return np.stack([np.asarray(r["out"]) for r in res.results], axis=0).astype(np.float32)
```
